# Optimizing a Trainium2 kernel written in Bass

```python
import math
import jax, jax.numpy as jnp
from jax import lax
import numpy as np

D_MODEL = 1024
BATCH = 32
SEQ = 2048
DEPTH = 1

SSM_WIDTH = D_MODEL // 2
SSM_GROUP = 16
SSM_GROUPS = SSM_WIDTH // SSM_GROUP
SSM_STATE = 64
ATTN_HEADS = 8
HEAD_DIM = (D_MODEL // 2) // ATTN_HEADS
ATTN_WIDTH = ATTN_HEADS * HEAD_DIM
MOBA_BLOCK = 256
MOBA_TOPK = 3
Q_CHUNK = 128
D_FF = 4 * D_MODEL
PLE_DIM = 256
RMS_EPS = 1e-6
DT_MIN = 1e-3
DT_MAX = 1e-1
NEG_INF = -1e30
OFF_U = 0
OFF_Q = OFF_U + SSM_WIDTH
OFF_K = OFF_Q + ATTN_WIDTH
OFF_V = OFF_K + ATTN_WIDTH
OFF_GA = OFF_V + ATTN_WIDTH
OFF_GB = OFF_GA + D_MODEL
IN_COLS = OFF_GB + D_MODEL

kernel_name = "hybrid_s5_moba_sqrelu_block"


def rmsnorm(x, g):
    xf = x.astype(jnp.float32)
    y = xf * lax.rsqrt(jnp.mean(xf * xf, axis=-1, keepdims=True) + RMS_EPS)
    return (y * g.astype(jnp.float32)).astype(x.dtype)


def _ssm_combine(e1, e2):
    a1, b1 = e1
    a2, b2 = e2
    return a1 * a2, a2 * b1 + b2


def s5_mixer(u, lam_re, lam_im, log_dt, b_re, b_im, c_re, c_im, d_skip, w_glu, b_glu):
    f32 = jnp.float32
    bsz, seq, _ = u.shape
    uf = u.astype(f32).reshape(bsz, seq, SSM_GROUPS, SSM_GROUP)
    lam = lax.complex(lam_re.astype(f32), lam_im.astype(f32))
    dt = jnp.exp(log_dt.astype(f32))[:, None]
    lam_bar = jnp.exp(lam * dt)
    b_mat = lax.complex(b_re.astype(f32), b_im.astype(f32))
    b_bar = ((lam_bar - 1.0) / lam)[..., None] * b_mat
    bu = jnp.einsum('bsgc,gpc->bsgp', uf.astype(jnp.complex64), b_bar)
    a = jnp.broadcast_to(lam_bar, (1, seq, SSM_GROUPS, SSM_STATE))
    _, states = lax.associative_scan(_ssm_combine, (a, bu), axis=1)
    c_mat = lax.complex(c_re.astype(f32), c_im.astype(f32))
    y = jnp.real(jnp.einsum('gcp,bsgp->bsgc', c_mat, states))
    y = y + d_skip.astype(f32).reshape(SSM_GROUPS, SSM_GROUP) * uf
    y = jax.nn.gelu(y.reshape(bsz, seq, SSM_WIDTH).astype(u.dtype))
    return y * jax.nn.sigmoid(y @ w_glu + b_glu)


def moba_attention(q, k, v):
    f32 = jnp.float32
    bsz, seq, nh, hd = q.shape
    nb = -(-seq // MOBA_BLOCK)
    s_pad = nb * MOBA_BLOCK
    pad = ((0, 0), (0, s_pad - seq), (0, 0), (0, 0))
    q = jnp.pad(q, pad)
    k = jnp.pad(k, pad)
    v = jnp.pad(v, pad)
    kb = k.reshape(bsz, nb, MOBA_BLOCK, nh, hd).transpose(0, 3, 1, 2, 4)
    vb = v.reshape(bsz, nb, MOBA_BLOCK, nh, hd).transpose(0, 3, 1, 2, 4)
    scale = hd ** -0.5
    n_sel = min(MOBA_TOPK, nb - 1)
    n_chunks = s_pad // Q_CHUNK
    qc = q.reshape(bsz * n_chunks, Q_CHUNK, nh, hd)
    b_ids = jnp.repeat(jnp.arange(bsz, dtype=jnp.int32), n_chunks)
    c_ids = jnp.tile(jnp.arange(n_chunks, dtype=jnp.int32), bsz)
    if n_sel > 0:
        k_mean = jnp.mean(kb.astype(f32), axis=3)
        gate = jnp.einsum('bshd,bhnd->bshn', q.astype(f32), k_mean)
        qblk = jnp.arange(s_pad) // MOBA_BLOCK
        past = jnp.arange(nb)[None, :] < qblk[:, None]
        gate = jnp.where(past[None, :, None, :], gate, NEG_INF)
        _, sel = lax.top_k(gate, n_sel)
        sel_c = sel.astype(jnp.int32).reshape(bsz * n_chunks, Q_CHUNK, nh, n_sel)
    else:
        sel_c = jnp.zeros((bsz * n_chunks, Q_CHUNK, nh, 0), jnp.int32)
    heads = jnp.arange(nh)[None, :, None]

    def step(args):
        qq, ss, b, c = args
        k_b = kb[b]
        v_b = vb[b]
        own = (c * Q_CHUNK) // MOBA_BLOCK
        pos_q = c * Q_CHUNK + jnp.arange(Q_CHUNK)
        pos_k = own * MOBA_BLOCK + jnp.arange(MOBA_BLOCK)
        k_own = k_b[:, own].astype(f32)
        v_own = v_b[:, own].astype(f32)
        qf = qq.astype(f32) * scale
        s_own = jnp.einsum('qhd,hkd->qhk', qf, k_own)
        causal = (pos_k[None, :] <= pos_q[:, None])[:, None, :]
        s_own = jnp.where(causal, s_own, NEG_INF)
        if n_sel > 0:
            k_sel = k_b[heads, ss].astype(f32)
            v_sel = v_b[heads, ss].astype(f32)
            s_sel = jnp.einsum('qhd,qhnkd->qhnk', qf, k_sel)
            valid = (ss < own)[..., None]
            s_sel = jnp.where(valid, s_sel, NEG_INF)
            n_k = n_sel * MOBA_BLOCK
            scores = jnp.concatenate([s_sel.reshape(Q_CHUNK, nh, n_k), s_own], axis=-1)
            probs = jax.nn.softmax(scores, axis=-1)
            p_sel = probs[..., :n_k].reshape(Q_CHUNK, nh, n_sel, MOBA_BLOCK)
            p_own = probs[..., n_k:]
            out = (jnp.einsum('qhnk,qhnkd->qhd', p_sel, v_sel)
                   + jnp.einsum('qhk,hkd->qhd', p_own, v_own))
        else:
            probs = jax.nn.softmax(s_own, axis=-1)
            out = jnp.einsum('qhk,hkd->qhd', probs, v_own)
        return out.astype(qq.dtype)

    outs = lax.map(step, (qc, sel_c, b_ids, c_ids))
    outs = outs.reshape(bsz, s_pad, nh * hd)
    return outs[:, :seq]


def setup_inputs(seed: int = 0) -> dict:
    key = jax.random.key(seed)
    ks = jax.random.split(key, 26)
    f32 = jnp.float32
    L = DEPTH

    def nrm(k, shape, scale):
        return jax.random.normal(k, shape, f32) * scale

    def gain(k):
        return 1.0 + 0.01 * jax.random.normal(k, (L, D_MODEL), f32)

    n_idx = jnp.arange(SSM_STATE, dtype=f32)
    return {
        "x": nrm(ks[0], (BATCH, SEQ, D_MODEL), 1.0),
        "p": nrm(ks[1], (L, BATCH, SEQ, PLE_DIM), 1.0),
        "g_pre_mix": gain(ks[2]),
        "w_in": nrm(ks[3], (L, D_MODEL, IN_COLS), D_MODEL ** -0.5),
        "ssm_lam_re": -0.5 + 0.01 * jax.random.normal(ks[4], (L, SSM_GROUPS, SSM_STATE), f32),
        "ssm_lam_im": math.pi * n_idx + 0.01 * jax.random.normal(ks[5], (L, SSM_GROUPS, SSM_STATE), f32),
        "ssm_log_dt": jax.random.uniform(ks[6], (L, SSM_GROUPS), f32, math.log(DT_MIN), math.log(DT_MAX)),
        "ssm_b_re": nrm(ks[7], (L, SSM_GROUPS, SSM_STATE, SSM_GROUP), (2 * SSM_GROUP) ** -0.5),
        "ssm_b_im": nrm(ks[8], (L, SSM_GROUPS, SSM_STATE, SSM_GROUP), (2 * SSM_GROUP) ** -0.5),
        "ssm_c_re": nrm(ks[9], (L, SSM_GROUPS, SSM_GROUP, SSM_STATE), (2 * SSM_STATE) ** -0.5),
        "ssm_c_im": nrm(ks[10], (L, SSM_GROUPS, SSM_GROUP, SSM_STATE), (2 * SSM_STATE) ** -0.5),
        "ssm_d": nrm(ks[11], (L, SSM_WIDTH), 1.0),
        "w_glu": nrm(ks[12], (L, SSM_WIDTH, SSM_WIDTH), SSM_WIDTH ** -0.5),
        "b_glu": nrm(ks[13], (L, SSM_WIDTH), 0.01),
        "w_branch_a": nrm(ks[14], (L, SSM_WIDTH, D_MODEL), SSM_WIDTH ** -0.5),
        "w_branch_b": nrm(ks[15], (L, ATTN_WIDTH, D_MODEL), ATTN_WIDTH ** -0.5),
        "w_out": nrm(ks[16], (L, D_MODEL, D_MODEL), D_MODEL ** -0.5),
        "g_post_mix": gain(ks[17]),
        "g_pre_mlp": gain(ks[18]),
        "w_mlp1": nrm(ks[19], (L, D_MODEL, D_FF), D_MODEL ** -0.5),
        "w_mlp2": nrm(ks[20], (L, D_FF, D_MODEL), D_FF ** -0.5),
        "g_post_mlp": gain(ks[21]),
        "w_ple": nrm(ks[22], (L, PLE_DIM, D_MODEL), PLE_DIM ** -0.5),
        "w_ple_gate": nrm(ks[23], (L, D_MODEL, D_MODEL), D_MODEL ** -0.5),
        "g_ple": gain(ks[24]),
    }


def reference(x, p, g_pre_mix, w_in, ssm_lam_re, ssm_lam_im, ssm_log_dt, ssm_b_re, ssm_b_im,
              ssm_c_re, ssm_c_im, ssm_d, w_glu, b_glu, w_branch_a, w_branch_b, w_out,
              g_post_mix, g_pre_mlp, w_mlp1, w_mlp2, g_post_mlp, w_ple, w_ple_gate, g_ple):
    bsz, seq, _ = x.shape
    for i in range(DEPTH):
        h = rmsnorm(x, g_pre_mix[i])
        z = h @ w_in[i]
        u = z[..., OFF_U:OFF_Q]
        q = z[..., OFF_Q:OFF_K].reshape(bsz, seq, ATTN_HEADS, HEAD_DIM)
        k = z[..., OFF_K:OFF_V].reshape(bsz, seq, ATTN_HEADS, HEAD_DIM)
        v = z[..., OFF_V:OFF_GA].reshape(bsz, seq, ATTN_HEADS, HEAD_DIM)
        gate_a = jax.nn.sigmoid(z[..., OFF_GA:OFF_GB])
        gate_b = jax.nn.sigmoid(z[..., OFF_GB:IN_COLS])
        y_a = s5_mixer(u, ssm_lam_re[i], ssm_lam_im[i], ssm_log_dt[i], ssm_b_re[i], ssm_b_im[i],
                       ssm_c_re[i], ssm_c_im[i], ssm_d[i], w_glu[i], b_glu[i]) @ w_branch_a[i]
        y_b = moba_attention(q, k, v) @ w_branch_b[i]
        mixed = (gate_a * y_a + gate_b * y_b) @ w_out[i]
        x = x + rmsnorm(mixed, g_post_mix[i])
        hm = rmsnorm(x, g_pre_mlp[i])
        f = jnp.square(jax.nn.relu(hm @ w_mlp1[i])) @ w_mlp2[i]
        x = x + rmsnorm(f, g_post_mlp[i])
        e = (p[i] @ w_ple[i]) * jax.nn.sigmoid(x @ w_ple_gate[i])
        x = x + rmsnorm(e, g_ple[i])
    return x
```

```python
import math
from contextlib import ExitStack
import numpy as np
import concourse.bass as bass
import concourse.mybir as mybir
from concourse.bass_utils import run_bass_kernel_spmd

F32 = mybir.dt.float32
BF16 = mybir.dt.bfloat16
I32 = mybir.dt.int32
AF = mybir.ActivationFunctionType
ALU = mybir.AluOpType
AX = mybir.AxisListType

NCORES = 8
D = 1024
SEQ = 2048
BPC = 4
T = BPC * SEQ
NTT = T // 128
G = 32
PST = 64
DFF = 4096
PLE = 256
EPS = 1e-6
NEG = -30000.0


class Buf:
    __slots__ = ("name", "w", "r", "ds")

    def __init__(self, name=""):
        self.name = name
        self.w = None
        self.r = []
        self.ds = None


class K:
    ENGS = ("pe", "dve", "act", "pool", "sp")

    def __init__(self, nc, es):
        self.nc = nc
        self.es = es
        self.esem = {e: es.enter_context(nc.semaphore("es_" + e)) for e in self.ENGS}
        self.cnt = {e: 0 for e in self.ENGS}
        self.dpool = [es.enter_context(nc.semaphore("ds%d" % i)) for i in range(48)]
        self.NHW = 36
        self.dcnt = [0] * len(self.dpool)
        self.ops = None
        with nc.Block() as block:
            def clr(eng):
                for sm in list(self.esem.values()) + self.dpool:
                    eng.sem_clear(sm)
            block.sync(clr)

    def begin(self):
        self.ops = {e: [] for e in self.ENGS}
        self.seen = {e: {} for e in self.ENGS}
        self.dnext = {False: 0, True: self.NHW}
        self.dused = set()
        self.phase_id = getattr(self, "phase_id", 0) + 1

    def _dsem(self, buf, sw):
        if buf.ds is None or buf.ds[0] != (self.phase_id, sw):
            lim = len(self.dpool) if sw else self.NHW
            assert self.dnext[sw] < lim, "out of dma sems"
            buf.ds = ((self.phase_id, sw), self.dnext[sw])
            self.dnext[sw] += 1
        return buf.ds[1]

    def _deps(self, eng, reads, writes):
        waits = {}
        def add(tok):
            if tok is None:
                return
            key, val = tok
            if key == ("e", "pe") and eng == "pe":
                return
            if self.seen[eng].get(key, 0) >= val:
                return
            if waits.get(key, 0) < val:
                waits[key] = val
        for b in reads:
            add(b.w)
        for b in writes:
            add(b.w)
            for t in b.r:
                add(t)
        for key, val in waits.items():
            self.seen[eng][key] = val
        return list(waits.items())

    def _mark(self, tok, reads, writes):
        for b in reads:
            b.r.append(tok)
        for b in writes:
            b.w = tok
            b.r = []

    def op(self, eng, fn, reads=(), writes=()):
        waits = self._deps(eng, reads, writes)
        self.cnt[eng] += 1
        tok = (("e", eng), self.cnt[eng])
        self._mark(tok, reads, writes)
        self.ops[eng].append(("c", fn, waits))

    def dma(self, eng, out, in_, sb, reads=(), writes=(), **kw):
        waits = self._deps(eng, reads, writes)
        si = self._dsem(sb, eng == "pool")
        self.dcnt[si] += 16
        self.dused.add(si)
        tok = (("d", si), self.dcnt[si])
        self._mark(tok, reads, writes)
        self.ops[eng].append(("d", (out, in_, si, kw), waits))

    def _sem(self, key):
        return self.esem[key[1]] if key[0] == "e" else self.dpool[key[1]]

    def end(self, name):
        nc = self.nc
        fin = [(("e", e), self.cnt[e]) for e in self.ENGS if e != "sp" and self.cnt[e] > 0]
        fin += [(("d", si), self.dcnt[si]) for si in sorted(self.dused)]
        ops = self.ops
        handles = {"pe": "tensor", "dve": "vector", "act": "scalar", "pool": "gpsimd", "sp": "sync"}
        with nc.Block() as block:
            for e in self.ENGS:
                def body(eng, e=e):
                    for kind, payload, waits in ops[e]:
                        for key, val in waits:
                            eng.wait_ge(self._sem(key), val)
                        if kind == "c":
                            ins = payload(eng)
                            ins.then_inc(self.esem[e], 1)
                        else:
                            out, in_, si, kw = payload
                            eng.dma_start(out=out, in_=in_, **kw).then_inc(self.dpool[si], 16)
                    if e == "sp":
                        for key, val in fin:
                            eng.wait_ge(self._sem(key), val)
                getattr(block, handles[e])(body)
        self.ops = None


def mm(k, items, reads, writes):
    def fn(eng):
        ins = None
        for (out, lhsT, rhs, st, sp) in items:
            ins = eng.matmul(out, lhsT, rhs, start=st, stop=sp)
        return ins
    k.op("pe", fn, reads, writes)


def tr(k, items, reads, writes):
    def fn(eng):
        ins = None
        for (out, in_, ident) in items:
            ins = eng.transpose(out, in_, ident)
        return ins
    k.op("pe", fn, reads, writes)


class Consts:
    pass


def make_consts(k, es):
    nc = k.nc
    c = Consts()
    c.ident = es.enter_context(nc.sbuf_tensor("ident", [128, 128], BF16))
    c.identf = es.enter_context(nc.sbuf_tensor("identf", [128, 128], F32))
    c.tri = es.enter_context(nc.sbuf_tensor("tri", [128, 128], BF16))
    c.nhalf = es.enter_context(nc.sbuf_tensor("nhalf", [128, 1], F32))
    c.ones = es.enter_context(nc.sbuf_tensor("onesb", [128, 128], BF16))
    io = es.enter_context(nc.sbuf_tensor("iota_i", [128, 128], I32))
    b_io, b_id, b_idf, b_tri, b_nh, b_on = (Buf() for _ in range(6))
    k.begin()
    k.op("pool", lambda e: e.iota(io[:, :], [[1, 128]], base=0, channel_multiplier=-1), (), (b_io,))
    k.op("dve", lambda e: e.tensor_scalar(out=c.ident[:, :], in0=io[:, :], scalar1=0.0, scalar2=None, op0=ALU.is_equal), (b_io,), (b_id,))
    k.op("dve", lambda e: e.tensor_scalar(out=c.identf[:, :], in0=io[:, :], scalar1=0.0, scalar2=None, op0=ALU.is_equal), (b_io,), (b_idf,))
    k.op("dve", lambda e: e.tensor_scalar(out=c.tri[:, :], in0=io[:, :], scalar1=0.0, scalar2=None, op0=ALU.is_gt), (b_io,), (b_tri,))
    k.op("dve", lambda e: e.memset(c.nhalf[:, :], -0.5), (), (b_nh,))
    k.op("dve", lambda e: e.memset(c.ones[:, :], 1.0), (), (b_on,))
    k.end("consts")
    return c


def rms_stats(k, c, x_ap, junk_ap, ss, rstd, bx, bj, bss, brs, n=1024):
    k.op("act", lambda e: e.activation(out=junk_ap, in_=x_ap, func=AF.Square, accum_out=ss), (bx,), (bj, bss))
    k.op("pool", lambda e: e.tensor_scalar(out=rstd, in0=ss, scalar1=1.0 / n, scalar2=EPS, op0=ALU.mult, op1=ALU.add), (bss,), (brs,))
    k.op("pool", lambda e: e.tensor_tensor(out=rstd, in0=rstd, in1=c.nhalf[:, :], op=ALU.pow), (brs,), (brs,))


def load_w_scaled(k, ps, w_d, kt, ncols, g_d, name, Wt, bW, half=2048, order=None):
    nc = k.nc
    gcol = ps.enter_context(nc.sbuf_tensor(name + "_g", [128, kt], F32))
    bg = Buf()
    k.dma("sp", gcol[:, :], g_d.rearrange("(f p) -> p f", p=128), bg, (), (bg,), allow_slow_non_contiguous=True)
    half = min(half, ncols)
    stg = [ps.enter_context(nc.sbuf_tensor(name + "_s%d" % i, [128, half], F32)) for i in range(2)]
    bs = [Buf(), Buf()]
    if isinstance(bW, list):
        assert half == 512
        pieces = [(f, ci * 512) for ci in (order or range(ncols // 512)) for f in range(kt)]
    else:
        pieces = [(f, c0) for f in range(kt) for c0 in range(0, ncols, half)]
    for i, (f, c0) in enumerate(pieces):
        s, b = stg[i % 2], bs[i % 2]
        bw = bW[c0 // 512] if isinstance(bW, list) else bW
        k.dma("sp", s[:, :], w_d[f * 128:(f + 1) * 128, c0:c0 + half], b, (), (b,))
        k.op("dve", lambda e, s=s, f=f, c0=c0: e.tensor_scalar(out=Wt[:, f, c0:c0 + half], in0=s[:, :], scalar1=gcol[:, f:f + 1], scalar2=None, op0=ALU.mult), (b, bg), (bw,))
    return stg, bs


def load_w_cast(k, ps, w_d, kt, ncols, name, Wt, bW, eng="act"):
    nc = k.nc
    half = 2048 if ncols > 2048 else ncols
    stg = [ps.enter_context(nc.sbuf_tensor(name + "_s%d" % i, [128, half], F32)) for i in range(2)]
    bs = [Buf(), Buf()]
    i = 0
    for f in range(kt):
        for c0 in range(0, ncols, half):
            s, b = stg[i % 2], bs[i % 2]
            k.dma("sp", s[:, :], w_d[f * 128:(f + 1) * 128, c0:c0 + half], b, (), (b,))
            if eng == "act":
                k.op("act", lambda e, s=s, f=f, c0=c0: e.copy(out=Wt[:, f, c0:c0 + half], in_=s[:, :]), (b,), (bW,))
            else:
                k.op("dve", lambda e, s=s, f=f, c0=c0: e.tensor_copy(out=Wt[:, f, c0:c0 + half], in_=s[:, :]), (b,), (bW,))
            i += 1


def phase_A(k, c, io):
    nc = k.nc
    with ExitStack() as ps:
        k.begin()
        sb = lambda n, s, d: ps.enter_context(nc.sbuf_tensor(n, s, d))
        W = sb("A_W", [128, 8, 4096], BF16)
        bWs = [Buf() for _ in range(8)]
        load_w_scaled(k, ps, io["w_in"], 8, 4096, io["g_pre_mix"], "A_w", W, bWs, half=512, order=[0, 3, 1, 2, 4, 5, 6, 7])
        xt = [sb("A_x%d" % i, [128, 1024], F32) for i in range(4)]
        bx = [Buf() for _ in range(4)]
        junk = sb("A_junk", [128, 1024], BF16)
        bj = Buf()
        ss = [sb("A_ss%d" % i, [128, 1], F32) for i in range(4)]
        rs = [sb("A_rs%d" % i, [128, 1], F32) for i in range(4)]
        bss = [Buf() for _ in range(4)]
        brs = [Buf() for _ in range(4)]
        hb = [sb("A_hb%d" % i, [128, 1024], BF16) for i in range(2)]
        bhb = [Buf(), Buf()]
        pT = [ps.enter_context(nc.psum_tensor("A_pT%d" % i, [128, 1024], BF16)) for i in range(2)]
        bpT = [Buf(), Buf()]
        hT = [sb("A_hT%d" % i, [128, 8, 512], BF16) for i in range(2)]
        bhT = [Buf(), Buf()]
        pm = [ps.enter_context(nc.psum_tensor("A_pm%d" % i, [128, 512], F32)) for i in range(6)]
        bpm = [Buf() for _ in range(6)]
        zst = [sb("A_z%d" % i, [128, 1024], BF16) for i in range(2)]
        bz = [Buf(), Buf()]
        fst = [sb("A_f%d" % i, [128, 8, 512], BF16) for i in range(3)]
        bf = [Buf() for _ in range(3)]
        ipm = 0
        ifs = 0
        for ch in range(T // 512):
            hTc, bhTc = hT[ch % 2], bhT[ch % 2]
            for j in range(4):
                t = ch * 4 + j
                xi = t % 4
                k.dma("sp", xt[xi][:, :], io["x"][t * 128:(t + 1) * 128, :], bx[xi], (), (bx[xi],))
                rms_stats(k, c, xt[xi][:, :], junk[:, :], ss[xi][:, :], rs[xi][:, :], bx[xi], bj, bss[xi], brs[xi])
                hbi = t % 2
                k.op("act", lambda e, xi=xi, hbi=hbi: e.activation(out=hb[hbi][:, :], in_=xt[xi][:, :], func=AF.Copy, scale=rs[xi][:, :]), (bx[xi], brs[xi]), (bhb[hbi],))
                tr(k, [(pT[hbi][:, f * 128:(f + 1) * 128], hb[hbi][:, f * 128:(f + 1) * 128], c.ident[:, :]) for f in range(8)], (bhb[hbi],), (bpT[hbi],))
                k.op("dve", lambda e, hbi=hbi, j=j, hTc=hTc: e.tensor_copy(out=hTc[:, :, j * 128:(j + 1) * 128], in_=pT[hbi][:, :].rearrange("p (f t) -> p f t", f=8)), (bpT[hbi],), (bhTc,))
            for j in range(4):
                t = ch * 4 + j
                zi = t % 2
                for n, c0 in enumerate((0, 1536)):
                    p = ipm % 6
                    ipm += 1
                    mm(k, [(pm[p][:, :], hTc[:, f, j * 128:(j + 1) * 128], W[:, f, c0:c0 + 512], f == 0, f == 7) for f in range(8)], (bhTc, bWs[c0 // 512]), (bpm[p],))
                    k.op("dve", lambda e, p=p, zi=zi, n=n: e.tensor_copy(out=zst[zi][:, n * 512:(n + 1) * 512], in_=pm[p][:, :]), (bpm[p],), (bz[zi],))
                k.dma("pool", io["z_s"][t * 128:(t + 1) * 128, :], zst[zi][:, :], bz[zi], (bz[zi],), ())
            for m in range(24):
                c0 = 512 + m * 128 if m < 8 else 2048 + (m - 8) * 128
                p = ipm % 6
                ipm += 1
                mm(k, [(pm[p][:, :], W[:, f, c0:c0 + 128], hTc[:, f, :], f == 0, f == 7) for f in range(8)], (bhTc, bWs[c0 // 512]), (bpm[p],))
                fi = ifs % 3
                if m < 8:
                    k.op("dve", lambda e, p=p, fi=fi, m=m: e.tensor_copy(out=fst[fi][:, m % 8, :], in_=pm[p][:, :]), (bpm[p],), (bf[fi],))
                else:
                    k.op("act", lambda e, p=p, fi=fi, m=m: e.activation(out=fst[fi][:, m % 8, :], in_=pm[p][:, :], func=AF.Sigmoid), (bpm[p],), (bf[fi],))
                if m % 8 == 7:
                    r0 = (m // 8) * 1024
                    k.dma("pool", io["fm_s"][r0:r0 + 1024, ch * 512:(ch + 1) * 512].rearrange("(m p) t -> p m t", p=128), fst[fi][:, :, :], bf[fi], (bf[fi],), ())
                    ifs += 1
        k.end("A")


IN_SPECS = [
    ("x", [T, D]), ("p", [T, PLE]), ("g_pre_mix", [D]), ("w_in", [D, 4096]),
    ("ssm_lam_re", [G, PST]), ("ssm_lam_im", [G, PST]), ("ssm_log_dt", [G]),
    ("ssm_b_re", [G, PST, 16]), ("ssm_b_im", [G, PST, 16]),
    ("ssm_c_re", [G, 16, PST]), ("ssm_c_im", [G, 16, PST]), ("ssm_d", [512]),
    ("w_glu", [512, 512]), ("b_glu", [512]), ("w_branch_a", [512, D]), ("w_branch_b", [512, D]),
    ("w_out", [D, D]), ("g_post_mix", [D]), ("g_pre_mlp", [D]), ("w_mlp1", [D, DFF]),
    ("w_mlp2", [DFF, D]), ("g_post_mlp", [D]), ("w_ple", [PLE, D]), ("w_ple_gate", [D, D]),
    ("g_ple", [D]),
]
SCRATCH = [
    ("z_s", [T, 1024], BF16),
    ("fm_s", [3072, T], BF16),
    ("s5_s", [512, T], BF16),
    ("at_s", [T, 512], BF16),
    ("x1_s", [T, D], F32),
    ("x2_s", [T, D], F32),
]


def build(phases="ASCDEF", debug=(), inject=()):
    nc = bass.Bass("TRN2", target_bir_lowering=False)
    io = {}
    for name, shape in IN_SPECS:
        io[name] = nc.dram_tensor(name, shape, F32, kind="ExternalInput").ap()
    for name, shape, dt in SCRATCH:
        kind = "ExternalOutput" if name in debug else ("ExternalInput" if name in inject else "Internal")
        io[name] = nc.dram_tensor(name, shape, dt, kind=kind).ap()
    io["out"] = nc.dram_tensor("out", [T, D], F32, kind="ExternalOutput").ap()
    with ExitStack() as es:
        k = K(nc, es)
        c = make_consts(k, es)
        if "A" in phases:
            phase_A(k, c, io)
        if "S" in phases:
            phase_S(k, c, io)
        if "C" in phases:
            phase_C(k, c, io)
        if "D" in phases:
            phase_D(k, c, io)
        if "E" in phases:
            phase_E(k, c, io)
        if "F" in phases:
            phase_F(k, c, io)
    return nc


def make_in_maps(inputs, ncores=NCORES):
    maps = []
    for ci in range(ncores):
        m = {}
        for name, shape in IN_SPECS:
            a = inputs[name]
            if name == "x":
                a = a[ci * BPC:(ci + 1) * BPC].reshape(T, D)
            elif name == "p":
                a = a[0, ci * BPC:(ci + 1) * BPC].reshape(T, PLE)
            else:
                a = a[0]
            m[name] = np.ascontiguousarray(a, dtype=np.float32).reshape(shape)
        maps.append(m)
    return maps


def kernel(**inputs):
    inputs = {k_: np.asarray(v) for k_, v in inputs.items()}
    nc = build()
    in_maps = make_in_maps(inputs)
    res = run_bass_kernel_spmd(nc, in_maps, core_ids=list(range(NCORES)))
    outs = [np.asarray(r["out"]).reshape(BPC, SEQ, D) for r in res.results]
    return np.concatenate(outs, axis=0).astype(np.float32)


def load_gb(k, ps, g_d, name):
    nc = k.nc
    gb = ps.enter_context(nc.sbuf_tensor(name, [128, D], F32))
    b = Buf()
    k.dma("sp", gb[:, :], g_d.partition_broadcast(128), b, (), (b,))
    return gb, b


class TailBufs:
    def __init__(self, k, sb, name, n=2, inplace=False):
        self.n = n
        self.inplace = inplace
        self.junk = [sb(name + "_junk%d" % i, [128, 1024], BF16) for i in range(n)]
        self.ss = [sb(name + "_tss%d" % i, [128, 1], F32) for i in range(n)]
        self.rs = [sb(name + "_trs%d" % i, [128, 1], F32) for i in range(n)]
        self.tmp = [sb(name + "_ttmp%d" % i, [128, 1024], F32) for i in range(n)]
        self.ost = self.tmp if inplace else [sb(name + "_tost%d" % i, [128, 1024], F32) for i in range(n)]
        self.b = [[Buf() for _ in range(5)] for _ in range(n)]


def norm_res_tail(k, c, tb, i, src_ap, bsrc, gb, bgb, xres, bxres, out_rows):
    i = i % tb.n
    bj, bss, brs, btmp, bost = tb.b[i]
    junk, ss, rs, tmp, ost = tb.junk[i], tb.ss[i], tb.rs[i], tb.tmp[i], tb.ost[i]
    rms_stats(k, c, src_ap, junk[:, :], ss[:, :], rs[:, :], bsrc, bj, bss, brs)
    k.op("dve", lambda e: e.scalar_tensor_tensor(out=tmp[:, :], in0=src_ap, scalar=rs[:, :], in1=gb[:, :], op0=ALU.mult, op1=ALU.mult), (bsrc, brs, bgb), (btmp,))
    if tb.inplace:
        bost = btmp
    k.op("pool", lambda e: e.tensor_tensor(out=ost[:, :], in0=tmp[:, :], in1=xres[:, :], op=ALU.add), (btmp, bxres), (bost,))
    k.dma("pool", out_rows, ost[:, :], bost, (bost,), ())


def phase_D(k, c, io):
    nc = k.nc
    with ExitStack() as ps:
        k.begin()
        sb = lambda n, s, d: ps.enter_context(nc.sbuf_tensor(n, s, d))
        Wa = sb("D_Wa", [128, 4, 1024], BF16)
        Wb = sb("D_Wb", [128, 4, 1024], BF16)
        Wo = sb("D_Wo", [128, 8, 1024], BF16)
        bWa, bWb, bWo = Buf(), Buf(), Buf()
        load_w_cast(k, ps, io["w_branch_a"], 4, 1024, "D_wa", Wa, bWa, "dve")
        load_w_cast(k, ps, io["w_branch_b"], 4, 1024, "D_wb", Wb, bWb, "act")
        load_w_cast(k, ps, io["w_out"], 8, 1024, "D_wo", Wo, bWo, "dve")
        g2, bg2 = load_gb(k, ps, io["g_post_mix"], "D_g2")
        gts = [sb("D_gt%d" % i, [128, 16, 512], BF16) for i in range(2)]
        bgt = [Buf(), Buf()]
        s5t = [sb("D_s5%d" % i, [128, 4, 512], BF16) for i in range(2)]
        bs5 = [Buf(), Buf()]
        att = [sb("D_at%d" % i, [128, 512], BF16) for i in range(4)]
        bat = [Buf() for _ in range(4)]
        atTs = [sb("D_atT%d" % i, [128, 4, 512], BF16) for i in range(2)]
        batTs = [Buf(), Buf()]
        pT = ps.enter_context(nc.psum_tensor("D_pT", [128, 1024], BF16))
        bpT = Buf()
        pm = [ps.enter_context(nc.psum_tensor("D_pm%d" % i, [128, 512], F32)) for i in range(3)]
        bpm = [Buf() for _ in range(3)]
        pmx = [ps.enter_context(nc.psum_tensor("D_px%d" % i, [128, 1024], F32)) for i in range(2)]
        bpx = [Buf(), Buf()]
        t1 = [sb("D_t1%d" % i, [128, 512], F32) for i in range(2)]
        bt1 = [Buf(), Buf()]
        t2 = [sb("D_t2%d" % i, [128, 512], F32) for i in range(2)]
        bt2 = [Buf(), Buf()]
        mTs = [sb("D_mT%d" % i, [128, 8, 512], BF16) for i in range(2)]
        bmTs = [Buf(), Buf()]
        xt = [sb("D_x%d" % i, [128, 1024], F32) for i in range(2)]
        bx = [Buf(), Buf()]
        tbf = TailBufs(k, sb, "D")
        ipm = [0]
        xt3 = xt + [sb("D_x2", [128, 1024], F32)]
        bx3 = bx + [Buf()]

        def head(ch):
            gi = ch % 2
            atT, batT = atTs[gi], batTs[gi]
            cs = slice(ch * 512, (ch + 1) * 512)
            k.dma("sp", gts[gi][:, :, :], io["fm_s"][1024:3072, cs].rearrange("(m p) t -> p m t", p=128), bgt[gi], (), (bgt[gi],))
            k.dma("sp", s5t[gi][:, :, :], io["s5_s"][:, cs].rearrange("(m p) t -> p m t", p=128), bs5[gi], (), (bs5[gi],))
            for j in range(4):
                t = ch * 4 + j
                k.dma("sp", att[j][:, :], io["at_s"][t * 128:(t + 1) * 128, :], bat[j], (), (bat[j],))
            for j in range(4):
                tr(k, [(pT[:, f * 128:(f + 1) * 128], att[j][:, f * 128:(f + 1) * 128], c.ident[:, :]) for f in range(4)], (bat[j],), (bpT,))
                k.op("dve", lambda e, j=j: e.tensor_copy(out=atT[:, :, j * 128:(j + 1) * 128], in_=pT[:, 0:512].rearrange("p (f t) -> p f t", f=4)), (bpT,), (batT,))

        def gate(ch):
            gi = ch % 2
            atT, batT = atTs[gi], batTs[gi]
            mT, bmT = mTs[gi], bmTs[gi]
            for m in range(8):
                p = ipm[0] % 3
                ipm[0] += 1
                mm(k, [(pm[p][:, :], Wa[:, f, m * 128:(m + 1) * 128], s5t[gi][:, f, :], f == 0, f == 3) for f in range(4)], (bs5[gi], bWa), (bpm[p],))
                i1 = m % 2
                k.op("dve", lambda e, p=p, i1=i1, m=m: e.tensor_tensor(out=t1[i1][:, :], in0=pm[p][:, :], in1=gts[gi][:, m, :], op=ALU.mult), (bpm[p], bgt[gi]), (bt1[i1],))
                p2 = ipm[0] % 3
                ipm[0] += 1
                mm(k, [(pm[p2][:, :], Wb[:, f, m * 128:(m + 1) * 128], atT[:, f, :], f == 0, f == 3) for f in range(4)], (batT, bWb), (bpm[p2],))
                k.op("dve", lambda e, p2=p2, i1=i1, m=m: e.tensor_tensor(out=t2[i1][:, :], in0=pm[p2][:, :], in1=gts[gi][:, 8 + m, :], op=ALU.mult), (bpm[p2], bgt[gi]), (bt2[i1],))
                k.op("pool", lambda e, i1=i1, m=m: e.tensor_tensor(out=mT[:, m, :], in0=t1[i1][:, :], in1=t2[i1][:, :], op=ALU.add), (bt1[i1], bt2[i1]), (bmT,))

        def mm2(t):
            ch, j = t // 4, t % 4
            mT, bmT = mTs[ch % 2], bmTs[ch % 2]
            xi, x3 = t % 2, t % 3
            k.dma("sp", xt3[x3][:, :], io["x"][t * 128:(t + 1) * 128, :], bx3[x3], (), (bx3[x3],))
            for n in range(2):
                mm(k, [(pmx[xi][:, n * 512:(n + 1) * 512], mT[:, f, j * 128:(j + 1) * 128], Wo[:, f, n * 512:(n + 1) * 512], f == 0, f == 7) for f in range(8)], (bmT, bWo), (bpx[xi],))

        def tail(t):
            xi, x3 = t % 2, t % 3
            norm_res_tail(k, c, tbf, t, pmx[xi][:, :], bpx[xi], g2, bg2, xt3[x3], bx3[x3], io["x1_s"][t * 128:(t + 1) * 128, :])

        NCH = T // 512
        head(0)
        for ch in range(NCH):
            gate(ch)
            for j in range(4):
                t = ch * 4 + j
                mm2(t)
                if t >= 1:
                    tail(t - 1)
                if j == 1 and ch + 1 < NCH:
                    head(ch + 1)
        tail(NTT - 1)
        k.end("D")


def phase_E(k, c, io):
    nc = k.nc
    CH = 512
    with ExitStack() as ps:
        k.begin()
        sb = lambda n, s, d: ps.enter_context(nc.sbuf_tensor(n, s, d))
        W1 = sb("E_W1", [128, 8, 4096], BF16)
        W2 = sb("E_W2", [128, 32, 1024], BF16)
        bW1s, bW2 = [Buf() for _ in range(8)], Buf()
        stg, bstg = load_w_scaled(k, ps, io["w_mlp1"], 8, 4096, io["g_pre_mlp"], "E_w1", W1, bW1s, half=512)
        for f2 in range(64):
            f, hf = f2 // 2, f2 % 2
            s, b = stg[f2 % 2], bstg[f2 % 2]
            k.dma("sp", s[:, :], io["w_mlp2"][f * 128:(f + 1) * 128, hf * 512:(hf + 1) * 512], b, (), (b,))
            if f2 % 2:
                k.op("act", lambda e, s=s, f=f, hf=hf: e.copy(out=W2[:, f, hf * 512:(hf + 1) * 512], in_=s[:, :]), (b,), (bW2,))
            else:
                k.op("dve", lambda e, s=s, f=f, hf=hf: e.tensor_copy(out=W2[:, f, hf * 512:(hf + 1) * 512], in_=s[:, :]), (b,), (bW2,))
        g4, bg4 = load_gb(k, ps, io["g_post_mlp"], "E_g4")
        xt = [sb("E_x%d" % i, [128, 1024], F32) for i in range(4)]
        bx = [Buf() for _ in range(4)]
        junk = sb("E_junk", [128, 1024], BF16)
        bj = Buf()
        ss = [sb("E_ss%d" % i, [128, 1], F32) for i in range(2)]
        rs = [sb("E_rs%d" % i, [128, 1], F32) for i in range(2)]
        bss = [Buf(), Buf()]
        brs = [Buf(), Buf()]
        hb = [sb("E_hb0", [128, 1024], BF16)] * 2
        bhb = [Buf()] * 2
        pT = ps.enter_context(nc.psum_tensor("E_pT", [128, 1024], BF16))
        bpT = Buf()
        hT = sb("E_hT", [128, 8, CH], BF16)
        bhT = Buf()
        pm = [ps.enter_context(nc.psum_tensor("E_pm%d" % i, [128, 512], F32)) for i in range(3)]
        bpm = [Buf() for _ in range(3)]
        pmx = [ps.enter_context(nc.psum_tensor("E_px%d" % i, [128, 1024], F32)) for i in range(2)]
        bpx = [Buf(), Buf()]
        rl = [sb("E_rl0", [128, CH], F32)] * 2
        brl = [Buf()] * 2
        f1T = sb("E_f1T", [128, 32, CH], BF16)
        bf1 = Buf()
        tbf = TailBufs(k, sb, "E", n=1, inplace=True)
        ipm = 0
        nj = CH // 128
        for ch in range(T // CH):
            for j in range(nj):
                t = ch * nj + j
                xi = t % 4
                hi = t % 2
                k.dma("sp", xt[xi][:, :], io["x1_s"][t * 128:(t + 1) * 128, :], bx[xi], (), (bx[xi],))
                rms_stats(k, c, xt[xi][:, :], junk[:, :], ss[hi][:, :], rs[hi][:, :], bx[xi], bj, bss[hi], brs[hi])
                k.op("act", lambda e, xi=xi, hi=hi: e.activation(out=hb[hi][:, :], in_=xt[xi][:, :], func=AF.Copy, scale=rs[hi][:, :]), (bx[xi], brs[hi]), (bhb[hi],))
                tr(k, [(pT[:, f * 128:(f + 1) * 128], hb[hi][:, f * 128:(f + 1) * 128], c.ident[:, :]) for f in range(8)], (bhb[hi],), (bpT,))
                k.op("dve", lambda e, j=j: e.tensor_copy(out=hT[:, :, j * 128:(j + 1) * 128], in_=pT[:, :].rearrange("p (f t) -> p f t", f=8)), (bpT,), (bhT,))
            for m in range(32):
                p = ipm % 3
                ipm += 1
                mm(k, [(pm[p][:, 0:CH], W1[:, f, m * 128:(m + 1) * 128], hT[:, f, :], f == 0, f == 7) for f in range(8)], (bhT, bW1s[m // 4]), (bpm[p],))
                ri = m % 2
                k.op("act", lambda e, p=p, ri=ri: e.activation(out=rl[ri][:, :], in_=pm[p][:, 0:CH], func=AF.Relu), (bpm[p],), (brl[ri],))
                k.op("pool", lambda e, ri=ri, m=m: e.tensor_tensor(out=f1T[:, m, :], in0=rl[ri][:, :], in1=rl[ri][:, :], op=ALU.mult), (brl[ri],), (bf1,))
            for j in range(nj):
                t = ch * nj + j
                xi = t % 4
                oi = t % 2
                for n in range(2):
                    mm(k, [(pmx[oi][:, n * 512:(n + 1) * 512], f1T[:, f, j * 128:(j + 1) * 128], W2[:, f, n * 512:(n + 1) * 512], f == 0, f == 31) for f in range(32)], (bf1, bW2), (bpx[oi],))
                norm_res_tail(k, c, tbf, t, pmx[oi][:, :], bpx[oi], g4, bg4, xt[xi], bx[xi], io["x2_s"][t * 128:(t + 1) * 128, :])
        k.end("E")


def phase_F(k, c, io):
    nc = k.nc
    with ExitStack() as ps:
        k.begin()
        sb = lambda n, s, d: ps.enter_context(nc.sbuf_tensor(n, s, d))
        Wp = sb("F_Wp", [128, 2, 1024], BF16)
        Wg = sb("F_Wg", [128, 8, 1024], BF16)
        bWp, bWg = Buf(), Buf()
        load_w_cast(k, ps, io["w_ple"], 2, 1024, "F_wp", Wp, bWp, "dve")
        load_w_cast(k, ps, io["w_ple_gate"], 8, 1024, "F_wg", Wg, bWg, "act")
        g5, bg5 = load_gb(k, ps, io["g_ple"], "F_g5")
        NB = 3
        xt = [sb("F_x%d" % i, [128, 1024], F32) for i in range(4)]
        bx = [Buf() for _ in range(4)]
        pt = [sb("F_p%d" % i, [128, 256], F32) for i in range(NB)]
        bp = [Buf() for _ in range(NB)]
        xb = [sb("F_xb%d" % i, [128, 1024], BF16) for i in range(NB)]
        bxb = [Buf() for _ in range(NB)]
        pb = [sb("F_pb%d" % i, [128, 256], BF16) for i in range(NB)]
        bpb = [Buf() for _ in range(NB)]
        pTx = ps.enter_context(nc.psum_tensor("F_pTx", [128, 1024], BF16))
        pTp = ps.enter_context(nc.psum_tensor("F_pTp", [128, 1024], BF16))
        bpTx, bpTp = Buf(), Buf()
        xT = [sb("F_xT%d" % i, [128, 8, 128], BF16) for i in range(NB)]
        bxT = [Buf() for _ in range(NB)]
        pT = [sb("F_pT%d" % i, [128, 2, 128], BF16) for i in range(NB)]
        bpT = [Buf() for _ in range(NB)]
        ppw = ps.enter_context(nc.psum_tensor("F_ppw", [128, 1024], F32))
        bppw = Buf()
        psg = [ps.enter_context(nc.psum_tensor("F_psg%d" % i, [128, 1024], F32)) for i in range(2)]
        bpsg = [Buf(), Buf()]
        sgss = [sb("F_sgs%d" % i, [128, 1024], F32) for i in range(2)]
        bsgss = [Buf(), Buf()]
        ees = [sb("F_e%d" % i, [128, 1024], F32) for i in range(2)]
        bees = [Buf(), Buf()]
        tbf = TailBufs(k, sb, "F")

        def head(t):
            xi, i3 = t % 4, t % NB
            k.dma("sp", xt[xi][:, :], io["x2_s"][t * 128:(t + 1) * 128, :], bx[xi], (), (bx[xi],))
            k.dma("sp", pt[i3][:, :], io["p"][t * 128:(t + 1) * 128, :], bp[i3], (), (bp[i3],))
            k.op("act", lambda e: e.copy(out=xb[i3][:, :], in_=xt[xi][:, :]), (bx[xi],), (bxb[i3],))
            k.op("dve", lambda e: e.tensor_copy(out=pb[i3][:, :], in_=pt[i3][:, :]), (bp[i3],), (bpb[i3],))
            tr(k, [(pTx[:, f * 128:(f + 1) * 128], xb[i3][:, f * 128:(f + 1) * 128], c.ident[:, :]) for f in range(8)], (bxb[i3],), (bpTx,))
            k.op("dve", lambda e: e.tensor_copy(out=xT[i3][:, :, :], in_=pTx[:, :].rearrange("p (f t) -> p f t", f=8)), (bpTx,), (bxT[i3],))
            tr(k, [(pTp[:, f * 128:(f + 1) * 128], pb[i3][:, f * 128:(f + 1) * 128], c.ident[:, :]) for f in range(2)], (bpb[i3],), (bpTp,))
            k.op("dve", lambda e: e.tensor_copy(out=pT[i3][:, :, :], in_=pTp[:, 0:256].rearrange("p (f t) -> p f t", f=2)), (bpTp,), (bpT[i3],))

        def mid_sg(t):
            i2, i3 = t % 2, t % NB
            for n in range(2):
                mm(k, [(psg[i2][:, n * 512:(n + 1) * 512], xT[i3][:, f, :], Wg[:, f, n * 512:(n + 1) * 512], f == 0, f == 7) for f in range(8)], (bxT[i3], bWg), (bpsg[i2],))

        def mid_pw(t):
            i3 = t % NB
            for n in range(2):
                mm(k, [(ppw[:, n * 512:(n + 1) * 512], pT[i3][:, f, :], Wp[:, f, n * 512:(n + 1) * 512], f == 0, f == 1) for f in range(2)], (bpT[i3], bWp), (bppw,))

        def tail(t):
            xi, i2 = t % 4, t % 2
            sgs, bsgs, ee, bee = sgss[i2], bsgss[i2], ees[i2], bees[i2]
            k.op("act", lambda e: e.activation(out=sgs[:, :], in_=psg[i2][:, :], func=AF.Sigmoid), (bpsg[i2],), (bsgs,))
            k.op("dve", lambda e: e.tensor_tensor(out=ee[:, :], in0=ppw[:, :], in1=sgs[:, :], op=ALU.mult), (bppw, bsgs), (bee,))
            norm_res_tail(k, c, tbf, t, ee[:, :], bee, g5, bg5, xt[xi], bx[xi], io["out"][t * 128:(t + 1) * 128, :])

        head(0)
        head(1)
        for t in range(NTT):
            mid_sg(t)
            if t >= 1:
                tail(t - 1)
            mid_pw(t)
            if t + 2 < NTT:
                head(t + 2)
        tail(NTT - 1)
        k.end("F")


def phase_C(k, c, io):
    nc = k.nc
    with ExitStack() as ps:
        k.begin()
        sb = lambda n, s, d: ps.enter_context(nc.sbuf_tensor(n, s, d))
        ioi = sb("C_ioi", [128, 64], I32)
        io2 = sb("C_io2", [128, 256], I32)
        io3 = sb("C_io3", [128, 2048], I32)
        negp = sb("C_negp", [128, 8, 64], F32)
        cm = sb("C_cm", [128, 2, 256], BF16)
        oh = sb("C_oh", [128, 64 * 128], BF16)
        bio, bnegp, bcm, boh, bio3 = Buf(), Buf(), Buf(), Buf(), Buf()
        k.op("pool", lambda e: e.iota(ioi[:, :], [[0, 8], [1, 8]], base=0, channel_multiplier=0), (), (bio,))
        for qb in range(8):
            k.op("dve", lambda e, qb=qb: e.tensor_scalar(out=negp[:, qb, :], in0=ioi[:, :], scalar1=float(qb), scalar2=-1e30, op0=ALU.is_ge, op1=ALU.mult), (bio,), (bnegp,))
        for h2 in range(2):
            k.op("pool", lambda e, h2=h2: e.iota(io2[:, :], [[1, 256]], base=-128 * h2, channel_multiplier=-1), (bcm,), (bio,))
            k.op("dve", lambda e, h2=h2: e.tensor_scalar(out=cm[:, h2, :], in0=io2[:, :], scalar1=0.0, scalar2=NEG, op0=ALU.is_lt, op1=ALU.mult), (bio,), (bcm,))
        for q4 in range(4):
            k.op("pool", lambda e, q4=q4: e.iota(io3[:, :], [[1, 16], [0, 128]], base=16 * q4, channel_multiplier=-1), (boh,), (bio3,))
            k.op("dve", lambda e, q4=q4: e.tensor_scalar(out=oh[:, q4 * 2048:(q4 + 1) * 2048], in0=io3[:, :], scalar1=0.0, scalar2=None, op0=ALU.is_equal), (bio3,), (boh,))
        qT = [sb("C_qT%d" % i, [128, 4, SEQ], BF16) for i in range(2)]
        kTzs = [sb("C_kTz%d" % i, [128, 8, SEQ], BF16) for i in range(2)]
        Va = [sb("C_V%d" % i, [128, 16, 8, 66], BF16) for i in range(2)]
        bq, bv, bks = [Buf(), Buf()], [Buf(), Buf()], [Buf(), Buf()]
        for i in range(2):
            k.op("pool", lambda e, i=i: e.memset(kTzs[i][:, :, :], 0.0), (), (bks[i],))
            k.op("pool", lambda e, i=i: e.memset(Va[i][:, :, :, :], 1.0), (), (bv[i],))
        kms = sb("C_kms", [128, 64], F32)
        kmbs = [sb("C_kmb%d" % i, [128, 8, 8], BF16) for i in range(2)]
        bkms, bkmbs = Buf(), [Buf(), Buf()]
        pgb = ps.enter_context(nc.psum_tensor("C_pgb", [128, 512], F32))
        bpg = [Buf(), Buf()]
        bpbt = [Buf(), Buf()]
        gms = [sb("C_gm%d" % i, [128, 64], F32) for i in range(2)]
        cmps = [sb("C_cmp%d" % i, [128, 512], F32) for i in range(2)]
        ranks = [sb("C_rank%d" % i, [128, 64], F32) for i in range(2)]
        biasbs = [sb("C_biasb%d" % i, [128, 128], F32) for i in range(2)]
        bgms, bcmps, branks, bbiasbs = ([Buf(), Buf()] for _ in range(4))
        biasTs = [sb("C_biasT%d" % i, [128, SEQ], BF16) for i in range(2)]
        bbTs = [Buf(), Buf()]
        for i in range(2):
            k.op("pool", lambda e, i=i: e.memset(biasbs[i][:, :], 0.0), (), (bbiasbs[i],))
        pss = [ps.enter_context(nc.psum_tensor("C_ps%d" % i, [128, 512], F32)) for i in range(3)]
        bps = [Buf(), Buf(), Buf()]
        po = [[ps.enter_context(nc.psum_tensor("C_po%d%d" % (i, j), [128, 512], F32)) for j in range(2)] for i in range(2)]
        bpo = [[Buf(), Buf()], [Buf(), Buf()]]
        pe_ = [sb("C_pe%d" % i, [128, 256], BF16) for i in range(3)]
        bpe = [Buf() for _ in range(3)]
        rden = sb("C_rden", [128, 1], F32)
        brd = Buf()
        atm = [sb("C_atm%d" % i, [128, 16, 512], BF16) for i in range(2)]
        batm = [Buf(), Buf()]
        cnt = {"ips": 0, "ipe": 0, "ipo": 0}

        def load_seq(s):
            si = s % 2
            tb = s * SEQ
            kTz, bk = kTzs[si], bks[si]
            k.dma("sp", qT[si][:, :, :], io["fm_s"][0:512, tb:tb + SEQ].rearrange("(m p) t -> p m t", p=128), bq[si], (), (bq[si],))
            for h in range(8):
                hp = slice((h % 2) * 64, (h % 2) * 64 + 64)
                k.dma("sp", kTz[hp, h, :], io["fm_s"][512 + h * 64:512 + (h + 1) * 64, tb:tb + SEQ], bk, (), (bk,))
            for kt in range(16):
                k.dma("sp", Va[si][:, kt, :, 0:64], io["z_s"][tb + kt * 128:tb + (kt + 1) * 128, 512:1024].rearrange("p (h d) -> p h d", h=8), bv[si], (), (bv[si],))
            k.op("dve", lambda e: e.tensor_reduce(out=kms[:, :], in_=kTz[:, :, :].rearrange("p h (b t) -> p (h b) t", b=8), axis=AX.X, op=ALU.add), (bk,), (bkms,))
            k.op("dve", lambda e: e.tensor_copy(out=kmbs[si][:, :, :], in_=kms[:, :].rearrange("p (h b) -> p h b", h=8)), (bkms,), (bkmbs[si],))

        def gate_step(s, qt):
            si = s % 2
            g2 = qt % 2
            qb = qt // 2
            gm, cmp, rank, biasb = gms[g2], cmps[g2], ranks[g2], biasbs[g2]
            pg = pgb[:, g2 * 64:(g2 + 1) * 64]
            pbt = pgb[:, 256 + g2 * 128:256 + (g2 + 1) * 128]

            def gfn(eng):
                ins = None
                for h in range(8):
                    ins = eng.matmul(pg[:, h * 8:(h + 1) * 8], qT[si][:, h // 2, qt * 128:(qt + 1) * 128], kmbs[si][:, h, :], start=True, stop=True)
                return ins
            k.op("pe", gfn, (bq[si], bkmbs[si]), (bpg[g2],))
            k.op("dve", lambda e: e.tensor_tensor(out=gm[:, :], in0=pg, in1=negp[:, qb, :], op=ALU.add), (bpg[g2], bnegp), (bgms[g2],))
            g3 = gm[:, :].rearrange("p (h b) -> p h b", h=8)
            k.op("dve", lambda e: e.tensor_tensor(out=cmp[:, :].rearrange("p (h b c) -> p h b c", h=8, b=8), in0=g3.unsqueeze(2).broadcast_to([128, 8, 8, 8]), in1=g3.unsqueeze(3).broadcast_to([128, 8, 8, 8]), op=ALU.is_gt), (bgms[g2],), (bcmps[g2],))
            k.op("dve", lambda e: e.tensor_reduce(out=rank[:, :], in_=cmp[:, :].rearrange("p (a c) -> p a c", c=8), axis=AX.X, op=ALU.add), (bcmps[g2],), (branks[g2],))
            k.op("dve", lambda e: e.tensor_scalar(out=biasb[:, 0:64], in0=rank[:, :], scalar1=2.5, scalar2=NEG, op0=ALU.is_gt, op1=ALU.mult), (branks[g2],), (bbiasbs[g2],))
            tr(k, [(pbt, biasb[:, :], c.identf[:, :])], (bbiasbs[g2],), (bpbt[g2],))
            k.op("dve", lambda e: e.tensor_copy(out=biasTs[si][:, qt * 128:(qt + 1) * 128], in_=pbt), (bpbt[g2],), (bbTs[si],))

        def sweep(s, inserts):
            si = s % 2
            kTz, bk = kTzs[si], bks[si]
            biasT, bbT = biasTs[si], bbTs[si]
            units = [(h, qb, kt) for h in range(8) for qb in range(8) for kt in range(2 * qb + 2)]

            def score(u, p):
                h, qb, kt = u
                m = h // 2
                b = kt // 2
                qs = slice(qb * 256, (qb + 1) * 256)
                last = (b != qb) and (qb <= 3)
                first = (pss[p][:, 0:256], kTz[:, h, kt * 128:(kt + 1) * 128], qT[si][:, m, qs], True, last)
                if b == qb:
                    items = [first, (pss[p][:, 0:256], c.ident[:, :], cm[:, kt % 2, :], False, True)]
                    rd = (bk, bq[si], bcm)
                elif last:
                    items = [first]
                    rd = (bk, bq[si])
                else:
                    r = h * 8 + b
                    items = [first, (pss[p][:, 0:256], oh[:, r * 128:(r + 1) * 128], biasT[:, qs], False, True)]
                    rd = (bk, bq[si], boh, bbT)
                mm(k, items, rd, (bps[p],))

            ips = cnt["ips"]
            score(units[0], ips % 3)
            score(units[1], (ips + 1) % 3)
            oi = 0
            for ui, u in enumerate(units):
                h, qb, kt = u
                nkt = 2 * qb + 2
                p = ips % 3
                ips += 1
                if ui + 2 < len(units):
                    score(units[ui + 2], (ips + 1) % 3)
                if kt == 0:
                    oi = cnt["ipo"] % 2
                    cnt["ipo"] += 1
                e_i = cnt["ipe"] % 3
                cnt["ipe"] += 1
                k.op("act", lambda e, p=p, e_i=e_i: e.activation(out=pe_[e_i][:, :], in_=pss[p][:, 0:256], func=AF.Exp, scale=0.125), (bps[p],), (bpe[e_i],))
                mm(k, [(po[oi][qh][:, 0:65], pe_[e_i][:, qh * 128:(qh + 1) * 128], Va[si][:, kt, h, 0:65], kt == 0, kt == nkt - 1) for qh in range(2)], (bpe[e_i], bv[si]), (bpo[oi][0], bpo[oi][1]))
                if kt == nkt - 1:
                    for qh in range(2):
                        qt = qb * 2 + qh
                        k.op("dve", lambda e, oi=oi, qh=qh: e.reciprocal(out=rden[:, :], in_=po[oi][qh][:, 64:65]), (bpo[oi][qh],), (brd,))
                        k.op("dve", lambda e, oi=oi, qh=qh, qt=qt, h=h: e.tensor_scalar(out=atm[si][:, qt, h * 64:(h + 1) * 64], in0=po[oi][qh][:, 0:64], scalar1=rden[:, :], scalar2=None, op0=ALU.mult), (bpo[oi][qh], brd), (batm[si],))
                if ui in inserts:
                    inserts[ui]()
            cnt["ips"] = ips
            tb = s * SEQ
            k.dma("pool", io["at_s"][tb:tb + SEQ, :].rearrange("(q p) c -> p q c", p=128), atm[si][:, :, :], batm[si], (batm[si],), ())

        load_seq(0)
        for qt in range(16):
            gate_step(0, qt)
        for s in range(BPC):
            inserts = {}
            if s + 1 < BPC:
                inserts[8] = (lambda s=s: load_seq(s + 1))
                for qt in range(16):
                    inserts[40 + qt * 30] = (lambda s=s, qt=qt: gate_step(s + 1, qt))
            sweep(s, inserts)
        k.end("C")


TWO_PI = 2.0 * math.pi


def cis(k, scr, first, out_c, out_s, barg, bout):
    t, ti, y, m1 = scr
    bt = Buf()
    k.op("dve", lambda e: first(e, t), (barg,), (bt,))
    k.op("dve", lambda e: e.tensor_copy(out=ti, in_=t), (bt,), (bt,))
    k.op("dve", lambda e: e.tensor_copy(out=y, in_=ti), (bt,), (bt,))
    k.op("dve", lambda e: e.tensor_tensor(out=t, in0=t, in1=y, op=ALU.subtract), (bt,), (bt,))
    for shift, dst in ((0.0, out_s), (math.pi / 2, out_c)):
        k.op("dve", lambda e, shift=shift: e.tensor_scalar(out=y, in0=t, scalar1=TWO_PI, scalar2=shift, op0=ALU.mult, op1=ALU.add), (bt,), (bt,))
        k.op("dve", lambda e: e.tensor_scalar(out=m1, in0=y, scalar1=math.pi, scalar2=-TWO_PI, op0=ALU.is_gt, op1=ALU.mult), (bt,), (bt,))
        k.op("dve", lambda e: e.tensor_tensor(out=m1, in0=m1, in1=y, op=ALU.add), (bt,), (bt,))
        k.op("dve", lambda e: e.tensor_scalar(out=y, in0=y, scalar1=-math.pi, scalar2=TWO_PI, op0=ALU.is_lt, op1=ALU.mult), (bt,), (bt,))
        k.op("dve", lambda e: e.tensor_tensor(out=y, in0=m1, in1=y, op=ALU.add), (bt,), (bt,))
        k.op("dve", lambda e: e.tensor_scalar(out=y, in0=y, scalar1=math.pi, scalar2=-math.pi, op0=ALU.min, op1=ALU.max), (bt,), (bt,))
        k.op("act", lambda e, dst=dst: e.activation(out=dst, in_=y, func=AF.Sin), (bt,), (bout, bt))


def cmul(k, eng, out_r, out_i, ar, ai, br, bi, t1, t2, rd, wr, bt, neg_i=False):
    E = eng
    k.op(E, lambda e: e.tensor_tensor(out=t1, in0=ar, in1=br, op=ALU.mult), rd, (bt,))
    k.op(E, lambda e: e.tensor_tensor(out=t2, in0=ai, in1=bi, op=ALU.mult), rd + (bt,), (bt,))
    k.op(E, lambda e: e.tensor_tensor(out=out_r, in0=t1, in1=t2, op=ALU.subtract), (bt,), wr + (bt,))
    k.op(E, lambda e: e.tensor_tensor(out=t1, in0=ar, in1=bi, op=ALU.mult), rd + (bt,), (bt,))
    k.op(E, lambda e: e.tensor_tensor(out=t2, in0=ai, in1=br, op=ALU.mult), rd + (bt,), (bt,))
    if neg_i:
        k.op(E, lambda e: e.scalar_tensor_tensor(out=out_i, in0=t1, scalar=-1.0, in1=t2, op0=ALU.mult, op1=ALU.subtract), (bt,), wr + (bt,))
    else:
        k.op(E, lambda e: e.tensor_tensor(out=out_i, in0=t1, in1=t2, op=ALU.add), (bt,), wr + (bt,))


def phase_S(k, c, io):
    nc = k.nc
    with ExitStack() as outer:
        osb = lambda n, s, d: outer.enter_context(nc.sbuf_tensor(n, s, d))
        GT = osb("S_GT", [128, G, 2, 128], BF16)
        TT = osb("S_TT", [128, G, 2, 256], BF16)
        H = osb("S_H", [128, G, 256], BF16)
        Mt = osb("S_M", [128, 2, 2048], F32)
        Dt = osb("S_D", [128, 2, 2048], F32)
        s5_setup(k, c, io, GT, TT, H, Mt, Dt)
        s5_main(k, c, io, GT, TT, H, Mt, Dt)


def s5_setup(k, c, io, GT, TT, H, Mt, Dt):
    nc = k.nc
    with ExitStack() as ps:
        k.begin()
        sb = lambda n, s, d: ps.enter_context(nc.sbuf_tensor(n, s, d))
        sb2 = lambda n, s, d: ps.enter_context(nc.sbuf_tensor(n, [s[0], int(np.prod(s[1:]))], d))
        B0 = Buf()
        lg = sb("u_lg", [32, 128], F32)
        k.dma("sp", lg[:, 0:64], io["ssm_lam_re"], B0, (), (B0,))
        k.dma("sp", lg[:, 64:128], io["ssm_lam_im"], B0, (), (B0,))
        ldt = sb("u_ldt", [64, 32], F32)
        k.dma("sp", ldt[:, :], io["ssm_log_dt"].partition_broadcast(64), B0, (), (B0,))
        Bre = sb("u_Bre", [64, G, 16], F32)
        Bim = sb("u_Bim", [64, G, 16], F32)
        k.dma("sp", Bre[:, :, :], io["ssm_b_re"].rearrange("g p c -> p g c"), B0, (), (B0,))
        k.dma("sp", Bim[:, :, :], io["ssm_b_im"].rearrange("g p c -> p g c"), B0, (), (B0,))
        cg = [sb("u_cg%d" % i, [128, 4, 64], F32) for i in range(2)]
        k.dma("sp", cg[0][:, :, :], io["ssm_c_re"].rearrange("(a b) c p -> (b c) a p", a=4), B0, (), (B0,))
        k.dma("sp", cg[1][:, :, :], io["ssm_c_im"].rearrange("(a b) c p -> (b c) a p", a=4), B0, (), (B0,))
        dcol = sb("u_dcol", [128, G], F32)
        for j in range(8):
            k.dma("sp", dcol[j * 16:(j + 1) * 16, :], io["ssm_d"].rearrange("(g c) -> c g", c=16), B0, (), (B0,), allow_slow_non_contiguous=True)
        pp = ps.enter_context(nc.psum_tensor("u_pp", [128, 512], F32))
        pq = ps.enter_context(nc.psum_tensor("u_pq", [128, 512], F32))
        Bp = Buf()
        lre = sb("u_lre", [64, G], F32)
        lim = sb("u_lim", [64, G], F32)
        tr(k, [(pp[0:64, 0:32], lg[:, 0:64], c.identf[0:32, 0:32]), (pp[0:64, 32:64], lg[:, 64:128], c.identf[0:32, 0:32])], (B0,), (Bp,))
        k.op("dve", lambda e: e.tensor_copy(out=lre[:, :], in_=pp[0:64, 0:32]), (Bp,), (B0,))
        k.op("dve", lambda e: e.tensor_copy(out=lim[:, :], in_=pp[0:64, 32:64]), (Bp,), (B0, Bp))
        Cre = sb("u_Cre", [64, G, 16], F32)
        Cim = sb("u_Cim", [64, G, 16], F32)
        for i, Cx in enumerate((Cre, Cim)):
            tr(k, [(pp[0:64, a * 128:(a + 1) * 128], cg[i][:, a, :], c.identf[:, :]) for a in range(4)], (B0,), (Bp,))
            k.op("dve", lambda e, Cx=Cx: e.tensor_copy(out=Cx[:, :, :], in_=pp[0:64, :].rearrange("p (g c) -> p g c", c=16)), (Bp,), (B0, Bp))
        dt = sb("u_dt", [64, G], F32)
        k.op("act", lambda e: e.activation(out=dt[:, :], in_=ldt[:, :], func=AF.Exp), (B0,), (B0,))
        rho = sb("u_rho", [64, G], F32)
        th = sb("u_th", [64, G], F32)
        k.op("dve", lambda e: e.tensor_tensor(out=rho[:, :], in0=lre[:, :], in1=dt[:, :], op=ALU.mult), (B0,), (B0,))
        k.op("dve", lambda e: e.tensor_tensor(out=th[:, :], in0=lim[:, :], in1=dt[:, :], op=ALU.mult), (B0,), (B0,))
        nvi = sb("u_nvi", [64, 17], I32)
        nv = sb("u_nv", [64, 17], F32)
        k.op("pool", lambda e: e.iota(nvi[:, :], [[1, 17]], base=0, channel_multiplier=0), (B0,), (B0,))
        k.op("dve", lambda e: e.tensor_copy(out=nv[:, :], in_=nvi[:, :]), (B0,), (B0,))
        NP = G * 17
        argt = sb("u_argt", [64, NP], F32)
        argr = sb("u_argr", [64, NP], F32)
        a3 = lambda t: t[:, :].rearrange("p (g n) -> p g n", n=17)
        bth = th[:, :].unsqueeze(2).broadcast_to([64, G, 17])
        brho = rho[:, :].unsqueeze(2).broadcast_to([64, G, 17])
        bnv = nv[:, :].unsqueeze(1).broadcast_to([64, G, 17])
        k.op("dve", lambda e: e.tensor_tensor(out=a3(argt), in0=bth, in1=bnv, op=ALU.mult), (B0,), (B0,))
        k.op("dve", lambda e: e.tensor_tensor(out=a3(argr), in0=brho, in1=bnv, op=ALU.mult), (B0,), (B0,))
        cs = sb("u_cs", [64, NP], F32)
        sn = sb("u_sn", [64, NP], F32)
        scr = (sb("u_c1", [64, NP], F32)[:, :], sb("u_c2", [64, NP], I32)[:, :], sb("u_c3", [64, NP], F32)[:, :], sb("u_c4", [64, NP], F32)[:, :])
        cis(k, scr, lambda e, t: e.tensor_scalar(out=t, in0=argt[:, :], scalar1=1.0 / TWO_PI, scalar2=None, op0=ALU.mult), cs[:, :], sn[:, :], B0, B0)
        mg = sb("u_mg", [64, NP], F32)
        mgi = sb("u_mgi", [64, NP], F32)
        k.op("act", lambda e: e.activation(out=mg[:, :], in_=argr[:, :], func=AF.Exp), (B0,), (B0,))
        k.op("act", lambda e: e.activation(out=mgi[:, :], in_=argr[:, :], func=AF.Exp, scale=-1.0), (B0,), (B0,))
        Pr = sb("u_Pr", [64, G, 17], F32)
        Pi = sb("u_Pi", [64, G, 17], F32)
        Qr = sb("u_Qr", [64, G, 17], F32)
        Qi = sb("u_Qi", [64, G, 17], F32)
        f2 = lambda t: t[:, :, :].rearrange("p g n -> p (g n)")
        k.op("dve", lambda e: e.tensor_tensor(out=f2(Pr), in0=mg[:, :], in1=cs[:, :], op=ALU.mult), (B0,), (B0,))
        k.op("dve", lambda e: e.tensor_tensor(out=f2(Pi), in0=mg[:, :], in1=sn[:, :], op=ALU.mult), (B0,), (B0,))
        k.op("dve", lambda e: e.tensor_tensor(out=f2(Qr), in0=mgi[:, :], in1=cs[:, :], op=ALU.mult), (B0,), (B0,))
        k.op("dve", lambda e: e.scalar_tensor_tensor(out=f2(Qi), in0=mgi[:, :], scalar=-1.0, in1=sn[:, :], op0=ALU.mult, op1=ALU.mult), (B0,), (B0,))
        den = sb("u_den", [64, G], F32)
        tA = sb("u_tA", [64, G], F32)
        tB = sb("u_tB", [64, G], F32)
        nr = sb("u_nr", [64, G], F32)
        cr = sb("u_cr", [64, G], F32)
        ci = sb("u_ci", [64, G], F32)
        k.op("dve", lambda e: e.tensor_tensor(out=den[:, :], in0=lre[:, :], in1=lre[:, :], op=ALU.mult), (B0,), (B0,))
        k.op("dve", lambda e: e.tensor_tensor(out=tA[:, :], in0=lim[:, :], in1=lim[:, :], op=ALU.mult), (B0,), (B0,))
        k.op("dve", lambda e: e.tensor_tensor(out=den[:, :], in0=den[:, :], in1=tA[:, :], op=ALU.add), (B0,), (B0,))
        k.op("dve", lambda e: e.reciprocal(out=den[:, :], in_=den[:, :]), (B0,), (B0,))
        k.op("dve", lambda e: e.tensor_scalar(out=nr[:, :], in0=Pr[:, :, 1], scalar1=-1.0, scalar2=None, op0=ALU.add), (B0,), (B0,))
        k.op("dve", lambda e: e.tensor_tensor(out=tA[:, :], in0=nr[:, :], in1=lre[:, :], op=ALU.mult), (B0,), (B0,))
        k.op("dve", lambda e: e.tensor_tensor(out=tB[:, :], in0=Pi[:, :, 1], in1=lim[:, :], op=ALU.mult), (B0,), (B0,))
        k.op("dve", lambda e: e.tensor_tensor(out=tA[:, :], in0=tA[:, :], in1=tB[:, :], op=ALU.add), (B0,), (B0,))
        k.op("dve", lambda e: e.tensor_tensor(out=cr[:, :], in0=tA[:, :], in1=den[:, :], op=ALU.mult), (B0,), (B0,))
        k.op("dve", lambda e: e.tensor_tensor(out=tA[:, :], in0=Pi[:, :, 1], in1=lre[:, :], op=ALU.mult), (B0,), (B0,))
        k.op("dve", lambda e: e.tensor_tensor(out=tB[:, :], in0=nr[:, :], in1=lim[:, :], op=ALU.mult), (B0,), (B0,))
        k.op("dve", lambda e: e.tensor_tensor(out=tA[:, :], in0=tA[:, :], in1=tB[:, :], op=ALU.subtract), (B0,), (B0,))
        k.op("dve", lambda e: e.tensor_tensor(out=ci[:, :], in0=tA[:, :], in1=den[:, :], op=ALU.mult), (B0,), (B0,))
        bbr = sb("u_bbr", [64, G, 16], F32)
        bbi = sb("u_bbi", [64, G, 16], F32)
        w1 = sb("u_w1", [64, G, 16], F32)
        w2 = sb("u_w2", [64, G, 16], F32)
        bc16 = lambda t: t[:, :].unsqueeze(2).broadcast_to([64, G, 16])
        cmul(k, "dve", bbr[:, :, :], bbi[:, :, :], bc16(cr), bc16(ci), Bre[:, :, :], Bim[:, :, :], w1[:, :, :], w2[:, :, :], (B0,), (B0,), B0)
        mki = sb("u_mki", [128, 2, 256], I32)
        mask = sb("u_mask", [128, 2, 256], F32)
        idc = sb("u_idc", [128, 2, 256], F32)
        shid = sb("u_shid", [64, 128], F32)
        for ch in range(2):
            k.op("pool", lambda e, ch=ch: e.iota(mki[:, ch, :], [[16, 16], [0, 16]], base=15 - 128 * ch, channel_multiplier=-1), (B0,), (B0,))
        k.op("dve", lambda e: e.tensor_scalar(out=mask[:, :, :], in0=mki[:, :, :], scalar1=0.0, scalar2=None, op0=ALU.is_ge), (B0,), (B0,))
        for ch in range(2):
            k.op("pool", lambda e, ch=ch: e.iota(mki[:, ch, :], [[16, 16], [1, 16]], base=-128 * ch, channel_multiplier=-1), (B0,), (B0,))
        k.op("dve", lambda e: e.tensor_scalar(out=idc[:, :, :], in0=mki[:, :, :], scalar1=0.0, scalar2=None, op0=ALU.is_equal), (B0,), (B0,))
        k.op("pool", lambda e: e.iota(mki[0:64, 0, 0:128], [[1, 128]], base=-64, channel_multiplier=-1), (B0,), (B0,))
        k.op("dve", lambda e: e.tensor_scalar(out=shid[:, :], in0=mki[0:64, 0, 0:128], scalar1=0.0, scalar2=None, op0=ALU.is_equal), (B0,), (B0,))
        GC = 4
        Fr = sb("u_Fr", [64, GC, 16, 16], F32)
        Fi = sb("u_Fi", [64, GC, 16, 16], F32)
        Er = sb("u_Er", [64, GC, 17, 16], F32)
        nEi = sb("u_nEi", [64, GC, 17, 16], F32)
        Lr = sb("u_Lr", [64, GC, 16, 16], F32)
        Li = sb("u_Li", [64, GC, 16, 16], F32)
        x1 = sb("u_x1", [64, GC, 17, 16], F32)
        x2 = sb("u_x2", [64, GC, 17, 16], F32)
        tmask = sb("u_tmask", [128, 256], F32)
        for gc in range(G // GC):
            gs = slice(gc * GC, (gc + 1) * GC)
            qr = Qr[:, gs, 0:16].unsqueeze(3).broadcast_to([64, GC, 16, 16])
            qi = Qi[:, gs, 0:16].unsqueeze(3).broadcast_to([64, GC, 16, 16])
            br_ = bbr[:, gs, :].unsqueeze(2).broadcast_to([64, GC, 16, 16])
            bi_ = bbi[:, gs, :].unsqueeze(2).broadcast_to([64, GC, 16, 16])
            cmul(k, "dve", Fr[:, :, :, :], Fi[:, :, :, :], qr, qi, br_, bi_, x1[:, :, 0:16, :], x2[:, :, 0:16, :], (B0,), (B0,), B0)
            pr = Pr[:, gs, :].unsqueeze(3).broadcast_to([64, GC, 17, 16])
            pi = Pi[:, gs, :].unsqueeze(3).broadcast_to([64, GC, 17, 16])
            cr_ = Cre[:, gs, :].unsqueeze(2).broadcast_to([64, GC, 17, 16])
            ci_ = Cim[:, gs, :].unsqueeze(2).broadcast_to([64, GC, 17, 16])
            cmul(k, "dve", Er[:, :, :, :], nEi[:, :, :, :], pr, pi, cr_, ci_, x1[:, :, :, :], x2[:, :, :, :], (B0,), (B0,), B0, neg_i=True)
            l15r = Pr[:, gs, 15:16].unsqueeze(3).broadcast_to([64, GC, 16, 16])
            l15i = Pi[:, gs, 15:16].unsqueeze(3).broadcast_to([64, GC, 16, 16])
            cmul(k, "dve", Lr[:, :, :, :], Li[:, :, :, :], l15r, l15i, Fr[:, :, :, :], Fi[:, :, :, :], x1[:, :, 0:16, :], x2[:, :, 0:16, :], (B0,), (B0,), B0)
            for gl in range(GC):
                g = gc * GC + gl
                items = []
                for ch in range(2):
                    items.append((pp[:, ch * 128:ch * 128 + 64], Lr[:, gl, ch * 8:(ch + 1) * 8, :].rearrange("p j c -> p (j c)"), c.identf[0:64, 0:64]))
                    items.append((pp[:, ch * 128 + 64:ch * 128 + 128], Li[:, gl, ch * 8:(ch + 1) * 8, :].rearrange("p j c -> p (j c)"), c.identf[0:64, 0:64]))
                tr(k, items, (B0,), (Bp,))
                k.op("act", lambda e, g=g: e.copy(out=GT[:, g, :, :], in_=pp[:, 0:256].rearrange("p (a b) -> p a b", a=2)), (Bp,), (B0, Bp))
                for ch in range(2):
                    fr_ = Fr[:, gl, ch * 8:(ch + 1) * 8, :].rearrange("p j c -> p (j c)")
                    fi_ = Fi[:, gl, ch * 8:(ch + 1) * 8, :].rearrange("p j c -> p (j c)")
                    er_ = Er[:, gl, 0:16, :].rearrange("p i c -> p (i c)")
                    ei_ = nEi[:, gl, 0:16, :].rearrange("p i c -> p (i c)")
                    Bq = Buf()
                    mm(k, [(pq[:, 0:256], fr_, er_, True, False), (pq[:, 0:256], fi_, ei_, False, True)], (B0,), (Bq, Bp))
                    k.op("dve", lambda e, ch=ch: e.tensor_tensor(out=tmask[:, :], in0=pq[:, 0:256], in1=mask[:, ch, :], op=ALU.mult), (Bq, B0), (B0, Bp))
                    k.op("dve", lambda e, ch=ch, g=g: e.scalar_tensor_tensor(out=TT[:, g, ch, :], in0=idc[:, ch, :], scalar=dcol[:, g:g + 1], in1=tmask[:, :], op0=ALU.mult, op1=ALU.add), (B0,), (B0,))
                er1 = Er[:, gl, 1:17, :].rearrange("p i c -> p (i c)")
                ei1 = nEi[:, gl, 1:17, :].rearrange("p i c -> p (i c)")
                mm(k, [(pq[:, 256:512], c.identf[0:64, :], er1, True, False), (pq[:, 256:512], shid[:, :], ei1, False, True)], (B0,), (Bp,))
                k.op("act", lambda e, g=g: e.copy(out=H[:, g, :], in_=pq[:, 256:512]), (Bp,), (B0, Bp))
        k.end("S_setup")
    with ExitStack() as ps:
        k.begin()
        sb = lambda n, s, d: ps.enter_context(nc.sbuf_tensor(n, s, d))
        B0 = Buf()
        lb = sb("w_lb", [128, 2, 2048], F32)
        k.dma("sp", lb[:, 0, :], io["ssm_lam_re"].rearrange("g p -> (g p)").partition_broadcast(128), B0, (), (B0,))
        k.dma("sp", lb[:, 1, :], io["ssm_lam_im"].rearrange("g p -> (g p)").partition_broadcast(128), B0, (), (B0,))
        dtb = sb("w_dtb", [128, G], F32)
        k.dma("sp", dtb[:, :], io["ssm_log_dt"].partition_broadcast(128), B0, (), (B0,))
        k.op("act", lambda e: e.activation(out=dtb[:, :], in_=dtb[:, :], func=AF.Exp), (B0,), (B0,))
        for i in range(2):
            k.op("dve", lambda e, i=i: e.tensor_tensor(out=lb[:, i, :].rearrange("p (g q) -> p g q", q=64), in0=lb[:, i, :].rearrange("p (g q) -> p g q", q=64), in1=dtb[:, :].unsqueeze(2).broadcast_to([128, G, 64]), op=ALU.mult), (B0,), (B0,))
        nki = sb("w_nki", [128, 2], I32)
        nk = sb("w_nk", [128, 2], F32)
        nk2 = sb("w_nk2", [128, 2], F32)
        k.op("pool", lambda e: e.iota(nki[:, 0:1], [[0, 1]], base=1024, channel_multiplier=-16), (B0,), (B0,))
        k.op("pool", lambda e: e.iota(nki[:, 1:2], [[0, 1]], base=-1040, channel_multiplier=16), (B0,), (B0,))
        k.op("dve", lambda e: e.tensor_copy(out=nk[:, :], in_=nki[:, :]), (B0,), (B0,))
        k.op("dve", lambda e: e.tensor_scalar(out=nk2[:, :], in0=nk[:, :], scalar1=1.0 / TWO_PI, scalar2=None, op0=ALU.mult), (B0,), (B0,))
        wc = sb("w_c", [128, 2048], F32)
        ws = sb("w_s", [128, 2048], F32)
        scr = (sb("w_c1", [128, 2048], F32)[:, :], sb("w_c2", [128, 2048], I32)[:, :], sb("w_c3", [128, 2048], F32)[:, :], sb("w_c4", [128, 2048], F32)[:, :])
        for i, Tb in enumerate((Mt, Dt)):
            cis(k, scr, lambda e, t, i=i: e.tensor_scalar(out=t, in0=lb[:, 1, :], scalar1=nk2[:, i:i + 1], scalar2=None, op0=ALU.mult), wc[:, :], ws[:, :], B0, B0)
            k.op("act", lambda e, i=i, Tb=Tb: e.activation(out=Tb[:, 1, :], in_=lb[:, 0, :], func=AF.Exp, scale=nk[:, i:i + 1]), (B0,), (B0,))
            k.op("dve", lambda e, Tb=Tb: e.tensor_tensor(out=Tb[:, 0, :], in0=Tb[:, 1, :], in1=wc[:, :], op=ALU.mult), (B0,), (B0,))
            k.op("dve", lambda e, Tb=Tb: e.tensor_tensor(out=Tb[:, 1, :], in0=Tb[:, 1, :], in1=ws[:, :], op=ALU.mult), (B0,), (B0,))
        k.end("S_tables")


def s5_main(k, c, io, GT, TT, H, Mt, Dt):
    nc = k.nc
    with ExitStack() as ps:
        k.begin()
        sb = lambda n, s, d: ps.enter_context(nc.sbuf_tensor(n, s, d))
        bTab = Buf()
        Wg = sb("S_Wg", [128, 4, 512], BF16)
        bWg = Buf()
        load_w_cast(k, ps, io["w_glu"], 4, 512, "S_wg", Wg, bWg, "dve")
        bgl = sb("S_bgl", [128, 4], F32)
        bbgl = Buf()
        k.dma("sp", bgl[:, :], io["b_glu"].rearrange("(m p) -> p m", p=128), bbgl, (), (bbgl,), allow_slow_non_contiguous=True)
        UA = sb("S_UA", [128, 8192], BF16)
        UB = sb("S_UB", [128, 8192], BF16)
        bUA, bUB = Buf(), Buf()
        U16 = sb("S_U16", [128, G, 2, 128], BF16)
        bU16 = Buf()
        X = sb("S_X", [128, G, 128], BF16)
        bX = [Buf() for _ in range(8)]
        Stm = sb("S_Stm", [128, G, 128], BF16)
        bStm = [Buf() for _ in range(8)]
        ST = sb("S_ST", [128, G, 128], BF16)
        bST = [Buf() for _ in range(4)]
        pT = [ps.enter_context(nc.psum_tensor("S_pT%d" % i, [128, 1024], BF16)) for i in range(2)]
        bpT = [Buf(), Buf()]
        NPD = 2
        pd = [ps.enter_context(nc.psum_tensor("S_pd%d" % i, [128, 512], F32)) for i in range(NPD)]
        bpd = [Buf() for _ in range(NPD)]
        py = [ps.enter_context(nc.psum_tensor("S_py%d" % i, [128, 512], F32)) for i in range(3)]
        bpy = [Buf() for _ in range(3)]
        pgl = ps.enter_context(nc.psum_tensor("S_pgl", [128, 512], F32))
        bpgl = Buf()
        tq = [sb("S_tq%d" % i, [128, 256], F32) for i in range(4)]
        btq = [Buf() for _ in range(4)]
        gx2 = [sb("S_gx2%d" % i, [128, 512], F32) for i in range(3)]
        gu = [sb("S_gu%d" % i, [128, 512], F32) for i in range(3)]
        bgx = [Buf() for _ in range(3)]
        bgu = [Buf() for _ in range(3)]
        sgl = sb("S_sgl", [128, 512], F32)
        bsgl = Buf()
        s5st = [sb("S_s5%d" % i, [128, 4, 512], BF16) for i in range(2)]
        bs5 = [Buf(), Buf()]
        ipT = 0
        ipd = 0
        ipy = 0

        def cmod(pbank, Tb, dst, g0, bsrc, bdst):
            src = pbank[:, :].rearrange("p (g x) -> p g x", g=4)
            sre, sim = src[:, :, 0:64], src[:, :, 64:128]
            tre = Tb[:, 0, g0 * 64:(g0 + 4) * 64].rearrange("p (g q) -> p g q", g=4)
            tim = Tb[:, 1, g0 * 64:(g0 + 4) * 64].rearrange("p (g q) -> p g q", g=4)
            v = lambda t: t[:, :].rearrange("p (g q) -> p g q", g=4)
            k.op("dve", lambda e: e.tensor_tensor(out=v(tq[0]), in0=sre, in1=tre, op=ALU.mult), (bsrc, bTab), (btq[0],))
            k.op("dve", lambda e: e.tensor_tensor(out=v(tq[1]), in0=sim, in1=tim, op=ALU.mult), (bsrc, bTab), (btq[1],))
            k.op("dve", lambda e: e.tensor_tensor(out=v(tq[2]), in0=sre, in1=tim, op=ALU.mult), (bsrc, bTab), (btq[2],))
            k.op("dve", lambda e: e.tensor_tensor(out=v(tq[3]), in0=sim, in1=tre, op=ALU.mult), (bsrc, bTab), (btq[3],))
            k.op("pool", lambda e: e.tensor_tensor(out=dst[:, g0:g0 + 4, 0:64], in0=v(tq[0]), in1=v(tq[1]), op=ALU.subtract), (btq[0], btq[1]), (bdst,))
            k.op("pool", lambda e: e.tensor_tensor(out=dst[:, g0:g0 + 4, 64:128], in0=v(tq[2]), in1=v(tq[3]), op=ALU.add), (btq[2], btq[3]), (bdst,))

        for s in range(BPC):
            tb = s * SEQ
            Utm = UA[:, :].rearrange("p (j c) -> p j c", j=16)
            k.dma("sp", Utm, io["z_s"][tb:tb + SEQ, 0:512].rearrange("(k j) c -> k j c", j=16), bUA, (), (bUA,))
            for hf in range(2):
                k.op("dve", lambda e, hf=hf: e.tensor_copy(
                    out=UB[:, hf * 4096:(hf + 1) * 4096].rearrange("p (g j c) -> p g j c", g=16, j=16),
                    in_=UA[:, :].rearrange("p (j g c) -> p g j c", j=16, g=32)[:, hf * 16:(hf + 1) * 16, :, :]), (bUA,), (bUB,))
            Ug = UB[:, :].rearrange("p (g x) -> p g x", g=32)
            for g4 in range(8):
                p = ipT % 2
                ipT += 1
                tr(k, [(pT[p][:, (gl * 2 + ch) * 128:(gl * 2 + ch + 1) * 128], Ug[:, g4 * 4 + gl, ch * 128:(ch + 1) * 128], c.ident[:, :]) for gl in range(4) for ch in range(2)], (bUB,), (bpT[p],))
                k.op("act", lambda e, p=p, g4=g4: e.copy(out=U16[:, g4 * 4:(g4 + 1) * 4, :, :], in_=pT[p][:, :].rearrange("p (g a k) -> p g a k", g=4, a=2)), (bpT[p],), (bU16,))
            for g4 in range(8):
                p = ipd % NPD
                ipd += 1
                items = []
                for gl in range(4):
                    g = g4 * 4 + gl
                    for ch in range(2):
                        items.append((pd[p][:, gl * 128:(gl + 1) * 128], U16[:, g, ch, :], GT[:, g, ch, :], ch == 0, ch == 1))
                mm(k, items, (bU16, bTab), (bpd[p],))
                cmod(pd[p], Mt, X, g4 * 4, bpd[p], bX[g4])
            for g4 in range(8):
                p = ipd % NPD
                ipd += 1
                mm(k, [(pd[p][:, :], c.tri[:, :], X[:, g4 * 4:(g4 + 1) * 4, :].rearrange("p g x -> p (g x)"), True, True)], (bX[g4],), (bpd[p],))
                cmod(pd[p], Dt, Stm, g4 * 4, bpd[p], bStm[g4])
            for g8 in range(4):
                p = ipT % 2
                ipT += 1
                tr(k, [(pT[p][:, gl * 128:(gl + 1) * 128], Stm[:, g8 * 8 + gl, :], c.ident[:, :]) for gl in range(8)], (bStm[2 * g8], bStm[2 * g8 + 1]), (bpT[p],))
                k.op("act", lambda e, p=p, g8=g8: e.copy(out=ST[:, g8 * 8:(g8 + 1) * 8, :], in_=pT[p][:, :].rearrange("p (g k) -> p g k", g=8)), (bpT[p],), (bST[g8],))
            ygtm = UA[:, :].rearrange("p (i c) -> p i c", i=16)
            def y_front(gp):
                p = gp % 3
                items = []
                for g2 in range(2):
                    g = gp * 2 + g2
                    o = py[p][:, g2 * 256:(g2 + 1) * 256]
                    items += [(o, U16[:, g, 0, :], TT[:, g, 0, :], True, False), (o, U16[:, g, 1, :], TT[:, g, 1, :], False, False), (o, ST[:, g, :], H[:, g, :], False, True)]
                mm(k, items, (bU16, bST[gp // 4], bTab), (bpy[p],))
                k.op("act", lambda e: e.activation(out=gx2[p][:, :], in_=py[p][:, :], func=AF.Square), (bpy[p],), (bgx[p],))
                k.op("pool", lambda e: e.tensor_scalar(out=gx2[p][:, :], in0=gx2[p][:, :], scalar1=0.044715, scalar2=1.0, op0=ALU.mult, op1=ALU.add), (bgx[p],), (bgx[p],))

            def y_back(gp):
                p = gp % 3
                k.op("dve", lambda e: e.tensor_tensor(out=gu[p][:, :], in0=py[p][:, :], in1=gx2[p][:, :], op=ALU.mult), (bpy[p], bgx[p]), (bgu[p],))
                k.op("act", lambda e: e.activation(out=gu[p][:, :], in_=gu[p][:, :], func=AF.Sigmoid, scale=1.5957691216057308), (bgu[p],), (bgu[p],))
                k.op("dve", lambda e: e.tensor_tensor(
                    out=ygtm[:, :, gp * 32:(gp + 1) * 32].rearrange("p i (g c) -> p g i c", g=2),
                    in0=py[p][:, :].rearrange("p (g i c) -> p g i c", g=2, i=16),
                    in1=gu[p][:, :].rearrange("p (g i c) -> p g i c", g=2, i=16), op=ALU.mult), (bpy[p], bgu[p]), (bUA,))

            y_front(0)
            for gp in range(16):
                if gp + 1 < 16:
                    y_front(gp + 1)
                y_back(gp)
            ygT = UB[:, :].rearrange("p (ct t) -> p ct t", ct=4)
            for ib in range(8):
                p = ipT % 2
                ipT += 1
                tr(k, [(pT[p][:, (i2 * 4 + ct) * 128:(i2 * 4 + ct + 1) * 128], ygtm[:, ib * 2 + i2, ct * 128:(ct + 1) * 128], c.ident[:, :]) for i2 in range(2) for ct in range(4)], (bUA,), (bpT[p],))
                k.op("act", lambda e, p=p, ib=ib: e.copy(
                    out=UB[:, :].rearrange("p (ct k i) -> p i ct k", ct=4, i=16)[:, ib * 2:(ib + 1) * 2, :, :],
                    in_=pT[p][:, :].rearrange("p (i ct k) -> p i ct k", i=2, ct=4)), (bpT[p],), (bUB,))
            for ch in range(4):
                sidx = (s * 4 + ch) % 2
                for m in range(4):
                    mm(k, [(pgl[:, :], Wg[:, f, m * 128:(m + 1) * 128], ygT[:, f, ch * 512:(ch + 1) * 512], f == 0, f == 3) for f in range(4)], (bUB, bWg), (bpgl,))
                    k.op("act", lambda e, m=m: e.activation(out=sgl[:, :], in_=pgl[:, :], func=AF.Sigmoid, bias=bgl[:, m:m + 1]), (bpgl, bbgl), (bsgl,))
                    k.op("dve", lambda e, m=m, ch=ch, sidx=sidx: e.tensor_tensor(out=s5st[sidx][:, m, :], in0=sgl[:, :], in1=ygT[:, m, ch * 512:(ch + 1) * 512], op=ALU.mult), (bsgl, bUB), (bs5[sidx],))
                k.dma("pool", io["s5_s"][:, tb + ch * 512:tb + (ch + 1) * 512].rearrange("(m p) t -> p m t", p=128), s5st[sidx][:, :, :], bs5[sidx], (bs5[sidx],), ())
        k.end("S_main")
```

```python
import math
from contextlib import ExitStack
import numpy as np
import concourse.bass as bass
import concourse.mybir as mybir
from concourse.bass_utils import run_bass_kernel_spmd

F32 = mybir.dt.float32
BF16 = mybir.dt.bfloat16
I32 = mybir.dt.int32
AF = mybir.ActivationFunctionType
ALU = mybir.AluOpType
AX = mybir.AxisListType

NCORES = 8
D = 1024
SEQ = 2048
BPC = 4
T = BPC * SEQ
NTT = T // 128
G = 32
PST = 64
DFF = 4096
PLE = 256
EPS = 1e-6
NEG = -30000.0


class Buf:
    __slots__ = ("name", "w", "r", "ds")

    def __init__(self, name=""):
        self.name = name
        self.w = None
        self.r = []
        self.ds = None


class K:
    ENGS = ("pe", "dve", "act", "pool", "sp")

    def __init__(self, nc, es):
        self.nc = nc
        self.es = es
        self.esem = {e: es.enter_context(nc.semaphore("es_" + e)) for e in self.ENGS}
        self.cnt = {e: 0 for e in self.ENGS}
        self.dpool = [es.enter_context(nc.semaphore("ds%d" % i)) for i in range(48)]
        self.NHW = 36
        self.dcnt = [0] * len(self.dpool)
        self.ops = None
        with nc.Block() as block:
            def clr(eng):
                for sm in list(self.esem.values()) + self.dpool:
                    eng.sem_clear(sm)
            block.sync(clr)

    def begin(self):
        self.ops = {e: [] for e in self.ENGS}
        self.seen = {e: {} for e in self.ENGS}
        self.dnext = {False: 0, True: self.NHW}
        self.dused = set()
        self.phase_id = getattr(self, "phase_id", 0) + 1

    def _dsem(self, buf, sw):
        if buf.ds is None or buf.ds[0] != (self.phase_id, sw):
            lim = len(self.dpool) if sw else self.NHW
            assert self.dnext[sw] < lim, "out of dma sems"
            buf.ds = ((self.phase_id, sw), self.dnext[sw])
            self.dnext[sw] += 1
        return buf.ds[1]

    def _deps(self, eng, reads, writes):
        waits = {}
        def add(tok):
            if tok is None:
                return
            key, val = tok
            if key == ("e", "pe") and eng == "pe":
                return
            if self.seen[eng].get(key, 0) >= val:
                return
            if waits.get(key, 0) < val:
                waits[key] = val
        for b in reads:
            add(b.w)
        for b in writes:
            add(b.w)
            for t in b.r:
                add(t)
        for key, val in waits.items():
            self.seen[eng][key] = val
        return list(waits.items())

    def _mark(self, tok, reads, writes):
        for b in reads:
            b.r.append(tok)
        for b in writes:
            b.w = tok
            b.r = []

    def op(self, eng, fn, reads=(), writes=()):
        waits = self._deps(eng, reads, writes)
        self.cnt[eng] += 1
        tok = (("e", eng), self.cnt[eng])
        self._mark(tok, reads, writes)
        self.ops[eng].append(("c", fn, waits))

    def dma(self, eng, out, in_, sb, reads=(), writes=(), **kw):
        waits = self._deps(eng, reads, writes)
        si = self._dsem(sb, eng == "pool")
        self.dcnt[si] += 16
        self.dused.add(si)
        tok = (("d", si), self.dcnt[si])
        self._mark(tok, reads, writes)
        self.ops[eng].append(("d", (out, in_, si, kw), waits))

    def _sem(self, key):
        return self.esem[key[1]] if key[0] == "e" else self.dpool[key[1]]

    def end(self, name):
        nc = self.nc
        fin = [(("e", e), self.cnt[e]) for e in self.ENGS if e != "sp" and self.cnt[e] > 0]
        fin += [(("d", si), self.dcnt[si]) for si in sorted(self.dused)]
        ops = self.ops
        handles = {"pe": "tensor", "dve": "vector", "act": "scalar", "pool": "gpsimd", "sp": "sync"}
        with nc.Block() as block:
            for e in self.ENGS:
                def body(eng, e=e):
                    for kind, payload, waits in ops[e]:
                        for key, val in waits:
                            eng.wait_ge(self._sem(key), val)
                        if kind == "c":
                            ins = payload(eng)
                            ins.then_inc(self.esem[e], 1)
                        else:
                            out, in_, si, kw = payload
                            eng.dma_start(out=out, in_=in_, **kw).then_inc(self.dpool[si], 16)
                    if e == "sp":
                        for key, val in fin:
                            eng.wait_ge(self._sem(key), val)
                getattr(block, handles[e])(body)
        self.ops = None


def mm(k, items, reads, writes):
    def fn(eng):
        ins = None
        for (out, lhsT, rhs, st, sp) in items:
            ins = eng.matmul(out, lhsT, rhs, start=st, stop=sp)
        return ins
    k.op("pe", fn, reads, writes)


def tr(k, items, reads, writes):
    def fn(eng):
        ins = None
        for (out, in_, ident) in items:
            ins = eng.transpose(out, in_, ident)
        return ins
    k.op("pe", fn, reads, writes)


class Consts:
    pass


def make_consts(k, es):
    nc = k.nc
    c = Consts()
    c.ident = es.enter_context(nc.sbuf_tensor("ident", [128, 128], BF16))
    c.identf = es.enter_context(nc.sbuf_tensor("identf", [128, 128], F32))
    c.tri = es.enter_context(nc.sbuf_tensor("tri", [128, 128], BF16))
    c.nhalf = es.enter_context(nc.sbuf_tensor("nhalf", [128, 1], F32))
    c.ones = es.enter_context(nc.sbuf_tensor("onesb", [128, 128], BF16))
    io = es.enter_context(nc.sbuf_tensor("iota_i", [128, 128], I32))
    b_io, b_id, b_idf, b_tri, b_nh, b_on = (Buf() for _ in range(6))
    k.begin()
    k.op("pool", lambda e: e.iota(io[:, :], [[1, 128]], base=0, channel_multiplier=-1), (), (b_io,))
    k.op("dve", lambda e: e.tensor_scalar(out=c.ident[:, :], in0=io[:, :], scalar1=0.0, scalar2=None, op0=ALU.is_equal), (b_io,), (b_id,))
    k.op("dve", lambda e: e.tensor_scalar(out=c.identf[:, :], in0=io[:, :], scalar1=0.0, scalar2=None, op0=ALU.is_equal), (b_io,), (b_idf,))
    k.op("dve", lambda e: e.tensor_scalar(out=c.tri[:, :], in0=io[:, :], scalar1=0.0, scalar2=None, op0=ALU.is_gt), (b_io,), (b_tri,))
    k.op("dve", lambda e: e.memset(c.nhalf[:, :], -0.5), (), (b_nh,))
    k.op("dve", lambda e: e.memset(c.ones[:, :], 1.0), (), (b_on,))
    k.end("consts")
    return c


def rms_stats(k, c, x_ap, junk_ap, ss, rstd, bx, bj, bss, brs, n=1024):
    k.op("act", lambda e: e.activation(out=junk_ap, in_=x_ap, func=AF.Square, accum_out=ss), (bx,), (bj, bss))
    k.op("pool", lambda e: e.tensor_scalar(out=rstd, in0=ss, scalar1=1.0 / n, scalar2=EPS, op0=ALU.mult, op1=ALU.add), (bss,), (brs,))
    k.op("pool", lambda e: e.tensor_tensor(out=rstd, in0=rstd, in1=c.nhalf[:, :], op=ALU.pow), (brs,), (brs,))


def load_w_scaled(k, ps, w_d, kt, ncols, g_d, name, Wt, bW, half=2048, order=None):
    nc = k.nc
    gcol = ps.enter_context(nc.sbuf_tensor(name + "_g", [128, kt], F32))
    bg = Buf()
    k.dma("sp", gcol[:, :], g_d.rearrange("(f p) -> p f", p=128), bg, (), (bg,), allow_slow_non_contiguous=True)
    half = min(half, ncols)
    stg = [ps.enter_context(nc.sbuf_tensor(name + "_s%d" % i, [128, half], F32)) for i in range(2)]
    bs = [Buf(), Buf()]
    if isinstance(bW, list):
        assert half == 512
        pieces = [(f, ci * 512) for ci in (order or range(ncols // 512)) for f in range(kt)]
    else:
        pieces = [(f, c0) for f in range(kt) for c0 in range(0, ncols, half)]
    for i, (f, c0) in enumerate(pieces):
        s, b = stg[i % 2], bs[i % 2]
        bw = bW[c0 // 512] if isinstance(bW, list) else bW
        k.dma("sp", s[:, :], w_d[f * 128:(f + 1) * 128, c0:c0 + half], b, (), (b,))
        k.op("dve", lambda e, s=s, f=f, c0=c0: e.tensor_scalar(out=Wt[:, f, c0:c0 + half], in0=s[:, :], scalar1=gcol[:, f:f + 1], scalar2=None, op0=ALU.mult), (b, bg), (bw,))
    return stg, bs


def load_w_cast(k, ps, w_d, kt, ncols, name, Wt, bW, eng="act"):
    nc = k.nc
    half = 2048 if ncols > 2048 else ncols
    stg = [ps.enter_context(nc.sbuf_tensor(name + "_s%d" % i, [128, half], F32)) for i in range(2)]
    bs = [Buf(), Buf()]
    i = 0
    for f in range(kt):
        for c0 in range(0, ncols, half):
            s, b = stg[i % 2], bs[i % 2]
            k.dma("sp", s[:, :], w_d[f * 128:(f + 1) * 128, c0:c0 + half], b, (), (b,))
            if eng == "act":
                k.op("act", lambda e, s=s, f=f, c0=c0: e.copy(out=Wt[:, f, c0:c0 + half], in_=s[:, :]), (b,), (bW,))
            else:
                k.op("dve", lambda e, s=s, f=f, c0=c0: e.tensor_copy(out=Wt[:, f, c0:c0 + half], in_=s[:, :]), (b,), (bW,))
            i += 1


def phase_A(k, c, io):
    nc = k.nc
    with ExitStack() as ps:
        k.begin()
        sb = lambda n, s, d: ps.enter_context(nc.sbuf_tensor(n, s, d))
        W = sb("A_W", [128, 8, 4096], BF16)
        bWs = [Buf() for _ in range(8)]
        load_w_scaled(k, ps, io["w_in"], 8, 4096, io["g_pre_mix"], "A_w", W, bWs, half=512, order=[0, 3, 1, 2, 4, 5, 6, 7])
        xt = [sb("A_x%d" % i, [128, 1024], F32) for i in range(4)]
        bx = [Buf() for _ in range(4)]
        junk = sb("A_junk", [128, 1024], BF16)
        bj = Buf()
        ss = [sb("A_ss%d" % i, [128, 1], F32) for i in range(4)]
        rs = [sb("A_rs%d" % i, [128, 1], F32) for i in range(4)]
        bss = [Buf() for _ in range(4)]
        brs = [Buf() for _ in range(4)]
        hb = [sb("A_hb%d" % i, [128, 1024], BF16) for i in range(2)]
        bhb = [Buf(), Buf()]
        pT = [ps.enter_context(nc.psum_tensor("A_pT%d" % i, [128, 1024], BF16)) for i in range(2)]
        bpT = [Buf(), Buf()]
        hT = [sb("A_hT%d" % i, [128, 8, 512], BF16) for i in range(2)]
        bhT = [Buf(), Buf()]
        pm = [ps.enter_context(nc.psum_tensor("A_pm%d" % i, [128, 512], F32)) for i in range(6)]
        bpm = [Buf() for _ in range(6)]
        zst = [sb("A_z%d" % i, [128, 1024], BF16) for i in range(2)]
        bz = [Buf(), Buf()]
        fst = [sb("A_f%d" % i, [128, 8, 512], BF16) for i in range(3)]
        bf = [Buf() for _ in range(3)]
        ipm = 0
        ifs = 0
        def head_tile(ch, j):
            hTc, bhTc = hT[ch % 2], bhT[ch % 2]
            t = ch * 4 + j
            xi = t % 4
            hbi = t % 2
            k.dma("sp", xt[xi][:, :], io["x"][t * 128:(t + 1) * 128, :], bx[xi], (), (bx[xi],))
            rms_stats(k, c, xt[xi][:, :], junk[:, :], ss[xi][:, :], rs[xi][:, :], bx[xi], bj, bss[xi], brs[xi])
            k.op("act", lambda e: e.activation(out=hb[hbi][:, :], in_=xt[xi][:, :], func=AF.Copy, scale=rs[xi][:, :]), (bx[xi], brs[xi]), (bhb[hbi],))
            tr(k, [(pT[hbi][:, f * 128:(f + 1) * 128], hb[hbi][:, f * 128:(f + 1) * 128], c.ident[:, :]) for f in range(8)], (bhb[hbi],), (bpT[hbi],))
            k.op("dve", lambda e: e.tensor_copy(out=hTc[:, :, j * 128:(j + 1) * 128], in_=pT[hbi][:, :].rearrange("p (f t) -> p f t", f=8)), (bpT[hbi],), (bhTc,))

        for j in range(4):
            head_tile(0, j)
        for ch in range(T // 512):
            hTc, bhTc = hT[ch % 2], bhT[ch % 2]
            for j in range(4):
                t = ch * 4 + j
                zi = t % 2
                for n, c0 in enumerate((0, 1536)):
                    p = ipm % 6
                    ipm += 1
                    mm(k, [(pm[p][:, :], hTc[:, f, j * 128:(j + 1) * 128], W[:, f, c0:c0 + 512], f == 0, f == 7) for f in range(8)], (bhTc, bWs[c0 // 512]), (bpm[p],))
                    k.op("dve", lambda e, p=p, zi=zi, n=n: e.tensor_copy(out=zst[zi][:, n * 512:(n + 1) * 512], in_=pm[p][:, :]), (bpm[p],), (bz[zi],))
                k.dma("pool", io["z_s"][t * 128:(t + 1) * 128, :], zst[zi][:, :], bz[zi], (bz[zi],), ())
            for m in range(24):
                c0 = 512 + m * 128 if m < 8 else 2048 + (m - 8) * 128
                p = ipm % 6
                ipm += 1
                mm(k, [(pm[p][:, :], W[:, f, c0:c0 + 128], hTc[:, f, :], f == 0, f == 7) for f in range(8)], (bhTc, bWs[c0 // 512]), (bpm[p],))
                fi = ifs % 3
                if m < 8:
                    k.op("dve", lambda e, p=p, fi=fi, m=m: e.tensor_copy(out=fst[fi][:, m % 8, :], in_=pm[p][:, :]), (bpm[p],), (bf[fi],))
                else:
                    k.op("act", lambda e, p=p, fi=fi, m=m: e.activation(out=fst[fi][:, m % 8, :], in_=pm[p][:, :], func=AF.Sigmoid), (bpm[p],), (bf[fi],))
                if m % 8 == 7:
                    r0 = (m // 8) * 1024
                    k.dma("pool", io["fm_s"][r0:r0 + 1024, ch * 512:(ch + 1) * 512].rearrange("(m p) t -> p m t", p=128), fst[fi][:, :, :], bf[fi], (bf[fi],), ())
                    ifs += 1
                if m in (3, 8, 13, 18) and ch + 1 < T // 512:
                    head_tile(ch + 1, (3, 8, 13, 18).index(m))
        k.end("A")


IN_SPECS = [
    ("x", [T, D]), ("p", [T, PLE]), ("g_pre_mix", [D]), ("w_in", [D, 4096]),
    ("ssm_lam_re", [G, PST]), ("ssm_lam_im", [G, PST]), ("ssm_log_dt", [G]),
    ("ssm_b_re", [G, PST, 16]), ("ssm_b_im", [G, PST, 16]),
    ("ssm_c_re", [G, 16, PST]), ("ssm_c_im", [G, 16, PST]), ("ssm_d", [512]),
    ("w_glu", [512, 512]), ("b_glu", [512]), ("w_branch_a", [512, D]), ("w_branch_b", [512, D]),
    ("w_out", [D, D]), ("g_post_mix", [D]), ("g_pre_mlp", [D]), ("w_mlp1", [D, DFF]),
    ("w_mlp2", [DFF, D]), ("g_post_mlp", [D]), ("w_ple", [PLE, D]), ("w_ple_gate", [D, D]),
    ("g_ple", [D]),
]
SCRATCH = [
    ("z_s", [T, 1024], BF16),
    ("fm_s", [3072, T], BF16),
    ("s5_s", [512, T], BF16),
    ("at_s", [T, 512], BF16),
    ("x1_s", [T, D], F32),
    ("x2_s", [T, D], F32),
]


def build(phases="ASCDEF", debug=(), inject=()):
    nc = bass.Bass("TRN2", target_bir_lowering=False)
    io = {}
    for name, shape in IN_SPECS:
        io[name] = nc.dram_tensor(name, shape, F32, kind="ExternalInput").ap()
    for name, shape, dt in SCRATCH:
        kind = "ExternalOutput" if name in debug else ("ExternalInput" if name in inject else "Internal")
        io[name] = nc.dram_tensor(name, shape, dt, kind=kind).ap()
    io["out"] = nc.dram_tensor("out", [T, D], F32, kind="ExternalOutput").ap()
    with ExitStack() as es:
        k = K(nc, es)
        c = make_consts(k, es)
        if "A" in phases:
            phase_A(k, c, io)
        if "S" in phases:
            phase_S(k, c, io)
        if "C" in phases:
            phase_C(k, c, io)
        if "D" in phases:
            phase_D(k, c, io)
        if "E" in phases:
            phase_E(k, c, io)
        if "F" in phases:
            phase_F(k, c, io)
    return nc


def make_in_maps(inputs, ncores=NCORES):
    maps = []
    for ci in range(ncores):
        m = {}
        for name, shape in IN_SPECS:
            a = inputs[name]
            if name == "x":
                a = a[ci * BPC:(ci + 1) * BPC].reshape(T, D)
            elif name == "p":
                a = a[0, ci * BPC:(ci + 1) * BPC].reshape(T, PLE)
            else:
                a = a[0]
            m[name] = np.ascontiguousarray(a, dtype=np.float32).reshape(shape)
        maps.append(m)
    return maps


def kernel(**inputs):
    inputs = {k_: np.asarray(v) for k_, v in inputs.items()}
    nc = build()
    in_maps = make_in_maps(inputs)
    res = run_bass_kernel_spmd(nc, in_maps, core_ids=list(range(NCORES)))
    outs = [np.asarray(r["out"]).reshape(BPC, SEQ, D) for r in res.results]
    return np.concatenate(outs, axis=0).astype(np.float32)


def load_gb(k, ps, g_d, name):
    nc = k.nc
    gb = ps.enter_context(nc.sbuf_tensor(name, [128, D], F32))
    b = Buf()
    k.dma("sp", gb[:, :], g_d.partition_broadcast(128), b, (), (b,))
    return gb, b


class TailBufs:
    def __init__(self, k, sb, name, n=2, inplace=False):
        self.n = n
        self.inplace = inplace
        self.junk = [sb(name + "_junk%d" % i, [128, 1024], BF16) for i in range(n)]
        self.ss = [sb(name + "_tss%d" % i, [128, 1], F32) for i in range(n)]
        self.rs = [sb(name + "_trs%d" % i, [128, 1], F32) for i in range(n)]
        self.tmp = [sb(name + "_ttmp%d" % i, [128, 1024], F32) for i in range(n)]
        self.ost = self.tmp if inplace else [sb(name + "_tost%d" % i, [128, 1024], F32) for i in range(n)]
        self.b = [[Buf() for _ in range(5)] for _ in range(n)]


def norm_res_tail(k, c, tb, i, src_ap, bsrc, gb, bgb, xres, bxres, out_rows):
    i = i % tb.n
    bj, bss, brs, btmp, bost = tb.b[i]
    junk, ss, rs, tmp, ost = tb.junk[i], tb.ss[i], tb.rs[i], tb.tmp[i], tb.ost[i]
    rms_stats(k, c, src_ap, junk[:, :], ss[:, :], rs[:, :], bsrc, bj, bss, brs)
    k.op("dve", lambda e: e.scalar_tensor_tensor(out=tmp[:, :], in0=src_ap, scalar=rs[:, :], in1=gb[:, :], op0=ALU.mult, op1=ALU.mult), (bsrc, brs, bgb), (btmp,))
    if tb.inplace:
        bost = btmp
    k.op("pool", lambda e: e.tensor_tensor(out=ost[:, :], in0=tmp[:, :], in1=xres[:, :], op=ALU.add), (btmp, bxres), (bost,))
    k.dma("pool", out_rows, ost[:, :], bost, (bost,), ())


def phase_D(k, c, io):
    nc = k.nc
    with ExitStack() as ps:
        k.begin()
        sb = lambda n, s, d: ps.enter_context(nc.sbuf_tensor(n, s, d))
        Wa = sb("D_Wa", [128, 4, 1024], BF16)
        Wb = sb("D_Wb", [128, 4, 1024], BF16)
        Wo = sb("D_Wo", [128, 8, 1024], BF16)
        bWa, bWb, bWo = Buf(), Buf(), Buf()
        load_w_cast(k, ps, io["w_branch_a"], 4, 1024, "D_wa", Wa, bWa, "dve")
        load_w_cast(k, ps, io["w_branch_b"], 4, 1024, "D_wb", Wb, bWb, "act")
        load_w_cast(k, ps, io["w_out"], 8, 1024, "D_wo", Wo, bWo, "dve")
        g2, bg2 = load_gb(k, ps, io["g_post_mix"], "D_g2")
        gts = [sb("D_gt%d" % i, [128, 16, 512], BF16) for i in range(2)]
        bgt = [Buf(), Buf()]
        s5t = [sb("D_s5%d" % i, [128, 4, 512], BF16) for i in range(2)]
        bs5 = [Buf(), Buf()]
        att = [sb("D_at%d" % i, [128, 512], BF16) for i in range(4)]
        bat = [Buf() for _ in range(4)]
        atTs = [sb("D_atT%d" % i, [128, 4, 512], BF16) for i in range(2)]
        batTs = [Buf(), Buf()]
        pT = ps.enter_context(nc.psum_tensor("D_pT", [128, 1024], BF16))
        bpT = Buf()
        pm = [ps.enter_context(nc.psum_tensor("D_pm%d" % i, [128, 512], F32)) for i in range(3)]
        bpm = [Buf() for _ in range(3)]
        pmx = [ps.enter_context(nc.psum_tensor("D_px%d" % i, [128, 1024], F32)) for i in range(2)]
        bpx = [Buf(), Buf()]
        t1 = [sb("D_t1%d" % i, [128, 512], F32) for i in range(2)]
        bt1 = [Buf(), Buf()]
        t2 = [sb("D_t2%d" % i, [128, 512], F32) for i in range(2)]
        bt2 = [Buf(), Buf()]
        mTs = [sb("D_mT%d" % i, [128, 8, 512], BF16) for i in range(2)]
        bmTs = [Buf(), Buf()]
        xt = [sb("D_x%d" % i, [128, 1024], F32) for i in range(2)]
        bx = [Buf(), Buf()]
        tbf = TailBufs(k, sb, "D")
        ipm = [0]
        xt3 = xt + [sb("D_x2", [128, 1024], F32)]
        bx3 = bx + [Buf()]

        def head(ch):
            gi = ch % 2
            atT, batT = atTs[gi], batTs[gi]
            cs = slice(ch * 512, (ch + 1) * 512)
            k.dma("sp", gts[gi][:, :, :], io["fm_s"][1024:3072, cs].rearrange("(m p) t -> p m t", p=128), bgt[gi], (), (bgt[gi],))
            k.dma("sp", s5t[gi][:, :, :], io["s5_s"][:, cs].rearrange("(m p) t -> p m t", p=128), bs5[gi], (), (bs5[gi],))
            for j in range(4):
                t = ch * 4 + j
                k.dma("sp", att[j][:, :], io["at_s"][t * 128:(t + 1) * 128, :], bat[j], (), (bat[j],))
            for j in range(4):
                tr(k, [(pT[:, f * 128:(f + 1) * 128], att[j][:, f * 128:(f + 1) * 128], c.ident[:, :]) for f in range(4)], (bat[j],), (bpT,))
                k.op("dve", lambda e, j=j: e.tensor_copy(out=atT[:, :, j * 128:(j + 1) * 128], in_=pT[:, 0:512].rearrange("p (f t) -> p f t", f=4)), (bpT,), (batT,))

        def gate(ch):
            gi = ch % 2
            atT, batT = atTs[gi], batTs[gi]
            mT, bmT = mTs[gi], bmTs[gi]
            for m in range(8):
                p = ipm[0] % 3
                ipm[0] += 1
                mm(k, [(pm[p][:, :], Wa[:, f, m * 128:(m + 1) * 128], s5t[gi][:, f, :], f == 0, f == 3) for f in range(4)], (bs5[gi], bWa), (bpm[p],))
                i1 = m % 2
                k.op("dve", lambda e, p=p, i1=i1, m=m: e.tensor_tensor(out=t1[i1][:, :], in0=pm[p][:, :], in1=gts[gi][:, m, :], op=ALU.mult), (bpm[p], bgt[gi]), (bt1[i1],))
                p2 = ipm[0] % 3
                ipm[0] += 1
                mm(k, [(pm[p2][:, :], Wb[:, f, m * 128:(m + 1) * 128], atT[:, f, :], f == 0, f == 3) for f in range(4)], (batT, bWb), (bpm[p2],))
                k.op("dve", lambda e, p2=p2, i1=i1, m=m: e.tensor_tensor(out=t2[i1][:, :], in0=pm[p2][:, :], in1=gts[gi][:, 8 + m, :], op=ALU.mult), (bpm[p2], bgt[gi]), (bt2[i1],))
                k.op("pool", lambda e, i1=i1, m=m: e.tensor_tensor(out=mT[:, m, :], in0=t1[i1][:, :], in1=t2[i1][:, :], op=ALU.add), (bt1[i1], bt2[i1]), (bmT,))

        def mm2(t):
            ch, j = t // 4, t % 4
            mT, bmT = mTs[ch % 2], bmTs[ch % 2]
            xi, x3 = t % 2, t % 3
            k.dma("sp", xt3[x3][:, :], io["x"][t * 128:(t + 1) * 128, :], bx3[x3], (), (bx3[x3],))
            for n in range(2):
                mm(k, [(pmx[xi][:, n * 512:(n + 1) * 512], mT[:, f, j * 128:(j + 1) * 128], Wo[:, f, n * 512:(n + 1) * 512], f == 0, f == 7) for f in range(8)], (bmT, bWo), (bpx[xi],))

        def tail(t):
            xi, x3 = t % 2, t % 3
            norm_res_tail(k, c, tbf, t, pmx[xi][:, :], bpx[xi], g2, bg2, xt3[x3], bx3[x3], io["x1_s"][t * 128:(t + 1) * 128, :])

        NCH = T // 512
        head(0)
        for ch in range(NCH):
            gate(ch)
            for j in range(4):
                t = ch * 4 + j
                mm2(t)
                if t >= 1:
                    tail(t - 1)
                if j == 1 and ch + 1 < NCH:
                    head(ch + 1)
        tail(NTT - 1)
        k.end("D")


def phase_E(k, c, io):
    nc = k.nc
    CH = 512
    with ExitStack() as ps:
        k.begin()
        sb = lambda n, s, d: ps.enter_context(nc.sbuf_tensor(n, s, d))
        W1 = sb("E_W1", [128, 8, 4096], BF16)
        W2 = sb("E_W2", [128, 32, 1024], BF16)
        bW1s, bW2 = [Buf() for _ in range(8)], Buf()
        stg, bstg = load_w_scaled(k, ps, io["w_mlp1"], 8, 4096, io["g_pre_mlp"], "E_w1", W1, bW1s, half=512)
        for f2 in range(64):
            f, hf = f2 // 2, f2 % 2
            s, b = stg[f2 % 2], bstg[f2 % 2]
            k.dma("sp", s[:, :], io["w_mlp2"][f * 128:(f + 1) * 128, hf * 512:(hf + 1) * 512], b, (), (b,))
            if f2 % 2:
                k.op("act", lambda e, s=s, f=f, hf=hf: e.copy(out=W2[:, f, hf * 512:(hf + 1) * 512], in_=s[:, :]), (b,), (bW2,))
            else:
                k.op("dve", lambda e, s=s, f=f, hf=hf: e.tensor_copy(out=W2[:, f, hf * 512:(hf + 1) * 512], in_=s[:, :]), (b,), (bW2,))
        g4, bg4 = load_gb(k, ps, io["g_post_mlp"], "E_g4")
        xt = [sb("E_x%d" % i, [128, 1024], F32) for i in range(4)]
        bx = [Buf() for _ in range(4)]
        junk = sb("E_junk", [128, 1024], BF16)
        bj = Buf()
        ss = [sb("E_ss%d" % i, [128, 1], F32) for i in range(2)]
        rs = [sb("E_rs%d" % i, [128, 1], F32) for i in range(2)]
        bss = [Buf(), Buf()]
        brs = [Buf(), Buf()]
        hb = [sb("E_hb0", [128, 1024], BF16)] * 2
        bhb = [Buf()] * 2
        pT = ps.enter_context(nc.psum_tensor("E_pT", [128, 1024], BF16))
        bpT = Buf()
        hT = sb("E_hT", [128, 8, CH], BF16)
        bhT = Buf()
        pm = [ps.enter_context(nc.psum_tensor("E_pm%d" % i, [128, 512], F32)) for i in range(3)]
        bpm = [Buf() for _ in range(3)]
        pmx = [ps.enter_context(nc.psum_tensor("E_px%d" % i, [128, 1024], F32)) for i in range(2)]
        bpx = [Buf(), Buf()]
        rl = [sb("E_rl0", [128, CH], F32)] * 2
        brl = [Buf()] * 2
        f1T = sb("E_f1T", [128, 32, CH], BF16)
        bf1 = Buf()
        tbf = TailBufs(k, sb, "E", n=1, inplace=True)
        ipm = 0
        nj = CH // 128
        for ch in range(T // CH):
            for j in range(nj):
                t = ch * nj + j
                xi = t % 4
                hi = t % 2
                k.dma("sp", xt[xi][:, :], io["x1_s"][t * 128:(t + 1) * 128, :], bx[xi], (), (bx[xi],))
                rms_stats(k, c, xt[xi][:, :], junk[:, :], ss[hi][:, :], rs[hi][:, :], bx[xi], bj, bss[hi], brs[hi])
                k.op("act", lambda e, xi=xi, hi=hi: e.activation(out=hb[hi][:, :], in_=xt[xi][:, :], func=AF.Copy, scale=rs[hi][:, :]), (bx[xi], brs[hi]), (bhb[hi],))
                tr(k, [(pT[:, f * 128:(f + 1) * 128], hb[hi][:, f * 128:(f + 1) * 128], c.ident[:, :]) for f in range(8)], (bhb[hi],), (bpT,))
                k.op("dve", lambda e, j=j: e.tensor_copy(out=hT[:, :, j * 128:(j + 1) * 128], in_=pT[:, :].rearrange("p (f t) -> p f t", f=8)), (bpT,), (bhT,))
            for m in range(32):
                p = ipm % 3
                ipm += 1
                mm(k, [(pm[p][:, 0:CH], W1[:, f, m * 128:(m + 1) * 128], hT[:, f, :], f == 0, f == 7) for f in range(8)], (bhT, bW1s[m // 4]), (bpm[p],))
                ri = m % 2
                k.op("act", lambda e, p=p, ri=ri: e.activation(out=rl[ri][:, :], in_=pm[p][:, 0:CH], func=AF.Relu), (bpm[p],), (brl[ri],))
                k.op("pool", lambda e, ri=ri, m=m: e.tensor_tensor(out=f1T[:, m, :], in0=rl[ri][:, :], in1=rl[ri][:, :], op=ALU.mult), (brl[ri],), (bf1,))
            for j in range(nj):
                t = ch * nj + j
                xi = t % 4
                oi = t % 2
                for n in range(2):
                    mm(k, [(pmx[oi][:, n * 512:(n + 1) * 512], f1T[:, f, j * 128:(j + 1) * 128], W2[:, f, n * 512:(n + 1) * 512], f == 0, f == 31) for f in range(32)], (bf1, bW2), (bpx[oi],))
                norm_res_tail(k, c, tbf, t, pmx[oi][:, :], bpx[oi], g4, bg4, xt[xi], bx[xi], io["x2_s"][t * 128:(t + 1) * 128, :])
        k.end("E")


def phase_F(k, c, io):
    nc = k.nc
    with ExitStack() as ps:
        k.begin()
        sb = lambda n, s, d: ps.enter_context(nc.sbuf_tensor(n, s, d))
        Wp = sb("F_Wp", [128, 2, 1024], BF16)
        Wg = sb("F_Wg", [128, 8, 1024], BF16)
        bWp, bWg = Buf(), Buf()
        load_w_cast(k, ps, io["w_ple"], 2, 1024, "F_wp", Wp, bWp, "dve")
        load_w_cast(k, ps, io["w_ple_gate"], 8, 1024, "F_wg", Wg, bWg, "act")
        g5, bg5 = load_gb(k, ps, io["g_ple"], "F_g5")
        NB = 3
        xt = [sb("F_x%d" % i, [128, 1024], F32) for i in range(4)]
        bx = [Buf() for _ in range(4)]
        pt = [sb("F_p%d" % i, [128, 256], F32) for i in range(NB)]
        bp = [Buf() for _ in range(NB)]
        xb = [sb("F_xb%d" % i, [128, 1024], BF16) for i in range(NB)]
        bxb = [Buf() for _ in range(NB)]
        pb = [sb("F_pb%d" % i, [128, 256], BF16) for i in range(NB)]
        bpb = [Buf() for _ in range(NB)]
        pTx = ps.enter_context(nc.psum_tensor("F_pTx", [128, 1024], BF16))
        pTp = ps.enter_context(nc.psum_tensor("F_pTp", [128, 1024], BF16))
        bpTx, bpTp = Buf(), Buf()
        xT = [sb("F_xT%d" % i, [128, 8, 128], BF16) for i in range(NB)]
        bxT = [Buf() for _ in range(NB)]
        pT = [sb("F_pT%d" % i, [128, 2, 128], BF16) for i in range(NB)]
        bpT = [Buf() for _ in range(NB)]
        ppw = ps.enter_context(nc.psum_tensor("F_ppw", [128, 1024], F32))
        bppw = Buf()
        psg = [ps.enter_context(nc.psum_tensor("F_psg%d" % i, [128, 1024], F32)) for i in range(2)]
        bpsg = [Buf(), Buf()]
        sgss = [sb("F_sgs%d" % i, [128, 1024], F32) for i in range(2)]
        bsgss = [Buf(), Buf()]
        ees = [sb("F_e%d" % i, [128, 1024], F32) for i in range(2)]
        bees = [Buf(), Buf()]
        tbf = TailBufs(k, sb, "F")

        def head(t):
            xi, i3 = t % 4, t % NB
            k.dma("sp", xt[xi][:, :], io["x2_s"][t * 128:(t + 1) * 128, :], bx[xi], (), (bx[xi],))
            k.dma("sp", pt[i3][:, :], io["p"][t * 128:(t + 1) * 128, :], bp[i3], (), (bp[i3],))
            k.op("act", lambda e: e.copy(out=xb[i3][:, :], in_=xt[xi][:, :]), (bx[xi],), (bxb[i3],))
            k.op("dve", lambda e: e.tensor_copy(out=pb[i3][:, :], in_=pt[i3][:, :]), (bp[i3],), (bpb[i3],))
            tr(k, [(pTx[:, f * 128:(f + 1) * 128], xb[i3][:, f * 128:(f + 1) * 128], c.ident[:, :]) for f in range(8)], (bxb[i3],), (bpTx,))
            k.op("dve", lambda e: e.tensor_copy(out=xT[i3][:, :, :], in_=pTx[:, :].rearrange("p (f t) -> p f t", f=8)), (bpTx,), (bxT[i3],))
            tr(k, [(pTp[:, f * 128:(f + 1) * 128], pb[i3][:, f * 128:(f + 1) * 128], c.ident[:, :]) for f in range(2)], (bpb[i3],), (bpTp,))
            k.op("dve", lambda e: e.tensor_copy(out=pT[i3][:, :, :], in_=pTp[:, 0:256].rearrange("p (f t) -> p f t", f=2)), (bpTp,), (bpT[i3],))

        def mid_sg(t):
            i2, i3 = t % 2, t % NB
            for n in range(2):
                mm(k, [(psg[i2][:, n * 512:(n + 1) * 512], xT[i3][:, f, :], Wg[:, f, n * 512:(n + 1) * 512], f == 0, f == 7) for f in range(8)], (bxT[i3], bWg), (bpsg[i2],))

        def mid_pw(t):
            i3 = t % NB
            for n in range(2):
                mm(k, [(ppw[:, n * 512:(n + 1) * 512], pT[i3][:, f, :], Wp[:, f, n * 512:(n + 1) * 512], f == 0, f == 1) for f in range(2)], (bpT[i3], bWp), (bppw,))

        def tail(t):
            xi, i2 = t % 4, t % 2
            sgs, bsgs, ee, bee = sgss[i2], bsgss[i2], ees[i2], bees[i2]
            k.op("act", lambda e: e.activation(out=sgs[:, :], in_=psg[i2][:, :], func=AF.Sigmoid), (bpsg[i2],), (bsgs,))
            k.op("dve", lambda e: e.tensor_tensor(out=ee[:, :], in0=ppw[:, :], in1=sgs[:, :], op=ALU.mult), (bppw, bsgs), (bee,))
            norm_res_tail(k, c, tbf, t, ee[:, :], bee, g5, bg5, xt[xi], bx[xi], io["out"][t * 128:(t + 1) * 128, :])

        head(0)
        head(1)
        for t in range(NTT):
            mid_sg(t)
            if t >= 1:
                tail(t - 1)
            mid_pw(t)
            if t + 2 < NTT:
                head(t + 2)
        tail(NTT - 1)
        k.end("F")


def phase_C(k, c, io):
    nc = k.nc
    with ExitStack() as ps:
        k.begin()
        sb = lambda n, s, d: ps.enter_context(nc.sbuf_tensor(n, s, d))
        ioi = sb("C_ioi", [128, 64], I32)
        io2 = sb("C_io2", [128, 256], I32)
        io3 = sb("C_io3", [128, 2048], I32)
        negp = sb("C_negp", [128, 8, 64], F32)
        cm = sb("C_cm", [128, 2, 256], BF16)
        oh = sb("C_oh", [128, 64 * 128], BF16)
        bio, bnegp, bcm, boh, bio3 = Buf(), Buf(), Buf(), Buf(), Buf()
        k.op("pool", lambda e: e.iota(ioi[:, :], [[0, 8], [1, 8]], base=0, channel_multiplier=0), (), (bio,))
        for qb in range(8):
            k.op("dve", lambda e, qb=qb: e.tensor_scalar(out=negp[:, qb, :], in0=ioi[:, :], scalar1=float(qb), scalar2=-1e30, op0=ALU.is_ge, op1=ALU.mult), (bio,), (bnegp,))
        for h2 in range(2):
            k.op("pool", lambda e, h2=h2: e.iota(io2[:, :], [[1, 256]], base=-128 * h2, channel_multiplier=-1), (bcm,), (bio,))
            k.op("dve", lambda e, h2=h2: e.tensor_scalar(out=cm[:, h2, :], in0=io2[:, :], scalar1=0.0, scalar2=NEG, op0=ALU.is_lt, op1=ALU.mult), (bio,), (bcm,))
        for q4 in range(4):
            k.op("pool", lambda e, q4=q4: e.iota(io3[:, :], [[1, 16], [0, 128]], base=16 * q4, channel_multiplier=-1), (boh,), (bio3,))
            k.op("dve", lambda e, q4=q4: e.tensor_scalar(out=oh[:, q4 * 2048:(q4 + 1) * 2048], in0=io3[:, :], scalar1=0.0, scalar2=None, op0=ALU.is_equal), (bio3,), (boh,))
        qT = [sb("C_qT%d" % i, [128, 4, SEQ], BF16) for i in range(2)]
        kTzs = [sb("C_kTz%d" % i, [128, 8, SEQ], BF16) for i in range(2)]
        Va = [sb("C_V%d" % i, [128, 16, 8, 66], BF16) for i in range(2)]
        bq, bv, bks = [Buf(), Buf()], [Buf(), Buf()], [Buf(), Buf()]
        for i in range(2):
            k.op("pool", lambda e, i=i: e.memset(kTzs[i][:, :, :], 0.0), (), (bks[i],))
            k.op("pool", lambda e, i=i: e.memset(Va[i][:, :, :, :], 1.0), (), (bv[i],))
        kms = sb("C_kms", [128, 64], F32)
        kmbs = [sb("C_kmb%d" % i, [128, 8, 8], BF16) for i in range(2)]
        bkms, bkmbs = Buf(), [Buf(), Buf()]
        pgb = ps.enter_context(nc.psum_tensor("C_pgb", [128, 512], F32))
        bpg = [Buf(), Buf()]
        bpbt = [Buf(), Buf()]
        gms = [sb("C_gm%d" % i, [128, 64], F32) for i in range(2)]
        cmps = [sb("C_cmp%d" % i, [128, 512], F32) for i in range(2)]
        ranks = [sb("C_rank%d" % i, [128, 64], F32) for i in range(2)]
        biasbs = [sb("C_biasb%d" % i, [128, 128], F32) for i in range(2)]
        bgms, bcmps, branks, bbiasbs = ([Buf(), Buf()] for _ in range(4))
        biasTs = [sb("C_biasT%d" % i, [128, SEQ], BF16) for i in range(2)]
        bbTs = [Buf(), Buf()]
        for i in range(2):
            k.op("pool", lambda e, i=i: e.memset(biasbs[i][:, :], 0.0), (), (bbiasbs[i],))
        pss = [ps.enter_context(nc.psum_tensor("C_ps%d" % i, [128, 512], F32)) for i in range(3)]
        bps = [Buf(), Buf(), Buf()]
        po = [[ps.enter_context(nc.psum_tensor("C_po%d%d" % (i, j), [128, 512], F32)) for j in range(2)] for i in range(2)]
        bpo = [[Buf(), Buf()], [Buf(), Buf()]]
        pe_ = [sb("C_pe%d" % i, [128, 256], BF16) for i in range(3)]
        bpe = [Buf() for _ in range(3)]
        rden = sb("C_rden", [128, 1], F32)
        brd = Buf()
        atm = [sb("C_atm%d" % i, [128, 16, 512], BF16) for i in range(2)]
        batm = [Buf(), Buf()]
        cnt = {"ips": 0, "ipe": 0, "ipo": 0}

        def load_seq(s):
            si = s % 2
            tb = s * SEQ
            kTz, bk = kTzs[si], bks[si]
            k.dma("sp", qT[si][:, :, :], io["fm_s"][0:512, tb:tb + SEQ].rearrange("(m p) t -> p m t", p=128), bq[si], (), (bq[si],))
            for h in range(8):
                hp = slice((h % 2) * 64, (h % 2) * 64 + 64)
                k.dma("sp", kTz[hp, h, :], io["fm_s"][512 + h * 64:512 + (h + 1) * 64, tb:tb + SEQ], bk, (), (bk,))
            for kt in range(16):
                k.dma("sp", Va[si][:, kt, :, 0:64], io["z_s"][tb + kt * 128:tb + (kt + 1) * 128, 512:1024].rearrange("p (h d) -> p h d", h=8), bv[si], (), (bv[si],))
            k.op("dve", lambda e: e.tensor_reduce(out=kms[:, :], in_=kTz[:, :, :].rearrange("p h (b t) -> p (h b) t", b=8), axis=AX.X, op=ALU.add), (bk,), (bkms,))
            k.op("dve", lambda e: e.tensor_copy(out=kmbs[si][:, :, :], in_=kms[:, :].rearrange("p (h b) -> p h b", h=8)), (bkms,), (bkmbs[si],))

        def gate_step(s, qt):
            si = s % 2
            g2 = qt % 2
            qb = qt // 2
            gm, cmp, rank, biasb = gms[g2], cmps[g2], ranks[g2], biasbs[g2]
            pg = pgb[:, g2 * 64:(g2 + 1) * 64]
            pbt = pgb[:, 256 + g2 * 128:256 + (g2 + 1) * 128]

            def gfn(eng):
                ins = None
                for h in range(8):
                    ins = eng.matmul(pg[:, h * 8:(h + 1) * 8], qT[si][:, h // 2, qt * 128:(qt + 1) * 128], kmbs[si][:, h, :], start=True, stop=True)
                return ins
            k.op("pe", gfn, (bq[si], bkmbs[si]), (bpg[g2],))
            k.op("dve", lambda e: e.tensor_tensor(out=gm[:, :], in0=pg, in1=negp[:, qb, :], op=ALU.add), (bpg[g2], bnegp), (bgms[g2],))
            g3 = gm[:, :].rearrange("p (h b) -> p h b", h=8)
            k.op("dve", lambda e: e.tensor_tensor(out=cmp[:, :].rearrange("p (h b c) -> p h b c", h=8, b=8), in0=g3.unsqueeze(2).broadcast_to([128, 8, 8, 8]), in1=g3.unsqueeze(3).broadcast_to([128, 8, 8, 8]), op=ALU.is_gt), (bgms[g2],), (bcmps[g2],))
            k.op("dve", lambda e: e.tensor_reduce(out=rank[:, :], in_=cmp[:, :].rearrange("p (a c) -> p a c", c=8), axis=AX.X, op=ALU.add), (bcmps[g2],), (branks[g2],))
            k.op("dve", lambda e: e.tensor_scalar(out=biasb[:, 0:64], in0=rank[:, :], scalar1=2.5, scalar2=NEG, op0=ALU.is_gt, op1=ALU.mult), (branks[g2],), (bbiasbs[g2],))
            tr(k, [(pbt, biasb[:, :], c.identf[:, :])], (bbiasbs[g2],), (bpbt[g2],))
            k.op("dve", lambda e: e.tensor_copy(out=biasTs[si][:, qt * 128:(qt + 1) * 128], in_=pbt), (bpbt[g2],), (bbTs[si],))

        def sweep(s, inserts):
            si = s % 2
            kTz, bk = kTzs[si], bks[si]
            biasT, bbT = biasTs[si], bbTs[si]
            units = [(h, qb, kt) for h in range(8) for qb in range(8) for kt in range(2 * qb + 2)]

            def score(u, p):
                h, qb, kt = u
                m = h // 2
                b = kt // 2
                qs = slice(qb * 256, (qb + 1) * 256)
                last = (b != qb) and (qb <= 3)
                first = (pss[p][:, 0:256], kTz[:, h, kt * 128:(kt + 1) * 128], qT[si][:, m, qs], True, last)
                if b == qb:
                    items = [first, (pss[p][:, 0:256], c.ident[:, :], cm[:, kt % 2, :], False, True)]
                    rd = (bk, bq[si], bcm)
                elif last:
                    items = [first]
                    rd = (bk, bq[si])
                else:
                    r = h * 8 + b
                    items = [first, (pss[p][:, 0:256], oh[:, r * 128:(r + 1) * 128], biasT[:, qs], False, True)]
                    rd = (bk, bq[si], boh, bbT)
                mm(k, items, rd, (bps[p],))

            ips = cnt["ips"]
            score(units[0], ips % 3)
            score(units[1], (ips + 1) % 3)
            oi = 0
            for ui, u in enumerate(units):
                h, qb, kt = u
                nkt = 2 * qb + 2
                p = ips % 3
                ips += 1
                if ui + 2 < len(units):
                    score(units[ui + 2], (ips + 1) % 3)
                if kt == 0:
                    oi = cnt["ipo"] % 2
                    cnt["ipo"] += 1
                e_i = cnt["ipe"] % 3
                cnt["ipe"] += 1
                k.op("act", lambda e, p=p, e_i=e_i: e.activation(out=pe_[e_i][:, :], in_=pss[p][:, 0:256], func=AF.Exp, scale=0.125), (bps[p],), (bpe[e_i],))
                mm(k, [(po[oi][qh][:, 0:65], pe_[e_i][:, qh * 128:(qh + 1) * 128], Va[si][:, kt, h, 0:65], kt == 0, kt == nkt - 1) for qh in range(2)], (bpe[e_i], bv[si]), (bpo[oi][0], bpo[oi][1]))
                if kt == nkt - 1:
                    for qh in range(2):
                        qt = qb * 2 + qh
                        k.op("dve", lambda e, oi=oi, qh=qh: e.reciprocal(out=rden[:, :], in_=po[oi][qh][:, 64:65]), (bpo[oi][qh],), (brd,))
                        k.op("dve", lambda e, oi=oi, qh=qh, qt=qt, h=h: e.tensor_scalar(out=atm[si][:, qt, h * 64:(h + 1) * 64], in0=po[oi][qh][:, 0:64], scalar1=rden[:, :], scalar2=None, op0=ALU.mult), (bpo[oi][qh], brd), (batm[si],))
                if ui in inserts:
                    inserts[ui]()
            cnt["ips"] = ips
            tb = s * SEQ
            k.dma("pool", io["at_s"][tb:tb + SEQ, :].rearrange("(q p) c -> p q c", p=128), atm[si][:, :, :], batm[si], (batm[si],), ())

        load_seq(0)
        for qt in range(16):
            gate_step(0, qt)
        for s in range(BPC):
            inserts = {}
            if s + 1 < BPC:
                inserts[8] = (lambda s=s: load_seq(s + 1))
                for qt in range(16):
                    inserts[40 + qt * 30] = (lambda s=s, qt=qt: gate_step(s + 1, qt))
            sweep(s, inserts)
        k.end("C")


TWO_PI = 2.0 * math.pi


def cis(k, scr, first, out_c, out_s, barg, bout):
    t, ti, y, m1 = scr
    bt = Buf()
    k.op("dve", lambda e: first(e, t), (barg,), (bt,))
    k.op("dve", lambda e: e.tensor_copy(out=ti, in_=t), (bt,), (bt,))
    k.op("dve", lambda e: e.tensor_copy(out=y, in_=ti), (bt,), (bt,))
    k.op("dve", lambda e: e.tensor_tensor(out=t, in0=t, in1=y, op=ALU.subtract), (bt,), (bt,))
    for shift, dst in ((0.0, out_s), (math.pi / 2, out_c)):
        k.op("dve", lambda e, shift=shift: e.tensor_scalar(out=y, in0=t, scalar1=TWO_PI, scalar2=shift, op0=ALU.mult, op1=ALU.add), (bt,), (bt,))
        k.op("dve", lambda e: e.tensor_scalar(out=m1, in0=y, scalar1=math.pi, scalar2=-TWO_PI, op0=ALU.is_gt, op1=ALU.mult), (bt,), (bt,))
        k.op("dve", lambda e: e.tensor_tensor(out=m1, in0=m1, in1=y, op=ALU.add), (bt,), (bt,))
        k.op("dve", lambda e: e.tensor_scalar(out=y, in0=y, scalar1=-math.pi, scalar2=TWO_PI, op0=ALU.is_lt, op1=ALU.mult), (bt,), (bt,))
        k.op("dve", lambda e: e.tensor_tensor(out=y, in0=m1, in1=y, op=ALU.add), (bt,), (bt,))
        k.op("dve", lambda e: e.tensor_scalar(out=y, in0=y, scalar1=math.pi, scalar2=-math.pi, op0=ALU.min, op1=ALU.max), (bt,), (bt,))
        k.op("act", lambda e, dst=dst: e.activation(out=dst, in_=y, func=AF.Sin), (bt,), (bout, bt))


def cmul(k, eng, out_r, out_i, ar, ai, br, bi, t1, t2, rd, wr, bt, neg_i=False):
    E = eng
    k.op(E, lambda e: e.tensor_tensor(out=t1, in0=ar, in1=br, op=ALU.mult), rd, (bt,))
    k.op(E, lambda e: e.tensor_tensor(out=t2, in0=ai, in1=bi, op=ALU.mult), rd + (bt,), (bt,))
    k.op(E, lambda e: e.tensor_tensor(out=out_r, in0=t1, in1=t2, op=ALU.subtract), (bt,), wr + (bt,))
    k.op(E, lambda e: e.tensor_tensor(out=t1, in0=ar, in1=bi, op=ALU.mult), rd + (bt,), (bt,))
    k.op(E, lambda e: e.tensor_tensor(out=t2, in0=ai, in1=br, op=ALU.mult), rd + (bt,), (bt,))
    if neg_i:
        k.op(E, lambda e: e.scalar_tensor_tensor(out=out_i, in0=t1, scalar=-1.0, in1=t2, op0=ALU.mult, op1=ALU.subtract), (bt,), wr + (bt,))
    else:
        k.op(E, lambda e: e.tensor_tensor(out=out_i, in0=t1, in1=t2, op=ALU.add), (bt,), wr + (bt,))


def phase_S(k, c, io):
    nc = k.nc
    with ExitStack() as outer:
        osb = lambda n, s, d: outer.enter_context(nc.sbuf_tensor(n, s, d))
        GT = osb("S_GT", [128, G, 2, 128], BF16)
        TT = osb("S_TT", [128, G, 2, 256], BF16)
        H = osb("S_H", [128, G, 256], BF16)
        Mt = osb("S_M", [128, 2, 2048], F32)
        Dt = osb("S_D", [128, 2, 2048], F32)
        s5_setup(k, c, io, GT, TT, H, Mt, Dt)
        s5_main(k, c, io, GT, TT, H, Mt, Dt)


def s5_setup(k, c, io, GT, TT, H, Mt, Dt):
    nc = k.nc
    with ExitStack() as ps:
        k.begin()
        sb = lambda n, s, d: ps.enter_context(nc.sbuf_tensor(n, s, d))
        sb2 = lambda n, s, d: ps.enter_context(nc.sbuf_tensor(n, [s[0], int(np.prod(s[1:]))], d))
        B0 = Buf()
        lg = sb("u_lg", [32, 128], F32)
        k.dma("sp", lg[:, 0:64], io["ssm_lam_re"], B0, (), (B0,))
        k.dma("sp", lg[:, 64:128], io["ssm_lam_im"], B0, (), (B0,))
        ldt = sb("u_ldt", [64, 32], F32)
        k.dma("sp", ldt[:, :], io["ssm_log_dt"].partition_broadcast(64), B0, (), (B0,))
        Bre = sb("u_Bre", [64, G, 16], F32)
        Bim = sb("u_Bim", [64, G, 16], F32)
        k.dma("sp", Bre[:, :, :], io["ssm_b_re"].rearrange("g p c -> p g c"), B0, (), (B0,))
        k.dma("sp", Bim[:, :, :], io["ssm_b_im"].rearrange("g p c -> p g c"), B0, (), (B0,))
        cg = [sb("u_cg%d" % i, [128, 4, 64], F32) for i in range(2)]
        k.dma("sp", cg[0][:, :, :], io["ssm_c_re"].rearrange("(a b) c p -> (b c) a p", a=4), B0, (), (B0,))
        k.dma("sp", cg[1][:, :, :], io["ssm_c_im"].rearrange("(a b) c p -> (b c) a p", a=4), B0, (), (B0,))
        dcol = sb("u_dcol", [128, G], F32)
        for j in range(8):
            k.dma("sp", dcol[j * 16:(j + 1) * 16, :], io["ssm_d"].rearrange("(g c) -> c g", c=16), B0, (), (B0,), allow_slow_non_contiguous=True)
        pp = ps.enter_context(nc.psum_tensor("u_pp", [128, 512], F32))
        pq = ps.enter_context(nc.psum_tensor("u_pq", [128, 512], F32))
        Bp = Buf()
        lre = sb("u_lre", [64, G], F32)
        lim = sb("u_lim", [64, G], F32)
        tr(k, [(pp[0:64, 0:32], lg[:, 0:64], c.identf[0:32, 0:32]), (pp[0:64, 32:64], lg[:, 64:128], c.identf[0:32, 0:32])], (B0,), (Bp,))
        k.op("dve", lambda e: e.tensor_copy(out=lre[:, :], in_=pp[0:64, 0:32]), (Bp,), (B0,))
        k.op("dve", lambda e: e.tensor_copy(out=lim[:, :], in_=pp[0:64, 32:64]), (Bp,), (B0, Bp))
        Cre = sb("u_Cre", [64, G, 16], F32)
        Cim = sb("u_Cim", [64, G, 16], F32)
        for i, Cx in enumerate((Cre, Cim)):
            tr(k, [(pp[0:64, a * 128:(a + 1) * 128], cg[i][:, a, :], c.identf[:, :]) for a in range(4)], (B0,), (Bp,))
            k.op("dve", lambda e, Cx=Cx: e.tensor_copy(out=Cx[:, :, :], in_=pp[0:64, :].rearrange("p (g c) -> p g c", c=16)), (Bp,), (B0, Bp))
        dt = sb("u_dt", [64, G], F32)
        k.op("act", lambda e: e.activation(out=dt[:, :], in_=ldt[:, :], func=AF.Exp), (B0,), (B0,))
        rho = sb("u_rho", [64, G], F32)
        th = sb("u_th", [64, G], F32)
        k.op("dve", lambda e: e.tensor_tensor(out=rho[:, :], in0=lre[:, :], in1=dt[:, :], op=ALU.mult), (B0,), (B0,))
        k.op("dve", lambda e: e.tensor_tensor(out=th[:, :], in0=lim[:, :], in1=dt[:, :], op=ALU.mult), (B0,), (B0,))
        nvi = sb("u_nvi", [64, 17], I32)
        nv = sb("u_nv", [64, 17], F32)
        k.op("pool", lambda e: e.iota(nvi[:, :], [[1, 17]], base=0, channel_multiplier=0), (B0,), (B0,))
        k.op("dve", lambda e: e.tensor_copy(out=nv[:, :], in_=nvi[:, :]), (B0,), (B0,))
        NP = G * 17
        argt = sb("u_argt", [64, NP], F32)
        argr = sb("u_argr", [64, NP], F32)
        a3 = lambda t: t[:, :].rearrange("p (g n) -> p g n", n=17)
        bth = th[:, :].unsqueeze(2).broadcast_to([64, G, 17])
        brho = rho[:, :].unsqueeze(2).broadcast_to([64, G, 17])
        bnv = nv[:, :].unsqueeze(1).broadcast_to([64, G, 17])
        k.op("dve", lambda e: e.tensor_tensor(out=a3(argt), in0=bth, in1=bnv, op=ALU.mult), (B0,), (B0,))
        k.op("dve", lambda e: e.tensor_tensor(out=a3(argr), in0=brho, in1=bnv, op=ALU.mult), (B0,), (B0,))
        cs = sb("u_cs", [64, NP], F32)
        sn = sb("u_sn", [64, NP], F32)
        scr = (sb("u_c1", [64, NP], F32)[:, :], sb("u_c2", [64, NP], I32)[:, :], sb("u_c3", [64, NP], F32)[:, :], sb("u_c4", [64, NP], F32)[:, :])
        cis(k, scr, lambda e, t: e.tensor_scalar(out=t, in0=argt[:, :], scalar1=1.0 / TWO_PI, scalar2=None, op0=ALU.mult), cs[:, :], sn[:, :], B0, B0)
        mg = sb("u_mg", [64, NP], F32)
        mgi = sb("u_mgi", [64, NP], F32)
        k.op("act", lambda e: e.activation(out=mg[:, :], in_=argr[:, :], func=AF.Exp), (B0,), (B0,))
        k.op("act", lambda e: e.activation(out=mgi[:, :], in_=argr[:, :], func=AF.Exp, scale=-1.0), (B0,), (B0,))
        Pr = sb("u_Pr", [64, G, 17], F32)
        Pi = sb("u_Pi", [64, G, 17], F32)
        Qr = sb("u_Qr", [64, G, 17], F32)
        Qi = sb("u_Qi", [64, G, 17], F32)
        f2 = lambda t: t[:, :, :].rearrange("p g n -> p (g n)")
        k.op("dve", lambda e: e.tensor_tensor(out=f2(Pr), in0=mg[:, :], in1=cs[:, :], op=ALU.mult), (B0,), (B0,))
        k.op("dve", lambda e: e.tensor_tensor(out=f2(Pi), in0=mg[:, :], in1=sn[:, :], op=ALU.mult), (B0,), (B0,))
        k.op("dve", lambda e: e.tensor_tensor(out=f2(Qr), in0=mgi[:, :], in1=cs[:, :], op=ALU.mult), (B0,), (B0,))
        k.op("dve", lambda e: e.scalar_tensor_tensor(out=f2(Qi), in0=mgi[:, :], scalar=-1.0, in1=sn[:, :], op0=ALU.mult, op1=ALU.mult), (B0,), (B0,))
        den = sb("u_den", [64, G], F32)
        tA = sb("u_tA", [64, G], F32)
        tB = sb("u_tB", [64, G], F32)
        nr = sb("u_nr", [64, G], F32)
        cr = sb("u_cr", [64, G], F32)
        ci = sb("u_ci", [64, G], F32)
        k.op("dve", lambda e: e.tensor_tensor(out=den[:, :], in0=lre[:, :], in1=lre[:, :], op=ALU.mult), (B0,), (B0,))
        k.op("dve", lambda e: e.tensor_tensor(out=tA[:, :], in0=lim[:, :], in1=lim[:, :], op=ALU.mult), (B0,), (B0,))
        k.op("dve", lambda e: e.tensor_tensor(out=den[:, :], in0=den[:, :], in1=tA[:, :], op=ALU.add), (B0,), (B0,))
        k.op("dve", lambda e: e.reciprocal(out=den[:, :], in_=den[:, :]), (B0,), (B0,))
        k.op("dve", lambda e: e.tensor_scalar(out=nr[:, :], in0=Pr[:, :, 1], scalar1=-1.0, scalar2=None, op0=ALU.add), (B0,), (B0,))
        k.op("dve", lambda e: e.tensor_tensor(out=tA[:, :], in0=nr[:, :], in1=lre[:, :], op=ALU.mult), (B0,), (B0,))
        k.op("dve", lambda e: e.tensor_tensor(out=tB[:, :], in0=Pi[:, :, 1], in1=lim[:, :], op=ALU.mult), (B0,), (B0,))
        k.op("dve", lambda e: e.tensor_tensor(out=tA[:, :], in0=tA[:, :], in1=tB[:, :], op=ALU.add), (B0,), (B0,))
        k.op("dve", lambda e: e.tensor_tensor(out=cr[:, :], in0=tA[:, :], in1=den[:, :], op=ALU.mult), (B0,), (B0,))
        k.op("dve", lambda e: e.tensor_tensor(out=tA[:, :], in0=Pi[:, :, 1], in1=lre[:, :], op=ALU.mult), (B0,), (B0,))
        k.op("dve", lambda e: e.tensor_tensor(out=tB[:, :], in0=nr[:, :], in1=lim[:, :], op=ALU.mult), (B0,), (B0,))
        k.op("dve", lambda e: e.tensor_tensor(out=tA[:, :], in0=tA[:, :], in1=tB[:, :], op=ALU.subtract), (B0,), (B0,))
        k.op("dve", lambda e: e.tensor_tensor(out=ci[:, :], in0=tA[:, :], in1=den[:, :], op=ALU.mult), (B0,), (B0,))
        bbr = sb("u_bbr", [64, G, 16], F32)
        bbi = sb("u_bbi", [64, G, 16], F32)
        w1 = sb("u_w1", [64, G, 16], F32)
        w2 = sb("u_w2", [64, G, 16], F32)
        bc16 = lambda t: t[:, :].unsqueeze(2).broadcast_to([64, G, 16])
        cmul(k, "dve", bbr[:, :, :], bbi[:, :, :], bc16(cr), bc16(ci), Bre[:, :, :], Bim[:, :, :], w1[:, :, :], w2[:, :, :], (B0,), (B0,), B0)
        mki = sb("u_mki", [128, 2, 256], I32)
        mask = sb("u_mask", [128, 2, 256], F32)
        idc = sb("u_idc", [128, 2, 256], F32)
        shid = sb("u_shid", [64, 128], F32)
        for ch in range(2):
            k.op("pool", lambda e, ch=ch: e.iota(mki[:, ch, :], [[16, 16], [0, 16]], base=15 - 128 * ch, channel_multiplier=-1), (B0,), (B0,))
        k.op("dve", lambda e: e.tensor_scalar(out=mask[:, :, :], in0=mki[:, :, :], scalar1=0.0, scalar2=None, op0=ALU.is_ge), (B0,), (B0,))
        for ch in range(2):
            k.op("pool", lambda e, ch=ch: e.iota(mki[:, ch, :], [[16, 16], [1, 16]], base=-128 * ch, channel_multiplier=-1), (B0,), (B0,))
        k.op("dve", lambda e: e.tensor_scalar(out=idc[:, :, :], in0=mki[:, :, :], scalar1=0.0, scalar2=None, op0=ALU.is_equal), (B0,), (B0,))
        k.op("pool", lambda e: e.iota(mki[0:64, 0, 0:128], [[1, 128]], base=-64, channel_multiplier=-1), (B0,), (B0,))
        k.op("dve", lambda e: e.tensor_scalar(out=shid[:, :], in0=mki[0:64, 0, 0:128], scalar1=0.0, scalar2=None, op0=ALU.is_equal), (B0,), (B0,))
        GC = 4
        Fr = sb("u_Fr", [64, GC, 16, 16], F32)
        Fi = sb("u_Fi", [64, GC, 16, 16], F32)
        Er = sb("u_Er", [64, GC, 17, 16], F32)
        nEi = sb("u_nEi", [64, GC, 17, 16], F32)
        Lr = sb("u_Lr", [64, GC, 16, 16], F32)
        Li = sb("u_Li", [64, GC, 16, 16], F32)
        x1 = sb("u_x1", [64, GC, 17, 16], F32)
        x2 = sb("u_x2", [64, GC, 17, 16], F32)
        tmask = sb("u_tmask", [128, 256], F32)
        for gc in range(G // GC):
            gs = slice(gc * GC, (gc + 1) * GC)
            qr = Qr[:, gs, 0:16].unsqueeze(3).broadcast_to([64, GC, 16, 16])
            qi = Qi[:, gs, 0:16].unsqueeze(3).broadcast_to([64, GC, 16, 16])
            br_ = bbr[:, gs, :].unsqueeze(2).broadcast_to([64, GC, 16, 16])
            bi_ = bbi[:, gs, :].unsqueeze(2).broadcast_to([64, GC, 16, 16])
            cmul(k, "dve", Fr[:, :, :, :], Fi[:, :, :, :], qr, qi, br_, bi_, x1[:, :, 0:16, :], x2[:, :, 0:16, :], (B0,), (B0,), B0)
            pr = Pr[:, gs, :].unsqueeze(3).broadcast_to([64, GC, 17, 16])
            pi = Pi[:, gs, :].unsqueeze(3).broadcast_to([64, GC, 17, 16])
            cr_ = Cre[:, gs, :].unsqueeze(2).broadcast_to([64, GC, 17, 16])
            ci_ = Cim[:, gs, :].unsqueeze(2).broadcast_to([64, GC, 17, 16])
            cmul(k, "dve", Er[:, :, :, :], nEi[:, :, :, :], pr, pi, cr_, ci_, x1[:, :, :, :], x2[:, :, :, :], (B0,), (B0,), B0, neg_i=True)
            l15r = Pr[:, gs, 15:16].unsqueeze(3).broadcast_to([64, GC, 16, 16])
            l15i = Pi[:, gs, 15:16].unsqueeze(3).broadcast_to([64, GC, 16, 16])
            cmul(k, "dve", Lr[:, :, :, :], Li[:, :, :, :], l15r, l15i, Fr[:, :, :, :], Fi[:, :, :, :], x1[:, :, 0:16, :], x2[:, :, 0:16, :], (B0,), (B0,), B0)
            for gl in range(GC):
                g = gc * GC + gl
                items = []
                for ch in range(2):
                    items.append((pp[:, ch * 128:ch * 128 + 64], Lr[:, gl, ch * 8:(ch + 1) * 8, :].rearrange("p j c -> p (j c)"), c.identf[0:64, 0:64]))
                    items.append((pp[:, ch * 128 + 64:ch * 128 + 128], Li[:, gl, ch * 8:(ch + 1) * 8, :].rearrange("p j c -> p (j c)"), c.identf[0:64, 0:64]))
                tr(k, items, (B0,), (Bp,))
                k.op("act", lambda e, g=g: e.copy(out=GT[:, g, :, :], in_=pp[:, 0:256].rearrange("p (a b) -> p a b", a=2)), (Bp,), (B0, Bp))
                for ch in range(2):
                    fr_ = Fr[:, gl, ch * 8:(ch + 1) * 8, :].rearrange("p j c -> p (j c)")
                    fi_ = Fi[:, gl, ch * 8:(ch + 1) * 8, :].rearrange("p j c -> p (j c)")
                    er_ = Er[:, gl, 0:16, :].rearrange("p i c -> p (i c)")
                    ei_ = nEi[:, gl, 0:16, :].rearrange("p i c -> p (i c)")
                    Bq = Buf()
                    mm(k, [(pq[:, 0:256], fr_, er_, True, False), (pq[:, 0:256], fi_, ei_, False, True)], (B0,), (Bq, Bp))
                    k.op("dve", lambda e, ch=ch: e.tensor_tensor(out=tmask[:, :], in0=pq[:, 0:256], in1=mask[:, ch, :], op=ALU.mult), (Bq, B0), (B0, Bp))
                    k.op("dve", lambda e, ch=ch, g=g: e.scalar_tensor_tensor(out=TT[:, g, ch, :], in0=idc[:, ch, :], scalar=dcol[:, g:g + 1], in1=tmask[:, :], op0=ALU.mult, op1=ALU.add), (B0,), (B0,))
                er1 = Er[:, gl, 1:17, :].rearrange("p i c -> p (i c)")
                ei1 = nEi[:, gl, 1:17, :].rearrange("p i c -> p (i c)")
                mm(k, [(pq[:, 256:512], c.identf[0:64, :], er1, True, False), (pq[:, 256:512], shid[:, :], ei1, False, True)], (B0,), (Bp,))
                k.op("act", lambda e, g=g: e.copy(out=H[:, g, :], in_=pq[:, 256:512]), (Bp,), (B0, Bp))
        k.end("S_setup")
    with ExitStack() as ps:
        k.begin()
        sb = lambda n, s, d: ps.enter_context(nc.sbuf_tensor(n, s, d))
        B0 = Buf()
        lb = sb("w_lb", [128, 2, 2048], F32)
        k.dma("sp", lb[:, 0, :], io["ssm_lam_re"].rearrange("g p -> (g p)").partition_broadcast(128), B0, (), (B0,))
        k.dma("sp", lb[:, 1, :], io["ssm_lam_im"].rearrange("g p -> (g p)").partition_broadcast(128), B0, (), (B0,))
        dtb = sb("w_dtb", [128, G], F32)
        k.dma("sp", dtb[:, :], io["ssm_log_dt"].partition_broadcast(128), B0, (), (B0,))
        k.op("act", lambda e: e.activation(out=dtb[:, :], in_=dtb[:, :], func=AF.Exp), (B0,), (B0,))
        for i in range(2):
            k.op("dve", lambda e, i=i: e.tensor_tensor(out=lb[:, i, :].rearrange("p (g q) -> p g q", q=64), in0=lb[:, i, :].rearrange("p (g q) -> p g q", q=64), in1=dtb[:, :].unsqueeze(2).broadcast_to([128, G, 64]), op=ALU.mult), (B0,), (B0,))
        nki = sb("w_nki", [128, 2], I32)
        nk = sb("w_nk", [128, 2], F32)
        nk2 = sb("w_nk2", [128, 2], F32)
        k.op("pool", lambda e: e.iota(nki[:, 0:1], [[0, 1]], base=1024, channel_multiplier=-16), (B0,), (B0,))
        k.op("pool", lambda e: e.iota(nki[:, 1:2], [[0, 1]], base=-1040, channel_multiplier=16), (B0,), (B0,))
        k.op("dve", lambda e: e.tensor_copy(out=nk[:, :], in_=nki[:, :]), (B0,), (B0,))
        k.op("dve", lambda e: e.tensor_scalar(out=nk2[:, :], in0=nk[:, :], scalar1=1.0 / TWO_PI, scalar2=None, op0=ALU.mult), (B0,), (B0,))
        wc = sb("w_c", [128, 2048], F32)
        ws = sb("w_s", [128, 2048], F32)
        scr = (sb("w_c1", [128, 2048], F32)[:, :], sb("w_c2", [128, 2048], I32)[:, :], sb("w_c3", [128, 2048], F32)[:, :], sb("w_c4", [128, 2048], F32)[:, :])
        for i, Tb in enumerate((Mt, Dt)):
            cis(k, scr, lambda e, t, i=i: e.tensor_scalar(out=t, in0=lb[:, 1, :], scalar1=nk2[:, i:i + 1], scalar2=None, op0=ALU.mult), wc[:, :], ws[:, :], B0, B0)
            k.op("act", lambda e, i=i, Tb=Tb: e.activation(out=Tb[:, 1, :], in_=lb[:, 0, :], func=AF.Exp, scale=nk[:, i:i + 1]), (B0,), (B0,))
            k.op("dve", lambda e, Tb=Tb: e.tensor_tensor(out=Tb[:, 0, :], in0=Tb[:, 1, :], in1=wc[:, :], op=ALU.mult), (B0,), (B0,))
            k.op("dve", lambda e, Tb=Tb: e.tensor_tensor(out=Tb[:, 1, :], in0=Tb[:, 1, :], in1=ws[:, :], op=ALU.mult), (B0,), (B0,))
        k.end("S_tables")


def s5_main(k, c, io, GT, TT, H, Mt, Dt):
    nc = k.nc
    with ExitStack() as ps:
        k.begin()
        sb = lambda n, s, d: ps.enter_context(nc.sbuf_tensor(n, s, d))
        bTab = Buf()
        Wg = sb("S_Wg", [128, 4, 512], BF16)
        bWg = Buf()
        load_w_cast(k, ps, io["w_glu"], 4, 512, "S_wg", Wg, bWg, "dve")
        bgl = sb("S_bgl", [128, 4], F32)
        bbgl = Buf()
        k.dma("sp", bgl[:, :], io["b_glu"].rearrange("(m p) -> p m", p=128), bbgl, (), (bbgl,), allow_slow_non_contiguous=True)
        UA = sb("S_UA", [128, 8192], BF16)
        UB = sb("S_UB", [128, 8192], BF16)
        bUA, bUB = Buf(), Buf()
        U16 = sb("S_U16", [128, G, 2, 128], BF16)
        bU16 = Buf()
        X = sb("S_X", [128, G, 128], BF16)
        bX = [Buf() for _ in range(8)]
        Stm = sb("S_Stm", [128, G, 128], BF16)
        bStm = [Buf() for _ in range(8)]
        ST = sb("S_ST", [128, G, 128], BF16)
        bST = [Buf() for _ in range(4)]
        pT = [ps.enter_context(nc.psum_tensor("S_pT%d" % i, [128, 1024], BF16)) for i in range(2)]
        bpT = [Buf(), Buf()]
        NPD = 2
        pd = [ps.enter_context(nc.psum_tensor("S_pd%d" % i, [128, 512], F32)) for i in range(NPD)]
        bpd = [Buf() for _ in range(NPD)]
        py = [ps.enter_context(nc.psum_tensor("S_py%d" % i, [128, 512], F32)) for i in range(3)]
        bpy = [Buf() for _ in range(3)]
        pgl = ps.enter_context(nc.psum_tensor("S_pgl", [128, 512], F32))
        bpgl = Buf()
        tq = [sb("S_tq%d" % i, [128, 256], F32) for i in range(4)]
        btq = [Buf() for _ in range(4)]
        gx2 = [sb("S_gx2%d" % i, [128, 512], F32) for i in range(3)]
        gu = [sb("S_gu%d" % i, [128, 512], F32) for i in range(3)]
        bgx = [Buf() for _ in range(3)]
        bgu = [Buf() for _ in range(3)]
        sgl = sb("S_sgl", [128, 512], F32)
        bsgl = Buf()
        s5st = [sb("S_s5%d" % i, [128, 4, 512], BF16) for i in range(2)]
        bs5 = [Buf(), Buf()]
        ipT = 0
        ipd = 0
        ipy = 0

        def cmod(pbank, Tb, dst, g0, bsrc, bdst):
            src = pbank[:, :].rearrange("p (g x) -> p g x", g=4)
            sre, sim = src[:, :, 0:64], src[:, :, 64:128]
            tre = Tb[:, 0, g0 * 64:(g0 + 4) * 64].rearrange("p (g q) -> p g q", g=4)
            tim = Tb[:, 1, g0 * 64:(g0 + 4) * 64].rearrange("p (g q) -> p g q", g=4)
            v = lambda t: t[:, :].rearrange("p (g q) -> p g q", g=4)
            k.op("dve", lambda e: e.tensor_tensor(out=v(tq[0]), in0=sre, in1=tre, op=ALU.mult), (bsrc, bTab), (btq[0],))
            k.op("dve", lambda e: e.tensor_tensor(out=v(tq[1]), in0=sim, in1=tim, op=ALU.mult), (bsrc, bTab), (btq[1],))
            k.op("dve", lambda e: e.tensor_tensor(out=v(tq[2]), in0=sre, in1=tim, op=ALU.mult), (bsrc, bTab), (btq[2],))
            k.op("dve", lambda e: e.tensor_tensor(out=v(tq[3]), in0=sim, in1=tre, op=ALU.mult), (bsrc, bTab), (btq[3],))
            k.op("pool", lambda e: e.tensor_tensor(out=dst[:, g0:g0 + 4, 0:64], in0=v(tq[0]), in1=v(tq[1]), op=ALU.subtract), (btq[0], btq[1]), (bdst,))
            k.op("pool", lambda e: e.tensor_tensor(out=dst[:, g0:g0 + 4, 64:128], in0=v(tq[2]), in1=v(tq[3]), op=ALU.add), (btq[2], btq[3]), (bdst,))

        for s in range(BPC):
            tb = s * SEQ
            Utm = UA[:, :].rearrange("p (j c) -> p j c", j=16)
            k.dma("sp", Utm, io["z_s"][tb:tb + SEQ, 0:512].rearrange("(k j) c -> k j c", j=16), bUA, (), (bUA,))
            for hf in range(2):
                k.op("dve", lambda e, hf=hf: e.tensor_copy(
                    out=UB[:, hf * 4096:(hf + 1) * 4096].rearrange("p (g j c) -> p g j c", g=16, j=16),
                    in_=UA[:, :].rearrange("p (j g c) -> p g j c", j=16, g=32)[:, hf * 16:(hf + 1) * 16, :, :]), (bUA,), (bUB,))
            Ug = UB[:, :].rearrange("p (g x) -> p g x", g=32)
            for g4 in range(8):
                p = ipT % 2
                ipT += 1
                tr(k, [(pT[p][:, (gl * 2 + ch) * 128:(gl * 2 + ch + 1) * 128], Ug[:, g4 * 4 + gl, ch * 128:(ch + 1) * 128], c.ident[:, :]) for gl in range(4) for ch in range(2)], (bUB,), (bpT[p],))
                k.op("act", lambda e, p=p, g4=g4: e.copy(out=U16[:, g4 * 4:(g4 + 1) * 4, :, :], in_=pT[p][:, :].rearrange("p (g a k) -> p g a k", g=4, a=2)), (bpT[p],), (bU16,))
            for g4 in range(8):
                p = ipd % NPD
                ipd += 1
                items = []
                for gl in range(4):
                    g = g4 * 4 + gl
                    for ch in range(2):
                        items.append((pd[p][:, gl * 128:(gl + 1) * 128], U16[:, g, ch, :], GT[:, g, ch, :], ch == 0, ch == 1))
                mm(k, items, (bU16, bTab), (bpd[p],))
                cmod(pd[p], Mt, X, g4 * 4, bpd[p], bX[g4])
            for g4 in range(8):
                p = ipd % NPD
                ipd += 1
                mm(k, [(pd[p][:, :], c.tri[:, :], X[:, g4 * 4:(g4 + 1) * 4, :].rearrange("p g x -> p (g x)"), True, True)], (bX[g4],), (bpd[p],))
                cmod(pd[p], Dt, Stm, g4 * 4, bpd[p], bStm[g4])
            for g8 in range(4):
                p = ipT % 2
                ipT += 1
                tr(k, [(pT[p][:, gl * 128:(gl + 1) * 128], Stm[:, g8 * 8 + gl, :], c.ident[:, :]) for gl in range(8)], (bStm[2 * g8], bStm[2 * g8 + 1]), (bpT[p],))
                k.op("act", lambda e, p=p, g8=g8: e.copy(out=ST[:, g8 * 8:(g8 + 1) * 8, :], in_=pT[p][:, :].rearrange("p (g k) -> p g k", g=8)), (bpT[p],), (bST[g8],))
            ygtm = UA[:, :].rearrange("p (i c) -> p i c", i=16)
            def y_front(gp):
                p = gp % 3
                items = []
                for g2 in range(2):
                    g = gp * 2 + g2
                    o = py[p][:, g2 * 256:(g2 + 1) * 256]
                    items += [(o, U16[:, g, 0, :], TT[:, g, 0, :], True, False), (o, U16[:, g, 1, :], TT[:, g, 1, :], False, False), (o, ST[:, g, :], H[:, g, :], False, True)]
                mm(k, items, (bU16, bST[gp // 4], bTab), (bpy[p],))
                k.op("act", lambda e: e.activation(out=gx2[p][:, :], in_=py[p][:, :], func=AF.Square), (bpy[p],), (bgx[p],))
                k.op("pool", lambda e: e.tensor_scalar(out=gx2[p][:, :], in0=gx2[p][:, :], scalar1=0.044715, scalar2=1.0, op0=ALU.mult, op1=ALU.add), (bgx[p],), (bgx[p],))

            def y_back(gp):
                p = gp % 3
                k.op("dve", lambda e: e.tensor_tensor(out=gu[p][:, :], in0=py[p][:, :], in1=gx2[p][:, :], op=ALU.mult), (bpy[p], bgx[p]), (bgu[p],))
                k.op("act", lambda e: e.activation(out=gu[p][:, :], in_=gu[p][:, :], func=AF.Sigmoid, scale=1.5957691216057308), (bgu[p],), (bgu[p],))
                k.op("dve", lambda e: e.tensor_tensor(
                    out=ygtm[:, :, gp * 32:(gp + 1) * 32].rearrange("p i (g c) -> p g i c", g=2),
                    in0=py[p][:, :].rearrange("p (g i c) -> p g i c", g=2, i=16),
                    in1=gu[p][:, :].rearrange("p (g i c) -> p g i c", g=2, i=16), op=ALU.mult), (bpy[p], bgu[p]), (bUA,))

            y_front(0)
            for gp in range(16):
                if gp + 1 < 16:
                    y_front(gp + 1)
                y_back(gp)
            ygT = UB[:, :].rearrange("p (ct t) -> p ct t", ct=4)
            for ib in range(8):
                p = ipT % 2
                ipT += 1
                tr(k, [(pT[p][:, (i2 * 4 + ct) * 128:(i2 * 4 + ct + 1) * 128], ygtm[:, ib * 2 + i2, ct * 128:(ct + 1) * 128], c.ident[:, :]) for i2 in range(2) for ct in range(4)], (bUA,), (bpT[p],))
                k.op("act", lambda e, p=p, ib=ib: e.copy(
                    out=UB[:, :].rearrange("p (ct k i) -> p i ct k", ct=4, i=16)[:, ib * 2:(ib + 1) * 2, :, :],
                    in_=pT[p][:, :].rearrange("p (i ct k) -> p i ct k", i=2, ct=4)), (bpT[p],), (bUB,))
            for ch in range(4):
                sidx = (s * 4 + ch) % 2
                for m in range(4):
                    mm(k, [(pgl[:, :], Wg[:, f, m * 128:(m + 1) * 128], ygT[:, f, ch * 512:(ch + 1) * 512], f == 0, f == 3) for f in range(4)], (bUB, bWg), (bpgl,))
                    k.op("act", lambda e, m=m: e.activation(out=sgl[:, :], in_=pgl[:, :], func=AF.Sigmoid, bias=bgl[:, m:m + 1]), (bpgl, bbgl), (bsgl,))
                    k.op("dve", lambda e, m=m, ch=ch, sidx=sidx: e.tensor_tensor(out=s5st[sidx][:, m, :], in0=sgl[:, :], in1=ygT[:, m, ch * 512:(ch + 1) * 512], op=ALU.mult), (bsgl, bUB), (bs5[sidx],))
                k.dma("pool", io["s5_s"][:, tb + ch * 512:tb + (ch + 1) * 512].rearrange("(m p) t -> p m t", p=128), s5st[sidx][:, :, :], bs5[sidx], (bs5[sidx],), ())
        k.end("S_main")
```

```python
import math
from contextlib import ExitStack
import numpy as np
import concourse.bass as bass
import concourse.mybir as mybir
from concourse.bass_utils import run_bass_kernel_spmd

F32 = mybir.dt.float32
BF16 = mybir.dt.bfloat16
I32 = mybir.dt.int32
AF = mybir.ActivationFunctionType
ALU = mybir.AluOpType
AX = mybir.AxisListType

NCORES = 8
D = 1024
SEQ = 2048
BPC = 4
T = BPC * SEQ
NTT = T // 128
G = 32
PST = 64
DFF = 4096
PLE = 256
EPS = 1e-6
NEG = -30000.0


class Buf:
    __slots__ = ("name", "w", "r", "ds")

    def __init__(self, name=""):
        self.name = name
        self.w = None
        self.r = []
        self.ds = None


class K:
    ENGS = ("pe", "dve", "act", "pool", "sp")

    def __init__(self, nc, es):
        self.nc = nc
        self.es = es
        self.esem = {e: es.enter_context(nc.semaphore("es_" + e)) for e in self.ENGS}
        self.cnt = {e: 0 for e in self.ENGS}
        self.dpool = [es.enter_context(nc.semaphore("ds%d" % i)) for i in range(48)]
        self.NHW = 36
        self.dcnt = [0] * len(self.dpool)
        self.ops = None
        with nc.Block() as block:
            def clr(eng):
                for sm in list(self.esem.values()) + self.dpool:
                    eng.sem_clear(sm)
            block.sync(clr)

    def begin(self):
        self.ops = {e: [] for e in self.ENGS}
        self.seen = {e: {} for e in self.ENGS}
        self.dnext = {False: 0, True: self.NHW}
        self.dused = set()
        self.phase_id = getattr(self, "phase_id", 0) + 1

    def _dsem(self, buf, sw):
        if buf.ds is None or buf.ds[0] != (self.phase_id, sw):
            lim = len(self.dpool) if sw else self.NHW
            assert self.dnext[sw] < lim, "out of dma sems"
            buf.ds = ((self.phase_id, sw), self.dnext[sw])
            self.dnext[sw] += 1
        return buf.ds[1]

    def _deps(self, eng, reads, writes):
        waits = {}
        def add(tok):
            if tok is None:
                return
            key, val = tok
            if key == ("e", "pe") and eng == "pe":
                return
            if self.seen[eng].get(key, 0) >= val:
                return
            if waits.get(key, 0) < val:
                waits[key] = val
        for b in reads:
            add(b.w)
        for b in writes:
            add(b.w)
            for t in b.r:
                add(t)
        for key, val in waits.items():
            self.seen[eng][key] = val
        return list(waits.items())

    def _mark(self, tok, reads, writes):
        for b in reads:
            b.r.append(tok)
        for b in writes:
            b.w = tok
            b.r = []

    def op(self, eng, fn, reads=(), writes=()):
        waits = self._deps(eng, reads, writes)
        self.cnt[eng] += 1
        tok = (("e", eng), self.cnt[eng])
        self._mark(tok, reads, writes)
        self.ops[eng].append(("c", fn, waits))

    def dma(self, eng, out, in_, sb, reads=(), writes=(), **kw):
        waits = self._deps(eng, reads, writes)
        si = self._dsem(sb, eng == "pool")
        self.dcnt[si] += 16
        self.dused.add(si)
        tok = (("d", si), self.dcnt[si])
        self._mark(tok, reads, writes)
        self.ops[eng].append(("d", (out, in_, si, kw), waits))

    def _sem(self, key):
        return self.esem[key[1]] if key[0] == "e" else self.dpool[key[1]]

    def end(self, name):
        nc = self.nc
        fin = [(("e", e), self.cnt[e]) for e in self.ENGS if e != "sp" and self.cnt[e] > 0]
        fin += [(("d", si), self.dcnt[si]) for si in sorted(self.dused)]
        ops = self.ops
        handles = {"pe": "tensor", "dve": "vector", "act": "scalar", "pool": "gpsimd", "sp": "sync"}
        with nc.Block() as block:
            for e in self.ENGS:
                def body(eng, e=e):
                    for kind, payload, waits in ops[e]:
                        for key, val in waits:
                            eng.wait_ge(self._sem(key), val)
                        if kind == "c":
                            ins = payload(eng)
                            ins.then_inc(self.esem[e], 1)
                        else:
                            out, in_, si, kw = payload
                            eng.dma_start(out=out, in_=in_, **kw).then_inc(self.dpool[si], 16)
                    if e == "sp":
                        for key, val in fin:
                            eng.wait_ge(self._sem(key), val)
                getattr(block, handles[e])(body)
        self.ops = None


def mm(k, items, reads, writes):
    def fn(eng):
        ins = None
        for (out, lhsT, rhs, st, sp) in items:
            ins = eng.matmul(out, lhsT, rhs, start=st, stop=sp)
        return ins
    k.op("pe", fn, reads, writes)


def tr(k, items, reads, writes):
    def fn(eng):
        ins = None
        for (out, in_, ident) in items:
            ins = eng.transpose(out, in_, ident)
        return ins
    k.op("pe", fn, reads, writes)


class Consts:
    pass


def make_consts(k, es):
    nc = k.nc
    c = Consts()
    c.ident = es.enter_context(nc.sbuf_tensor("ident", [128, 128], BF16))
    c.identf = es.enter_context(nc.sbuf_tensor("identf", [128, 128], F32))
    c.tri = es.enter_context(nc.sbuf_tensor("tri", [128, 128], BF16))
    c.nhalf = es.enter_context(nc.sbuf_tensor("nhalf", [128, 1], F32))
    c.ones = es.enter_context(nc.sbuf_tensor("onesb", [128, 128], BF16))
    io = es.enter_context(nc.sbuf_tensor("iota_i", [128, 128], I32))
    b_io, b_id, b_idf, b_tri, b_nh, b_on = (Buf() for _ in range(6))
    k.begin()
    k.op("pool", lambda e: e.iota(io[:, :], [[1, 128]], base=0, channel_multiplier=-1), (), (b_io,))
    k.op("dve", lambda e: e.tensor_scalar(out=c.ident[:, :], in0=io[:, :], scalar1=0.0, scalar2=None, op0=ALU.is_equal), (b_io,), (b_id,))
    k.op("dve", lambda e: e.tensor_scalar(out=c.identf[:, :], in0=io[:, :], scalar1=0.0, scalar2=None, op0=ALU.is_equal), (b_io,), (b_idf,))
    k.op("dve", lambda e: e.tensor_scalar(out=c.tri[:, :], in0=io[:, :], scalar1=0.0, scalar2=None, op0=ALU.is_gt), (b_io,), (b_tri,))
    k.op("dve", lambda e: e.memset(c.nhalf[:, :], -0.5), (), (b_nh,))
    k.op("dve", lambda e: e.memset(c.ones[:, :], 1.0), (), (b_on,))
    k.end("consts")
    return c


def rms_stats(k, c, x_ap, junk_ap, ss, rstd, bx, bj, bss, brs, n=1024):
    k.op("act", lambda e: e.activation(out=junk_ap, in_=x_ap, func=AF.Square, accum_out=ss), (bx,), (bj, bss))
    k.op("pool", lambda e: e.tensor_scalar(out=rstd, in0=ss, scalar1=1.0 / n, scalar2=EPS, op0=ALU.mult, op1=ALU.add), (bss,), (brs,))
    k.op("pool", lambda e: e.tensor_tensor(out=rstd, in0=rstd, in1=c.nhalf[:, :], op=ALU.pow), (brs,), (brs,))


def load_w_scaled(k, ps, w_d, kt, ncols, g_d, name, Wt, bW, half=2048, order=None):
    nc = k.nc
    gcol = ps.enter_context(nc.sbuf_tensor(name + "_g", [128, kt], F32))
    bg = Buf()
    k.dma("sp", gcol[:, :], g_d.rearrange("(f p) -> p f", p=128), bg, (), (bg,), allow_slow_non_contiguous=True)
    half = min(half, ncols)
    stg = [ps.enter_context(nc.sbuf_tensor(name + "_s%d" % i, [128, half], F32)) for i in range(2)]
    bs = [Buf(), Buf()]
    if isinstance(bW, list):
        assert half == 512
        pieces = [(f, ci * 512) for ci in (order or range(ncols // 512)) for f in range(kt)]
    else:
        pieces = [(f, c0) for f in range(kt) for c0 in range(0, ncols, half)]
    for i, (f, c0) in enumerate(pieces):
        s, b = stg[i % 2], bs[i % 2]
        bw = bW[c0 // 512] if isinstance(bW, list) else bW
        k.dma("sp", s[:, :], w_d[f * 128:(f + 1) * 128, c0:c0 + half], b, (), (b,))
        k.op("dve", lambda e, s=s, f=f, c0=c0: e.tensor_scalar(out=Wt[:, f, c0:c0 + half], in0=s[:, :], scalar1=gcol[:, f:f + 1], scalar2=None, op0=ALU.mult), (b, bg), (bw,))
    return stg, bs


def load_w_cast(k, ps, w_d, kt, ncols, name, Wt, bW, eng="act"):
    nc = k.nc
    half = 2048 if ncols > 2048 else ncols
    stg = [ps.enter_context(nc.sbuf_tensor(name + "_s%d" % i, [128, half], F32)) for i in range(2)]
    bs = [Buf(), Buf()]
    i = 0
    for f in range(kt):
        for c0 in range(0, ncols, half):
            s, b = stg[i % 2], bs[i % 2]
            k.dma("sp", s[:, :], w_d[f * 128:(f + 1) * 128, c0:c0 + half], b, (), (b,))
            if eng == "act":
                k.op("act", lambda e, s=s, f=f, c0=c0: e.copy(out=Wt[:, f, c0:c0 + half], in_=s[:, :]), (b,), (bW,))
            else:
                k.op("dve", lambda e, s=s, f=f, c0=c0: e.tensor_copy(out=Wt[:, f, c0:c0 + half], in_=s[:, :]), (b,), (bW,))
            i += 1


def phase_A(k, c, io):
    nc = k.nc
    with ExitStack() as ps:
        k.begin()
        sb = lambda n, s, d: ps.enter_context(nc.sbuf_tensor(n, s, d))
        W = sb("A_W", [128, 8, 4096], BF16)
        bWs = [Buf() for _ in range(8)]
        load_w_scaled(k, ps, io["w_in"], 8, 4096, io["g_pre_mix"], "A_w", W, bWs, half=512, order=[0, 3, 1, 2, 4, 5, 6, 7])
        xt = [sb("A_x%d" % i, [128, 1024], F32) for i in range(4)]
        bx = [Buf() for _ in range(4)]
        junk = sb("A_junk", [128, 1024], BF16)
        bj = Buf()
        ss = [sb("A_ss%d" % i, [128, 1], F32) for i in range(4)]
        rs = [sb("A_rs%d" % i, [128, 1], F32) for i in range(4)]
        bss = [Buf() for _ in range(4)]
        brs = [Buf() for _ in range(4)]
        hb = [sb("A_hb%d" % i, [128, 1024], BF16) for i in range(2)]
        bhb = [Buf(), Buf()]
        pT = [ps.enter_context(nc.psum_tensor("A_pT%d" % i, [128, 1024], BF16)) for i in range(2)]
        bpT = [Buf(), Buf()]
        hT = [sb("A_hT%d" % i, [128, 8, 512], BF16) for i in range(2)]
        bhT = [Buf(), Buf()]
        pm = [ps.enter_context(nc.psum_tensor("A_pm%d" % i, [128, 512], F32)) for i in range(6)]
        bpm = [Buf() for _ in range(6)]
        zst = [sb("A_z%d" % i, [128, 1024], BF16) for i in range(2)]
        bz = [Buf(), Buf()]
        fst = [sb("A_f%d" % i, [128, 8, 512], BF16) for i in range(3)]
        bf = [Buf() for _ in range(3)]
        ipm = 0
        ifs = 0
        def head_tile(ch, j):
            hTc, bhTc = hT[ch % 2], bhT[ch % 2]
            t = ch * 4 + j
            xi = t % 4
            hbi = t % 2
            k.dma("sp", xt[xi][:, :], io["x"][t * 128:(t + 1) * 128, :], bx[xi], (), (bx[xi],))
            rms_stats(k, c, xt[xi][:, :], junk[:, :], ss[xi][:, :], rs[xi][:, :], bx[xi], bj, bss[xi], brs[xi])
            k.op("act", lambda e: e.activation(out=hb[hbi][:, :], in_=xt[xi][:, :], func=AF.Copy, scale=rs[xi][:, :]), (bx[xi], brs[xi]), (bhb[hbi],))
            tr(k, [(pT[hbi][:, f * 128:(f + 1) * 128], hb[hbi][:, f * 128:(f + 1) * 128], c.ident[:, :]) for f in range(8)], (bhb[hbi],), (bpT[hbi],))
            k.op("dve", lambda e: e.tensor_copy(out=hTc[:, :, j * 128:(j + 1) * 128], in_=pT[hbi][:, :].rearrange("p (f t) -> p f t", f=8)), (bpT[hbi],), (bhTc,))

        for j in range(4):
            head_tile(0, j)
        for ch in range(T // 512):
            hTc, bhTc = hT[ch % 2], bhT[ch % 2]
            for j in range(4):
                t = ch * 4 + j
                zi = t % 2
                for n, c0 in enumerate((0, 1536)):
                    p = ipm % 6
                    ipm += 1
                    mm(k, [(pm[p][:, :], hTc[:, f, j * 128:(j + 1) * 128], W[:, f, c0:c0 + 512], f == 0, f == 7) for f in range(8)], (bhTc, bWs[c0 // 512]), (bpm[p],))
                    k.op("dve", lambda e, p=p, zi=zi, n=n: e.tensor_copy(out=zst[zi][:, n * 512:(n + 1) * 512], in_=pm[p][:, :]), (bpm[p],), (bz[zi],))
                k.dma("pool", io["z_s"][t * 128:(t + 1) * 128, :], zst[zi][:, :], bz[zi], (bz[zi],), ())
            for m in range(24):
                c0 = 512 + m * 128 if m < 8 else 2048 + (m - 8) * 128
                p = ipm % 6
                ipm += 1
                mm(k, [(pm[p][:, :], W[:, f, c0:c0 + 128], hTc[:, f, :], f == 0, f == 7) for f in range(8)], (bhTc, bWs[c0 // 512]), (bpm[p],))
                fi = ifs % 3
                if m < 8:
                    k.op("dve", lambda e, p=p, fi=fi, m=m: e.tensor_copy(out=fst[fi][:, m % 8, :], in_=pm[p][:, :]), (bpm[p],), (bf[fi],))
                else:
                    k.op("act", lambda e, p=p, fi=fi, m=m: e.activation(out=fst[fi][:, m % 8, :], in_=pm[p][:, :], func=AF.Sigmoid), (bpm[p],), (bf[fi],))
                if m % 8 == 7:
                    r0 = (m // 8) * 1024
                    k.dma("pool", io["fm_s"][r0:r0 + 1024, ch * 512:(ch + 1) * 512].rearrange("(m p) t -> p m t", p=128), fst[fi][:, :, :], bf[fi], (bf[fi],), ())
                    ifs += 1
                if m in (3, 8, 13, 18) and ch + 1 < T // 512:
                    head_tile(ch + 1, (3, 8, 13, 18).index(m))
        k.end("A")


IN_SPECS = [
    ("x", [T, D]), ("p", [T, PLE]), ("g_pre_mix", [D]), ("w_in", [D, 4096]),
    ("ssm_lam_re", [G, PST]), ("ssm_lam_im", [G, PST]), ("ssm_log_dt", [G]),
    ("ssm_b_re", [G, PST, 16]), ("ssm_b_im", [G, PST, 16]),
    ("ssm_c_re", [G, 16, PST]), ("ssm_c_im", [G, 16, PST]), ("ssm_d", [512]),
    ("w_glu", [512, 512]), ("b_glu", [512]), ("w_branch_a", [512, D]), ("w_branch_b", [512, D]),
    ("w_out", [D, D]), ("g_post_mix", [D]), ("g_pre_mlp", [D]), ("w_mlp1", [D, DFF]),
    ("w_mlp2", [DFF, D]), ("g_post_mlp", [D]), ("w_ple", [PLE, D]), ("w_ple_gate", [D, D]),
    ("g_ple", [D]),
]
SCRATCH = [
    ("z_s", [T, 1024], BF16),
    ("fm_s", [3072, T], BF16),
    ("s5_s", [512, T], BF16),
    ("at_s", [T, 512], BF16),
    ("x1_s", [T, D], F32),
    ("x2_s", [T, D], F32),
]


def build(phases="ASCDEF", debug=(), inject=()):
    nc = bass.Bass("TRN2", target_bir_lowering=False)
    io = {}
    for name, shape in IN_SPECS:
        io[name] = nc.dram_tensor(name, shape, F32, kind="ExternalInput").ap()
    for name, shape, dt in SCRATCH:
        kind = "ExternalOutput" if name in debug else ("ExternalInput" if name in inject else "Internal")
        io[name] = nc.dram_tensor(name, shape, dt, kind=kind).ap()
    io["out"] = nc.dram_tensor("out", [T, D], F32, kind="ExternalOutput").ap()
    with ExitStack() as es:
        k = K(nc, es)
        c = make_consts(k, es)
        if "A" in phases:
            phase_A(k, c, io)
        if "S" in phases:
            phase_S(k, c, io)
        if "C" in phases:
            phase_C(k, c, io)
        if "D" in phases:
            phase_D(k, c, io)
        if "E" in phases:
            phase_E(k, c, io)
        if "F" in phases:
            phase_F(k, c, io)
    return nc


def make_in_maps(inputs, ncores=NCORES):
    maps = []
    for ci in range(ncores):
        m = {}
        for name, shape in IN_SPECS:
            a = inputs[name]
            if name == "x":
                a = a[ci * BPC:(ci + 1) * BPC].reshape(T, D)
            elif name == "p":
                a = a[0, ci * BPC:(ci + 1) * BPC].reshape(T, PLE)
            else:
                a = a[0]
            m[name] = np.ascontiguousarray(a, dtype=np.float32).reshape(shape)
        maps.append(m)
    return maps


def kernel(**inputs):
    inputs = {k_: np.asarray(v) for k_, v in inputs.items()}
    nc = build()
    in_maps = make_in_maps(inputs)
    res = run_bass_kernel_spmd(nc, in_maps, core_ids=list(range(NCORES)))
    outs = [np.asarray(r["out"]).reshape(BPC, SEQ, D) for r in res.results]
    return np.concatenate(outs, axis=0).astype(np.float32)


def load_gb(k, ps, g_d, name):
    nc = k.nc
    gb = ps.enter_context(nc.sbuf_tensor(name, [128, D], F32))
    b = Buf()
    k.dma("sp", gb[:, :], g_d.partition_broadcast(128), b, (), (b,))
    return gb, b


class TailBufs:
    def __init__(self, k, sb, name, n=2, inplace=False):
        self.n = n
        self.inplace = inplace
        self.junk = [sb(name + "_junk%d" % i, [128, 1024], BF16) for i in range(n)]
        self.ss = [sb(name + "_tss%d" % i, [128, 1], F32) for i in range(n)]
        self.rs = [sb(name + "_trs%d" % i, [128, 1], F32) for i in range(n)]
        self.tmp = [sb(name + "_ttmp%d" % i, [128, 1024], F32) for i in range(n)]
        self.ost = self.tmp if inplace else [sb(name + "_tost%d" % i, [128, 1024], F32) for i in range(n)]
        self.b = [[Buf() for _ in range(5)] for _ in range(n)]


def norm_res_tail(k, c, tb, i, src_ap, bsrc, gb, bgb, xres, bxres, out_rows):
    i = i % tb.n
    bj, bss, brs, btmp, bost = tb.b[i]
    junk, ss, rs, tmp, ost = tb.junk[i], tb.ss[i], tb.rs[i], tb.tmp[i], tb.ost[i]
    rms_stats(k, c, src_ap, junk[:, :], ss[:, :], rs[:, :], bsrc, bj, bss, brs)
    k.op("dve", lambda e: e.scalar_tensor_tensor(out=tmp[:, :], in0=src_ap, scalar=rs[:, :], in1=gb[:, :], op0=ALU.mult, op1=ALU.mult), (bsrc, brs, bgb), (btmp,))
    if tb.inplace:
        bost = btmp
    k.op("pool", lambda e: e.tensor_tensor(out=ost[:, :], in0=tmp[:, :], in1=xres[:, :], op=ALU.add), (btmp, bxres), (bost,))
    k.dma("pool", out_rows, ost[:, :], bost, (bost,), ())


def phase_D(k, c, io):
    nc = k.nc
    with ExitStack() as ps:
        k.begin()
        sb = lambda n, s, d: ps.enter_context(nc.sbuf_tensor(n, s, d))
        Wa = sb("D_Wa", [128, 4, 1024], BF16)
        Wb = sb("D_Wb", [128, 4, 1024], BF16)
        Wo = sb("D_Wo", [128, 8, 1024], BF16)
        bWa, bWb, bWo = Buf(), Buf(), Buf()
        load_w_cast(k, ps, io["w_branch_a"], 4, 1024, "D_wa", Wa, bWa, "dve")
        load_w_cast(k, ps, io["w_branch_b"], 4, 1024, "D_wb", Wb, bWb, "act")
        load_w_cast(k, ps, io["w_out"], 8, 1024, "D_wo", Wo, bWo, "dve")
        g2, bg2 = load_gb(k, ps, io["g_post_mix"], "D_g2")
        gts = [sb("D_gt%d" % i, [128, 16, 512], BF16) for i in range(2)]
        bgt = [Buf(), Buf()]
        s5t = [sb("D_s5%d" % i, [128, 4, 512], BF16) for i in range(2)]
        bs5 = [Buf(), Buf()]
        att = [sb("D_at%d" % i, [128, 512], BF16) for i in range(4)]
        bat = [Buf() for _ in range(4)]
        atTs = [sb("D_atT%d" % i, [128, 4, 512], BF16) for i in range(2)]
        batTs = [Buf(), Buf()]
        pT = ps.enter_context(nc.psum_tensor("D_pT", [128, 1024], BF16))
        bpT = Buf()
        pm = [ps.enter_context(nc.psum_tensor("D_pm%d" % i, [128, 512], F32)) for i in range(3)]
        bpm = [Buf() for _ in range(3)]
        pmx = [ps.enter_context(nc.psum_tensor("D_px%d" % i, [128, 1024], F32)) for i in range(2)]
        bpx = [Buf(), Buf()]
        t1 = [sb("D_t1%d" % i, [128, 512], F32) for i in range(2)]
        bt1 = [Buf(), Buf()]
        t2 = [sb("D_t2%d" % i, [128, 512], F32) for i in range(2)]
        bt2 = [Buf(), Buf()]
        mTs = [sb("D_mT%d" % i, [128, 8, 512], BF16) for i in range(2)]
        bmTs = [Buf(), Buf()]
        xt = [sb("D_x%d" % i, [128, 1024], F32) for i in range(2)]
        bx = [Buf(), Buf()]
        tbf = TailBufs(k, sb, "D")
        ipm = [0]
        xt3 = xt + [sb("D_x2", [128, 1024], F32)]
        bx3 = bx + [Buf()]

        def head(ch):
            gi = ch % 2
            atT, batT = atTs[gi], batTs[gi]
            cs = slice(ch * 512, (ch + 1) * 512)
            k.dma("sp", gts[gi][:, :, :], io["fm_s"][1024:3072, cs].rearrange("(m p) t -> p m t", p=128), bgt[gi], (), (bgt[gi],))
            k.dma("sp", s5t[gi][:, :, :], io["s5_s"][:, cs].rearrange("(m p) t -> p m t", p=128), bs5[gi], (), (bs5[gi],))
            for j in range(4):
                t = ch * 4 + j
                k.dma("sp", att[j][:, :], io["at_s"][t * 128:(t + 1) * 128, :], bat[j], (), (bat[j],))
            for j in range(4):
                tr(k, [(pT[:, f * 128:(f + 1) * 128], att[j][:, f * 128:(f + 1) * 128], c.ident[:, :]) for f in range(4)], (bat[j],), (bpT,))
                k.op("dve", lambda e, j=j: e.tensor_copy(out=atT[:, :, j * 128:(j + 1) * 128], in_=pT[:, 0:512].rearrange("p (f t) -> p f t", f=4)), (bpT,), (batT,))

        def gate(ch):
            gi = ch % 2
            atT, batT = atTs[gi], batTs[gi]
            mT, bmT = mTs[gi], bmTs[gi]
            for m in range(8):
                p = ipm[0] % 3
                ipm[0] += 1
                mm(k, [(pm[p][:, :], Wa[:, f, m * 128:(m + 1) * 128], s5t[gi][:, f, :], f == 0, f == 3) for f in range(4)], (bs5[gi], bWa), (bpm[p],))
                i1 = m % 2
                k.op("dve", lambda e, p=p, i1=i1, m=m: e.tensor_tensor(out=t1[i1][:, :], in0=pm[p][:, :], in1=gts[gi][:, m, :], op=ALU.mult), (bpm[p], bgt[gi]), (bt1[i1],))
                p2 = ipm[0] % 3
                ipm[0] += 1
                mm(k, [(pm[p2][:, :], Wb[:, f, m * 128:(m + 1) * 128], atT[:, f, :], f == 0, f == 3) for f in range(4)], (batT, bWb), (bpm[p2],))
                k.op("dve", lambda e, p2=p2, i1=i1, m=m: e.tensor_tensor(out=t2[i1][:, :], in0=pm[p2][:, :], in1=gts[gi][:, 8 + m, :], op=ALU.mult), (bpm[p2], bgt[gi]), (bt2[i1],))
                k.op("pool", lambda e, i1=i1, m=m: e.tensor_tensor(out=mT[:, m, :], in0=t1[i1][:, :], in1=t2[i1][:, :], op=ALU.add), (bt1[i1], bt2[i1]), (bmT,))

        def mm2(t):
            ch, j = t // 4, t % 4
            mT, bmT = mTs[ch % 2], bmTs[ch % 2]
            xi, x3 = t % 2, t % 3
            k.dma("sp", xt3[x3][:, :], io["x"][t * 128:(t + 1) * 128, :], bx3[x3], (), (bx3[x3],))
            for n in range(2):
                mm(k, [(pmx[xi][:, n * 512:(n + 1) * 512], mT[:, f, j * 128:(j + 1) * 128], Wo[:, f, n * 512:(n + 1) * 512], f == 0, f == 7) for f in range(8)], (bmT, bWo), (bpx[xi],))

        def tail(t):
            xi, x3 = t % 2, t % 3
            norm_res_tail(k, c, tbf, t, pmx[xi][:, :], bpx[xi], g2, bg2, xt3[x3], bx3[x3], io["x1_s"][t * 128:(t + 1) * 128, :])

        NCH = T // 512
        head(0)
        for ch in range(NCH):
            gate(ch)
            for j in range(4):
                t = ch * 4 + j
                mm2(t)
                if t >= 1:
                    tail(t - 1)
                if j == 1 and ch + 1 < NCH:
                    head(ch + 1)
        tail(NTT - 1)
        k.end("D")


def phase_E(k, c, io):
    nc = k.nc
    CH = 512
    with ExitStack() as ps:
        k.begin()
        sb = lambda n, s, d: ps.enter_context(nc.sbuf_tensor(n, s, d))
        W1 = sb("E_W1", [128, 8, 4096], BF16)
        W2 = sb("E_W2", [128, 32, 1024], BF16)
        bW1s, bW2 = [Buf() for _ in range(8)], Buf()
        stg, bstg = load_w_scaled(k, ps, io["w_mlp1"], 8, 4096, io["g_pre_mlp"], "E_w1", W1, bW1s, half=512)
        for f2 in range(64):
            f, hf = f2 // 2, f2 % 2
            s, b = stg[f2 % 2], bstg[f2 % 2]
            k.dma("sp", s[:, :], io["w_mlp2"][f * 128:(f + 1) * 128, hf * 512:(hf + 1) * 512], b, (), (b,))
            if f2 % 2:
                k.op("act", lambda e, s=s, f=f, hf=hf: e.copy(out=W2[:, f, hf * 512:(hf + 1) * 512], in_=s[:, :]), (b,), (bW2,))
            else:
                k.op("dve", lambda e, s=s, f=f, hf=hf: e.tensor_copy(out=W2[:, f, hf * 512:(hf + 1) * 512], in_=s[:, :]), (b,), (bW2,))
        g4, bg4 = load_gb(k, ps, io["g_post_mlp"], "E_g4")
        xt = [sb("E_x%d" % i, [128, 1024], F32) for i in range(4)]
        bx = [Buf() for _ in range(4)]
        junk = sb("E_junk", [128, 1024], BF16)
        bj = Buf()
        ss = [sb("E_ss%d" % i, [128, 1], F32) for i in range(2)]
        rs = [sb("E_rs%d" % i, [128, 1], F32) for i in range(2)]
        bss = [Buf(), Buf()]
        brs = [Buf(), Buf()]
        hb = [sb("E_hb0", [128, 1024], BF16)] * 2
        bhb = [Buf()] * 2
        pT = ps.enter_context(nc.psum_tensor("E_pT", [128, 1024], BF16))
        bpT = Buf()
        hT = sb("E_hT", [128, 8, CH], BF16)
        bhT = Buf()
        pm = [ps.enter_context(nc.psum_tensor("E_pm%d" % i, [128, 512], F32)) for i in range(3)]
        bpm = [Buf() for _ in range(3)]
        pmx = [ps.enter_context(nc.psum_tensor("E_px%d" % i, [128, 1024], F32)) for i in range(2)]
        bpx = [Buf(), Buf()]
        rl = [sb("E_rl0", [128, CH], F32)] * 2
        brl = [Buf()] * 2
        f1T = sb("E_f1T", [128, 32, CH], BF16)
        bf1 = Buf()
        tbf = TailBufs(k, sb, "E", n=1, inplace=True)
        ipm = 0
        nj = CH // 128
        for ch in range(T // CH):
            for j in range(nj):
                t = ch * nj + j
                xi = t % 4
                hi = t % 2
                k.dma("sp", xt[xi][:, :], io["x1_s"][t * 128:(t + 1) * 128, :], bx[xi], (), (bx[xi],))
                rms_stats(k, c, xt[xi][:, :], junk[:, :], ss[hi][:, :], rs[hi][:, :], bx[xi], bj, bss[hi], brs[hi])
                k.op("act", lambda e, xi=xi, hi=hi: e.activation(out=hb[hi][:, :], in_=xt[xi][:, :], func=AF.Copy, scale=rs[hi][:, :]), (bx[xi], brs[hi]), (bhb[hi],))
                tr(k, [(pT[:, f * 128:(f + 1) * 128], hb[hi][:, f * 128:(f + 1) * 128], c.ident[:, :]) for f in range(8)], (bhb[hi],), (bpT,))
                k.op("dve", lambda e, j=j: e.tensor_copy(out=hT[:, :, j * 128:(j + 1) * 128], in_=pT[:, :].rearrange("p (f t) -> p f t", f=8)), (bpT,), (bhT,))
            for m in range(32):
                p = ipm % 3
                ipm += 1
                mm(k, [(pm[p][:, 0:CH], W1[:, f, m * 128:(m + 1) * 128], hT[:, f, :], f == 0, f == 7) for f in range(8)], (bhT, bW1s[m // 4]), (bpm[p],))
                ri = m % 2
                k.op("act", lambda e, p=p, ri=ri: e.activation(out=rl[ri][:, :], in_=pm[p][:, 0:CH], func=AF.Relu), (bpm[p],), (brl[ri],))
                k.op("pool", lambda e, ri=ri, m=m: e.tensor_tensor(out=f1T[:, m, :], in0=rl[ri][:, :], in1=rl[ri][:, :], op=ALU.mult), (brl[ri],), (bf1,))
            for j in range(nj):
                t = ch * nj + j
                xi = t % 4
                oi = t % 2
                for n in range(2):
                    mm(k, [(pmx[oi][:, n * 512:(n + 1) * 512], f1T[:, f, j * 128:(j + 1) * 128], W2[:, f, n * 512:(n + 1) * 512], f == 0, f == 31) for f in range(32)], (bf1, bW2), (bpx[oi],))
                norm_res_tail(k, c, tbf, t, pmx[oi][:, :], bpx[oi], g4, bg4, xt[xi], bx[xi], io["x2_s"][t * 128:(t + 1) * 128, :])
        k.end("E")


def phase_F(k, c, io):
    nc = k.nc
    with ExitStack() as ps:
        k.begin()
        sb = lambda n, s, d: ps.enter_context(nc.sbuf_tensor(n, s, d))
        Wp = sb("F_Wp", [128, 2, 1024], BF16)
        Wg = sb("F_Wg", [128, 8, 1024], BF16)
        bWp, bWg = Buf(), Buf()
        load_w_cast(k, ps, io["w_ple"], 2, 1024, "F_wp", Wp, bWp, "dve")
        load_w_cast(k, ps, io["w_ple_gate"], 8, 1024, "F_wg", Wg, bWg, "act")
        g5, bg5 = load_gb(k, ps, io["g_ple"], "F_g5")
        NB = 3
        xt = [sb("F_x%d" % i, [128, 1024], F32) for i in range(4)]
        bx = [Buf() for _ in range(4)]
        pt = [sb("F_p%d" % i, [128, 256], F32) for i in range(NB)]
        bp = [Buf() for _ in range(NB)]
        xb = [sb("F_xb%d" % i, [128, 1024], BF16) for i in range(NB)]
        bxb = [Buf() for _ in range(NB)]
        pb = [sb("F_pb%d" % i, [128, 256], BF16) for i in range(NB)]
        bpb = [Buf() for _ in range(NB)]
        pTx = ps.enter_context(nc.psum_tensor("F_pTx", [128, 1024], BF16))
        pTp = ps.enter_context(nc.psum_tensor("F_pTp", [128, 1024], BF16))
        bpTx, bpTp = Buf(), Buf()
        xT = [sb("F_xT%d" % i, [128, 8, 128], BF16) for i in range(NB)]
        bxT = [Buf() for _ in range(NB)]
        pT = [sb("F_pT%d" % i, [128, 2, 128], BF16) for i in range(NB)]
        bpT = [Buf() for _ in range(NB)]
        ppw = ps.enter_context(nc.psum_tensor("F_ppw", [128, 1024], F32))
        bppw = Buf()
        psg = [ps.enter_context(nc.psum_tensor("F_psg%d" % i, [128, 1024], F32)) for i in range(2)]
        bpsg = [Buf(), Buf()]
        sgss = [sb("F_sgs%d" % i, [128, 1024], F32) for i in range(2)]
        bsgss = [Buf(), Buf()]
        ees = [sb("F_e%d" % i, [128, 1024], F32) for i in range(2)]
        bees = [Buf(), Buf()]
        tbf = TailBufs(k, sb, "F")

        def head(t):
            xi, i3 = t % 4, t % NB
            k.dma("sp", xt[xi][:, :], io["x2_s"][t * 128:(t + 1) * 128, :], bx[xi], (), (bx[xi],))
            k.dma("sp", pt[i3][:, :], io["p"][t * 128:(t + 1) * 128, :], bp[i3], (), (bp[i3],))
            k.op("act", lambda e: e.copy(out=xb[i3][:, :], in_=xt[xi][:, :]), (bx[xi],), (bxb[i3],))
            k.op("dve", lambda e: e.tensor_copy(out=pb[i3][:, :], in_=pt[i3][:, :]), (bp[i3],), (bpb[i3],))
            tr(k, [(pTx[:, f * 128:(f + 1) * 128], xb[i3][:, f * 128:(f + 1) * 128], c.ident[:, :]) for f in range(8)], (bxb[i3],), (bpTx,))
            k.op("dve", lambda e: e.tensor_copy(out=xT[i3][:, :, :], in_=pTx[:, :].rearrange("p (f t) -> p f t", f=8)), (bpTx,), (bxT[i3],))
            tr(k, [(pTp[:, f * 128:(f + 1) * 128], pb[i3][:, f * 128:(f + 1) * 128], c.ident[:, :]) for f in range(2)], (bpb[i3],), (bpTp,))
            k.op("dve", lambda e: e.tensor_copy(out=pT[i3][:, :, :], in_=pTp[:, 0:256].rearrange("p (f t) -> p f t", f=2)), (bpTp,), (bpT[i3],))

        def mid_sg(t):
            i2, i3 = t % 2, t % NB
            for n in range(2):
                mm(k, [(psg[i2][:, n * 512:(n + 1) * 512], xT[i3][:, f, :], Wg[:, f, n * 512:(n + 1) * 512], f == 0, f == 7) for f in range(8)], (bxT[i3], bWg), (bpsg[i2],))

        def mid_pw(t):
            i3 = t % NB
            for n in range(2):
                mm(k, [(ppw[:, n * 512:(n + 1) * 512], pT[i3][:, f, :], Wp[:, f, n * 512:(n + 1) * 512], f == 0, f == 1) for f in range(2)], (bpT[i3], bWp), (bppw,))

        def tail(t):
            xi, i2 = t % 4, t % 2
            sgs, bsgs, ee, bee = sgss[i2], bsgss[i2], ees[i2], bees[i2]
            k.op("act", lambda e: e.activation(out=sgs[:, :], in_=psg[i2][:, :], func=AF.Sigmoid), (bpsg[i2],), (bsgs,))
            k.op("dve", lambda e: e.tensor_tensor(out=ee[:, :], in0=ppw[:, :], in1=sgs[:, :], op=ALU.mult), (bppw, bsgs), (bee,))
            norm_res_tail(k, c, tbf, t, ee[:, :], bee, g5, bg5, xt[xi], bx[xi], io["out"][t * 128:(t + 1) * 128, :])

        head(0)
        head(1)
        for t in range(NTT):
            mid_sg(t)
            if t >= 1:
                tail(t - 1)
            mid_pw(t)
            if t + 2 < NTT:
                head(t + 2)
        tail(NTT - 1)
        k.end("F")


def phase_C(k, c, io):
    nc = k.nc
    with ExitStack() as ps:
        k.begin()
        sb = lambda n, s, d: ps.enter_context(nc.sbuf_tensor(n, s, d))
        ioi = sb("C_ioi", [128, 64], I32)
        io2 = sb("C_io2", [128, 256], I32)
        io3 = sb("C_io3", [128, 2048], I32)
        negp = sb("C_negp", [128, 8, 64], F32)
        cm = sb("C_cm", [128, 2, 256], BF16)
        oh = sb("C_oh", [128, 64 * 128], BF16)
        bio, bnegp, bcm, boh, bio3 = Buf(), Buf(), Buf(), Buf(), Buf()
        k.op("pool", lambda e: e.iota(ioi[:, :], [[0, 8], [1, 8]], base=0, channel_multiplier=0), (), (bio,))
        for qb in range(8):
            k.op("dve", lambda e, qb=qb: e.tensor_scalar(out=negp[:, qb, :], in0=ioi[:, :], scalar1=float(qb), scalar2=-1e30, op0=ALU.is_ge, op1=ALU.mult), (bio,), (bnegp,))
        for h2 in range(2):
            k.op("pool", lambda e, h2=h2: e.iota(io2[:, :], [[1, 256]], base=-128 * h2, channel_multiplier=-1), (bcm,), (bio,))
            k.op("dve", lambda e, h2=h2: e.tensor_scalar(out=cm[:, h2, :], in0=io2[:, :], scalar1=0.0, scalar2=NEG, op0=ALU.is_lt, op1=ALU.mult), (bio,), (bcm,))
        for q4 in range(4):
            k.op("pool", lambda e, q4=q4: e.iota(io3[:, :], [[1, 16], [0, 128]], base=16 * q4, channel_multiplier=-1), (boh,), (bio3,))
            k.op("dve", lambda e, q4=q4: e.tensor_scalar(out=oh[:, q4 * 2048:(q4 + 1) * 2048], in0=io3[:, :], scalar1=0.0, scalar2=None, op0=ALU.is_equal), (bio3,), (boh,))
        qT = [sb("C_qT%d" % i, [128, 4, SEQ], BF16) for i in range(2)]
        kTzs = [sb("C_kTz%d" % i, [128, 8, SEQ], BF16) for i in range(2)]
        Va = [sb("C_V%d" % i, [128, 16, 8, 66], BF16) for i in range(2)]
        bq, bv, bks = [Buf(), Buf()], [Buf(), Buf()], [Buf(), Buf()]
        for i in range(2):
            k.op("pool", lambda e, i=i: e.memset(kTzs[i][:, :, :], 0.0), (), (bks[i],))
            k.op("pool", lambda e, i=i: e.memset(Va[i][:, :, :, :], 1.0), (), (bv[i],))
        kms = sb("C_kms", [128, 64], F32)
        kmbs = [sb("C_kmb%d" % i, [128, 8, 8], BF16) for i in range(2)]
        bkms, bkmbs = Buf(), [Buf(), Buf()]
        pgb = ps.enter_context(nc.psum_tensor("C_pgb", [128, 512], F32))
        bpg = [Buf(), Buf()]
        bpbt = [Buf(), Buf()]
        gms = [sb("C_gm%d" % i, [128, 64], F32) for i in range(2)]
        cmps = [sb("C_cmp%d" % i, [128, 512], F32) for i in range(2)]
        ranks = [sb("C_rank%d" % i, [128, 64], F32) for i in range(2)]
        biasbs = [sb("C_biasb%d" % i, [128, 128], F32) for i in range(2)]
        bgms, bcmps, branks, bbiasbs = ([Buf(), Buf()] for _ in range(4))
        biasTs = [sb("C_biasT%d" % i, [128, SEQ], BF16) for i in range(2)]
        bbTs = [Buf(), Buf()]
        for i in range(2):
            k.op("pool", lambda e, i=i: e.memset(biasbs[i][:, :], 0.0), (), (bbiasbs[i],))
        pss = [ps.enter_context(nc.psum_tensor("C_ps%d" % i, [128, 512], F32)) for i in range(3)]
        bps = [Buf(), Buf(), Buf()]
        po = [[ps.enter_context(nc.psum_tensor("C_po%d%d" % (i, j), [128, 512], F32)) for j in range(2)] for i in range(2)]
        bpo = [[Buf(), Buf()], [Buf(), Buf()]]
        pe_ = [sb("C_pe%d" % i, [128, 256], BF16) for i in range(3)]
        bpe = [Buf() for _ in range(3)]
        rden = sb("C_rden", [128, 1], F32)
        brd = Buf()
        atm = [sb("C_atm%d" % i, [128, 16, 512], BF16) for i in range(2)]
        batm = [Buf(), Buf()]
        cnt = {"ips": 0, "ipe": 0, "ipo": 0}

        def load_seq(s):
            si = s % 2
            tb = s * SEQ
            kTz, bk = kTzs[si], bks[si]
            k.dma("sp", qT[si][:, :, :], io["fm_s"][0:512, tb:tb + SEQ].rearrange("(m p) t -> p m t", p=128), bq[si], (), (bq[si],))
            for h in range(8):
                hp = slice((h % 2) * 64, (h % 2) * 64 + 64)
                k.dma("sp", kTz[hp, h, :], io["fm_s"][512 + h * 64:512 + (h + 1) * 64, tb:tb + SEQ], bk, (), (bk,))
            for kt in range(16):
                k.dma("sp", Va[si][:, kt, :, 0:64], io["z_s"][tb + kt * 128:tb + (kt + 1) * 128, 512:1024].rearrange("p (h d) -> p h d", h=8), bv[si], (), (bv[si],))
            k.op("dve", lambda e: e.tensor_reduce(out=kms[:, :], in_=kTz[:, :, :].rearrange("p h (b t) -> p (h b) t", b=8), axis=AX.X, op=ALU.add), (bk,), (bkms,))
            k.op("dve", lambda e: e.tensor_copy(out=kmbs[si][:, :, :], in_=kms[:, :].rearrange("p (h b) -> p h b", h=8)), (bkms,), (bkmbs[si],))

        def gate_step(s, qt):
            si = s % 2
            g2 = qt % 2
            qb = qt // 2
            gm, cmp, rank, biasb = gms[g2], cmps[g2], ranks[g2], biasbs[g2]
            pg = pgb[:, g2 * 64:(g2 + 1) * 64]
            pbt = pgb[:, 256 + g2 * 128:256 + (g2 + 1) * 128]

            def gfn(eng):
                ins = None
                for h in range(8):
                    ins = eng.matmul(pg[:, h * 8:(h + 1) * 8], qT[si][:, h // 2, qt * 128:(qt + 1) * 128], kmbs[si][:, h, :], start=True, stop=True)
                return ins
            k.op("pe", gfn, (bq[si], bkmbs[si]), (bpg[g2],))
            k.op("dve", lambda e: e.tensor_tensor(out=gm[:, :], in0=pg, in1=negp[:, qb, :], op=ALU.add), (bpg[g2], bnegp), (bgms[g2],))
            g3 = gm[:, :].rearrange("p (h b) -> p h b", h=8)
            k.op("dve", lambda e: e.tensor_tensor(out=cmp[:, :].rearrange("p (h b c) -> p h b c", h=8, b=8), in0=g3.unsqueeze(2).broadcast_to([128, 8, 8, 8]), in1=g3.unsqueeze(3).broadcast_to([128, 8, 8, 8]), op=ALU.is_gt), (bgms[g2],), (bcmps[g2],))
            k.op("dve", lambda e: e.tensor_reduce(out=rank[:, :], in_=cmp[:, :].rearrange("p (a c) -> p a c", c=8), axis=AX.X, op=ALU.add), (bcmps[g2],), (branks[g2],))
            k.op("dve", lambda e: e.tensor_scalar(out=biasb[:, 0:64], in0=rank[:, :], scalar1=2.5, scalar2=NEG, op0=ALU.is_gt, op1=ALU.mult), (branks[g2],), (bbiasbs[g2],))
            tr(k, [(pbt, biasb[:, :], c.identf[:, :])], (bbiasbs[g2],), (bpbt[g2],))
            k.op("dve", lambda e: e.tensor_copy(out=biasTs[si][:, qt * 128:(qt + 1) * 128], in_=pbt), (bpbt[g2],), (bbTs[si],))

        def sweep(s, inserts):
            si = s % 2
            kTz, bk = kTzs[si], bks[si]
            biasT, bbT = biasTs[si], bbTs[si]
            units = [(h, qb, kt) for h in range(8) for qb in range(8) for kt in range(2 * qb + 2)]

            def score(u, p):
                h, qb, kt = u
                m = h // 2
                b = kt // 2
                qs = slice(qb * 256, (qb + 1) * 256)
                last = (b != qb) and (qb <= 3)
                first = (pss[p][:, 0:256], kTz[:, h, kt * 128:(kt + 1) * 128], qT[si][:, m, qs], True, last)
                if b == qb:
                    items = [first, (pss[p][:, 0:256], c.ident[:, :], cm[:, kt % 2, :], False, True)]
                    rd = (bk, bq[si], bcm)
                elif last:
                    items = [first]
                    rd = (bk, bq[si])
                else:
                    r = h * 8 + b
                    items = [first, (pss[p][:, 0:256], oh[:, r * 128:(r + 1) * 128], biasT[:, qs], False, True)]
                    rd = (bk, bq[si], boh, bbT)
                mm(k, items, rd, (bps[p],))

            ips = cnt["ips"]
            score(units[0], ips % 3)
            score(units[1], (ips + 1) % 3)
            oi = 0
            for ui, u in enumerate(units):
                h, qb, kt = u
                nkt = 2 * qb + 2
                p = ips % 3
                ips += 1
                if ui + 2 < len(units):
                    score(units[ui + 2], (ips + 1) % 3)
                if kt == 0:
                    oi = cnt["ipo"] % 2
                    cnt["ipo"] += 1
                e_i = cnt["ipe"] % 3
                cnt["ipe"] += 1
                k.op("act", lambda e, p=p, e_i=e_i: e.activation(out=pe_[e_i][:, :], in_=pss[p][:, 0:256], func=AF.Exp, scale=0.125), (bps[p],), (bpe[e_i],))
                mm(k, [(po[oi][qh][:, 0:65], pe_[e_i][:, qh * 128:(qh + 1) * 128], Va[si][:, kt, h, 0:65], kt == 0, kt == nkt - 1) for qh in range(2)], (bpe[e_i], bv[si]), (bpo[oi][0], bpo[oi][1]))
                if kt == nkt - 1:
                    for qh in range(2):
                        qt = qb * 2 + qh
                        k.op("dve", lambda e, oi=oi, qh=qh: e.reciprocal(out=rden[:, :], in_=po[oi][qh][:, 64:65]), (bpo[oi][qh],), (brd,))
                        k.op("dve", lambda e, oi=oi, qh=qh, qt=qt, h=h: e.tensor_scalar(out=atm[si][:, qt, h * 64:(h + 1) * 64], in0=po[oi][qh][:, 0:64], scalar1=rden[:, :], scalar2=None, op0=ALU.mult), (bpo[oi][qh], brd), (batm[si],))
                if ui in inserts:
                    inserts[ui]()
            cnt["ips"] = ips
            tb = s * SEQ
            k.dma("pool", io["at_s"][tb:tb + SEQ, :].rearrange("(q p) c -> p q c", p=128), atm[si][:, :, :], batm[si], (batm[si],), ())

        load_seq(0)
        for qt in range(16):
            gate_step(0, qt)
        for s in range(BPC):
            inserts = {}
            if s + 1 < BPC:
                inserts[8] = (lambda s=s: load_seq(s + 1))
                for qt in range(16):
                    inserts[40 + qt * 30] = (lambda s=s, qt=qt: gate_step(s + 1, qt))
            sweep(s, inserts)
        k.end("C")


TWO_PI = 2.0 * math.pi


def cis(k, scr, first, out_c, out_s, barg, bout):
    t, ti, y, m1 = scr
    bt = Buf()
    k.op("dve", lambda e: first(e, t), (barg,), (bt,))
    k.op("dve", lambda e: e.tensor_copy(out=ti, in_=t), (bt,), (bt,))
    k.op("dve", lambda e: e.tensor_copy(out=y, in_=ti), (bt,), (bt,))
    k.op("dve", lambda e: e.tensor_tensor(out=t, in0=t, in1=y, op=ALU.subtract), (bt,), (bt,))
    for shift, dst in ((0.0, out_s), (math.pi / 2, out_c)):
        k.op("dve", lambda e, shift=shift: e.tensor_scalar(out=y, in0=t, scalar1=TWO_PI, scalar2=shift, op0=ALU.mult, op1=ALU.add), (bt,), (bt,))
        k.op("dve", lambda e: e.tensor_scalar(out=m1, in0=y, scalar1=math.pi, scalar2=-TWO_PI, op0=ALU.is_gt, op1=ALU.mult), (bt,), (bt,))
        k.op("dve", lambda e: e.tensor_tensor(out=m1, in0=m1, in1=y, op=ALU.add), (bt,), (bt,))
        k.op("dve", lambda e: e.tensor_scalar(out=y, in0=y, scalar1=-math.pi, scalar2=TWO_PI, op0=ALU.is_lt, op1=ALU.mult), (bt,), (bt,))
        k.op("dve", lambda e: e.tensor_tensor(out=y, in0=m1, in1=y, op=ALU.add), (bt,), (bt,))
        k.op("dve", lambda e: e.tensor_scalar(out=y, in0=y, scalar1=math.pi, scalar2=-math.pi, op0=ALU.min, op1=ALU.max), (bt,), (bt,))
        k.op("act", lambda e, dst=dst: e.activation(out=dst, in_=y, func=AF.Sin), (bt,), (bout, bt))


def cmul(k, eng, out_r, out_i, ar, ai, br, bi, t1, t2, rd, wr, bt, neg_i=False):
    E = eng
    k.op(E, lambda e: e.tensor_tensor(out=t1, in0=ar, in1=br, op=ALU.mult), rd, (bt,))
    k.op(E, lambda e: e.tensor_tensor(out=t2, in0=ai, in1=bi, op=ALU.mult), rd + (bt,), (bt,))
    k.op(E, lambda e: e.tensor_tensor(out=out_r, in0=t1, in1=t2, op=ALU.subtract), (bt,), wr + (bt,))
    k.op(E, lambda e: e.tensor_tensor(out=t1, in0=ar, in1=bi, op=ALU.mult), rd + (bt,), (bt,))
    k.op(E, lambda e: e.tensor_tensor(out=t2, in0=ai, in1=br, op=ALU.mult), rd + (bt,), (bt,))
    if neg_i:
        k.op(E, lambda e: e.scalar_tensor_tensor(out=out_i, in0=t1, scalar=-1.0, in1=t2, op0=ALU.mult, op1=ALU.subtract), (bt,), wr + (bt,))
    else:
        k.op(E, lambda e: e.tensor_tensor(out=out_i, in0=t1, in1=t2, op=ALU.add), (bt,), wr + (bt,))


def phase_S(k, c, io):
    nc = k.nc
    with ExitStack() as outer:
        osb = lambda n, s, d: outer.enter_context(nc.sbuf_tensor(n, s, d))
        GT = osb("S_GT", [128, G, 2, 128], BF16)
        TT = osb("S_TT", [128, G, 2, 256], BF16)
        H = osb("S_H", [128, G, 256], BF16)
        Mt = osb("S_M", [128, 2, 2048], F32)
        Dt = osb("S_D", [128, 2, 2048], F32)
        s5_setup(k, c, io, GT, TT, H, Mt, Dt)
        s5_main(k, c, io, GT, TT, H, Mt, Dt)


def s5_setup(k, c, io, GT, TT, H, Mt, Dt):
    nc = k.nc
    with ExitStack() as ps:
        k.begin()
        sb = lambda n, s, d: ps.enter_context(nc.sbuf_tensor(n, s, d))
        sb2 = lambda n, s, d: ps.enter_context(nc.sbuf_tensor(n, [s[0], int(np.prod(s[1:]))], d))
        B0 = Buf()
        lg = sb("u_lg", [32, 128], F32)
        k.dma("sp", lg[:, 0:64], io["ssm_lam_re"], B0, (), (B0,))
        k.dma("sp", lg[:, 64:128], io["ssm_lam_im"], B0, (), (B0,))
        ldt = sb("u_ldt", [64, 32], F32)
        k.dma("sp", ldt[:, :], io["ssm_log_dt"].partition_broadcast(64), B0, (), (B0,))
        Bre = sb("u_Bre", [64, G, 16], F32)
        Bim = sb("u_Bim", [64, G, 16], F32)
        k.dma("sp", Bre[:, :, :], io["ssm_b_re"].rearrange("g p c -> p g c"), B0, (), (B0,))
        k.dma("sp", Bim[:, :, :], io["ssm_b_im"].rearrange("g p c -> p g c"), B0, (), (B0,))
        cg = [sb("u_cg%d" % i, [128, 4, 64], F32) for i in range(2)]
        k.dma("sp", cg[0][:, :, :], io["ssm_c_re"].rearrange("(a b) c p -> (b c) a p", a=4), B0, (), (B0,))
        k.dma("sp", cg[1][:, :, :], io["ssm_c_im"].rearrange("(a b) c p -> (b c) a p", a=4), B0, (), (B0,))
        dcol = sb("u_dcol", [128, G], F32)
        for j in range(8):
            k.dma("sp", dcol[j * 16:(j + 1) * 16, :], io["ssm_d"].rearrange("(g c) -> c g", c=16), B0, (), (B0,), allow_slow_non_contiguous=True)
        pp = ps.enter_context(nc.psum_tensor("u_pp", [128, 512], F32))
        pq = ps.enter_context(nc.psum_tensor("u_pq", [128, 512], F32))
        Bp = Buf()
        lre = sb("u_lre", [64, G], F32)
        lim = sb("u_lim", [64, G], F32)
        tr(k, [(pp[0:64, 0:32], lg[:, 0:64], c.identf[0:32, 0:32]), (pp[0:64, 32:64], lg[:, 64:128], c.identf[0:32, 0:32])], (B0,), (Bp,))
        k.op("dve", lambda e: e.tensor_copy(out=lre[:, :], in_=pp[0:64, 0:32]), (Bp,), (B0,))
        k.op("dve", lambda e: e.tensor_copy(out=lim[:, :], in_=pp[0:64, 32:64]), (Bp,), (B0, Bp))
        Cre = sb("u_Cre", [64, G, 16], F32)
        Cim = sb("u_Cim", [64, G, 16], F32)
        for i, Cx in enumerate((Cre, Cim)):
            tr(k, [(pp[0:64, a * 128:(a + 1) * 128], cg[i][:, a, :], c.identf[:, :]) for a in range(4)], (B0,), (Bp,))
            k.op("dve", lambda e, Cx=Cx: e.tensor_copy(out=Cx[:, :, :], in_=pp[0:64, :].rearrange("p (g c) -> p g c", c=16)), (Bp,), (B0, Bp))
        dt = sb("u_dt", [64, G], F32)
        k.op("act", lambda e: e.activation(out=dt[:, :], in_=ldt[:, :], func=AF.Exp), (B0,), (B0,))
        rho = sb("u_rho", [64, G], F32)
        th = sb("u_th", [64, G], F32)
        k.op("dve", lambda e: e.tensor_tensor(out=rho[:, :], in0=lre[:, :], in1=dt[:, :], op=ALU.mult), (B0,), (B0,))
        k.op("dve", lambda e: e.tensor_tensor(out=th[:, :], in0=lim[:, :], in1=dt[:, :], op=ALU.mult), (B0,), (B0,))
        nvi = sb("u_nvi", [64, 17], I32)
        nv = sb("u_nv", [64, 17], F32)
        k.op("pool", lambda e: e.iota(nvi[:, :], [[1, 17]], base=0, channel_multiplier=0), (B0,), (B0,))
        k.op("dve", lambda e: e.tensor_copy(out=nv[:, :], in_=nvi[:, :]), (B0,), (B0,))
        NP = G * 17
        argt = sb("u_argt", [64, NP], F32)
        argr = sb("u_argr", [64, NP], F32)
        a3 = lambda t: t[:, :].rearrange("p (g n) -> p g n", n=17)
        bth = th[:, :].unsqueeze(2).broadcast_to([64, G, 17])
        brho = rho[:, :].unsqueeze(2).broadcast_to([64, G, 17])
        bnv = nv[:, :].unsqueeze(1).broadcast_to([64, G, 17])
        k.op("dve", lambda e: e.tensor_tensor(out=a3(argt), in0=bth, in1=bnv, op=ALU.mult), (B0,), (B0,))
        k.op("dve", lambda e: e.tensor_tensor(out=a3(argr), in0=brho, in1=bnv, op=ALU.mult), (B0,), (B0,))
        cs = sb("u_cs", [64, NP], F32)
        sn = sb("u_sn", [64, NP], F32)
        scr = (sb("u_c1", [64, NP], F32)[:, :], sb("u_c2", [64, NP], I32)[:, :], sb("u_c3", [64, NP], F32)[:, :], sb("u_c4", [64, NP], F32)[:, :])
        cis(k, scr, lambda e, t: e.tensor_scalar(out=t, in0=argt[:, :], scalar1=1.0 / TWO_PI, scalar2=None, op0=ALU.mult), cs[:, :], sn[:, :], B0, B0)
        mg = sb("u_mg", [64, NP], F32)
        mgi = sb("u_mgi", [64, NP], F32)
        k.op("act", lambda e: e.activation(out=mg[:, :], in_=argr[:, :], func=AF.Exp), (B0,), (B0,))
        k.op("act", lambda e: e.activation(out=mgi[:, :], in_=argr[:, :], func=AF.Exp, scale=-1.0), (B0,), (B0,))
        Pr = sb("u_Pr", [64, G, 17], F32)
        Pi = sb("u_Pi", [64, G, 17], F32)
        Qr = sb("u_Qr", [64, G, 17], F32)
        Qi = sb("u_Qi", [64, G, 17], F32)
        f2 = lambda t: t[:, :, :].rearrange("p g n -> p (g n)")
        k.op("dve", lambda e: e.tensor_tensor(out=f2(Pr), in0=mg[:, :], in1=cs[:, :], op=ALU.mult), (B0,), (B0,))
        k.op("dve", lambda e: e.tensor_tensor(out=f2(Pi), in0=mg[:, :], in1=sn[:, :], op=ALU.mult), (B0,), (B0,))
        k.op("dve", lambda e: e.tensor_tensor(out=f2(Qr), in0=mgi[:, :], in1=cs[:, :], op=ALU.mult), (B0,), (B0,))
        k.op("dve", lambda e: e.scalar_tensor_tensor(out=f2(Qi), in0=mgi[:, :], scalar=-1.0, in1=sn[:, :], op0=ALU.mult, op1=ALU.mult), (B0,), (B0,))
        den = sb("u_den", [64, G], F32)
        tA = sb("u_tA", [64, G], F32)
        tB = sb("u_tB", [64, G], F32)
        nr = sb("u_nr", [64, G], F32)
        cr = sb("u_cr", [64, G], F32)
        ci = sb("u_ci", [64, G], F32)
        k.op("dve", lambda e: e.tensor_tensor(out=den[:, :], in0=lre[:, :], in1=lre[:, :], op=ALU.mult), (B0,), (B0,))
        k.op("dve", lambda e: e.tensor_tensor(out=tA[:, :], in0=lim[:, :], in1=lim[:, :], op=ALU.mult), (B0,), (B0,))
        k.op("dve", lambda e: e.tensor_tensor(out=den[:, :], in0=den[:, :], in1=tA[:, :], op=ALU.add), (B0,), (B0,))
        k.op("dve", lambda e: e.reciprocal(out=den[:, :], in_=den[:, :]), (B0,), (B0,))
        k.op("dve", lambda e: e.tensor_scalar(out=nr[:, :], in0=Pr[:, :, 1], scalar1=-1.0, scalar2=None, op0=ALU.add), (B0,), (B0,))
        k.op("dve", lambda e: e.tensor_tensor(out=tA[:, :], in0=nr[:, :], in1=lre[:, :], op=ALU.mult), (B0,), (B0,))
        k.op("dve", lambda e: e.tensor_tensor(out=tB[:, :], in0=Pi[:, :, 1], in1=lim[:, :], op=ALU.mult), (B0,), (B0,))
        k.op("dve", lambda e: e.tensor_tensor(out=tA[:, :], in0=tA[:, :], in1=tB[:, :], op=ALU.add), (B0,), (B0,))
        k.op("dve", lambda e: e.tensor_tensor(out=cr[:, :], in0=tA[:, :], in1=den[:, :], op=ALU.mult), (B0,), (B0,))
        k.op("dve", lambda e: e.tensor_tensor(out=tA[:, :], in0=Pi[:, :, 1], in1=lre[:, :], op=ALU.mult), (B0,), (B0,))
        k.op("dve", lambda e: e.tensor_tensor(out=tB[:, :], in0=nr[:, :], in1=lim[:, :], op=ALU.mult), (B0,), (B0,))
        k.op("dve", lambda e: e.tensor_tensor(out=tA[:, :], in0=tA[:, :], in1=tB[:, :], op=ALU.subtract), (B0,), (B0,))
        k.op("dve", lambda e: e.tensor_tensor(out=ci[:, :], in0=tA[:, :], in1=den[:, :], op=ALU.mult), (B0,), (B0,))
        bbr = sb("u_bbr", [64, G, 16], F32)
        bbi = sb("u_bbi", [64, G, 16], F32)
        w1 = sb("u_w1", [64, G, 16], F32)
        w2 = sb("u_w2", [64, G, 16], F32)
        bc16 = lambda t: t[:, :].unsqueeze(2).broadcast_to([64, G, 16])
        cmul(k, "dve", bbr[:, :, :], bbi[:, :, :], bc16(cr), bc16(ci), Bre[:, :, :], Bim[:, :, :], w1[:, :, :], w2[:, :, :], (B0,), (B0,), B0)
        mki = sb("u_mki", [128, 2, 256], I32)
        mask = sb("u_mask", [128, 2, 256], F32)
        idc = sb("u_idc", [128, 2, 256], F32)
        shid = sb("u_shid", [64, 128], F32)
        for ch in range(2):
            k.op("pool", lambda e, ch=ch: e.iota(mki[:, ch, :], [[16, 16], [0, 16]], base=15 - 128 * ch, channel_multiplier=-1), (B0,), (B0,))
        k.op("dve", lambda e: e.tensor_scalar(out=mask[:, :, :], in0=mki[:, :, :], scalar1=0.0, scalar2=None, op0=ALU.is_ge), (B0,), (B0,))
        for ch in range(2):
            k.op("pool", lambda e, ch=ch: e.iota(mki[:, ch, :], [[16, 16], [1, 16]], base=-128 * ch, channel_multiplier=-1), (B0,), (B0,))
        k.op("dve", lambda e: e.tensor_scalar(out=idc[:, :, :], in0=mki[:, :, :], scalar1=0.0, scalar2=None, op0=ALU.is_equal), (B0,), (B0,))
        k.op("pool", lambda e: e.iota(mki[0:64, 0, 0:128], [[1, 128]], base=-64, channel_multiplier=-1), (B0,), (B0,))
        k.op("dve", lambda e: e.tensor_scalar(out=shid[:, :], in0=mki[0:64, 0, 0:128], scalar1=0.0, scalar2=None, op0=ALU.is_equal), (B0,), (B0,))
        GC = 4
        Fr = sb("u_Fr", [64, GC, 16, 16], F32)
        Fi = sb("u_Fi", [64, GC, 16, 16], F32)
        Er = sb("u_Er", [64, GC, 17, 16], F32)
        nEi = sb("u_nEi", [64, GC, 17, 16], F32)
        Lr = sb("u_Lr", [64, GC, 16, 16], F32)
        Li = sb("u_Li", [64, GC, 16, 16], F32)
        x1 = sb("u_x1", [64, GC, 17, 16], F32)
        x2 = sb("u_x2", [64, GC, 17, 16], F32)
        tmask2 = sb("u_tmask2", [128, 512], F32)
        for gc in range(G // GC):
            gs = slice(gc * GC, (gc + 1) * GC)
            qr = Qr[:, gs, 0:16].unsqueeze(3).broadcast_to([64, GC, 16, 16])
            qi = Qi[:, gs, 0:16].unsqueeze(3).broadcast_to([64, GC, 16, 16])
            br_ = bbr[:, gs, :].unsqueeze(2).broadcast_to([64, GC, 16, 16])
            bi_ = bbi[:, gs, :].unsqueeze(2).broadcast_to([64, GC, 16, 16])
            cmul(k, "dve", Fr[:, :, :, :], Fi[:, :, :, :], qr, qi, br_, bi_, x1[:, :, 0:16, :], x2[:, :, 0:16, :], (B0,), (B0,), B0)
            pr = Pr[:, gs, :].unsqueeze(3).broadcast_to([64, GC, 17, 16])
            pi = Pi[:, gs, :].unsqueeze(3).broadcast_to([64, GC, 17, 16])
            cr_ = Cre[:, gs, :].unsqueeze(2).broadcast_to([64, GC, 17, 16])
            ci_ = Cim[:, gs, :].unsqueeze(2).broadcast_to([64, GC, 17, 16])
            cmul(k, "dve", Er[:, :, :, :], nEi[:, :, :, :], pr, pi, cr_, ci_, x1[:, :, :, :], x2[:, :, :, :], (B0,), (B0,), B0, neg_i=True)
            l15r = Pr[:, gs, 15:16].unsqueeze(3).broadcast_to([64, GC, 16, 16])
            l15i = Pi[:, gs, 15:16].unsqueeze(3).broadcast_to([64, GC, 16, 16])
            cmul(k, "dve", Lr[:, :, :, :], Li[:, :, :, :], l15r, l15i, Fr[:, :, :, :], Fi[:, :, :, :], x1[:, :, 0:16, :], x2[:, :, 0:16, :], (B0,), (B0,), B0)
            for gl in range(0, GC, 2):
                g = gc * GC + gl
                items = []
                for g2 in range(2):
                    for ch in range(2):
                        o0 = g2 * 256 + ch * 128
                        items.append((pp[:, o0:o0 + 64], Lr[:, gl + g2, ch * 8:(ch + 1) * 8, :].rearrange("p j c -> p (j c)"), c.identf[0:64, 0:64]))
                        items.append((pp[:, o0 + 64:o0 + 128], Li[:, gl + g2, ch * 8:(ch + 1) * 8, :].rearrange("p j c -> p (j c)"), c.identf[0:64, 0:64]))
                tr(k, items, (B0,), (Bp,))
                k.op("act", lambda e, g=g: e.copy(out=GT[:, g:g + 2, :, :], in_=pp[:, :].rearrange("p (g a b) -> p g a b", g=2, a=2)), (Bp,), (B0, Bp))
                for ch in range(2):
                    items = []
                    for g2 in range(2):
                        fr_ = Fr[:, gl + g2, ch * 8:(ch + 1) * 8, :].rearrange("p j c -> p (j c)")
                        fi_ = Fi[:, gl + g2, ch * 8:(ch + 1) * 8, :].rearrange("p j c -> p (j c)")
                        er_ = Er[:, gl + g2, 0:16, :].rearrange("p i c -> p (i c)")
                        ei_ = nEi[:, gl + g2, 0:16, :].rearrange("p i c -> p (i c)")
                        o = pq[:, g2 * 256:(g2 + 1) * 256]
                        items += [(o, fr_, er_, True, False), (o, fi_, ei_, False, True)]
                    Bq = Buf()
                    mm(k, items, (B0,), (Bq, Bp))
                    k.op("dve", lambda e, ch=ch: e.tensor_tensor(out=tmask2[:, :].rearrange("p (g x) -> p g x", g=2), in0=pq[:, :].rearrange("p (g x) -> p g x", g=2), in1=mask[:, ch, :].unsqueeze(1).broadcast_to([128, 2, 256]), op=ALU.mult), (Bq, B0), (B0, Bp))
                    for g2 in range(2):
                        k.op("dve", lambda e, ch=ch, g=g, g2=g2: e.scalar_tensor_tensor(out=TT[:, g + g2, ch, :], in0=idc[:, ch, :], scalar=dcol[:, g + g2:g + g2 + 1], in1=tmask2[:, g2 * 256:(g2 + 1) * 256], op0=ALU.mult, op1=ALU.add), (B0,), (B0,))
                items = []
                for g2 in range(2):
                    er1 = Er[:, gl + g2, 1:17, :].rearrange("p i c -> p (i c)")
                    ei1 = nEi[:, gl + g2, 1:17, :].rearrange("p i c -> p (i c)")
                    o = pq[:, g2 * 256:(g2 + 1) * 256]
                    items += [(o, c.identf[0:64, :], er1, True, False), (o, shid[:, :], ei1, False, True)]
                mm(k, items, (B0,), (Bp,))
                k.op("act", lambda e, g=g: e.copy(out=H[:, g:g + 2, :], in_=pq[:, :].rearrange("p (g x) -> p g x", g=2)), (Bp,), (B0, Bp))
        k.end("S_setup")
    with ExitStack() as ps:
        k.begin()
        sb = lambda n, s, d: ps.enter_context(nc.sbuf_tensor(n, s, d))
        B0 = Buf()
        lb = sb("w_lb", [128, 2, 2048], F32)
        k.dma("sp", lb[:, 0, :], io["ssm_lam_re"].rearrange("g p -> (g p)").partition_broadcast(128), B0, (), (B0,))
        k.dma("sp", lb[:, 1, :], io["ssm_lam_im"].rearrange("g p -> (g p)").partition_broadcast(128), B0, (), (B0,))
        dtb = sb("w_dtb", [128, G], F32)
        k.dma("sp", dtb[:, :], io["ssm_log_dt"].partition_broadcast(128), B0, (), (B0,))
        k.op("act", lambda e: e.activation(out=dtb[:, :], in_=dtb[:, :], func=AF.Exp), (B0,), (B0,))
        for i in range(2):
            k.op("dve", lambda e, i=i: e.tensor_tensor(out=lb[:, i, :].rearrange("p (g q) -> p g q", q=64), in0=lb[:, i, :].rearrange("p (g q) -> p g q", q=64), in1=dtb[:, :].unsqueeze(2).broadcast_to([128, G, 64]), op=ALU.mult), (B0,), (B0,))
        nki = sb("w_nki", [128, 2], I32)
        nk = sb("w_nk", [128, 2], F32)
        nk2 = sb("w_nk2", [128, 2], F32)
        k.op("pool", lambda e: e.iota(nki[:, 0:1], [[0, 1]], base=1024, channel_multiplier=-16), (B0,), (B0,))
        k.op("pool", lambda e: e.iota(nki[:, 1:2], [[0, 1]], base=-1040, channel_multiplier=16), (B0,), (B0,))
        k.op("dve", lambda e: e.tensor_copy(out=nk[:, :], in_=nki[:, :]), (B0,), (B0,))
        k.op("dve", lambda e: e.tensor_scalar(out=nk2[:, :], in0=nk[:, :], scalar1=1.0 / TWO_PI, scalar2=None, op0=ALU.mult), (B0,), (B0,))
        wc = sb("w_c", [128, 2048], F32)
        ws = sb("w_s", [128, 2048], F32)
        scr = (sb("w_c1", [128, 2048], F32)[:, :], sb("w_c2", [128, 2048], I32)[:, :], sb("w_c3", [128, 2048], F32)[:, :], sb("w_c4", [128, 2048], F32)[:, :])
        for i, Tb in enumerate((Mt, Dt)):
            cis(k, scr, lambda e, t, i=i: e.tensor_scalar(out=t, in0=lb[:, 1, :], scalar1=nk2[:, i:i + 1], scalar2=None, op0=ALU.mult), wc[:, :], ws[:, :], B0, B0)
            k.op("act", lambda e, i=i, Tb=Tb: e.activation(out=Tb[:, 1, :], in_=lb[:, 0, :], func=AF.Exp, scale=nk[:, i:i + 1]), (B0,), (B0,))
            k.op("dve", lambda e, Tb=Tb: e.tensor_tensor(out=Tb[:, 0, :], in0=Tb[:, 1, :], in1=wc[:, :], op=ALU.mult), (B0,), (B0,))
            k.op("dve", lambda e, Tb=Tb: e.tensor_tensor(out=Tb[:, 1, :], in0=Tb[:, 1, :], in1=ws[:, :], op=ALU.mult), (B0,), (B0,))
        k.end("S_tables")


def s5_main(k, c, io, GT, TT, H, Mt, Dt):
    nc = k.nc
    with ExitStack() as ps:
        k.begin()
        sb = lambda n, s, d: ps.enter_context(nc.sbuf_tensor(n, s, d))
        bTab = Buf()
        Wg = sb("S_Wg", [128, 4, 512], BF16)
        bWg = Buf()
        load_w_cast(k, ps, io["w_glu"], 4, 512, "S_wg", Wg, bWg, "dve")
        bgl = sb("S_bgl", [128, 4], F32)
        bbgl = Buf()
        k.dma("sp", bgl[:, :], io["b_glu"].rearrange("(m p) -> p m", p=128), bbgl, (), (bbgl,), allow_slow_non_contiguous=True)
        UA = sb("S_UA", [128, 8192], BF16)
        UB = sb("S_UB", [128, 8192], BF16)
        bUA, bUB = Buf(), Buf()
        U16 = sb("S_U16", [128, G, 2, 128], BF16)
        bU16 = Buf()
        X = sb("S_X", [128, G, 128], BF16)
        bX = [Buf() for _ in range(8)]
        Stm = sb("S_Stm", [128, G, 128], BF16)
        bStm = [Buf() for _ in range(8)]
        ST = sb("S_ST", [128, G, 128], BF16)
        bST = [Buf() for _ in range(4)]
        pT = [ps.enter_context(nc.psum_tensor("S_pT%d" % i, [128, 1024], BF16)) for i in range(2)]
        bpT = [Buf(), Buf()]
        NPD = 2
        pd = [ps.enter_context(nc.psum_tensor("S_pd%d" % i, [128, 512], F32)) for i in range(NPD)]
        bpd = [Buf() for _ in range(NPD)]
        py = [ps.enter_context(nc.psum_tensor("S_py%d" % i, [128, 512], F32)) for i in range(3)]
        bpy = [Buf() for _ in range(3)]
        pgl = ps.enter_context(nc.psum_tensor("S_pgl", [128, 512], F32))
        bpgl = Buf()
        tq = [sb("S_tq%d" % i, [128, 256], F32) for i in range(4)]
        btq = [Buf() for _ in range(4)]
        gx2 = [sb("S_gx2%d" % i, [128, 512], F32) for i in range(3)]
        gu = [sb("S_gu%d" % i, [128, 512], F32) for i in range(3)]
        bgx = [Buf() for _ in range(3)]
        bgu = [Buf() for _ in range(3)]
        sgl = sb("S_sgl", [128, 512], F32)
        bsgl = Buf()
        s5st = [sb("S_s5%d" % i, [128, 4, 512], BF16) for i in range(2)]
        bs5 = [Buf(), Buf()]
        ipT = 0
        ipd = 0
        ipy = 0

        def cmod(pbank, Tb, dst, g0, bsrc, bdst):
            src = pbank[:, :].rearrange("p (g x) -> p g x", g=4)
            sre, sim = src[:, :, 0:64], src[:, :, 64:128]
            tre = Tb[:, 0, g0 * 64:(g0 + 4) * 64].rearrange("p (g q) -> p g q", g=4)
            tim = Tb[:, 1, g0 * 64:(g0 + 4) * 64].rearrange("p (g q) -> p g q", g=4)
            v = lambda t: t[:, :].rearrange("p (g q) -> p g q", g=4)
            k.op("dve", lambda e: e.tensor_tensor(out=v(tq[0]), in0=sre, in1=tre, op=ALU.mult), (bsrc, bTab), (btq[0],))
            k.op("dve", lambda e: e.tensor_tensor(out=v(tq[1]), in0=sim, in1=tim, op=ALU.mult), (bsrc, bTab), (btq[1],))
            k.op("dve", lambda e: e.tensor_tensor(out=v(tq[2]), in0=sre, in1=tim, op=ALU.mult), (bsrc, bTab), (btq[2],))
            k.op("dve", lambda e: e.tensor_tensor(out=v(tq[3]), in0=sim, in1=tre, op=ALU.mult), (bsrc, bTab), (btq[3],))
            k.op("pool", lambda e: e.tensor_tensor(out=dst[:, g0:g0 + 4, 0:64], in0=v(tq[0]), in1=v(tq[1]), op=ALU.subtract), (btq[0], btq[1]), (bdst,))
            k.op("pool", lambda e: e.tensor_tensor(out=dst[:, g0:g0 + 4, 64:128], in0=v(tq[2]), in1=v(tq[3]), op=ALU.add), (btq[2], btq[3]), (bdst,))

        for s in range(BPC):
            tb = s * SEQ
            Utm = UA[:, :].rearrange("p (j c) -> p j c", j=16)
            k.dma("sp", Utm, io["z_s"][tb:tb + SEQ, 0:512].rearrange("(k j) c -> k j c", j=16), bUA, (), (bUA,))
            for hf in range(2):
                k.op("dve", lambda e, hf=hf: e.tensor_copy(
                    out=UB[:, hf * 4096:(hf + 1) * 4096].rearrange("p (g j c) -> p g j c", g=16, j=16),
                    in_=UA[:, :].rearrange("p (j g c) -> p g j c", j=16, g=32)[:, hf * 16:(hf + 1) * 16, :, :]), (bUA,), (bUB,))
            Ug = UB[:, :].rearrange("p (g x) -> p g x", g=32)
            for g4 in range(8):
                p = ipT % 2
                ipT += 1
                tr(k, [(pT[p][:, (gl * 2 + ch) * 128:(gl * 2 + ch + 1) * 128], Ug[:, g4 * 4 + gl, ch * 128:(ch + 1) * 128], c.ident[:, :]) for gl in range(4) for ch in range(2)], (bUB,), (bpT[p],))
                k.op("act", lambda e, p=p, g4=g4: e.copy(out=U16[:, g4 * 4:(g4 + 1) * 4, :, :], in_=pT[p][:, :].rearrange("p (g a k) -> p g a k", g=4, a=2)), (bpT[p],), (bU16,))
            for g4 in range(8):
                p = ipd % NPD
                ipd += 1
                items = []
                for gl in range(4):
                    g = g4 * 4 + gl
                    for ch in range(2):
                        items.append((pd[p][:, gl * 128:(gl + 1) * 128], U16[:, g, ch, :], GT[:, g, ch, :], ch == 0, ch == 1))
                mm(k, items, (bU16, bTab), (bpd[p],))
                cmod(pd[p], Mt, X, g4 * 4, bpd[p], bX[g4])
            for g4 in range(8):
                p = ipd % NPD
                ipd += 1
                mm(k, [(pd[p][:, :], c.tri[:, :], X[:, g4 * 4:(g4 + 1) * 4, :].rearrange("p g x -> p (g x)"), True, True)], (bX[g4],), (bpd[p],))
                cmod(pd[p], Dt, Stm, g4 * 4, bpd[p], bStm[g4])
            for g8 in range(4):
                p = ipT % 2
                ipT += 1
                tr(k, [(pT[p][:, gl * 128:(gl + 1) * 128], Stm[:, g8 * 8 + gl, :], c.ident[:, :]) for gl in range(8)], (bStm[2 * g8], bStm[2 * g8 + 1]), (bpT[p],))
                k.op("act", lambda e, p=p, g8=g8: e.copy(out=ST[:, g8 * 8:(g8 + 1) * 8, :], in_=pT[p][:, :].rearrange("p (g k) -> p g k", g=8)), (bpT[p],), (bST[g8],))
            ygtm = UA[:, :].rearrange("p (i c) -> p i c", i=16)
            def y_front(gp):
                p = gp % 3
                items = []
                for g2 in range(2):
                    g = gp * 2 + g2
                    o = py[p][:, g2 * 256:(g2 + 1) * 256]
                    items += [(o, U16[:, g, 0, :], TT[:, g, 0, :], True, False), (o, U16[:, g, 1, :], TT[:, g, 1, :], False, False), (o, ST[:, g, :], H[:, g, :], False, True)]
                mm(k, items, (bU16, bST[gp // 4], bTab), (bpy[p],))
                k.op("act", lambda e: e.activation(out=gx2[p][:, :], in_=py[p][:, :], func=AF.Square), (bpy[p],), (bgx[p],))
                k.op("pool", lambda e: e.tensor_scalar(out=gx2[p][:, :], in0=gx2[p][:, :], scalar1=0.044715, scalar2=1.0, op0=ALU.mult, op1=ALU.add), (bgx[p],), (bgx[p],))

            def y_back(gp):
                p = gp % 3
                k.op("dve", lambda e: e.tensor_tensor(out=gu[p][:, :], in0=py[p][:, :], in1=gx2[p][:, :], op=ALU.mult), (bpy[p], bgx[p]), (bgu[p],))
                k.op("act", lambda e: e.activation(out=gu[p][:, :], in_=gu[p][:, :], func=AF.Sigmoid, scale=1.5957691216057308), (bgu[p],), (bgu[p],))
                k.op("dve", lambda e: e.tensor_tensor(
                    out=ygtm[:, :, gp * 32:(gp + 1) * 32].rearrange("p i (g c) -> p g i c", g=2),
                    in0=py[p][:, :].rearrange("p (g i c) -> p g i c", g=2, i=16),
                    in1=gu[p][:, :].rearrange("p (g i c) -> p g i c", g=2, i=16), op=ALU.mult), (bpy[p], bgu[p]), (bUA,))

            y_front(0)
            for gp in range(16):
                if gp + 1 < 16:
                    y_front(gp + 1)
                y_back(gp)
            ygT = UB[:, :].rearrange("p (ct t) -> p ct t", ct=4)
            for ib in range(8):
                p = ipT % 2
                ipT += 1
                tr(k, [(pT[p][:, (i2 * 4 + ct) * 128:(i2 * 4 + ct + 1) * 128], ygtm[:, ib * 2 + i2, ct * 128:(ct + 1) * 128], c.ident[:, :]) for i2 in range(2) for ct in range(4)], (bUA,), (bpT[p],))
                k.op("act", lambda e, p=p, ib=ib: e.copy(
                    out=UB[:, :].rearrange("p (ct k i) -> p i ct k", ct=4, i=16)[:, ib * 2:(ib + 1) * 2, :, :],
                    in_=pT[p][:, :].rearrange("p (i ct k) -> p i ct k", i=2, ct=4)), (bpT[p],), (bUB,))
            for ch in range(4):
                sidx = (s * 4 + ch) % 2
                for m in range(4):
                    mm(k, [(pgl[:, :], Wg[:, f, m * 128:(m + 1) * 128], ygT[:, f, ch * 512:(ch + 1) * 512], f == 0, f == 3) for f in range(4)], (bUB, bWg), (bpgl,))
                    k.op("act", lambda e, m=m: e.activation(out=sgl[:, :], in_=pgl[:, :], func=AF.Sigmoid, bias=bgl[:, m:m + 1]), (bpgl, bbgl), (bsgl,))
                    k.op("dve", lambda e, m=m, ch=ch, sidx=sidx: e.tensor_tensor(out=s5st[sidx][:, m, :], in0=sgl[:, :], in1=ygT[:, m, ch * 512:(ch + 1) * 512], op=ALU.mult), (bsgl, bUB), (bs5[sidx],))
                k.dma("pool", io["s5_s"][:, tb + ch * 512:tb + (ch + 1) * 512].rearrange("(m p) t -> p m t", p=128), s5st[sidx][:, :, :], bs5[sidx], (bs5[sidx],), ())
        k.end("S_main")
```

```python
import math
from contextlib import ExitStack
import numpy as np
import concourse.bass as bass
import concourse.mybir as mybir
from concourse.bass_utils import run_bass_kernel_spmd

F32 = mybir.dt.float32
BF16 = mybir.dt.bfloat16
I32 = mybir.dt.int32
AF = mybir.ActivationFunctionType
ALU = mybir.AluOpType
AX = mybir.AxisListType

NCORES = 8
D = 1024
SEQ = 2048
BPC = 4
T = BPC * SEQ
NTT = T // 128
G = 32
PST = 64
DFF = 4096
PLE = 256
EPS = 1e-6
NEG = -30000.0


class Buf:
    __slots__ = ("name", "w", "r", "ds")

    def __init__(self, name=""):
        self.name = name
        self.w = None
        self.r = []
        self.ds = None


class K:
    ENGS = ("pe", "dve", "act", "pool", "sp")

    def __init__(self, nc, es):
        self.nc = nc
        self.es = es
        self.esem = {e: es.enter_context(nc.semaphore("es_" + e)) for e in self.ENGS}
        self.cnt = {e: 0 for e in self.ENGS}
        self.dpool = [es.enter_context(nc.semaphore("ds%d" % i)) for i in range(48)]
        self.NHW = 36
        self.dcnt = [0] * len(self.dpool)
        self.ops = None
        with nc.Block() as block:
            def clr(eng):
                for sm in list(self.esem.values()) + self.dpool:
                    eng.sem_clear(sm)
            block.sync(clr)

    def begin(self):
        self.ops = {e: [] for e in self.ENGS}
        self.seen = {e: {} for e in self.ENGS}
        self.dnext = {False: 0, True: self.NHW}
        self.dused = set()
        self.phase_id = getattr(self, "phase_id", 0) + 1

    def _dsem(self, buf, sw):
        if buf.ds is None or buf.ds[0] != (self.phase_id, sw):
            lim = len(self.dpool) if sw else self.NHW
            assert self.dnext[sw] < lim, "out of dma sems"
            buf.ds = ((self.phase_id, sw), self.dnext[sw])
            self.dnext[sw] += 1
        return buf.ds[1]

    def _deps(self, eng, reads, writes):
        waits = {}
        def add(tok):
            if tok is None:
                return
            key, val = tok
            if key == ("e", "pe") and eng == "pe":
                return
            if self.seen[eng].get(key, 0) >= val:
                return
            if waits.get(key, 0) < val:
                waits[key] = val
        for b in reads:
            add(b.w)
        for b in writes:
            add(b.w)
            for t in b.r:
                add(t)
        for key, val in waits.items():
            self.seen[eng][key] = val
        return list(waits.items())

    def _mark(self, tok, reads, writes):
        for b in reads:
            b.r.append(tok)
        for b in writes:
            b.w = tok
            b.r = []

    def op(self, eng, fn, reads=(), writes=()):
        waits = self._deps(eng, reads, writes)
        self.cnt[eng] += 1
        tok = (("e", eng), self.cnt[eng])
        self._mark(tok, reads, writes)
        self.ops[eng].append(("c", fn, waits))

    def dma(self, eng, out, in_, sb, reads=(), writes=(), **kw):
        waits = self._deps(eng, reads, writes)
        si = self._dsem(sb, eng == "pool")
        self.dcnt[si] += 16
        self.dused.add(si)
        tok = (("d", si), self.dcnt[si])
        self._mark(tok, reads, writes)
        self.ops[eng].append(("d", (out, in_, si, kw), waits))

    def _sem(self, key):
        return self.esem[key[1]] if key[0] == "e" else self.dpool[key[1]]

    def end(self, name):
        nc = self.nc
        fin = [(("e", e), self.cnt[e]) for e in self.ENGS if e != "sp" and self.cnt[e] > 0]
        fin += [(("d", si), self.dcnt[si]) for si in sorted(self.dused)]
        ops = self.ops
        handles = {"pe": "tensor", "dve": "vector", "act": "scalar", "pool": "gpsimd", "sp": "sync"}
        with nc.Block() as block:
            for e in self.ENGS:
                def body(eng, e=e):
                    for kind, payload, waits in ops[e]:
                        for key, val in waits:
                            eng.wait_ge(self._sem(key), val)
                        if kind == "c":
                            ins = payload(eng)
                            ins.then_inc(self.esem[e], 1)
                        else:
                            out, in_, si, kw = payload
                            eng.dma_start(out=out, in_=in_, **kw).then_inc(self.dpool[si], 16)
                    if e == "sp":
                        for key, val in fin:
                            eng.wait_ge(self._sem(key), val)
                getattr(block, handles[e])(body)
        self.ops = None


def mm(k, items, reads, writes):
    def fn(eng):
        ins = None
        for (out, lhsT, rhs, st, sp) in items:
            ins = eng.matmul(out, lhsT, rhs, start=st, stop=sp)
        return ins
    k.op("pe", fn, reads, writes)


def tr(k, items, reads, writes):
    def fn(eng):
        ins = None
        for (out, in_, ident) in items:
            ins = eng.transpose(out, in_, ident)
        return ins
    k.op("pe", fn, reads, writes)


class Consts:
    pass


def make_consts(k, es):
    nc = k.nc
    c = Consts()
    c.ident = es.enter_context(nc.sbuf_tensor("ident", [128, 128], BF16))
    c.identf = es.enter_context(nc.sbuf_tensor("identf", [128, 128], F32))
    c.tri = es.enter_context(nc.sbuf_tensor("tri", [128, 128], BF16))
    c.nhalf = es.enter_context(nc.sbuf_tensor("nhalf", [128, 1], F32))
    c.ones = es.enter_context(nc.sbuf_tensor("onesb", [128, 128], BF16))
    io = es.enter_context(nc.sbuf_tensor("iota_i", [128, 128], I32))
    b_io, b_id, b_idf, b_tri, b_nh, b_on = (Buf() for _ in range(6))
    k.begin()
    k.op("pool", lambda e: e.iota(io[:, :], [[1, 128]], base=0, channel_multiplier=-1), (), (b_io,))
    k.op("dve", lambda e: e.tensor_scalar(out=c.ident[:, :], in0=io[:, :], scalar1=0.0, scalar2=None, op0=ALU.is_equal), (b_io,), (b_id,))
    k.op("dve", lambda e: e.tensor_scalar(out=c.identf[:, :], in0=io[:, :], scalar1=0.0, scalar2=None, op0=ALU.is_equal), (b_io,), (b_idf,))
    k.op("dve", lambda e: e.tensor_scalar(out=c.tri[:, :], in0=io[:, :], scalar1=0.0, scalar2=None, op0=ALU.is_gt), (b_io,), (b_tri,))
    k.op("dve", lambda e: e.memset(c.nhalf[:, :], -0.5), (), (b_nh,))
    k.op("dve", lambda e: e.memset(c.ones[:, :], 1.0), (), (b_on,))
    k.end("consts")
    return c


def rms_stats(k, c, x_ap, junk_ap, ss, rstd, bx, bj, bss, brs, n=1024):
    k.op("act", lambda e: e.activation(out=junk_ap, in_=x_ap, func=AF.Square, accum_out=ss), (bx,), (bj, bss))
    k.op("pool", lambda e: e.tensor_scalar(out=rstd, in0=ss, scalar1=1.0 / n, scalar2=EPS, op0=ALU.mult, op1=ALU.add), (bss,), (brs,))
    k.op("pool", lambda e: e.tensor_tensor(out=rstd, in0=rstd, in1=c.nhalf[:, :], op=ALU.pow), (brs,), (brs,))


def load_w_scaled(k, ps, w_d, kt, ncols, g_d, name, Wt, bW, half=2048, order=None):
    nc = k.nc
    gcol = ps.enter_context(nc.sbuf_tensor(name + "_g", [128, kt], F32))
    bg = Buf()
    k.dma("sp", gcol[:, :], g_d.rearrange("(f p) -> p f", p=128), bg, (), (bg,), allow_slow_non_contiguous=True)
    half = min(half, ncols)
    stg = [ps.enter_context(nc.sbuf_tensor(name + "_s%d" % i, [128, half], F32)) for i in range(2)]
    bs = [Buf(), Buf()]
    if isinstance(bW, list):
        assert half == 512
        pieces = [(f, ci * 512) for ci in (order or range(ncols // 512)) for f in range(kt)]
    else:
        pieces = [(f, c0) for f in range(kt) for c0 in range(0, ncols, half)]
    for i, (f, c0) in enumerate(pieces):
        s, b = stg[i % 2], bs[i % 2]
        bw = bW[c0 // 512] if isinstance(bW, list) else bW
        k.dma("sp", s[:, :], w_d[f * 128:(f + 1) * 128, c0:c0 + half], b, (), (b,))
        k.op("dve", lambda e, s=s, f=f, c0=c0: e.tensor_scalar(out=Wt[:, f, c0:c0 + half], in0=s[:, :], scalar1=gcol[:, f:f + 1], scalar2=None, op0=ALU.mult), (b, bg), (bw,))
    return stg, bs


def load_w_cast(k, ps, w_d, kt, ncols, name, Wt, bW, eng="act"):
    nc = k.nc
    half = 2048 if ncols > 2048 else ncols
    stg = [ps.enter_context(nc.sbuf_tensor(name + "_s%d" % i, [128, half], F32)) for i in range(2)]
    bs = [Buf(), Buf()]
    i = 0
    for f in range(kt):
        for c0 in range(0, ncols, half):
            s, b = stg[i % 2], bs[i % 2]
            k.dma("sp", s[:, :], w_d[f * 128:(f + 1) * 128, c0:c0 + half], b, (), (b,))
            if eng == "act":
                k.op("act", lambda e, s=s, f=f, c0=c0: e.copy(out=Wt[:, f, c0:c0 + half], in_=s[:, :]), (b,), (bW,))
            else:
                k.op("dve", lambda e, s=s, f=f, c0=c0: e.tensor_copy(out=Wt[:, f, c0:c0 + half], in_=s[:, :]), (b,), (bW,))
            i += 1


def phase_A(k, c, io):
    nc = k.nc
    with ExitStack() as ps:
        k.begin()
        sb = lambda n, s, d: ps.enter_context(nc.sbuf_tensor(n, s, d))
        W = sb("A_W", [128, 8, 4096], BF16)
        bWs = [Buf() for _ in range(8)]
        load_w_scaled(k, ps, io["w_in"], 8, 4096, io["g_pre_mix"], "A_w", W, bWs, half=512, order=[0, 3, 1, 2, 4, 5, 6, 7])
        xt = [sb("A_x%d" % i, [128, 1024], F32) for i in range(4)]
        bx = [Buf() for _ in range(4)]
        junk = sb("A_junk", [128, 1024], BF16)
        bj = Buf()
        ss = [sb("A_ss%d" % i, [128, 1], F32) for i in range(4)]
        rs = [sb("A_rs%d" % i, [128, 1], F32) for i in range(4)]
        bss = [Buf() for _ in range(4)]
        brs = [Buf() for _ in range(4)]
        hb = [sb("A_hb%d" % i, [128, 1024], BF16) for i in range(2)]
        bhb = [Buf(), Buf()]
        pT = [ps.enter_context(nc.psum_tensor("A_pT%d" % i, [128, 1024], BF16)) for i in range(2)]
        bpT = [Buf(), Buf()]
        hT = [sb("A_hT%d" % i, [128, 8, 512], BF16) for i in range(2)]
        bhT = [Buf(), Buf()]
        pm = [ps.enter_context(nc.psum_tensor("A_pm%d" % i, [128, 512], F32)) for i in range(6)]
        bpm = [Buf() for _ in range(6)]
        zst = [sb("A_z%d" % i, [128, 1024], BF16) for i in range(2)]
        bz = [Buf(), Buf()]
        fst = [sb("A_f%d" % i, [128, 8, 512], BF16) for i in range(3)]
        bf = [Buf() for _ in range(3)]
        ipm = 0
        ifs = 0
        def head_tile(ch, j):
            hTc, bhTc = hT[ch % 2], bhT[ch % 2]
            t = ch * 4 + j
            xi = t % 4
            hbi = t % 2
            k.dma("sp", xt[xi][:, :], io["x"][t * 128:(t + 1) * 128, :], bx[xi], (), (bx[xi],))
            rms_stats(k, c, xt[xi][:, :], junk[:, :], ss[xi][:, :], rs[xi][:, :], bx[xi], bj, bss[xi], brs[xi])
            k.op("act", lambda e: e.activation(out=hb[hbi][:, :], in_=xt[xi][:, :], func=AF.Copy, scale=rs[xi][:, :]), (bx[xi], brs[xi]), (bhb[hbi],))
            tr(k, [(pT[hbi][:, f * 128:(f + 1) * 128], hb[hbi][:, f * 128:(f + 1) * 128], c.ident[:, :]) for f in range(8)], (bhb[hbi],), (bpT[hbi],))
            k.op("dve", lambda e: e.tensor_copy(out=hTc[:, :, j * 128:(j + 1) * 128], in_=pT[hbi][:, :].rearrange("p (f t) -> p f t", f=8)), (bpT[hbi],), (bhTc,))

        for j in range(4):
            head_tile(0, j)
        for ch in range(T // 512):
            hTc, bhTc = hT[ch % 2], bhT[ch % 2]
            for j in range(4):
                t = ch * 4 + j
                zi = t % 2
                for n, c0 in enumerate((0, 1536)):
                    p = ipm % 6
                    ipm += 1
                    mm(k, [(pm[p][:, :], hTc[:, f, j * 128:(j + 1) * 128], W[:, f, c0:c0 + 512], f == 0, f == 7) for f in range(8)], (bhTc, bWs[c0 // 512]), (bpm[p],))
                    k.op("dve", lambda e, p=p, zi=zi, n=n: e.tensor_copy(out=zst[zi][:, n * 512:(n + 1) * 512], in_=pm[p][:, :]), (bpm[p],), (bz[zi],))
                k.dma("pool", io["z_s"][t * 128:(t + 1) * 128, :], zst[zi][:, :], bz[zi], (bz[zi],), ())
            for m in range(24):
                c0 = 512 + m * 128 if m < 8 else 2048 + (m - 8) * 128
                p = ipm % 6
                ipm += 1
                mm(k, [(pm[p][:, :], W[:, f, c0:c0 + 128], hTc[:, f, :], f == 0, f == 7) for f in range(8)], (bhTc, bWs[c0 // 512]), (bpm[p],))
                fi = ifs % 3
                if m < 8:
                    k.op("dve", lambda e, p=p, fi=fi, m=m: e.tensor_copy(out=fst[fi][:, m % 8, :], in_=pm[p][:, :]), (bpm[p],), (bf[fi],))
                else:
                    k.op("act", lambda e, p=p, fi=fi, m=m: e.activation(out=fst[fi][:, m % 8, :], in_=pm[p][:, :], func=AF.Sigmoid), (bpm[p],), (bf[fi],))
                if m % 8 == 7:
                    r0 = (m // 8) * 1024
                    k.dma("pool", io["fm_s"][r0:r0 + 1024, ch * 512:(ch + 1) * 512].rearrange("(m p) t -> p m t", p=128), fst[fi][:, :, :], bf[fi], (bf[fi],), ())
                    ifs += 1
                if m in (3, 8, 13, 18) and ch + 1 < T // 512:
                    head_tile(ch + 1, (3, 8, 13, 18).index(m))
        k.end("A")


IN_SPECS = [
    ("x", [T, D]), ("p", [T, PLE]), ("g_pre_mix", [D]), ("w_in", [D, 4096]),
    ("ssm_lam_re", [G, PST]), ("ssm_lam_im", [G, PST]), ("ssm_log_dt", [G]),
    ("ssm_b_re", [G, PST, 16]), ("ssm_b_im", [G, PST, 16]),
    ("ssm_c_re", [G, 16, PST]), ("ssm_c_im", [G, 16, PST]), ("ssm_d", [512]),
    ("w_glu", [512, 512]), ("b_glu", [512]), ("w_branch_a", [512, D]), ("w_branch_b", [512, D]),
    ("w_out", [D, D]), ("g_post_mix", [D]), ("g_pre_mlp", [D]), ("w_mlp1", [D, DFF]),
    ("w_mlp2", [DFF, D]), ("g_post_mlp", [D]), ("w_ple", [PLE, D]), ("w_ple_gate", [D, D]),
    ("g_ple", [D]),
]
SCRATCH = [
    ("z_s", [T, 1024], BF16),
    ("fm_s", [3072, T], BF16),
    ("s5_s", [512, T], BF16),
    ("at_s", [T, 512], BF16),
    ("x1_s", [T, D], F32),
    ("x2_s", [T, D], F32),
]


def build(phases="ASCDEF", debug=(), inject=()):
    nc = bass.Bass("TRN2", target_bir_lowering=False)
    io = {}
    for name, shape in IN_SPECS:
        io[name] = nc.dram_tensor(name, shape, F32, kind="ExternalInput").ap()
    for name, shape, dt in SCRATCH:
        kind = "ExternalOutput" if name in debug else ("ExternalInput" if name in inject else "Internal")
        io[name] = nc.dram_tensor(name, shape, dt, kind=kind).ap()
    io["out"] = nc.dram_tensor("out", [T, D], F32, kind="ExternalOutput").ap()
    with ExitStack() as es:
        k = K(nc, es)
        c = make_consts(k, es)
        if "A" in phases:
            phase_A(k, c, io)
        if "S" in phases:
            phase_S(k, c, io)
        if "C" in phases:
            phase_C(k, c, io)
        if "D" in phases:
            phase_D(k, c, io)
        if "E" in phases:
            phase_E(k, c, io)
        if "F" in phases:
            phase_F(k, c, io)
    return nc


def make_in_maps(inputs, ncores=NCORES):
    maps = []
    for ci in range(ncores):
        m = {}
        for name, shape in IN_SPECS:
            a = inputs[name]
            if name == "x":
                a = a[ci * BPC:(ci + 1) * BPC].reshape(T, D)
            elif name == "p":
                a = a[0, ci * BPC:(ci + 1) * BPC].reshape(T, PLE)
            else:
                a = a[0]
            m[name] = np.ascontiguousarray(a, dtype=np.float32).reshape(shape)
        maps.append(m)
    return maps


def kernel(**inputs):
    inputs = {k_: np.asarray(v) for k_, v in inputs.items()}
    nc = build()
    in_maps = make_in_maps(inputs)
    res = run_bass_kernel_spmd(nc, in_maps, core_ids=list(range(NCORES)))
    outs = [np.asarray(r["out"]).reshape(BPC, SEQ, D) for r in res.results]
    return np.concatenate(outs, axis=0).astype(np.float32)


def load_gb(k, ps, g_d, name):
    nc = k.nc
    gb = ps.enter_context(nc.sbuf_tensor(name, [128, D], F32))
    b = Buf()
    k.dma("sp", gb[:, :], g_d.partition_broadcast(128), b, (), (b,))
    return gb, b


class TailBufs:
    def __init__(self, k, sb, name, n=2, inplace=False):
        self.n = n
        self.inplace = inplace
        self.junk = [sb(name + "_junk%d" % i, [128, 1024], BF16) for i in range(n)]
        self.ss = [sb(name + "_tss%d" % i, [128, 1], F32) for i in range(n)]
        self.rs = [sb(name + "_trs%d" % i, [128, 1], F32) for i in range(n)]
        self.tmp = [sb(name + "_ttmp%d" % i, [128, 1024], F32) for i in range(n)]
        self.ost = self.tmp if inplace else [sb(name + "_tost%d" % i, [128, 1024], F32) for i in range(n)]
        self.b = [[Buf() for _ in range(5)] for _ in range(n)]


def norm_res_tail(k, c, tb, i, src_ap, bsrc, gb, bgb, xres, bxres, out_rows):
    i = i % tb.n
    bj, bss, brs, btmp, bost = tb.b[i]
    junk, ss, rs, tmp, ost = tb.junk[i], tb.ss[i], tb.rs[i], tb.tmp[i], tb.ost[i]
    rms_stats(k, c, src_ap, junk[:, :], ss[:, :], rs[:, :], bsrc, bj, bss, brs)
    k.op("dve", lambda e: e.scalar_tensor_tensor(out=tmp[:, :], in0=src_ap, scalar=rs[:, :], in1=gb[:, :], op0=ALU.mult, op1=ALU.mult), (bsrc, brs, bgb), (btmp,))
    if tb.inplace:
        bost = btmp
    k.op("pool", lambda e: e.tensor_tensor(out=ost[:, :], in0=tmp[:, :], in1=xres[:, :], op=ALU.add), (btmp, bxres), (bost,))
    k.dma("pool", out_rows, ost[:, :], bost, (bost,), ())


def phase_D(k, c, io):
    nc = k.nc
    with ExitStack() as ps:
        k.begin()
        sb = lambda n, s, d: ps.enter_context(nc.sbuf_tensor(n, s, d))
        Wa = sb("D_Wa", [128, 4, 1024], BF16)
        Wb = sb("D_Wb", [128, 4, 1024], BF16)
        Wo = sb("D_Wo", [128, 8, 1024], BF16)
        bWa, bWb, bWo = Buf(), Buf(), Buf()
        load_w_cast(k, ps, io["w_branch_a"], 4, 1024, "D_wa", Wa, bWa, "dve")
        load_w_cast(k, ps, io["w_branch_b"], 4, 1024, "D_wb", Wb, bWb, "act")
        load_w_cast(k, ps, io["w_out"], 8, 1024, "D_wo", Wo, bWo, "dve")
        g2, bg2 = load_gb(k, ps, io["g_post_mix"], "D_g2")
        gts = [sb("D_gt%d" % i, [128, 16, 512], BF16) for i in range(2)]
        bgt = [Buf(), Buf()]
        s5t = [sb("D_s5%d" % i, [128, 4, 512], BF16) for i in range(2)]
        bs5 = [Buf(), Buf()]
        att = [sb("D_at%d" % i, [128, 512], BF16) for i in range(4)]
        bat = [Buf() for _ in range(4)]
        atTs = [sb("D_atT%d" % i, [128, 4, 512], BF16) for i in range(2)]
        batTs = [Buf(), Buf()]
        pT = ps.enter_context(nc.psum_tensor("D_pT", [128, 1024], BF16))
        bpT = Buf()
        pm = [ps.enter_context(nc.psum_tensor("D_pm%d" % i, [128, 512], F32)) for i in range(3)]
        bpm = [Buf() for _ in range(3)]
        pmx = [ps.enter_context(nc.psum_tensor("D_px%d" % i, [128, 1024], F32)) for i in range(2)]
        bpx = [Buf(), Buf()]
        t1 = [sb("D_t1%d" % i, [128, 512], F32) for i in range(2)]
        bt1 = [Buf(), Buf()]
        t2 = [sb("D_t2%d" % i, [128, 512], F32) for i in range(2)]
        bt2 = [Buf(), Buf()]
        mTs = [sb("D_mT%d" % i, [128, 8, 512], BF16) for i in range(2)]
        bmTs = [Buf(), Buf()]
        xt = [sb("D_x%d" % i, [128, 1024], F32) for i in range(2)]
        bx = [Buf(), Buf()]
        tbf = TailBufs(k, sb, "D")
        ipm = [0]
        xt3 = xt + [sb("D_x2", [128, 1024], F32)]
        bx3 = bx + [Buf()]

        def head(ch):
            gi = ch % 2
            atT, batT = atTs[gi], batTs[gi]
            cs = slice(ch * 512, (ch + 1) * 512)
            k.dma("sp", gts[gi][:, :, :], io["fm_s"][1024:3072, cs].rearrange("(m p) t -> p m t", p=128), bgt[gi], (), (bgt[gi],))
            k.dma("sp", s5t[gi][:, :, :], io["s5_s"][:, cs].rearrange("(m p) t -> p m t", p=128), bs5[gi], (), (bs5[gi],))
            for j in range(4):
                t = ch * 4 + j
                k.dma("sp", att[j][:, :], io["at_s"][t * 128:(t + 1) * 128, :], bat[j], (), (bat[j],))
            for j in range(4):
                tr(k, [(pT[:, f * 128:(f + 1) * 128], att[j][:, f * 128:(f + 1) * 128], c.ident[:, :]) for f in range(4)], (bat[j],), (bpT,))
                k.op("dve", lambda e, j=j: e.tensor_copy(out=atT[:, :, j * 128:(j + 1) * 128], in_=pT[:, 0:512].rearrange("p (f t) -> p f t", f=4)), (bpT,), (batT,))

        def gate(ch):
            gi = ch % 2
            atT, batT = atTs[gi], batTs[gi]
            mT, bmT = mTs[gi], bmTs[gi]
            for m in range(8):
                p = ipm[0] % 3
                ipm[0] += 1
                mm(k, [(pm[p][:, :], Wa[:, f, m * 128:(m + 1) * 128], s5t[gi][:, f, :], f == 0, f == 3) for f in range(4)], (bs5[gi], bWa), (bpm[p],))
                i1 = m % 2
                k.op("dve", lambda e, p=p, i1=i1, m=m: e.tensor_tensor(out=t1[i1][:, :], in0=pm[p][:, :], in1=gts[gi][:, m, :], op=ALU.mult), (bpm[p], bgt[gi]), (bt1[i1],))
                p2 = ipm[0] % 3
                ipm[0] += 1
                mm(k, [(pm[p2][:, :], Wb[:, f, m * 128:(m + 1) * 128], atT[:, f, :], f == 0, f == 3) for f in range(4)], (batT, bWb), (bpm[p2],))
                k.op("dve", lambda e, p2=p2, i1=i1, m=m: e.tensor_tensor(out=t2[i1][:, :], in0=pm[p2][:, :], in1=gts[gi][:, 8 + m, :], op=ALU.mult), (bpm[p2], bgt[gi]), (bt2[i1],))
                k.op("pool", lambda e, i1=i1, m=m: e.tensor_tensor(out=mT[:, m, :], in0=t1[i1][:, :], in1=t2[i1][:, :], op=ALU.add), (bt1[i1], bt2[i1]), (bmT,))

        def mm2(t):
            ch, j = t // 4, t % 4
            mT, bmT = mTs[ch % 2], bmTs[ch % 2]
            xi, x3 = t % 2, t % 3
            k.dma("sp", xt3[x3][:, :], io["x"][t * 128:(t + 1) * 128, :], bx3[x3], (), (bx3[x3],))
            for n in range(2):
                mm(k, [(pmx[xi][:, n * 512:(n + 1) * 512], mT[:, f, j * 128:(j + 1) * 128], Wo[:, f, n * 512:(n + 1) * 512], f == 0, f == 7) for f in range(8)], (bmT, bWo), (bpx[xi],))

        def tail(t):
            xi, x3 = t % 2, t % 3
            norm_res_tail(k, c, tbf, t, pmx[xi][:, :], bpx[xi], g2, bg2, xt3[x3], bx3[x3], io["x1_s"][t * 128:(t + 1) * 128, :])

        NCH = T // 512
        head(0)
        for ch in range(NCH):
            gate(ch)
            for j in range(4):
                t = ch * 4 + j
                mm2(t)
                if t >= 1:
                    tail(t - 1)
                if j == 1 and ch + 1 < NCH:
                    head(ch + 1)
        tail(NTT - 1)
        k.end("D")


def phase_E(k, c, io):
    nc = k.nc
    CH = 512
    with ExitStack() as ps:
        k.begin()
        sb = lambda n, s, d: ps.enter_context(nc.sbuf_tensor(n, s, d))
        W1 = sb("E_W1", [128, 8, 4096], BF16)
        W2 = sb("E_W2", [128, 32, 1024], BF16)
        bW1s, bW2 = [Buf() for _ in range(8)], Buf()
        stg, bstg = load_w_scaled(k, ps, io["w_mlp1"], 8, 4096, io["g_pre_mlp"], "E_w1", W1, bW1s, half=512)
        for f2 in range(64):
            f, hf = f2 // 2, f2 % 2
            s, b = stg[f2 % 2], bstg[f2 % 2]
            k.dma("sp", s[:, :], io["w_mlp2"][f * 128:(f + 1) * 128, hf * 512:(hf + 1) * 512], b, (), (b,))
            if f2 % 2:
                k.op("act", lambda e, s=s, f=f, hf=hf: e.copy(out=W2[:, f, hf * 512:(hf + 1) * 512], in_=s[:, :]), (b,), (bW2,))
            else:
                k.op("dve", lambda e, s=s, f=f, hf=hf: e.tensor_copy(out=W2[:, f, hf * 512:(hf + 1) * 512], in_=s[:, :]), (b,), (bW2,))
        g4, bg4 = load_gb(k, ps, io["g_post_mlp"], "E_g4")
        xt = [sb("E_x%d" % i, [128, 1024], F32) for i in range(4)]
        bx = [Buf() for _ in range(4)]
        junk = sb("E_junk", [128, 1024], BF16)
        bj = Buf()
        ss = [sb("E_ss%d" % i, [128, 1], F32) for i in range(2)]
        rs = [sb("E_rs%d" % i, [128, 1], F32) for i in range(2)]
        bss = [Buf(), Buf()]
        brs = [Buf(), Buf()]
        hb = [sb("E_hb0", [128, 1024], BF16)] * 2
        bhb = [Buf()] * 2
        pT = ps.enter_context(nc.psum_tensor("E_pT", [128, 1024], BF16))
        bpT = Buf()
        hT = sb("E_hT", [128, 8, CH], BF16)
        bhT = Buf()
        pm = [ps.enter_context(nc.psum_tensor("E_pm%d" % i, [128, 512], F32)) for i in range(3)]
        bpm = [Buf() for _ in range(3)]
        pmx = [ps.enter_context(nc.psum_tensor("E_px%d" % i, [128, 1024], F32)) for i in range(2)]
        bpx = [Buf(), Buf()]
        rl = [sb("E_rl0", [128, CH], F32)] * 2
        brl = [Buf()] * 2
        f1T = sb("E_f1T", [128, 32, CH], BF16)
        bf1 = Buf()
        tbf = TailBufs(k, sb, "E", n=1, inplace=True)
        ipm = 0
        nj = CH // 128
        for ch in range(T // CH):
            for j in range(nj):
                t = ch * nj + j
                xi = t % 4
                hi = t % 2
                k.dma("sp", xt[xi][:, :], io["x1_s"][t * 128:(t + 1) * 128, :], bx[xi], (), (bx[xi],))
                rms_stats(k, c, xt[xi][:, :], junk[:, :], ss[hi][:, :], rs[hi][:, :], bx[xi], bj, bss[hi], brs[hi])
                k.op("act", lambda e, xi=xi, hi=hi: e.activation(out=hb[hi][:, :], in_=xt[xi][:, :], func=AF.Copy, scale=rs[hi][:, :]), (bx[xi], brs[hi]), (bhb[hi],))
                tr(k, [(pT[:, f * 128:(f + 1) * 128], hb[hi][:, f * 128:(f + 1) * 128], c.ident[:, :]) for f in range(8)], (bhb[hi],), (bpT,))
                k.op("dve", lambda e, j=j: e.tensor_copy(out=hT[:, :, j * 128:(j + 1) * 128], in_=pT[:, :].rearrange("p (f t) -> p f t", f=8)), (bpT,), (bhT,))
            for m in range(32):
                p = ipm % 3
                ipm += 1
                mm(k, [(pm[p][:, 0:CH], W1[:, f, m * 128:(m + 1) * 128], hT[:, f, :], f == 0, f == 7) for f in range(8)], (bhT, bW1s[m // 4]), (bpm[p],))
                ri = m % 2
                k.op("act", lambda e, p=p, ri=ri: e.activation(out=rl[ri][:, :], in_=pm[p][:, 0:CH], func=AF.Relu), (bpm[p],), (brl[ri],))
                k.op("pool", lambda e, ri=ri, m=m: e.tensor_tensor(out=f1T[:, m, :], in0=rl[ri][:, :], in1=rl[ri][:, :], op=ALU.mult), (brl[ri],), (bf1,))
            for j in range(nj):
                t = ch * nj + j
                xi = t % 4
                oi = t % 2
                for n in range(2):
                    mm(k, [(pmx[oi][:, n * 512:(n + 1) * 512], f1T[:, f, j * 128:(j + 1) * 128], W2[:, f, n * 512:(n + 1) * 512], f == 0, f == 31) for f in range(32)], (bf1, bW2), (bpx[oi],))
                norm_res_tail(k, c, tbf, t, pmx[oi][:, :], bpx[oi], g4, bg4, xt[xi], bx[xi], io["x2_s"][t * 128:(t + 1) * 128, :])
        k.end("E")


def phase_F(k, c, io):
    nc = k.nc
    with ExitStack() as ps:
        k.begin()
        sb = lambda n, s, d: ps.enter_context(nc.sbuf_tensor(n, s, d))
        Wp = sb("F_Wp", [128, 2, 1024], BF16)
        Wg = sb("F_Wg", [128, 8, 1024], BF16)
        bWp, bWg = Buf(), Buf()
        load_w_cast(k, ps, io["w_ple"], 2, 1024, "F_wp", Wp, bWp, "dve")
        load_w_cast(k, ps, io["w_ple_gate"], 8, 1024, "F_wg", Wg, bWg, "act")
        g5, bg5 = load_gb(k, ps, io["g_ple"], "F_g5")
        NB = 3
        xt = [sb("F_x%d" % i, [128, 1024], F32) for i in range(4)]
        bx = [Buf() for _ in range(4)]
        pt = [sb("F_p%d" % i, [128, 256], F32) for i in range(NB)]
        bp = [Buf() for _ in range(NB)]
        xb = [sb("F_xb%d" % i, [128, 1024], BF16) for i in range(NB)]
        bxb = [Buf() for _ in range(NB)]
        pb = [sb("F_pb%d" % i, [128, 256], BF16) for i in range(NB)]
        bpb = [Buf() for _ in range(NB)]
        pTx = ps.enter_context(nc.psum_tensor("F_pTx", [128, 1024], BF16))
        pTp = ps.enter_context(nc.psum_tensor("F_pTp", [128, 1024], BF16))
        bpTx, bpTp = Buf(), Buf()
        xT = [sb("F_xT%d" % i, [128, 8, 128], BF16) for i in range(NB)]
        bxT = [Buf() for _ in range(NB)]
        pT = [sb("F_pT%d" % i, [128, 2, 128], BF16) for i in range(NB)]
        bpT = [Buf() for _ in range(NB)]
        ppw = ps.enter_context(nc.psum_tensor("F_ppw", [128, 1024], F32))
        bppw = Buf()
        psg = [ps.enter_context(nc.psum_tensor("F_psg%d" % i, [128, 1024], F32)) for i in range(2)]
        bpsg = [Buf(), Buf()]
        sgss = [sb("F_sgs%d" % i, [128, 1024], F32) for i in range(2)]
        bsgss = [Buf(), Buf()]
        ees = [sb("F_e%d" % i, [128, 1024], F32) for i in range(2)]
        bees = [Buf(), Buf()]
        tbf = TailBufs(k, sb, "F")

        def head(t):
            xi, i3 = t % 4, t % NB
            k.dma("sp", xt[xi][:, :], io["x2_s"][t * 128:(t + 1) * 128, :], bx[xi], (), (bx[xi],))
            k.dma("sp", pt[i3][:, :], io["p"][t * 128:(t + 1) * 128, :], bp[i3], (), (bp[i3],))
            k.op("act", lambda e: e.copy(out=xb[i3][:, :], in_=xt[xi][:, :]), (bx[xi],), (bxb[i3],))
            k.op("dve", lambda e: e.tensor_copy(out=pb[i3][:, :], in_=pt[i3][:, :]), (bp[i3],), (bpb[i3],))
            tr(k, [(pTx[:, f * 128:(f + 1) * 128], xb[i3][:, f * 128:(f + 1) * 128], c.ident[:, :]) for f in range(8)], (bxb[i3],), (bpTx,))
            k.op("dve", lambda e: e.tensor_copy(out=xT[i3][:, :, :], in_=pTx[:, :].rearrange("p (f t) -> p f t", f=8)), (bpTx,), (bxT[i3],))
            tr(k, [(pTp[:, f * 128:(f + 1) * 128], pb[i3][:, f * 128:(f + 1) * 128], c.ident[:, :]) for f in range(2)], (bpb[i3],), (bpTp,))
            k.op("dve", lambda e: e.tensor_copy(out=pT[i3][:, :, :], in_=pTp[:, 0:256].rearrange("p (f t) -> p f t", f=2)), (bpTp,), (bpT[i3],))

        def mid_sg(t):
            i2, i3 = t % 2, t % NB
            for n in range(2):
                mm(k, [(psg[i2][:, n * 512:(n + 1) * 512], xT[i3][:, f, :], Wg[:, f, n * 512:(n + 1) * 512], f == 0, f == 7) for f in range(8)], (bxT[i3], bWg), (bpsg[i2],))

        def mid_pw(t):
            i3 = t % NB
            for n in range(2):
                mm(k, [(ppw[:, n * 512:(n + 1) * 512], pT[i3][:, f, :], Wp[:, f, n * 512:(n + 1) * 512], f == 0, f == 1) for f in range(2)], (bpT[i3], bWp), (bppw,))

        def tail(t):
            xi, i2 = t % 4, t % 2
            sgs, bsgs, ee, bee = sgss[i2], bsgss[i2], ees[i2], bees[i2]
            k.op("act", lambda e: e.activation(out=sgs[:, :], in_=psg[i2][:, :], func=AF.Sigmoid), (bpsg[i2],), (bsgs,))
            k.op("dve", lambda e: e.tensor_tensor(out=ee[:, :], in0=ppw[:, :], in1=sgs[:, :], op=ALU.mult), (bppw, bsgs), (bee,))
            norm_res_tail(k, c, tbf, t, ee[:, :], bee, g5, bg5, xt[xi], bx[xi], io["out"][t * 128:(t + 1) * 128, :])

        head(0)
        head(1)
        for t in range(NTT):
            mid_sg(t)
            if t >= 1:
                tail(t - 1)
            mid_pw(t)
            if t + 2 < NTT:
                head(t + 2)
        tail(NTT - 1)
        k.end("F")


def phase_C(k, c, io):
    nc = k.nc
    with ExitStack() as ps:
        k.begin()
        sb = lambda n, s, d: ps.enter_context(nc.sbuf_tensor(n, s, d))
        ioi = sb("C_ioi", [128, 64], I32)
        io2 = sb("C_io2", [128, 256], I32)
        io3 = sb("C_io3", [128, 2048], I32)
        negp = sb("C_negp", [128, 8, 64], F32)
        cm = sb("C_cm", [128, 2, 256], BF16)
        oh = sb("C_oh", [128, 64 * 128], BF16)
        bio, bnegp, bcm, boh, bio3 = Buf(), Buf(), Buf(), Buf(), Buf()
        k.op("pool", lambda e: e.iota(ioi[:, :], [[0, 8], [1, 8]], base=0, channel_multiplier=0), (), (bio,))
        for qb in range(8):
            k.op("dve", lambda e, qb=qb: e.tensor_scalar(out=negp[:, qb, :], in0=ioi[:, :], scalar1=float(qb), scalar2=-1e30, op0=ALU.is_ge, op1=ALU.mult), (bio,), (bnegp,))
        for h2 in range(2):
            k.op("pool", lambda e, h2=h2: e.iota(io2[:, :], [[1, 256]], base=-128 * h2, channel_multiplier=-1), (bcm,), (bio,))
            k.op("dve", lambda e, h2=h2: e.tensor_scalar(out=cm[:, h2, :], in0=io2[:, :], scalar1=0.0, scalar2=NEG, op0=ALU.is_lt, op1=ALU.mult), (bio,), (bcm,))
        for q4 in range(4):
            k.op("pool", lambda e, q4=q4: e.iota(io3[:, :], [[1, 16], [0, 128]], base=16 * q4, channel_multiplier=-1), (boh,), (bio3,))
            k.op("dve", lambda e, q4=q4: e.tensor_scalar(out=oh[:, q4 * 2048:(q4 + 1) * 2048], in0=io3[:, :], scalar1=0.0, scalar2=None, op0=ALU.is_equal), (bio3,), (boh,))
        qT = [sb("C_qT%d" % i, [128, 4, SEQ], BF16) for i in range(2)]
        kTzs = [sb("C_kTz%d" % i, [128, 8, SEQ], BF16) for i in range(2)]
        Va = [sb("C_V%d" % i, [128, 16, 8, 66], BF16) for i in range(2)]
        bq, bv, bks = [Buf(), Buf()], [Buf(), Buf()], [Buf(), Buf()]
        for i in range(2):
            k.op("pool", lambda e, i=i: e.memset(kTzs[i][:, :, :], 0.0), (), (bks[i],))
            k.op("pool", lambda e, i=i: e.memset(Va[i][:, :, :, :], 1.0), (), (bv[i],))
        kms = sb("C_kms", [128, 64], F32)
        kmbs = [sb("C_kmb%d" % i, [128, 8, 8], BF16) for i in range(2)]
        bkms, bkmbs = Buf(), [Buf(), Buf()]
        pgb = ps.enter_context(nc.psum_tensor("C_pgb", [128, 512], F32))
        bpg = [Buf(), Buf()]
        bpbt = [Buf(), Buf()]
        gms = [sb("C_gm%d" % i, [128, 64], F32) for i in range(2)]
        cmps = [sb("C_cmp%d" % i, [128, 512], F32) for i in range(2)]
        ranks = [sb("C_rank%d" % i, [128, 64], F32) for i in range(2)]
        biasbs = [sb("C_biasb%d" % i, [128, 128], F32) for i in range(2)]
        bgms, bcmps, branks, bbiasbs = ([Buf(), Buf()] for _ in range(4))
        biasTs = [sb("C_biasT%d" % i, [128, SEQ], BF16) for i in range(2)]
        bbTs = [Buf(), Buf()]
        for i in range(2):
            k.op("pool", lambda e, i=i: e.memset(biasbs[i][:, :], 0.0), (), (bbiasbs[i],))
        pss = [ps.enter_context(nc.psum_tensor("C_ps%d" % i, [128, 512], F32)) for i in range(3)]
        bps = [Buf(), Buf(), Buf()]
        po = [[ps.enter_context(nc.psum_tensor("C_po%d%d" % (i, j), [128, 512], F32)) for j in range(2)] for i in range(2)]
        bpo = [[Buf(), Buf()], [Buf(), Buf()]]
        pe_ = [sb("C_pe%d" % i, [128, 256], BF16) for i in range(3)]
        bpe = [Buf() for _ in range(3)]
        rden = sb("C_rden", [128, 1], F32)
        brd = Buf()
        atm = [sb("C_atm%d" % i, [128, 16, 512], BF16) for i in range(2)]
        batm = [Buf(), Buf()]
        cnt = {"ips": 0, "ipe": 0, "ipo": 0}

        def load_seq(s):
            si = s % 2
            tb = s * SEQ
            kTz, bk = kTzs[si], bks[si]
            k.dma("sp", qT[si][:, :, :], io["fm_s"][0:512, tb:tb + SEQ].rearrange("(m p) t -> p m t", p=128), bq[si], (), (bq[si],))
            for h in range(8):
                hp = slice((h % 2) * 64, (h % 2) * 64 + 64)
                k.dma("sp", kTz[hp, h, :], io["fm_s"][512 + h * 64:512 + (h + 1) * 64, tb:tb + SEQ], bk, (), (bk,))
            for kt in range(16):
                k.dma("sp", Va[si][:, kt, :, 0:64], io["z_s"][tb + kt * 128:tb + (kt + 1) * 128, 512:1024].rearrange("p (h d) -> p h d", h=8), bv[si], (), (bv[si],))
            k.op("dve", lambda e: e.tensor_reduce(out=kms[:, :], in_=kTz[:, :, :].rearrange("p h (b t) -> p (h b) t", b=8), axis=AX.X, op=ALU.add), (bk,), (bkms,))
            k.op("dve", lambda e: e.tensor_copy(out=kmbs[si][:, :, :], in_=kms[:, :].rearrange("p (h b) -> p h b", h=8)), (bkms,), (bkmbs[si],))

        def gate_step(s, qt):
            si = s % 2
            g2 = qt % 2
            qb = qt // 2
            gm, cmp, rank, biasb = gms[g2], cmps[g2], ranks[g2], biasbs[g2]
            pg = pgb[:, g2 * 64:(g2 + 1) * 64]
            pbt = pgb[:, 256 + g2 * 128:256 + (g2 + 1) * 128]

            def gfn(eng):
                ins = None
                for h in range(8):
                    ins = eng.matmul(pg[:, h * 8:(h + 1) * 8], qT[si][:, h // 2, qt * 128:(qt + 1) * 128], kmbs[si][:, h, :], start=True, stop=True)
                return ins
            k.op("pe", gfn, (bq[si], bkmbs[si]), (bpg[g2],))
            k.op("dve", lambda e: e.tensor_tensor(out=gm[:, :], in0=pg, in1=negp[:, qb, :], op=ALU.add), (bpg[g2], bnegp), (bgms[g2],))
            g3 = gm[:, :].rearrange("p (h b) -> p h b", h=8)
            k.op("dve", lambda e: e.tensor_tensor(out=cmp[:, :].rearrange("p (h b c) -> p h b c", h=8, b=8), in0=g3.unsqueeze(2).broadcast_to([128, 8, 8, 8]), in1=g3.unsqueeze(3).broadcast_to([128, 8, 8, 8]), op=ALU.is_gt), (bgms[g2],), (bcmps[g2],))
            k.op("dve", lambda e: e.tensor_reduce(out=rank[:, :], in_=cmp[:, :].rearrange("p (a c) -> p a c", c=8), axis=AX.X, op=ALU.add), (bcmps[g2],), (branks[g2],))
            k.op("dve", lambda e: e.tensor_scalar(out=biasb[:, 0:64], in0=rank[:, :], scalar1=2.5, scalar2=NEG, op0=ALU.is_gt, op1=ALU.mult), (branks[g2],), (bbiasbs[g2],))
            tr(k, [(pbt, biasb[:, :], c.identf[:, :])], (bbiasbs[g2],), (bpbt[g2],))
            k.op("dve", lambda e: e.tensor_copy(out=biasTs[si][:, qt * 128:(qt + 1) * 128], in_=pbt), (bpbt[g2],), (bbTs[si],))

        def sweep(s, inserts):
            si = s % 2
            kTz, bk = kTzs[si], bks[si]
            biasT, bbT = biasTs[si], bbTs[si]
            units = [(h, qb, kt) for h in range(8) for qb in range(8) for kt in range(2 * qb + 2)]

            def score(u, p):
                h, qb, kt = u
                m = h // 2
                b = kt // 2
                qs = slice(qb * 256, (qb + 1) * 256)
                last = (b != qb) and (qb <= 3)
                first = (pss[p][:, 0:256], kTz[:, h, kt * 128:(kt + 1) * 128], qT[si][:, m, qs], True, last)
                if b == qb:
                    items = [first, (pss[p][:, 0:256], c.ident[:, :], cm[:, kt % 2, :], False, True)]
                    rd = (bk, bq[si], bcm)
                elif last:
                    items = [first]
                    rd = (bk, bq[si])
                else:
                    r = h * 8 + b
                    items = [first, (pss[p][:, 0:256], oh[:, r * 128:(r + 1) * 128], biasT[:, qs], False, True)]
                    rd = (bk, bq[si], boh, bbT)
                mm(k, items, rd, (bps[p],))

            ips = cnt["ips"]
            score(units[0], ips % 3)
            score(units[1], (ips + 1) % 3)
            oi = 0
            for ui, u in enumerate(units):
                h, qb, kt = u
                nkt = 2 * qb + 2
                p = ips % 3
                ips += 1
                if ui + 2 < len(units):
                    score(units[ui + 2], (ips + 1) % 3)
                if kt == 0:
                    oi = cnt["ipo"] % 2
                    cnt["ipo"] += 1
                e_i = cnt["ipe"] % 3
                cnt["ipe"] += 1
                k.op("act", lambda e, p=p, e_i=e_i: e.activation(out=pe_[e_i][:, :], in_=pss[p][:, 0:256], func=AF.Exp, scale=0.125), (bps[p],), (bpe[e_i],))
                mm(k, [(po[oi][qh][:, 0:65], pe_[e_i][:, qh * 128:(qh + 1) * 128], Va[si][:, kt, h, 0:65], kt == 0, kt == nkt - 1) for qh in range(2)], (bpe[e_i], bv[si]), (bpo[oi][0], bpo[oi][1]))
                if kt == nkt - 1:
                    for qh in range(2):
                        qt = qb * 2 + qh
                        k.op("dve", lambda e, oi=oi, qh=qh: e.reciprocal(out=rden[:, :], in_=po[oi][qh][:, 64:65]), (bpo[oi][qh],), (brd,))
                        k.op("dve", lambda e, oi=oi, qh=qh, qt=qt, h=h: e.tensor_scalar(out=atm[si][:, qt, h * 64:(h + 1) * 64], in0=po[oi][qh][:, 0:64], scalar1=rden[:, :], scalar2=None, op0=ALU.mult), (bpo[oi][qh], brd), (batm[si],))
                if ui in inserts:
                    inserts[ui]()
            cnt["ips"] = ips
            tb = s * SEQ
            k.dma("pool", io["at_s"][tb:tb + SEQ, :].rearrange("(q p) c -> p q c", p=128), atm[si][:, :, :], batm[si], (batm[si],), ())

        load_seq(0)
        for qt in range(16):
            gate_step(0, qt)
        for s in range(BPC):
            inserts = {}
            if s + 1 < BPC:
                inserts[8] = (lambda s=s: load_seq(s + 1))
                for qt in range(16):
                    inserts[40 + qt * 30] = (lambda s=s, qt=qt: gate_step(s + 1, qt))
            sweep(s, inserts)
        k.end("C")


TWO_PI = 2.0 * math.pi


def cis(k, scr, first, out_c, out_s, barg, bout):
    t, ti, y, m1 = scr
    bt = Buf()
    k.op("dve", lambda e: first(e, t), (barg,), (bt,))
    k.op("dve", lambda e: e.tensor_copy(out=ti, in_=t), (bt,), (bt,))
    k.op("dve", lambda e: e.tensor_copy(out=y, in_=ti), (bt,), (bt,))
    k.op("dve", lambda e: e.tensor_tensor(out=t, in0=t, in1=y, op=ALU.subtract), (bt,), (bt,))
    for shift, dst in ((0.0, out_s), (math.pi / 2, out_c)):
        k.op("dve", lambda e, shift=shift: e.tensor_scalar(out=y, in0=t, scalar1=TWO_PI, scalar2=shift, op0=ALU.mult, op1=ALU.add), (bt,), (bt,))
        k.op("dve", lambda e: e.tensor_scalar(out=m1, in0=y, scalar1=math.pi, scalar2=-TWO_PI, op0=ALU.is_gt, op1=ALU.mult), (bt,), (bt,))
        k.op("dve", lambda e: e.tensor_tensor(out=m1, in0=m1, in1=y, op=ALU.add), (bt,), (bt,))
        k.op("dve", lambda e: e.tensor_scalar(out=y, in0=y, scalar1=-math.pi, scalar2=TWO_PI, op0=ALU.is_lt, op1=ALU.mult), (bt,), (bt,))
        k.op("dve", lambda e: e.tensor_tensor(out=y, in0=m1, in1=y, op=ALU.add), (bt,), (bt,))
        k.op("dve", lambda e: e.tensor_scalar(out=y, in0=y, scalar1=math.pi, scalar2=-math.pi, op0=ALU.min, op1=ALU.max), (bt,), (bt,))
        k.op("act", lambda e, dst=dst: e.activation(out=dst, in_=y, func=AF.Sin), (bt,), (bout, bt))


def cmul(k, eng, out_r, out_i, ar, ai, br, bi, t1, t2, rd, wr, bt, neg_i=False):
    E = eng
    k.op(E, lambda e: e.tensor_tensor(out=t1, in0=ar, in1=br, op=ALU.mult), rd, (bt,))
    k.op(E, lambda e: e.tensor_tensor(out=t2, in0=ai, in1=bi, op=ALU.mult), rd + (bt,), (bt,))
    k.op(E, lambda e: e.tensor_tensor(out=out_r, in0=t1, in1=t2, op=ALU.subtract), (bt,), wr + (bt,))
    k.op(E, lambda e: e.tensor_tensor(out=t1, in0=ar, in1=bi, op=ALU.mult), rd + (bt,), (bt,))
    k.op(E, lambda e: e.tensor_tensor(out=t2, in0=ai, in1=br, op=ALU.mult), rd + (bt,), (bt,))
    if neg_i:
        k.op(E, lambda e: e.scalar_tensor_tensor(out=out_i, in0=t1, scalar=-1.0, in1=t2, op0=ALU.mult, op1=ALU.subtract), (bt,), wr + (bt,))
    else:
        k.op(E, lambda e: e.tensor_tensor(out=out_i, in0=t1, in1=t2, op=ALU.add), (bt,), wr + (bt,))


def phase_S(k, c, io):
    nc = k.nc
    with ExitStack() as outer:
        osb = lambda n, s, d: outer.enter_context(nc.sbuf_tensor(n, s, d))
        GT = osb("S_GT", [128, G, 2, 128], BF16)
        TT = osb("S_TT", [128, G, 2, 256], BF16)
        H = osb("S_H", [128, G, 256], BF16)
        Mt = osb("S_M", [128, 2, 2048], F32)
        Dt = osb("S_D", [128, 2, 2048], F32)
        s5_setup(k, c, io, GT, TT, H, Mt, Dt)
        s5_main(k, c, io, GT, TT, H, Mt, Dt)


def s5_setup(k, c, io, GT, TT, H, Mt, Dt):
    nc = k.nc
    with ExitStack() as ps:
        k.begin()
        sb = lambda n, s, d: ps.enter_context(nc.sbuf_tensor(n, s, d))
        sb2 = lambda n, s, d: ps.enter_context(nc.sbuf_tensor(n, [s[0], int(np.prod(s[1:]))], d))
        B0 = Buf()
        lg = sb("u_lg", [32, 128], F32)
        k.dma("sp", lg[:, 0:64], io["ssm_lam_re"], B0, (), (B0,))
        k.dma("sp", lg[:, 64:128], io["ssm_lam_im"], B0, (), (B0,))
        ldt = sb("u_ldt", [64, 32], F32)
        k.dma("sp", ldt[:, :], io["ssm_log_dt"].partition_broadcast(64), B0, (), (B0,))
        Bre = sb("u_Bre", [64, G, 16], F32)
        Bim = sb("u_Bim", [64, G, 16], F32)
        k.dma("sp", Bre[:, :, :], io["ssm_b_re"].rearrange("g p c -> p g c"), B0, (), (B0,))
        k.dma("sp", Bim[:, :, :], io["ssm_b_im"].rearrange("g p c -> p g c"), B0, (), (B0,))
        cg = [sb("u_cg%d" % i, [128, 4, 64], F32) for i in range(2)]
        k.dma("sp", cg[0][:, :, :], io["ssm_c_re"].rearrange("(a b) c p -> (b c) a p", a=4), B0, (), (B0,))
        k.dma("sp", cg[1][:, :, :], io["ssm_c_im"].rearrange("(a b) c p -> (b c) a p", a=4), B0, (), (B0,))
        dcol = sb("u_dcol", [128, G], F32)
        for j in range(8):
            k.dma("sp", dcol[j * 16:(j + 1) * 16, :], io["ssm_d"].rearrange("(g c) -> c g", c=16), B0, (), (B0,), allow_slow_non_contiguous=True)
        pp = ps.enter_context(nc.psum_tensor("u_pp", [128, 512], F32))
        pq = ps.enter_context(nc.psum_tensor("u_pq", [128, 512], F32))
        Bp = Buf()
        lre = sb("u_lre", [64, G], F32)
        lim = sb("u_lim", [64, G], F32)
        tr(k, [(pp[0:64, 0:32], lg[:, 0:64], c.identf[0:32, 0:32]), (pp[0:64, 32:64], lg[:, 64:128], c.identf[0:32, 0:32])], (B0,), (Bp,))
        k.op("dve", lambda e: e.tensor_copy(out=lre[:, :], in_=pp[0:64, 0:32]), (Bp,), (B0,))
        k.op("dve", lambda e: e.tensor_copy(out=lim[:, :], in_=pp[0:64, 32:64]), (Bp,), (B0, Bp))
        Cre = sb("u_Cre", [64, G, 16], F32)
        Cim = sb("u_Cim", [64, G, 16], F32)
        for i, Cx in enumerate((Cre, Cim)):
            tr(k, [(pp[0:64, a * 128:(a + 1) * 128], cg[i][:, a, :], c.identf[:, :]) for a in range(4)], (B0,), (Bp,))
            k.op("dve", lambda e, Cx=Cx: e.tensor_copy(out=Cx[:, :, :], in_=pp[0:64, :].rearrange("p (g c) -> p g c", c=16)), (Bp,), (B0, Bp))
        dt = sb("u_dt", [64, G], F32)
        k.op("act", lambda e: e.activation(out=dt[:, :], in_=ldt[:, :], func=AF.Exp), (B0,), (B0,))
        rho = sb("u_rho", [64, G], F32)
        th = sb("u_th", [64, G], F32)
        k.op("dve", lambda e: e.tensor_tensor(out=rho[:, :], in0=lre[:, :], in1=dt[:, :], op=ALU.mult), (B0,), (B0,))
        k.op("dve", lambda e: e.tensor_tensor(out=th[:, :], in0=lim[:, :], in1=dt[:, :], op=ALU.mult), (B0,), (B0,))
        nvi = sb("u_nvi", [64, 17], I32)
        nv = sb("u_nv", [64, 17], F32)
        k.op("pool", lambda e: e.iota(nvi[:, :], [[1, 17]], base=0, channel_multiplier=0), (B0,), (B0,))
        k.op("dve", lambda e: e.tensor_copy(out=nv[:, :], in_=nvi[:, :]), (B0,), (B0,))
        NP = G * 17
        argt = sb("u_argt", [64, NP], F32)
        argr = sb("u_argr", [64, NP], F32)
        a3 = lambda t: t[:, :].rearrange("p (g n) -> p g n", n=17)
        bth = th[:, :].unsqueeze(2).broadcast_to([64, G, 17])
        brho = rho[:, :].unsqueeze(2).broadcast_to([64, G, 17])
        bnv = nv[:, :].unsqueeze(1).broadcast_to([64, G, 17])
        k.op("dve", lambda e: e.tensor_tensor(out=a3(argt), in0=bth, in1=bnv, op=ALU.mult), (B0,), (B0,))
        k.op("dve", lambda e: e.tensor_tensor(out=a3(argr), in0=brho, in1=bnv, op=ALU.mult), (B0,), (B0,))
        cs = sb("u_cs", [64, NP], F32)
        sn = sb("u_sn", [64, NP], F32)
        scr = (sb("u_c1", [64, NP], F32)[:, :], sb("u_c2", [64, NP], I32)[:, :], sb("u_c3", [64, NP], F32)[:, :], sb("u_c4", [64, NP], F32)[:, :])
        cis(k, scr, lambda e, t: e.tensor_scalar(out=t, in0=argt[:, :], scalar1=1.0 / TWO_PI, scalar2=None, op0=ALU.mult), cs[:, :], sn[:, :], B0, B0)
        mg = sb("u_mg", [64, NP], F32)
        mgi = sb("u_mgi", [64, NP], F32)
        k.op("act", lambda e: e.activation(out=mg[:, :], in_=argr[:, :], func=AF.Exp), (B0,), (B0,))
        k.op("act", lambda e: e.activation(out=mgi[:, :], in_=argr[:, :], func=AF.Exp, scale=-1.0), (B0,), (B0,))
        Pr = sb("u_Pr", [64, G, 17], F32)
        Pi = sb("u_Pi", [64, G, 17], F32)
        Qr = sb("u_Qr", [64, G, 17], F32)
        Qi = sb("u_Qi", [64, G, 17], F32)
        f2 = lambda t: t[:, :, :].rearrange("p g n -> p (g n)")
        k.op("dve", lambda e: e.tensor_tensor(out=f2(Pr), in0=mg[:, :], in1=cs[:, :], op=ALU.mult), (B0,), (B0,))
        k.op("dve", lambda e: e.tensor_tensor(out=f2(Pi), in0=mg[:, :], in1=sn[:, :], op=ALU.mult), (B0,), (B0,))
        k.op("dve", lambda e: e.tensor_tensor(out=f2(Qr), in0=mgi[:, :], in1=cs[:, :], op=ALU.mult), (B0,), (B0,))
        k.op("dve", lambda e: e.scalar_tensor_tensor(out=f2(Qi), in0=mgi[:, :], scalar=-1.0, in1=sn[:, :], op0=ALU.mult, op1=ALU.mult), (B0,), (B0,))
        den = sb("u_den", [64, G], F32)
        tA = sb("u_tA", [64, G], F32)
        tB = sb("u_tB", [64, G], F32)
        nr = sb("u_nr", [64, G], F32)
        cr = sb("u_cr", [64, G], F32)
        ci = sb("u_ci", [64, G], F32)
        k.op("dve", lambda e: e.tensor_tensor(out=den[:, :], in0=lre[:, :], in1=lre[:, :], op=ALU.mult), (B0,), (B0,))
        k.op("dve", lambda e: e.tensor_tensor(out=tA[:, :], in0=lim[:, :], in1=lim[:, :], op=ALU.mult), (B0,), (B0,))
        k.op("dve", lambda e: e.tensor_tensor(out=den[:, :], in0=den[:, :], in1=tA[:, :], op=ALU.add), (B0,), (B0,))
        k.op("dve", lambda e: e.reciprocal(out=den[:, :], in_=den[:, :]), (B0,), (B0,))
        k.op("dve", lambda e: e.tensor_scalar(out=nr[:, :], in0=Pr[:, :, 1], scalar1=-1.0, scalar2=None, op0=ALU.add), (B0,), (B0,))
        k.op("dve", lambda e: e.tensor_tensor(out=tA[:, :], in0=nr[:, :], in1=lre[:, :], op=ALU.mult), (B0,), (B0,))
        k.op("dve", lambda e: e.tensor_tensor(out=tB[:, :], in0=Pi[:, :, 1], in1=lim[:, :], op=ALU.mult), (B0,), (B0,))
        k.op("dve", lambda e: e.tensor_tensor(out=tA[:, :], in0=tA[:, :], in1=tB[:, :], op=ALU.add), (B0,), (B0,))
        k.op("dve", lambda e: e.tensor_tensor(out=cr[:, :], in0=tA[:, :], in1=den[:, :], op=ALU.mult), (B0,), (B0,))
        k.op("dve", lambda e: e.tensor_tensor(out=tA[:, :], in0=Pi[:, :, 1], in1=lre[:, :], op=ALU.mult), (B0,), (B0,))
        k.op("dve", lambda e: e.tensor_tensor(out=tB[:, :], in0=nr[:, :], in1=lim[:, :], op=ALU.mult), (B0,), (B0,))
        k.op("dve", lambda e: e.tensor_tensor(out=tA[:, :], in0=tA[:, :], in1=tB[:, :], op=ALU.subtract), (B0,), (B0,))
        k.op("dve", lambda e: e.tensor_tensor(out=ci[:, :], in0=tA[:, :], in1=den[:, :], op=ALU.mult), (B0,), (B0,))
        bbr = sb("u_bbr", [64, G, 16], F32)
        bbi = sb("u_bbi", [64, G, 16], F32)
        w1 = sb("u_w1", [64, G, 16], F32)
        w2 = sb("u_w2", [64, G, 16], F32)
        bc16 = lambda t: t[:, :].unsqueeze(2).broadcast_to([64, G, 16])
        cmul(k, "dve", bbr[:, :, :], bbi[:, :, :], bc16(cr), bc16(ci), Bre[:, :, :], Bim[:, :, :], w1[:, :, :], w2[:, :, :], (B0,), (B0,), B0)
        mki = sb("u_mki", [128, 2, 256], I32)
        mask = sb("u_mask", [128, 2, 256], F32)
        idc = sb("u_idc", [128, 2, 256], F32)
        shid = sb("u_shid", [64, 128], F32)
        for ch in range(2):
            k.op("pool", lambda e, ch=ch: e.iota(mki[:, ch, :], [[16, 16], [0, 16]], base=15 - 128 * ch, channel_multiplier=-1), (B0,), (B0,))
        k.op("dve", lambda e: e.tensor_scalar(out=mask[:, :, :], in0=mki[:, :, :], scalar1=0.0, scalar2=None, op0=ALU.is_ge), (B0,), (B0,))
        for ch in range(2):
            k.op("pool", lambda e, ch=ch: e.iota(mki[:, ch, :], [[16, 16], [1, 16]], base=-128 * ch, channel_multiplier=-1), (B0,), (B0,))
        k.op("dve", lambda e: e.tensor_scalar(out=idc[:, :, :], in0=mki[:, :, :], scalar1=0.0, scalar2=None, op0=ALU.is_equal), (B0,), (B0,))
        k.op("pool", lambda e: e.iota(mki[0:64, 0, 0:128], [[1, 128]], base=-64, channel_multiplier=-1), (B0,), (B0,))
        k.op("dve", lambda e: e.tensor_scalar(out=shid[:, :], in0=mki[0:64, 0, 0:128], scalar1=0.0, scalar2=None, op0=ALU.is_equal), (B0,), (B0,))
        GC = 4
        Fr = sb("u_Fr", [64, GC, 16, 16], F32)
        Fi = sb("u_Fi", [64, GC, 16, 16], F32)
        Er = sb("u_Er", [64, GC, 17, 16], F32)
        nEi = sb("u_nEi", [64, GC, 17, 16], F32)
        Lr = sb("u_Lr", [64, GC, 16, 16], F32)
        Li = sb("u_Li", [64, GC, 16, 16], F32)
        x1 = sb("u_x1", [64, GC, 17, 16], F32)
        x2 = sb("u_x2", [64, GC, 17, 16], F32)
        tmask2 = sb("u_tmask2", [128, 512], F32)
        for gc in range(G // GC):
            gs = slice(gc * GC, (gc + 1) * GC)
            qr = Qr[:, gs, 0:16].unsqueeze(3).broadcast_to([64, GC, 16, 16])
            qi = Qi[:, gs, 0:16].unsqueeze(3).broadcast_to([64, GC, 16, 16])
            br_ = bbr[:, gs, :].unsqueeze(2).broadcast_to([64, GC, 16, 16])
            bi_ = bbi[:, gs, :].unsqueeze(2).broadcast_to([64, GC, 16, 16])
            cmul(k, "dve", Fr[:, :, :, :], Fi[:, :, :, :], qr, qi, br_, bi_, x1[:, :, 0:16, :], x2[:, :, 0:16, :], (B0,), (B0,), B0)
            pr = Pr[:, gs, :].unsqueeze(3).broadcast_to([64, GC, 17, 16])
            pi = Pi[:, gs, :].unsqueeze(3).broadcast_to([64, GC, 17, 16])
            cr_ = Cre[:, gs, :].unsqueeze(2).broadcast_to([64, GC, 17, 16])
            ci_ = Cim[:, gs, :].unsqueeze(2).broadcast_to([64, GC, 17, 16])
            cmul(k, "dve", Er[:, :, :, :], nEi[:, :, :, :], pr, pi, cr_, ci_, x1[:, :, :, :], x2[:, :, :, :], (B0,), (B0,), B0, neg_i=True)
            l15r = Pr[:, gs, 15:16].unsqueeze(3).broadcast_to([64, GC, 16, 16])
            l15i = Pi[:, gs, 15:16].unsqueeze(3).broadcast_to([64, GC, 16, 16])
            cmul(k, "dve", Lr[:, :, :, :], Li[:, :, :, :], l15r, l15i, Fr[:, :, :, :], Fi[:, :, :, :], x1[:, :, 0:16, :], x2[:, :, 0:16, :], (B0,), (B0,), B0)
            for gl in range(0, GC, 2):
                g = gc * GC + gl
                items = []
                for g2 in range(2):
                    for ch in range(2):
                        o0 = g2 * 256 + ch * 128
                        items.append((pp[:, o0:o0 + 64], Lr[:, gl + g2, ch * 8:(ch + 1) * 8, :].rearrange("p j c -> p (j c)"), c.identf[0:64, 0:64]))
                        items.append((pp[:, o0 + 64:o0 + 128], Li[:, gl + g2, ch * 8:(ch + 1) * 8, :].rearrange("p j c -> p (j c)"), c.identf[0:64, 0:64]))
                tr(k, items, (B0,), (Bp,))
                k.op("act", lambda e, g=g: e.copy(out=GT[:, g:g + 2, :, :], in_=pp[:, :].rearrange("p (g a b) -> p g a b", g=2, a=2)), (Bp,), (B0, Bp))
                for ch in range(2):
                    items = []
                    for g2 in range(2):
                        fr_ = Fr[:, gl + g2, ch * 8:(ch + 1) * 8, :].rearrange("p j c -> p (j c)")
                        fi_ = Fi[:, gl + g2, ch * 8:(ch + 1) * 8, :].rearrange("p j c -> p (j c)")
                        er_ = Er[:, gl + g2, 0:16, :].rearrange("p i c -> p (i c)")
                        ei_ = nEi[:, gl + g2, 0:16, :].rearrange("p i c -> p (i c)")
                        o = pq[:, g2 * 256:(g2 + 1) * 256]
                        items += [(o, fr_, er_, True, False), (o, fi_, ei_, False, True)]
                    Bq = Buf()
                    mm(k, items, (B0,), (Bq, Bp))
                    k.op("dve", lambda e, ch=ch: e.tensor_tensor(out=tmask2[:, :].rearrange("p (g x) -> p g x", g=2), in0=pq[:, :].rearrange("p (g x) -> p g x", g=2), in1=mask[:, ch, :].unsqueeze(1).broadcast_to([128, 2, 256]), op=ALU.mult), (Bq, B0), (B0, Bp))
                    for g2 in range(2):
                        k.op("dve", lambda e, ch=ch, g=g, g2=g2: e.scalar_tensor_tensor(out=TT[:, g + g2, ch, :], in0=idc[:, ch, :], scalar=dcol[:, g + g2:g + g2 + 1], in1=tmask2[:, g2 * 256:(g2 + 1) * 256], op0=ALU.mult, op1=ALU.add), (B0,), (B0,))
                items = []
                for g2 in range(2):
                    er1 = Er[:, gl + g2, 1:17, :].rearrange("p i c -> p (i c)")
                    ei1 = nEi[:, gl + g2, 1:17, :].rearrange("p i c -> p (i c)")
                    o = pq[:, g2 * 256:(g2 + 1) * 256]
                    items += [(o, c.identf[0:64, :], er1, True, False), (o, shid[:, :], ei1, False, True)]
                mm(k, items, (B0,), (Bp,))
                k.op("act", lambda e, g=g: e.copy(out=H[:, g:g + 2, :], in_=pq[:, :].rearrange("p (g x) -> p g x", g=2)), (Bp,), (B0, Bp))
        k.end("S_setup")
    with ExitStack() as ps:
        k.begin()
        sb = lambda n, s, d: ps.enter_context(nc.sbuf_tensor(n, s, d))
        B0 = Buf()
        lb = sb("w_lb", [128, 2, 2048], F32)
        k.dma("sp", lb[:, 0, :], io["ssm_lam_re"].rearrange("g p -> (g p)").partition_broadcast(128), B0, (), (B0,))
        k.dma("sp", lb[:, 1, :], io["ssm_lam_im"].rearrange("g p -> (g p)").partition_broadcast(128), B0, (), (B0,))
        dtb = sb("w_dtb", [128, G], F32)
        k.dma("sp", dtb[:, :], io["ssm_log_dt"].partition_broadcast(128), B0, (), (B0,))
        k.op("act", lambda e: e.activation(out=dtb[:, :], in_=dtb[:, :], func=AF.Exp), (B0,), (B0,))
        for i in range(2):
            k.op("dve", lambda e, i=i: e.tensor_tensor(out=lb[:, i, :].rearrange("p (g q) -> p g q", q=64), in0=lb[:, i, :].rearrange("p (g q) -> p g q", q=64), in1=dtb[:, :].unsqueeze(2).broadcast_to([128, G, 64]), op=ALU.mult), (B0,), (B0,))
        nki = sb("w_nki", [128, 2], I32)
        nk = sb("w_nk", [128, 2], F32)
        nk2 = sb("w_nk2", [128, 2], F32)
        k.op("pool", lambda e: e.iota(nki[:, 0:1], [[0, 1]], base=1024, channel_multiplier=-16), (B0,), (B0,))
        k.op("pool", lambda e: e.iota(nki[:, 1:2], [[0, 1]], base=-1040, channel_multiplier=16), (B0,), (B0,))
        k.op("dve", lambda e: e.tensor_copy(out=nk[:, :], in_=nki[:, :]), (B0,), (B0,))
        k.op("dve", lambda e: e.tensor_scalar(out=nk2[:, :], in0=nk[:, :], scalar1=1.0 / TWO_PI, scalar2=None, op0=ALU.mult), (B0,), (B0,))
        wc = sb("w_c", [128, 2048], F32)
        ws = sb("w_s", [128, 2048], F32)
        scr = (sb("w_c1", [128, 2048], F32)[:, :], sb("w_c2", [128, 2048], I32)[:, :], sb("w_c3", [128, 2048], F32)[:, :], sb("w_c4", [128, 2048], F32)[:, :])
        for i, Tb in enumerate((Mt, Dt)):
            cis(k, scr, lambda e, t, i=i: e.tensor_scalar(out=t, in0=lb[:, 1, :], scalar1=nk2[:, i:i + 1], scalar2=None, op0=ALU.mult), wc[:, :], ws[:, :], B0, B0)
            k.op("act", lambda e, i=i, Tb=Tb: e.activation(out=Tb[:, 1, :], in_=lb[:, 0, :], func=AF.Exp, scale=nk[:, i:i + 1]), (B0,), (B0,))
            k.op("dve", lambda e, Tb=Tb: e.tensor_tensor(out=Tb[:, 0, :], in0=Tb[:, 1, :], in1=wc[:, :], op=ALU.mult), (B0,), (B0,))
            k.op("dve", lambda e, Tb=Tb: e.tensor_tensor(out=Tb[:, 1, :], in0=Tb[:, 1, :], in1=ws[:, :], op=ALU.mult), (B0,), (B0,))
        k.end("S_tables")


def s5_main(k, c, io, GT, TT, H, Mt, Dt):
    nc = k.nc
    with ExitStack() as ps:
        k.begin()
        sb = lambda n, s, d: ps.enter_context(nc.sbuf_tensor(n, s, d))
        bTab = Buf()
        Wg = sb("S_Wg", [128, 4, 512], BF16)
        bWg = Buf()
        load_w_cast(k, ps, io["w_glu"], 4, 512, "S_wg", Wg, bWg, "dve")
        bgl = sb("S_bgl", [128, 4], F32)
        bbgl = Buf()
        k.dma("sp", bgl[:, :], io["b_glu"].rearrange("(m p) -> p m", p=128), bbgl, (), (bbgl,), allow_slow_non_contiguous=True)
        UA = sb("S_UA", [128, 8192], BF16)
        UB = sb("S_UB", [128, 8192], BF16)
        bUA, bUB = Buf(), Buf()
        U16 = sb("S_U16", [128, G, 2, 128], BF16)
        bU16 = Buf()
        X = sb("S_X", [128, G, 128], BF16)
        bX = [Buf() for _ in range(8)]
        Stm = sb("S_Stm", [128, G, 128], BF16)
        bStm = [Buf() for _ in range(8)]
        ST = sb("S_ST", [128, G, 128], BF16)
        bST = [Buf() for _ in range(4)]
        pT = [ps.enter_context(nc.psum_tensor("S_pT%d" % i, [128, 1024], BF16)) for i in range(2)]
        bpT = [Buf(), Buf()]
        NPD = 2
        pd = [ps.enter_context(nc.psum_tensor("S_pd%d" % i, [128, 512], F32)) for i in range(NPD)]
        bpd = [Buf() for _ in range(NPD)]
        py = [ps.enter_context(nc.psum_tensor("S_py%d" % i, [128, 512], F32)) for i in range(2)]
        bpy = [Buf() for _ in range(2)]
        pgls = [ps.enter_context(nc.psum_tensor("S_pgl%d" % i, [128, 512], F32)) for i in range(2)]
        bpgls = [Buf(), Buf()]
        tq = [sb("S_tq%d" % i, [128, 256], F32) for i in range(4)]
        btq = [Buf() for _ in range(4)]
        gx2 = [sb("S_gx2%d" % i, [128, 512], F32) for i in range(3)]
        gu = [sb("S_gu%d" % i, [128, 512], F32) for i in range(3)]
        bgx = [Buf() for _ in range(3)]
        bgu = [Buf() for _ in range(3)]
        sgls = [sb("S_sgl%d" % i, [128, 512], F32) for i in range(2)]
        bsgls = [Buf(), Buf()]
        s5st = [sb("S_s5%d" % i, [128, 4, 512], BF16) for i in range(2)]
        bs5 = [Buf(), Buf()]
        ipT = 0
        ipd = 0
        ipy = 0

        def cmod(pbank, Tb, dst, g0, bsrc, bdst):
            src = pbank[:, :].rearrange("p (g x) -> p g x", g=4)
            sre, sim = src[:, :, 0:64], src[:, :, 64:128]
            tre = Tb[:, 0, g0 * 64:(g0 + 4) * 64].rearrange("p (g q) -> p g q", g=4)
            tim = Tb[:, 1, g0 * 64:(g0 + 4) * 64].rearrange("p (g q) -> p g q", g=4)
            v = lambda t: t[:, :].rearrange("p (g q) -> p g q", g=4)
            k.op("dve", lambda e: e.tensor_tensor(out=v(tq[0]), in0=sre, in1=tre, op=ALU.mult), (bsrc, bTab), (btq[0],))
            k.op("dve", lambda e: e.tensor_tensor(out=v(tq[1]), in0=sim, in1=tim, op=ALU.mult), (bsrc, bTab), (btq[1],))
            k.op("dve", lambda e: e.tensor_tensor(out=v(tq[2]), in0=sre, in1=tim, op=ALU.mult), (bsrc, bTab), (btq[2],))
            k.op("dve", lambda e: e.tensor_tensor(out=v(tq[3]), in0=sim, in1=tre, op=ALU.mult), (bsrc, bTab), (btq[3],))
            k.op("pool", lambda e: e.tensor_tensor(out=dst[:, g0:g0 + 4, 0:64], in0=v(tq[0]), in1=v(tq[1]), op=ALU.subtract), (btq[0], btq[1]), (bdst,))
            k.op("pool", lambda e: e.tensor_tensor(out=dst[:, g0:g0 + 4, 64:128], in0=v(tq[2]), in1=v(tq[3]), op=ALU.add), (btq[2], btq[3]), (bdst,))

        for s in range(BPC):
            tb = s * SEQ
            Utm = UA[:, :].rearrange("p (j c) -> p j c", j=16)
            k.dma("sp", Utm, io["z_s"][tb:tb + SEQ, 0:512].rearrange("(k j) c -> k j c", j=16), bUA, (), (bUA,))
            for hf in range(2):
                k.op("dve", lambda e, hf=hf: e.tensor_copy(
                    out=UB[:, hf * 4096:(hf + 1) * 4096].rearrange("p (g j c) -> p g j c", g=16, j=16),
                    in_=UA[:, :].rearrange("p (j g c) -> p g j c", j=16, g=32)[:, hf * 16:(hf + 1) * 16, :, :]), (bUA,), (bUB,))
            Ug = UB[:, :].rearrange("p (g x) -> p g x", g=32)
            for g4 in range(8):
                p = ipT % 2
                ipT += 1
                tr(k, [(pT[p][:, (gl * 2 + ch) * 128:(gl * 2 + ch + 1) * 128], Ug[:, g4 * 4 + gl, ch * 128:(ch + 1) * 128], c.ident[:, :]) for gl in range(4) for ch in range(2)], (bUB,), (bpT[p],))
                k.op("act", lambda e, p=p, g4=g4: e.copy(out=U16[:, g4 * 4:(g4 + 1) * 4, :, :], in_=pT[p][:, :].rearrange("p (g a k) -> p g a k", g=4, a=2)), (bpT[p],), (bU16,))
            for g4 in range(8):
                p = ipd % NPD
                ipd += 1
                items = []
                for gl in range(4):
                    g = g4 * 4 + gl
                    for ch in range(2):
                        items.append((pd[p][:, gl * 128:(gl + 1) * 128], U16[:, g, ch, :], GT[:, g, ch, :], ch == 0, ch == 1))
                mm(k, items, (bU16, bTab), (bpd[p],))
                cmod(pd[p], Mt, X, g4 * 4, bpd[p], bX[g4])
            for g4 in range(8):
                p = ipd % NPD
                ipd += 1
                mm(k, [(pd[p][:, :], c.tri[:, :], X[:, g4 * 4:(g4 + 1) * 4, :].rearrange("p g x -> p (g x)"), True, True)], (bX[g4],), (bpd[p],))
                cmod(pd[p], Dt, Stm, g4 * 4, bpd[p], bStm[g4])
            for g8 in range(4):
                p = ipT % 2
                ipT += 1
                tr(k, [(pT[p][:, gl * 128:(gl + 1) * 128], Stm[:, g8 * 8 + gl, :], c.ident[:, :]) for gl in range(8)], (bStm[2 * g8], bStm[2 * g8 + 1]), (bpT[p],))
                k.op("act", lambda e, p=p, g8=g8: e.copy(out=ST[:, g8 * 8:(g8 + 1) * 8, :], in_=pT[p][:, :].rearrange("p (g k) -> p g k", g=8)), (bpT[p],), (bST[g8],))
            ygtm = UA[:, :].rearrange("p (i c) -> p i c", i=16)
            def y_front(gp):
                p = gp % 2
                items = []
                for g2 in range(2):
                    g = gp * 2 + g2
                    o = py[p][:, g2 * 256:(g2 + 1) * 256]
                    items += [(o, U16[:, g, 0, :], TT[:, g, 0, :], True, False), (o, U16[:, g, 1, :], TT[:, g, 1, :], False, False), (o, ST[:, g, :], H[:, g, :], False, True)]
                mm(k, items, (bU16, bST[gp // 4], bTab), (bpy[p],))
                k.op("act", lambda e: e.activation(out=gx2[p][:, :], in_=py[p][:, :], func=AF.Square), (bpy[p],), (bgx[p],))
                k.op("pool", lambda e: e.tensor_scalar(out=gx2[p][:, :], in0=gx2[p][:, :], scalar1=0.044715, scalar2=1.0, op0=ALU.mult, op1=ALU.add), (bgx[p],), (bgx[p],))

            def y_back(gp):
                p = gp % 2
                k.op("dve", lambda e: e.tensor_tensor(out=gu[p][:, :], in0=py[p][:, :], in1=gx2[p][:, :], op=ALU.mult), (bpy[p], bgx[p]), (bgu[p],))
                k.op("act", lambda e: e.activation(out=gu[p][:, :], in_=gu[p][:, :], func=AF.Sigmoid, scale=1.5957691216057308), (bgu[p],), (bgu[p],))
                k.op("dve", lambda e: e.tensor_tensor(
                    out=ygtm[:, :, gp * 32:(gp + 1) * 32].rearrange("p i (g c) -> p g i c", g=2),
                    in0=py[p][:, :].rearrange("p (g i c) -> p g i c", g=2, i=16),
                    in1=gu[p][:, :].rearrange("p (g i c) -> p g i c", g=2, i=16), op=ALU.mult), (bpy[p], bgu[p]), (bUA,))

            y_front(0)
            for gp in range(16):
                if gp + 1 < 16:
                    y_front(gp + 1)
                y_back(gp)
            ygT = UB[:, :].rearrange("p (ct t) -> p ct t", ct=4)
            for ib in range(8):
                p = ipT % 2
                ipT += 1
                tr(k, [(pT[p][:, (i2 * 4 + ct) * 128:(i2 * 4 + ct + 1) * 128], ygtm[:, ib * 2 + i2, ct * 128:(ct + 1) * 128], c.ident[:, :]) for i2 in range(2) for ct in range(4)], (bUA,), (bpT[p],))
                k.op("act", lambda e, p=p, ib=ib: e.copy(
                    out=UB[:, :].rearrange("p (ct k i) -> p i ct k", ct=4, i=16)[:, ib * 2:(ib + 1) * 2, :, :],
                    in_=pT[p][:, :].rearrange("p (i ct k) -> p i ct k", i=2, ct=4)), (bpT[p],), (bUB,))
            for ch in range(4):
                sidx = (s * 4 + ch) % 2
                for m in range(4):
                    gi2 = (ch * 4 + m) % 2
                    pgl, bpgl, sgl, bsgl = pgls[gi2], bpgls[gi2], sgls[gi2], bsgls[gi2]
                    mm(k, [(pgl[:, :], Wg[:, f, m * 128:(m + 1) * 128], ygT[:, f, ch * 512:(ch + 1) * 512], f == 0, f == 3) for f in range(4)], (bUB, bWg), (bpgl,))
                    k.op("act", lambda e, m=m, pgl=pgl, sgl=sgl: e.activation(out=sgl[:, :], in_=pgl[:, :], func=AF.Sigmoid, bias=bgl[:, m:m + 1]), (bpgl, bbgl), (bsgl,))
                    k.op("dve", lambda e, m=m, ch=ch, sidx=sidx, sgl=sgl: e.tensor_tensor(out=s5st[sidx][:, m, :], in0=sgl[:, :], in1=ygT[:, m, ch * 512:(ch + 1) * 512], op=ALU.mult), (bsgl, bUB), (bs5[sidx],))
                k.dma("pool", io["s5_s"][:, tb + ch * 512:tb + (ch + 1) * 512].rearrange("(m p) t -> p m t", p=128), s5st[sidx][:, :, :], bs5[sidx], (bs5[sidx],), ())
        k.end("S_main")
```

```python
import math
from contextlib import ExitStack
import numpy as np
import concourse.bass as bass
import concourse.mybir as mybir
from concourse.bass_utils import run_bass_kernel_spmd

F32 = mybir.dt.float32
BF16 = mybir.dt.bfloat16
I32 = mybir.dt.int32
AF = mybir.ActivationFunctionType
ALU = mybir.AluOpType
AX = mybir.AxisListType

NCORES = 8
D = 1024
SEQ = 2048
BPC = 4
T = BPC * SEQ
NTT = T // 128
G = 32
PST = 64
DFF = 4096
PLE = 256
EPS = 1e-6
NEG = -30000.0


class Buf:
    __slots__ = ("name", "w", "r", "ds")

    def __init__(self, name=""):
        self.name = name
        self.w = None
        self.r = []
        self.ds = None


class K:
    ENGS = ("pe", "dve", "act", "pool", "sp")

    def __init__(self, nc, es):
        self.nc = nc
        self.es = es
        self.esem = {e: es.enter_context(nc.semaphore("es_" + e)) for e in self.ENGS}
        self.cnt = {e: 0 for e in self.ENGS}
        self.dpool = [es.enter_context(nc.semaphore("ds%d" % i)) for i in range(48)]
        self.NHW = 36
        self.dcnt = [0] * len(self.dpool)
        self.ops = None
        with nc.Block() as block:
            def clr(eng):
                for sm in list(self.esem.values()) + self.dpool:
                    eng.sem_clear(sm)
            block.sync(clr)

    def begin(self):
        self.ops = {e: [] for e in self.ENGS}
        self.seen = {e: {} for e in self.ENGS}
        self.dnext = {False: 0, True: self.NHW}
        self.dused = set()
        self.phase_id = getattr(self, "phase_id", 0) + 1

    def _dsem(self, buf, sw):
        if buf.ds is None or buf.ds[0] != (self.phase_id, sw):
            lim = len(self.dpool) if sw else self.NHW
            assert self.dnext[sw] < lim, "out of dma sems"
            buf.ds = ((self.phase_id, sw), self.dnext[sw])
            self.dnext[sw] += 1
        return buf.ds[1]

    def _deps(self, eng, reads, writes):
        waits = {}
        def add(tok):
            if tok is None:
                return
            key, val = tok
            if key == ("e", "pe") and eng == "pe":
                return
            if self.seen[eng].get(key, 0) >= val:
                return
            if waits.get(key, 0) < val:
                waits[key] = val
        for b in reads:
            add(b.w)
        for b in writes:
            add(b.w)
            for t in b.r:
                add(t)
        for key, val in waits.items():
            self.seen[eng][key] = val
        return list(waits.items())

    def _mark(self, tok, reads, writes):
        for b in reads:
            b.r.append(tok)
        for b in writes:
            b.w = tok
            b.r = []

    def op(self, eng, fn, reads=(), writes=()):
        waits = self._deps(eng, reads, writes)
        self.cnt[eng] += 1
        tok = (("e", eng), self.cnt[eng])
        self._mark(tok, reads, writes)
        self.ops[eng].append(("c", fn, waits))

    def dma(self, eng, out, in_, sb, reads=(), writes=(), **kw):
        waits = self._deps(eng, reads, writes)
        si = self._dsem(sb, eng == "pool")
        self.dcnt[si] += 16
        self.dused.add(si)
        tok = (("d", si), self.dcnt[si])
        self._mark(tok, reads, writes)
        self.ops[eng].append(("d", (out, in_, si, kw), waits))

    def _sem(self, key):
        return self.esem[key[1]] if key[0] == "e" else self.dpool[key[1]]

    def end(self, name):
        nc = self.nc
        fin = [(("e", e), self.cnt[e]) for e in self.ENGS if e != "sp" and self.cnt[e] > 0]
        fin += [(("d", si), self.dcnt[si]) for si in sorted(self.dused)]
        ops = self.ops
        handles = {"pe": "tensor", "dve": "vector", "act": "scalar", "pool": "gpsimd", "sp": "sync"}
        with nc.Block() as block:
            for e in self.ENGS:
                def body(eng, e=e):
                    for kind, payload, waits in ops[e]:
                        for key, val in waits:
                            eng.wait_ge(self._sem(key), val)
                        if kind == "c":
                            ins = payload(eng)
                            ins.then_inc(self.esem[e], 1)
                        else:
                            out, in_, si, kw = payload
                            eng.dma_start(out=out, in_=in_, **kw).then_inc(self.dpool[si], 16)
                    if e == "sp":
                        for key, val in fin:
                            eng.wait_ge(self._sem(key), val)
                getattr(block, handles[e])(body)
        self.ops = None


def mm(k, items, reads, writes):
    def fn(eng):
        ins = None
        for (out, lhsT, rhs, st, sp) in items:
            ins = eng.matmul(out, lhsT, rhs, start=st, stop=sp)
        return ins
    k.op("pe", fn, reads, writes)


def tr(k, items, reads, writes):
    def fn(eng):
        ins = None
        for (out, in_, ident) in items:
            ins = eng.transpose(out, in_, ident)
        return ins
    k.op("pe", fn, reads, writes)


class Consts:
    pass


def make_consts(k, es):
    nc = k.nc
    c = Consts()
    c.ident = es.enter_context(nc.sbuf_tensor("ident", [128, 128], BF16))
    c.identf = es.enter_context(nc.sbuf_tensor("identf", [128, 128], F32))
    c.tri = es.enter_context(nc.sbuf_tensor("tri", [128, 128], BF16))
    c.nhalf = es.enter_context(nc.sbuf_tensor("nhalf", [128, 1], F32))
    c.ones = es.enter_context(nc.sbuf_tensor("onesb", [128, 128], BF16))
    io = es.enter_context(nc.sbuf_tensor("iota_i", [128, 128], I32))
    b_io, b_id, b_idf, b_tri, b_nh, b_on = (Buf() for _ in range(6))
    k.begin()
    k.op("pool", lambda e: e.iota(io[:, :], [[1, 128]], base=0, channel_multiplier=-1), (), (b_io,))
    k.op("dve", lambda e: e.tensor_scalar(out=c.ident[:, :], in0=io[:, :], scalar1=0.0, scalar2=None, op0=ALU.is_equal), (b_io,), (b_id,))
    k.op("dve", lambda e: e.tensor_scalar(out=c.identf[:, :], in0=io[:, :], scalar1=0.0, scalar2=None, op0=ALU.is_equal), (b_io,), (b_idf,))
    k.op("dve", lambda e: e.tensor_scalar(out=c.tri[:, :], in0=io[:, :], scalar1=0.0, scalar2=None, op0=ALU.is_gt), (b_io,), (b_tri,))
    k.op("dve", lambda e: e.memset(c.nhalf[:, :], -0.5), (), (b_nh,))
    k.op("dve", lambda e: e.memset(c.ones[:, :], 1.0), (), (b_on,))
    k.end("consts")
    return c


def rms_stats(k, c, x_ap, junk_ap, ss, rstd, bx, bj, bss, brs, n=1024):
    k.op("act", lambda e: e.activation(out=junk_ap, in_=x_ap, func=AF.Square, accum_out=ss), (bx,), (bj, bss))
    k.op("pool", lambda e: e.tensor_scalar(out=rstd, in0=ss, scalar1=1.0 / n, scalar2=EPS, op0=ALU.mult, op1=ALU.add), (bss,), (brs,))
    k.op("pool", lambda e: e.tensor_tensor(out=rstd, in0=rstd, in1=c.nhalf[:, :], op=ALU.pow), (brs,), (brs,))


def load_w_scaled(k, ps, w_d, kt, ncols, g_d, name, Wt, bW, half=2048, order=None):
    nc = k.nc
    gcol = ps.enter_context(nc.sbuf_tensor(name + "_g", [128, kt], F32))
    bg = Buf()
    k.dma("sp", gcol[:, :], g_d.rearrange("(f p) -> p f", p=128), bg, (), (bg,), allow_slow_non_contiguous=True)
    half = min(half, ncols)
    stg = [ps.enter_context(nc.sbuf_tensor(name + "_s%d" % i, [128, half], F32)) for i in range(2)]
    bs = [Buf(), Buf()]
    if isinstance(bW, list):
        assert half == 512
        pieces = [(f, ci * 512) for ci in (order or range(ncols // 512)) for f in range(kt)]
    else:
        pieces = [(f, c0) for f in range(kt) for c0 in range(0, ncols, half)]
    for i, (f, c0) in enumerate(pieces):
        s, b = stg[i % 2], bs[i % 2]
        bw = bW[c0 // 512] if isinstance(bW, list) else bW
        k.dma("sp", s[:, :], w_d[f * 128:(f + 1) * 128, c0:c0 + half], b, (), (b,))
        k.op("dve", lambda e, s=s, f=f, c0=c0: e.tensor_scalar(out=Wt[:, f, c0:c0 + half], in0=s[:, :], scalar1=gcol[:, f:f + 1], scalar2=None, op0=ALU.mult), (b, bg), (bw,))
    return stg, bs


def load_w_cast(k, ps, w_d, kt, ncols, name, Wt, bW, eng="act"):
    nc = k.nc
    half = 2048 if ncols > 2048 else ncols
    stg = [ps.enter_context(nc.sbuf_tensor(name + "_s%d" % i, [128, half], F32)) for i in range(2)]
    bs = [Buf(), Buf()]
    i = 0
    for f in range(kt):
        for c0 in range(0, ncols, half):
            s, b = stg[i % 2], bs[i % 2]
            k.dma("sp", s[:, :], w_d[f * 128:(f + 1) * 128, c0:c0 + half], b, (), (b,))
            if eng == "act":
                k.op("act", lambda e, s=s, f=f, c0=c0: e.copy(out=Wt[:, f, c0:c0 + half], in_=s[:, :]), (b,), (bW,))
            else:
                k.op("dve", lambda e, s=s, f=f, c0=c0: e.tensor_copy(out=Wt[:, f, c0:c0 + half], in_=s[:, :]), (b,), (bW,))
            i += 1


def phase_A(k, c, io):
    nc = k.nc
    with ExitStack() as ps:
        k.begin()
        sb = lambda n, s, d: ps.enter_context(nc.sbuf_tensor(n, s, d))
        W = sb("A_W", [128, 8, 4096], BF16)
        bWs = [Buf() for _ in range(8)]
        load_w_scaled(k, ps, io["w_in"], 8, 4096, io["g_pre_mix"], "A_w", W, bWs, half=512, order=[0, 3, 1, 2, 4, 5, 6, 7])
        xt = [sb("A_x%d" % i, [128, 1024], F32) for i in range(4)]
        bx = [Buf() for _ in range(4)]
        junk = sb("A_junk", [128, 1024], BF16)
        bj = Buf()
        ss = [sb("A_ss%d" % i, [128, 1], F32) for i in range(4)]
        rs = [sb("A_rs%d" % i, [128, 1], F32) for i in range(4)]
        bss = [Buf() for _ in range(4)]
        brs = [Buf() for _ in range(4)]
        hb = [sb("A_hb%d" % i, [128, 1024], BF16) for i in range(2)]
        bhb = [Buf(), Buf()]
        pT = [ps.enter_context(nc.psum_tensor("A_pT%d" % i, [128, 1024], BF16)) for i in range(2)]
        bpT = [Buf(), Buf()]
        hT = [sb("A_hT%d" % i, [128, 8, 512], BF16) for i in range(2)]
        bhT = [Buf(), Buf()]
        pm = [ps.enter_context(nc.psum_tensor("A_pm%d" % i, [128, 512], F32)) for i in range(6)]
        bpm = [Buf() for _ in range(6)]
        zst = [sb("A_z%d" % i, [128, 1024], BF16) for i in range(2)]
        bz = [Buf(), Buf()]
        fst = [sb("A_f%d" % i, [128, 8, 512], BF16) for i in range(3)]
        bf = [Buf() for _ in range(3)]
        ipm = 0
        ifs = 0
        def head_tile(ch, j):
            hTc, bhTc = hT[ch % 2], bhT[ch % 2]
            t = ch * 4 + j
            xi = t % 4
            hbi = t % 2
            k.dma("sp", xt[xi][:, :], io["x"][t * 128:(t + 1) * 128, :], bx[xi], (), (bx[xi],))
            rms_stats(k, c, xt[xi][:, :], junk[:, :], ss[xi][:, :], rs[xi][:, :], bx[xi], bj, bss[xi], brs[xi])
            k.op("act", lambda e: e.activation(out=hb[hbi][:, :], in_=xt[xi][:, :], func=AF.Copy, scale=rs[xi][:, :]), (bx[xi], brs[xi]), (bhb[hbi],))
            tr(k, [(pT[hbi][:, f * 128:(f + 1) * 128], hb[hbi][:, f * 128:(f + 1) * 128], c.ident[:, :]) for f in range(8)], (bhb[hbi],), (bpT[hbi],))
            k.op("dve", lambda e: e.tensor_copy(out=hTc[:, :, j * 128:(j + 1) * 128], in_=pT[hbi][:, :].rearrange("p (f t) -> p f t", f=8)), (bpT[hbi],), (bhTc,))

        for j in range(4):
            head_tile(0, j)
        for ch in range(T // 512):
            hTc, bhTc = hT[ch % 2], bhT[ch % 2]
            for j in range(4):
                t = ch * 4 + j
                zi = t % 2
                for n, c0 in enumerate((0, 1536)):
                    p = ipm % 6
                    ipm += 1
                    mm(k, [(pm[p][:, :], hTc[:, f, j * 128:(j + 1) * 128], W[:, f, c0:c0 + 512], f == 0, f == 7) for f in range(8)], (bhTc, bWs[c0 // 512]), (bpm[p],))
                    k.op("dve", lambda e, p=p, zi=zi, n=n: e.tensor_copy(out=zst[zi][:, n * 512:(n + 1) * 512], in_=pm[p][:, :]), (bpm[p],), (bz[zi],))
                k.dma("pool", io["z_s"][t * 128:(t + 1) * 128, :], zst[zi][:, :], bz[zi], (bz[zi],), ())
            for m in range(24):
                c0 = 512 + m * 128 if m < 8 else 2048 + (m - 8) * 128
                p = ipm % 6
                ipm += 1
                mm(k, [(pm[p][:, :], W[:, f, c0:c0 + 128], hTc[:, f, :], f == 0, f == 7) for f in range(8)], (bhTc, bWs[c0 // 512]), (bpm[p],))
                fi = ifs % 3
                if m < 8:
                    k.op("dve", lambda e, p=p, fi=fi, m=m: e.tensor_copy(out=fst[fi][:, m % 8, :], in_=pm[p][:, :]), (bpm[p],), (bf[fi],))
                else:
                    k.op("act", lambda e, p=p, fi=fi, m=m: e.activation(out=fst[fi][:, m % 8, :], in_=pm[p][:, :], func=AF.Sigmoid), (bpm[p],), (bf[fi],))
                if m % 8 == 7:
                    r0 = (m // 8) * 1024
                    k.dma("pool", io["fm_s"][r0:r0 + 1024, ch * 512:(ch + 1) * 512].rearrange("(m p) t -> p m t", p=128), fst[fi][:, :, :], bf[fi], (bf[fi],), ())
                    ifs += 1
                if m in (3, 8, 13, 18) and ch + 1 < T // 512:
                    head_tile(ch + 1, (3, 8, 13, 18).index(m))
        k.end("A")


IN_SPECS = [
    ("x", [T, D]), ("p", [T, PLE]), ("g_pre_mix", [D]), ("w_in", [D, 4096]),
    ("ssm_lam_re", [G, PST]), ("ssm_lam_im", [G, PST]), ("ssm_log_dt", [G]),
    ("ssm_b_re", [G, PST, 16]), ("ssm_b_im", [G, PST, 16]),
    ("ssm_c_re", [G, 16, PST]), ("ssm_c_im", [G, 16, PST]), ("ssm_d", [512]),
    ("w_glu", [512, 512]), ("b_glu", [512]), ("w_branch_a", [512, D]), ("w_branch_b", [512, D]),
    ("w_out", [D, D]), ("g_post_mix", [D]), ("g_pre_mlp", [D]), ("w_mlp1", [D, DFF]),
    ("w_mlp2", [DFF, D]), ("g_post_mlp", [D]), ("w_ple", [PLE, D]), ("w_ple_gate", [D, D]),
    ("g_ple", [D]),
]
SCRATCH = [
    ("z_s", [T, 1024], BF16),
    ("fm_s", [3072, T], BF16),
    ("s5_s", [512, T], BF16),
    ("at_s", [T, 512], BF16),
    ("x1_s", [T, D], F32),
    ("x2_s", [T, D], F32),
]


def build(phases="ASCDEF", debug=(), inject=()):
    nc = bass.Bass("TRN2", target_bir_lowering=False)
    io = {}
    for name, shape in IN_SPECS:
        io[name] = nc.dram_tensor(name, shape, F32, kind="ExternalInput").ap()
    for name, shape, dt in SCRATCH:
        kind = "ExternalOutput" if name in debug else ("ExternalInput" if name in inject else "Internal")
        io[name] = nc.dram_tensor(name, shape, dt, kind=kind).ap()
    io["out"] = nc.dram_tensor("out", [T, D], F32, kind="ExternalOutput").ap()
    with ExitStack() as es:
        k = K(nc, es)
        c = make_consts(k, es)
        if "A" in phases:
            phase_A(k, c, io)
        if "S" in phases:
            phase_S(k, c, io)
        if "C" in phases:
            phase_C(k, c, io)
        if "D" in phases:
            phase_D(k, c, io)
        if "E" in phases:
            phase_E(k, c, io)
        if "F" in phases:
            phase_F(k, c, io)
    return nc


def make_in_maps(inputs, ncores=NCORES):
    maps = []
    for ci in range(ncores):
        m = {}
        for name, shape in IN_SPECS:
            a = inputs[name]
            if name == "x":
                a = a[ci * BPC:(ci + 1) * BPC].reshape(T, D)
            elif name == "p":
                a = a[0, ci * BPC:(ci + 1) * BPC].reshape(T, PLE)
            else:
                a = a[0]
            m[name] = np.ascontiguousarray(a, dtype=np.float32).reshape(shape)
        maps.append(m)
    return maps


def kernel(**inputs):
    inputs = {k_: np.asarray(v) for k_, v in inputs.items()}
    nc = build()
    in_maps = make_in_maps(inputs)
    res = run_bass_kernel_spmd(nc, in_maps, core_ids=list(range(NCORES)))
    outs = [np.asarray(r["out"]).reshape(BPC, SEQ, D) for r in res.results]
    return np.concatenate(outs, axis=0).astype(np.float32)


def load_gb(k, ps, g_d, name):
    nc = k.nc
    gb = ps.enter_context(nc.sbuf_tensor(name, [128, D], F32))
    b = Buf()
    k.dma("sp", gb[:, :], g_d.partition_broadcast(128), b, (), (b,))
    return gb, b


class TailBufs:
    def __init__(self, k, sb, name, n=2, inplace=False):
        self.n = n
        self.inplace = inplace
        self.junk = [sb(name + "_junk%d" % i, [128, 1024], BF16) for i in range(n)]
        self.ss = [sb(name + "_tss%d" % i, [128, 1], F32) for i in range(n)]
        self.rs = [sb(name + "_trs%d" % i, [128, 1], F32) for i in range(n)]
        self.tmp = [sb(name + "_ttmp%d" % i, [128, 1024], F32) for i in range(n)]
        self.ost = self.tmp if inplace else [sb(name + "_tost%d" % i, [128, 1024], F32) for i in range(n)]
        self.b = [[Buf() for _ in range(5)] for _ in range(n)]


def norm_res_tail(k, c, tb, i, src_ap, bsrc, gb, bgb, xres, bxres, out_rows):
    i = i % tb.n
    bj, bss, brs, btmp, bost = tb.b[i]
    junk, ss, rs, tmp, ost = tb.junk[i], tb.ss[i], tb.rs[i], tb.tmp[i], tb.ost[i]
    rms_stats(k, c, src_ap, junk[:, :], ss[:, :], rs[:, :], bsrc, bj, bss, brs)
    k.op("dve", lambda e: e.scalar_tensor_tensor(out=tmp[:, :], in0=src_ap, scalar=rs[:, :], in1=gb[:, :], op0=ALU.mult, op1=ALU.mult), (bsrc, brs, bgb), (btmp,))
    if tb.inplace:
        bost = btmp
    k.op("pool", lambda e: e.tensor_tensor(out=ost[:, :], in0=tmp[:, :], in1=xres[:, :], op=ALU.add), (btmp, bxres), (bost,))
    k.dma("pool", out_rows, ost[:, :], bost, (bost,), ())


def phase_D(k, c, io):
    nc = k.nc
    with ExitStack() as ps:
        k.begin()
        sb = lambda n, s, d: ps.enter_context(nc.sbuf_tensor(n, s, d))
        Wa = sb("D_Wa", [128, 4, 1024], BF16)
        Wb = sb("D_Wb", [128, 4, 1024], BF16)
        Wo = sb("D_Wo", [128, 8, 1024], BF16)
        bWa, bWb, bWo = Buf(), Buf(), Buf()
        load_w_cast(k, ps, io["w_branch_a"], 4, 1024, "D_wa", Wa, bWa, "dve")
        load_w_cast(k, ps, io["w_branch_b"], 4, 1024, "D_wb", Wb, bWb, "act")
        load_w_cast(k, ps, io["w_out"], 8, 1024, "D_wo", Wo, bWo, "dve")
        g2, bg2 = load_gb(k, ps, io["g_post_mix"], "D_g2")
        gts = [sb("D_gt%d" % i, [128, 16, 512], BF16) for i in range(2)]
        bgt = [Buf(), Buf()]
        s5t = [sb("D_s5%d" % i, [128, 4, 512], BF16) for i in range(2)]
        bs5 = [Buf(), Buf()]
        att = [sb("D_at%d" % i, [128, 512], BF16) for i in range(4)]
        bat = [Buf() for _ in range(4)]
        atTs = [sb("D_atT%d" % i, [128, 4, 512], BF16) for i in range(2)]
        batTs = [Buf(), Buf()]
        pT = ps.enter_context(nc.psum_tensor("D_pT", [128, 1024], BF16))
        bpT = Buf()
        pm = [ps.enter_context(nc.psum_tensor("D_pm%d" % i, [128, 512], F32)) for i in range(3)]
        bpm = [Buf() for _ in range(3)]
        pmx = [ps.enter_context(nc.psum_tensor("D_px%d" % i, [128, 1024], F32)) for i in range(2)]
        bpx = [Buf(), Buf()]
        t1 = [sb("D_t1%d" % i, [128, 512], F32) for i in range(2)]
        bt1 = [Buf(), Buf()]
        t2 = [sb("D_t2%d" % i, [128, 512], F32) for i in range(2)]
        bt2 = [Buf(), Buf()]
        mTs = [sb("D_mT%d" % i, [128, 8, 512], BF16) for i in range(2)]
        bmTs = [Buf(), Buf()]
        xt = [sb("D_x%d" % i, [128, 1024], F32) for i in range(2)]
        bx = [Buf(), Buf()]
        tbf = TailBufs(k, sb, "D")
        ipm = [0]
        xt3 = xt + [sb("D_x2", [128, 1024], F32)]
        bx3 = bx + [Buf()]

        def head(ch):
            gi = ch % 2
            atT, batT = atTs[gi], batTs[gi]
            cs = slice(ch * 512, (ch + 1) * 512)
            k.dma("sp", gts[gi][:, :, :], io["fm_s"][1024:3072, cs].rearrange("(m p) t -> p m t", p=128), bgt[gi], (), (bgt[gi],))
            k.dma("sp", s5t[gi][:, :, :], io["s5_s"][:, cs].rearrange("(m p) t -> p m t", p=128), bs5[gi], (), (bs5[gi],))
            for j in range(4):
                t = ch * 4 + j
                k.dma("sp", att[j][:, :], io["at_s"][t * 128:(t + 1) * 128, :], bat[j], (), (bat[j],))
            for j in range(4):
                tr(k, [(pT[:, f * 128:(f + 1) * 128], att[j][:, f * 128:(f + 1) * 128], c.ident[:, :]) for f in range(4)], (bat[j],), (bpT,))
                k.op("dve", lambda e, j=j: e.tensor_copy(out=atT[:, :, j * 128:(j + 1) * 128], in_=pT[:, 0:512].rearrange("p (f t) -> p f t", f=4)), (bpT,), (batT,))

        def gate(ch):
            gi = ch % 2
            atT, batT = atTs[gi], batTs[gi]
            mT, bmT = mTs[gi], bmTs[gi]
            for m in range(8):
                p = ipm[0] % 3
                ipm[0] += 1
                mm(k, [(pm[p][:, :], Wa[:, f, m * 128:(m + 1) * 128], s5t[gi][:, f, :], f == 0, f == 3) for f in range(4)], (bs5[gi], bWa), (bpm[p],))
                i1 = m % 2
                k.op("dve", lambda e, p=p, i1=i1, m=m: e.tensor_tensor(out=t1[i1][:, :], in0=pm[p][:, :], in1=gts[gi][:, m, :], op=ALU.mult), (bpm[p], bgt[gi]), (bt1[i1],))
                p2 = ipm[0] % 3
                ipm[0] += 1
                mm(k, [(pm[p2][:, :], Wb[:, f, m * 128:(m + 1) * 128], atT[:, f, :], f == 0, f == 3) for f in range(4)], (batT, bWb), (bpm[p2],))
                k.op("dve", lambda e, p2=p2, i1=i1, m=m: e.tensor_tensor(out=t2[i1][:, :], in0=pm[p2][:, :], in1=gts[gi][:, 8 + m, :], op=ALU.mult), (bpm[p2], bgt[gi]), (bt2[i1],))
                k.op("pool", lambda e, i1=i1, m=m: e.tensor_tensor(out=mT[:, m, :], in0=t1[i1][:, :], in1=t2[i1][:, :], op=ALU.add), (bt1[i1], bt2[i1]), (bmT,))

        def mm2(t):
            ch, j = t // 4, t % 4
            mT, bmT = mTs[ch % 2], bmTs[ch % 2]
            xi, x3 = t % 2, t % 3
            k.dma("sp", xt3[x3][:, :], io["x"][t * 128:(t + 1) * 128, :], bx3[x3], (), (bx3[x3],))
            for n in range(2):
                mm(k, [(pmx[xi][:, n * 512:(n + 1) * 512], mT[:, f, j * 128:(j + 1) * 128], Wo[:, f, n * 512:(n + 1) * 512], f == 0, f == 7) for f in range(8)], (bmT, bWo), (bpx[xi],))

        def tail(t):
            xi, x3 = t % 2, t % 3
            norm_res_tail(k, c, tbf, t, pmx[xi][:, :], bpx[xi], g2, bg2, xt3[x3], bx3[x3], io["x1_s"][t * 128:(t + 1) * 128, :])

        NCH = T // 512
        head(0)
        for ch in range(NCH):
            gate(ch)
            for j in range(4):
                t = ch * 4 + j
                mm2(t)
                if t >= 1:
                    tail(t - 1)
                if j == 1 and ch + 1 < NCH:
                    head(ch + 1)
        tail(NTT - 1)
        k.end("D")


def phase_E(k, c, io):
    nc = k.nc
    CH = 512
    with ExitStack() as ps:
        k.begin()
        sb = lambda n, s, d: ps.enter_context(nc.sbuf_tensor(n, s, d))
        W1 = sb("E_W1", [128, 8, 4096], BF16)
        W2 = sb("E_W2", [128, 32, 1024], BF16)
        bW1s, bW2 = [Buf() for _ in range(8)], Buf()
        stg, bstg = load_w_scaled(k, ps, io["w_mlp1"], 8, 4096, io["g_pre_mlp"], "E_w1", W1, bW1s, half=512)
        for f2 in range(64):
            f, hf = f2 // 2, f2 % 2
            s, b = stg[f2 % 2], bstg[f2 % 2]
            k.dma("sp", s[:, :], io["w_mlp2"][f * 128:(f + 1) * 128, hf * 512:(hf + 1) * 512], b, (), (b,))
            if f2 % 2:
                k.op("act", lambda e, s=s, f=f, hf=hf: e.copy(out=W2[:, f, hf * 512:(hf + 1) * 512], in_=s[:, :]), (b,), (bW2,))
            else:
                k.op("dve", lambda e, s=s, f=f, hf=hf: e.tensor_copy(out=W2[:, f, hf * 512:(hf + 1) * 512], in_=s[:, :]), (b,), (bW2,))
        g4, bg4 = load_gb(k, ps, io["g_post_mlp"], "E_g4")
        xt = [sb("E_x%d" % i, [128, 1024], F32) for i in range(4)]
        bx = [Buf() for _ in range(4)]
        junk = sb("E_junk", [128, 1024], BF16)
        bj = Buf()
        ss = [sb("E_ss%d" % i, [128, 1], F32) for i in range(2)]
        rs = [sb("E_rs%d" % i, [128, 1], F32) for i in range(2)]
        bss = [Buf(), Buf()]
        brs = [Buf(), Buf()]
        hb = [sb("E_hb0", [128, 1024], BF16)] * 2
        bhb = [Buf()] * 2
        pT = ps.enter_context(nc.psum_tensor("E_pT", [128, 1024], BF16))
        bpT = Buf()
        hT = sb("E_hT", [128, 8, CH], BF16)
        bhT = Buf()
        pm = [ps.enter_context(nc.psum_tensor("E_pm%d" % i, [128, 512], F32)) for i in range(3)]
        bpm = [Buf() for _ in range(3)]
        pmx = [ps.enter_context(nc.psum_tensor("E_px%d" % i, [128, 1024], F32)) for i in range(2)]
        bpx = [Buf(), Buf()]
        rl = [sb("E_rl0", [128, CH], F32)] * 2
        brl = [Buf()] * 2
        f1T = sb("E_f1T", [128, 32, CH], BF16)
        bf1 = Buf()
        tbf = TailBufs(k, sb, "E", n=1, inplace=True)
        ipm = 0
        nj = CH // 128
        for ch in range(T // CH):
            for j in range(nj):
                t = ch * nj + j
                xi = t % 4
                hi = t % 2
                k.dma("sp", xt[xi][:, :], io["x1_s"][t * 128:(t + 1) * 128, :], bx[xi], (), (bx[xi],))
                rms_stats(k, c, xt[xi][:, :], junk[:, :], ss[hi][:, :], rs[hi][:, :], bx[xi], bj, bss[hi], brs[hi])
                k.op("act", lambda e, xi=xi, hi=hi: e.activation(out=hb[hi][:, :], in_=xt[xi][:, :], func=AF.Copy, scale=rs[hi][:, :]), (bx[xi], brs[hi]), (bhb[hi],))
                tr(k, [(pT[:, f * 128:(f + 1) * 128], hb[hi][:, f * 128:(f + 1) * 128], c.ident[:, :]) for f in range(8)], (bhb[hi],), (bpT,))
                k.op("dve", lambda e, j=j: e.tensor_copy(out=hT[:, :, j * 128:(j + 1) * 128], in_=pT[:, :].rearrange("p (f t) -> p f t", f=8)), (bpT,), (bhT,))
            for m in range(32):
                p = ipm % 3
                ipm += 1
                mm(k, [(pm[p][:, 0:CH], W1[:, f, m * 128:(m + 1) * 128], hT[:, f, :], f == 0, f == 7) for f in range(8)], (bhT, bW1s[m // 4]), (bpm[p],))
                ri = m % 2
                k.op("act", lambda e, p=p, ri=ri: e.activation(out=rl[ri][:, :], in_=pm[p][:, 0:CH], func=AF.Relu), (bpm[p],), (brl[ri],))
                k.op("pool", lambda e, ri=ri, m=m: e.tensor_tensor(out=f1T[:, m, :], in0=rl[ri][:, :], in1=rl[ri][:, :], op=ALU.mult), (brl[ri],), (bf1,))
            for j in range(nj):
                t = ch * nj + j
                xi = t % 4
                oi = t % 2
                for n in range(2):
                    mm(k, [(pmx[oi][:, n * 512:(n + 1) * 512], f1T[:, f, j * 128:(j + 1) * 128], W2[:, f, n * 512:(n + 1) * 512], f == 0, f == 31) for f in range(32)], (bf1, bW2), (bpx[oi],))
                norm_res_tail(k, c, tbf, t, pmx[oi][:, :], bpx[oi], g4, bg4, xt[xi], bx[xi], io["x2_s"][t * 128:(t + 1) * 128, :])
        k.end("E")


def phase_F(k, c, io):
    nc = k.nc
    with ExitStack() as ps:
        k.begin()
        sb = lambda n, s, d: ps.enter_context(nc.sbuf_tensor(n, s, d))
        Wp = sb("F_Wp", [128, 2, 1024], BF16)
        Wg = sb("F_Wg", [128, 8, 1024], BF16)
        bWp, bWg = Buf(), Buf()
        load_w_cast(k, ps, io["w_ple"], 2, 1024, "F_wp", Wp, bWp, "dve")
        load_w_cast(k, ps, io["w_ple_gate"], 8, 1024, "F_wg", Wg, bWg, "act")
        g5, bg5 = load_gb(k, ps, io["g_ple"], "F_g5")
        NB = 3
        xt = [sb("F_x%d" % i, [128, 1024], F32) for i in range(4)]
        bx = [Buf() for _ in range(4)]
        pt = [sb("F_p%d" % i, [128, 256], F32) for i in range(NB)]
        bp = [Buf() for _ in range(NB)]
        xb = [sb("F_xb%d" % i, [128, 1024], BF16) for i in range(NB)]
        bxb = [Buf() for _ in range(NB)]
        pb = [sb("F_pb%d" % i, [128, 256], BF16) for i in range(NB)]
        bpb = [Buf() for _ in range(NB)]
        pTx = ps.enter_context(nc.psum_tensor("F_pTx", [128, 1024], BF16))
        pTp = ps.enter_context(nc.psum_tensor("F_pTp", [128, 1024], BF16))
        bpTx, bpTp = Buf(), Buf()
        xT = [sb("F_xT%d" % i, [128, 8, 128], BF16) for i in range(NB)]
        bxT = [Buf() for _ in range(NB)]
        pT = [sb("F_pT%d" % i, [128, 2, 128], BF16) for i in range(NB)]
        bpT = [Buf() for _ in range(NB)]
        ppw = ps.enter_context(nc.psum_tensor("F_ppw", [128, 1024], F32))
        bppw = Buf()
        psg = [ps.enter_context(nc.psum_tensor("F_psg%d" % i, [128, 1024], F32)) for i in range(2)]
        bpsg = [Buf(), Buf()]
        sgss = [sb("F_sgs%d" % i, [128, 1024], F32) for i in range(2)]
        bsgss = [Buf(), Buf()]
        ees = [sb("F_e%d" % i, [128, 1024], F32) for i in range(2)]
        bees = [Buf(), Buf()]
        tbf = TailBufs(k, sb, "F")

        def head(t):
            xi, i3 = t % 4, t % NB
            k.dma("sp", xt[xi][:, :], io["x2_s"][t * 128:(t + 1) * 128, :], bx[xi], (), (bx[xi],))
            k.dma("sp", pt[i3][:, :], io["p"][t * 128:(t + 1) * 128, :], bp[i3], (), (bp[i3],))
            k.op("act", lambda e: e.copy(out=xb[i3][:, :], in_=xt[xi][:, :]), (bx[xi],), (bxb[i3],))
            k.op("dve", lambda e: e.tensor_copy(out=pb[i3][:, :], in_=pt[i3][:, :]), (bp[i3],), (bpb[i3],))
            tr(k, [(pTx[:, f * 128:(f + 1) * 128], xb[i3][:, f * 128:(f + 1) * 128], c.ident[:, :]) for f in range(8)], (bxb[i3],), (bpTx,))
            k.op("dve", lambda e: e.tensor_copy(out=xT[i3][:, :, :], in_=pTx[:, :].rearrange("p (f t) -> p f t", f=8)), (bpTx,), (bxT[i3],))
            tr(k, [(pTp[:, f * 128:(f + 1) * 128], pb[i3][:, f * 128:(f + 1) * 128], c.ident[:, :]) for f in range(2)], (bpb[i3],), (bpTp,))
            k.op("dve", lambda e: e.tensor_copy(out=pT[i3][:, :, :], in_=pTp[:, 0:256].rearrange("p (f t) -> p f t", f=2)), (bpTp,), (bpT[i3],))

        def mid_sg(t):
            i2, i3 = t % 2, t % NB
            for n in range(2):
                mm(k, [(psg[i2][:, n * 512:(n + 1) * 512], xT[i3][:, f, :], Wg[:, f, n * 512:(n + 1) * 512], f == 0, f == 7) for f in range(8)], (bxT[i3], bWg), (bpsg[i2],))

        def mid_pw(t):
            i3 = t % NB
            for n in range(2):
                mm(k, [(ppw[:, n * 512:(n + 1) * 512], pT[i3][:, f, :], Wp[:, f, n * 512:(n + 1) * 512], f == 0, f == 1) for f in range(2)], (bpT[i3], bWp), (bppw,))

        def tail(t):
            xi, i2 = t % 4, t % 2
            sgs, bsgs, ee, bee = sgss[i2], bsgss[i2], ees[i2], bees[i2]
            k.op("act", lambda e: e.activation(out=sgs[:, :], in_=psg[i2][:, :], func=AF.Sigmoid), (bpsg[i2],), (bsgs,))
            k.op("dve", lambda e: e.tensor_tensor(out=ee[:, :], in0=ppw[:, :], in1=sgs[:, :], op=ALU.mult), (bppw, bsgs), (bee,))
            norm_res_tail(k, c, tbf, t, ee[:, :], bee, g5, bg5, xt[xi], bx[xi], io["out"][t * 128:(t + 1) * 128, :])

        head(0)
        head(1)
        for t in range(NTT):
            mid_sg(t)
            if t >= 1:
                tail(t - 1)
            mid_pw(t)
            if t + 2 < NTT:
                head(t + 2)
        tail(NTT - 1)
        k.end("F")


def phase_C(k, c, io):
    nc = k.nc
    with ExitStack() as ps:
        k.begin()
        sb = lambda n, s, d: ps.enter_context(nc.sbuf_tensor(n, s, d))
        ioi = sb("C_ioi", [128, 64], I32)
        io2 = sb("C_io2", [128, 256], I32)
        io3 = sb("C_io3", [128, 2048], I32)
        negp = sb("C_negp", [128, 8, 64], F32)
        cm = sb("C_cm", [128, 2, 256], BF16)
        oh = sb("C_oh", [128, 64 * 128], BF16)
        bio, bnegp, bcm, boh, bio3 = Buf(), Buf(), Buf(), Buf(), Buf()
        k.op("pool", lambda e: e.iota(ioi[:, :], [[0, 8], [1, 8]], base=0, channel_multiplier=0), (), (bio,))
        for qb in range(8):
            k.op("dve", lambda e, qb=qb: e.tensor_scalar(out=negp[:, qb, :], in0=ioi[:, :], scalar1=float(qb), scalar2=-1e30, op0=ALU.is_ge, op1=ALU.mult), (bio,), (bnegp,))
        for h2 in range(2):
            k.op("pool", lambda e, h2=h2: e.iota(io2[:, :], [[1, 256]], base=-128 * h2, channel_multiplier=-1), (bcm,), (bio,))
            k.op("dve", lambda e, h2=h2: e.tensor_scalar(out=cm[:, h2, :], in0=io2[:, :], scalar1=0.0, scalar2=NEG, op0=ALU.is_lt, op1=ALU.mult), (bio,), (bcm,))
        for q4 in range(4):
            k.op("pool", lambda e, q4=q4: e.iota(io3[:, :], [[1, 16], [0, 128]], base=16 * q4, channel_multiplier=-1), (boh,), (bio3,))
            k.op("dve", lambda e, q4=q4: e.tensor_scalar(out=oh[:, q4 * 2048:(q4 + 1) * 2048], in0=io3[:, :], scalar1=0.0, scalar2=None, op0=ALU.is_equal), (bio3,), (boh,))
        qT = [sb("C_qT%d" % i, [128, 4, SEQ], BF16) for i in range(2)]
        kTzs = [sb("C_kTz%d" % i, [128, 8, SEQ], BF16) for i in range(2)]
        Va = [sb("C_V%d" % i, [128, 16, 8, 66], BF16) for i in range(2)]
        bq, bv, bks = [Buf(), Buf()], [Buf(), Buf()], [Buf(), Buf()]
        for i in range(2):
            k.op("pool", lambda e, i=i: e.memset(kTzs[i][:, :, :], 0.0), (), (bks[i],))
            k.op("pool", lambda e, i=i: e.memset(Va[i][:, :, :, :], 1.0), (), (bv[i],))
        kms = sb("C_kms", [128, 64], F32)
        kmbs = [sb("C_kmb%d" % i, [128, 8, 8], BF16) for i in range(2)]
        bkms, bkmbs = Buf(), [Buf(), Buf()]
        pgb = ps.enter_context(nc.psum_tensor("C_pgb", [128, 512], F32))
        bpg = [Buf(), Buf()]
        bpbt = [Buf(), Buf()]
        gms = [sb("C_gm%d" % i, [128, 64], F32) for i in range(2)]
        cmps = [sb("C_cmp%d" % i, [128, 512], F32) for i in range(2)]
        ranks = [sb("C_rank%d" % i, [128, 64], F32) for i in range(2)]
        biasbs = [sb("C_biasb%d" % i, [128, 128], F32) for i in range(2)]
        bgms, bcmps, branks, bbiasbs = ([Buf(), Buf()] for _ in range(4))
        biasTs = [sb("C_biasT%d" % i, [128, SEQ], BF16) for i in range(2)]
        bbTs = [Buf(), Buf()]
        for i in range(2):
            k.op("pool", lambda e, i=i: e.memset(biasbs[i][:, :], 0.0), (), (bbiasbs[i],))
        pss = [ps.enter_context(nc.psum_tensor("C_ps%d" % i, [128, 512], F32)) for i in range(3)]
        bps = [Buf(), Buf(), Buf()]
        po = [[ps.enter_context(nc.psum_tensor("C_po%d%d" % (i, j), [128, 512], F32)) for j in range(2)] for i in range(2)]
        bpo = [[Buf(), Buf()], [Buf(), Buf()]]
        pe_ = [sb("C_pe%d" % i, [128, 256], BF16) for i in range(3)]
        bpe = [Buf() for _ in range(3)]
        rden = sb("C_rden", [128, 1], F32)
        brd = Buf()
        atm = [sb("C_atm%d" % i, [128, 16, 512], BF16) for i in range(2)]
        batm = [Buf(), Buf()]
        cnt = {"ips": 0, "ipe": 0, "ipo": 0}

        def load_seq(s):
            si = s % 2
            tb = s * SEQ
            kTz, bk = kTzs[si], bks[si]
            k.dma("sp", qT[si][:, :, :], io["fm_s"][0:512, tb:tb + SEQ].rearrange("(m p) t -> p m t", p=128), bq[si], (), (bq[si],))
            for h in range(8):
                hp = slice((h % 2) * 64, (h % 2) * 64 + 64)
                k.dma("sp", kTz[hp, h, :], io["fm_s"][512 + h * 64:512 + (h + 1) * 64, tb:tb + SEQ], bk, (), (bk,))
            for kt in range(16):
                k.dma("sp", Va[si][:, kt, :, 0:64], io["z_s"][tb + kt * 128:tb + (kt + 1) * 128, 512:1024].rearrange("p (h d) -> p h d", h=8), bv[si], (), (bv[si],))
            k.op("dve", lambda e: e.tensor_reduce(out=kms[:, :], in_=kTz[:, :, :].rearrange("p h (b t) -> p (h b) t", b=8), axis=AX.X, op=ALU.add), (bk,), (bkms,))
            k.op("dve", lambda e: e.tensor_copy(out=kmbs[si][:, :, :], in_=kms[:, :].rearrange("p (h b) -> p h b", h=8)), (bkms,), (bkmbs[si],))

        def gate_step(s, qt):
            si = s % 2
            g2 = qt % 2
            qb = qt // 2
            gm, cmp, rank, biasb = gms[g2], cmps[g2], ranks[g2], biasbs[g2]
            pg = pgb[:, g2 * 64:(g2 + 1) * 64]
            pbt = pgb[:, 256 + g2 * 128:256 + (g2 + 1) * 128]

            def gfn(eng):
                ins = None
                for h in range(8):
                    ins = eng.matmul(pg[:, h * 8:(h + 1) * 8], qT[si][:, h // 2, qt * 128:(qt + 1) * 128], kmbs[si][:, h, :], start=True, stop=True)
                return ins
            k.op("pe", gfn, (bq[si], bkmbs[si]), (bpg[g2],))
            k.op("dve", lambda e: e.tensor_tensor(out=gm[:, :], in0=pg, in1=negp[:, qb, :], op=ALU.add), (bpg[g2], bnegp), (bgms[g2],))
            g3 = gm[:, :].rearrange("p (h b) -> p h b", h=8)
            k.op("dve", lambda e: e.tensor_tensor(out=cmp[:, :].rearrange("p (h b c) -> p h b c", h=8, b=8), in0=g3.unsqueeze(2).broadcast_to([128, 8, 8, 8]), in1=g3.unsqueeze(3).broadcast_to([128, 8, 8, 8]), op=ALU.is_gt), (bgms[g2],), (bcmps[g2],))
            k.op("dve", lambda e: e.tensor_reduce(out=rank[:, :], in_=cmp[:, :].rearrange("p (a c) -> p a c", c=8), axis=AX.X, op=ALU.add), (bcmps[g2],), (branks[g2],))
            k.op("dve", lambda e: e.tensor_scalar(out=biasb[:, 0:64], in0=rank[:, :], scalar1=2.5, scalar2=NEG, op0=ALU.is_gt, op1=ALU.mult), (branks[g2],), (bbiasbs[g2],))

        def gate_step_b(s, qt):
            si = s % 2
            g2 = qt % 2
            biasb = biasbs[g2]
            pbt = pgb[:, 256 + g2 * 128:256 + (g2 + 1) * 128]
            tr(k, [(pbt, biasb[:, :], c.identf[:, :])], (bbiasbs[g2],), (bpbt[g2],))
            k.op("dve", lambda e: e.tensor_copy(out=biasTs[si][:, qt * 128:(qt + 1) * 128], in_=pbt), (bpbt[g2],), (bbTs[si],))

        def sweep(s, inserts):
            si = s % 2
            kTz, bk = kTzs[si], bks[si]
            biasT, bbT = biasTs[si], bbTs[si]
            units = [(h, qb, kt) for h in range(8) for qb in range(8) for kt in range(2 * qb + 2)]

            def score(u, p):
                h, qb, kt = u
                m = h // 2
                b = kt // 2
                qs = slice(qb * 256, (qb + 1) * 256)
                last = (b != qb) and (qb <= 3)
                first = (pss[p][:, 0:256], kTz[:, h, kt * 128:(kt + 1) * 128], qT[si][:, m, qs], True, last)
                if b == qb:
                    items = [first, (pss[p][:, 0:256], c.ident[:, :], cm[:, kt % 2, :], False, True)]
                    rd = (bk, bq[si], bcm)
                elif last:
                    items = [first]
                    rd = (bk, bq[si])
                else:
                    r = h * 8 + b
                    items = [first, (pss[p][:, 0:256], oh[:, r * 128:(r + 1) * 128], biasT[:, qs], False, True)]
                    rd = (bk, bq[si], boh, bbT)
                mm(k, items, rd, (bps[p],))

            ips = cnt["ips"]
            score(units[0], ips % 3)
            score(units[1], (ips + 1) % 3)
            oi = 0
            for ui, u in enumerate(units):
                h, qb, kt = u
                nkt = 2 * qb + 2
                p = ips % 3
                ips += 1
                if ui + 2 < len(units):
                    score(units[ui + 2], (ips + 1) % 3)
                if kt == 0:
                    oi = cnt["ipo"] % 2
                    cnt["ipo"] += 1
                e_i = cnt["ipe"] % 3
                cnt["ipe"] += 1
                k.op("act", lambda e, p=p, e_i=e_i: e.activation(out=pe_[e_i][:, :], in_=pss[p][:, 0:256], func=AF.Exp, scale=0.125), (bps[p],), (bpe[e_i],))
                mm(k, [(po[oi][qh][:, 0:65], pe_[e_i][:, qh * 128:(qh + 1) * 128], Va[si][:, kt, h, 0:65], kt == 0, kt == nkt - 1) for qh in range(2)], (bpe[e_i], bv[si]), (bpo[oi][0], bpo[oi][1]))
                if kt == nkt - 1:
                    for qh in range(2):
                        qt = qb * 2 + qh
                        k.op("dve", lambda e, oi=oi, qh=qh: e.reciprocal(out=rden[:, :], in_=po[oi][qh][:, 64:65]), (bpo[oi][qh],), (brd,))
                        k.op("dve", lambda e, oi=oi, qh=qh, qt=qt, h=h: e.tensor_scalar(out=atm[si][:, qt, h * 64:(h + 1) * 64], in0=po[oi][qh][:, 0:64], scalar1=rden[:, :], scalar2=None, op0=ALU.mult), (bpo[oi][qh], brd), (batm[si],))
                if ui in inserts:
                    inserts[ui]()
            cnt["ips"] = ips
            tb = s * SEQ
            k.dma("pool", io["at_s"][tb:tb + SEQ, :].rearrange("(q p) c -> p q c", p=128), atm[si][:, :, :], batm[si], (batm[si],), ())

        load_seq(0)
        for qt in range(16):
            gate_step(0, qt)
            gate_step_b(0, qt)
        for s in range(BPC):
            inserts = {}
            if s + 1 < BPC:
                inserts[8] = (lambda s=s: load_seq(s + 1))
                for qt in range(16):
                    inserts[40 + qt * 30] = (lambda s=s, qt=qt: gate_step(s + 1, qt))
                    inserts[55 + qt * 30] = (lambda s=s, qt=qt: gate_step_b(s + 1, qt))
            sweep(s, inserts)
        k.end("C")


TWO_PI = 2.0 * math.pi


def cis(k, scr, first, out_c, out_s, barg, bout):
    t, ti, y, m1 = scr
    bt = Buf()
    k.op("dve", lambda e: first(e, t), (barg,), (bt,))
    k.op("dve", lambda e: e.tensor_copy(out=ti, in_=t), (bt,), (bt,))
    k.op("dve", lambda e: e.tensor_copy(out=y, in_=ti), (bt,), (bt,))
    k.op("dve", lambda e: e.tensor_tensor(out=t, in0=t, in1=y, op=ALU.subtract), (bt,), (bt,))
    for shift, dst in ((0.0, out_s), (math.pi / 2, out_c)):
        k.op("dve", lambda e, shift=shift: e.tensor_scalar(out=y, in0=t, scalar1=TWO_PI, scalar2=shift, op0=ALU.mult, op1=ALU.add), (bt,), (bt,))
        k.op("dve", lambda e: e.tensor_scalar(out=m1, in0=y, scalar1=math.pi, scalar2=-TWO_PI, op0=ALU.is_gt, op1=ALU.mult), (bt,), (bt,))
        k.op("dve", lambda e: e.tensor_tensor(out=m1, in0=m1, in1=y, op=ALU.add), (bt,), (bt,))
        k.op("dve", lambda e: e.tensor_scalar(out=y, in0=y, scalar1=-math.pi, scalar2=TWO_PI, op0=ALU.is_lt, op1=ALU.mult), (bt,), (bt,))
        k.op("dve", lambda e: e.tensor_tensor(out=y, in0=m1, in1=y, op=ALU.add), (bt,), (bt,))
        k.op("dve", lambda e: e.tensor_scalar(out=y, in0=y, scalar1=math.pi, scalar2=-math.pi, op0=ALU.min, op1=ALU.max), (bt,), (bt,))
        k.op("act", lambda e, dst=dst: e.activation(out=dst, in_=y, func=AF.Sin), (bt,), (bout, bt))


def cmul(k, eng, out_r, out_i, ar, ai, br, bi, t1, t2, rd, wr, bt, neg_i=False):
    E = eng
    k.op(E, lambda e: e.tensor_tensor(out=t1, in0=ar, in1=br, op=ALU.mult), rd, (bt,))
    k.op(E, lambda e: e.tensor_tensor(out=t2, in0=ai, in1=bi, op=ALU.mult), rd + (bt,), (bt,))
    k.op(E, lambda e: e.tensor_tensor(out=out_r, in0=t1, in1=t2, op=ALU.subtract), (bt,), wr + (bt,))
    k.op(E, lambda e: e.tensor_tensor(out=t1, in0=ar, in1=bi, op=ALU.mult), rd + (bt,), (bt,))
    k.op(E, lambda e: e.tensor_tensor(out=t2, in0=ai, in1=br, op=ALU.mult), rd + (bt,), (bt,))
    if neg_i:
        k.op(E, lambda e: e.scalar_tensor_tensor(out=out_i, in0=t1, scalar=-1.0, in1=t2, op0=ALU.mult, op1=ALU.subtract), (bt,), wr + (bt,))
    else:
        k.op(E, lambda e: e.tensor_tensor(out=out_i, in0=t1, in1=t2, op=ALU.add), (bt,), wr + (bt,))


def phase_S(k, c, io):
    nc = k.nc
    with ExitStack() as outer:
        osb = lambda n, s, d: outer.enter_context(nc.sbuf_tensor(n, s, d))
        GT = osb("S_GT", [128, G, 2, 128], BF16)
        TT = osb("S_TT", [128, G, 2, 256], BF16)
        H = osb("S_H", [128, G, 256], BF16)
        Mt = osb("S_M", [128, 2, 2048], F32)
        Dt = osb("S_D", [128, 2, 2048], F32)
        s5_setup(k, c, io, GT, TT, H, Mt, Dt)
        s5_main(k, c, io, GT, TT, H, Mt, Dt)


def s5_setup(k, c, io, GT, TT, H, Mt, Dt):
    nc = k.nc
    with ExitStack() as ps:
        k.begin()
        sb = lambda n, s, d: ps.enter_context(nc.sbuf_tensor(n, s, d))
        sb2 = lambda n, s, d: ps.enter_context(nc.sbuf_tensor(n, [s[0], int(np.prod(s[1:]))], d))
        B0 = Buf()
        lg = sb("u_lg", [32, 128], F32)
        k.dma("sp", lg[:, 0:64], io["ssm_lam_re"], B0, (), (B0,))
        k.dma("sp", lg[:, 64:128], io["ssm_lam_im"], B0, (), (B0,))
        ldt = sb("u_ldt", [64, 32], F32)
        k.dma("sp", ldt[:, :], io["ssm_log_dt"].partition_broadcast(64), B0, (), (B0,))
        Bre = sb("u_Bre", [64, G, 16], F32)
        Bim = sb("u_Bim", [64, G, 16], F32)
        k.dma("sp", Bre[:, :, :], io["ssm_b_re"].rearrange("g p c -> p g c"), B0, (), (B0,))
        k.dma("sp", Bim[:, :, :], io["ssm_b_im"].rearrange("g p c -> p g c"), B0, (), (B0,))
        cg = [sb("u_cg%d" % i, [128, 4, 64], F32) for i in range(2)]
        k.dma("sp", cg[0][:, :, :], io["ssm_c_re"].rearrange("(a b) c p -> (b c) a p", a=4), B0, (), (B0,))
        k.dma("sp", cg[1][:, :, :], io["ssm_c_im"].rearrange("(a b) c p -> (b c) a p", a=4), B0, (), (B0,))
        dcol = sb("u_dcol", [128, G], F32)
        for j in range(8):
            k.dma("sp", dcol[j * 16:(j + 1) * 16, :], io["ssm_d"].rearrange("(g c) -> c g", c=16), B0, (), (B0,), allow_slow_non_contiguous=True)
        pp = ps.enter_context(nc.psum_tensor("u_pp", [128, 512], F32))
        pq = ps.enter_context(nc.psum_tensor("u_pq", [128, 512], F32))
        Bp = Buf()
        lre = sb("u_lre", [64, G], F32)
        lim = sb("u_lim", [64, G], F32)
        tr(k, [(pp[0:64, 0:32], lg[:, 0:64], c.identf[0:32, 0:32]), (pp[0:64, 32:64], lg[:, 64:128], c.identf[0:32, 0:32])], (B0,), (Bp,))
        k.op("dve", lambda e: e.tensor_copy(out=lre[:, :], in_=pp[0:64, 0:32]), (Bp,), (B0,))
        k.op("dve", lambda e: e.tensor_copy(out=lim[:, :], in_=pp[0:64, 32:64]), (Bp,), (B0, Bp))
        Cre = sb("u_Cre", [64, G, 16], F32)
        Cim = sb("u_Cim", [64, G, 16], F32)
        for i, Cx in enumerate((Cre, Cim)):
            tr(k, [(pp[0:64, a * 128:(a + 1) * 128], cg[i][:, a, :], c.identf[:, :]) for a in range(4)], (B0,), (Bp,))
            k.op("dve", lambda e, Cx=Cx: e.tensor_copy(out=Cx[:, :, :], in_=pp[0:64, :].rearrange("p (g c) -> p g c", c=16)), (Bp,), (B0, Bp))
        dt = sb("u_dt", [64, G], F32)
        k.op("act", lambda e: e.activation(out=dt[:, :], in_=ldt[:, :], func=AF.Exp), (B0,), (B0,))
        rho = sb("u_rho", [64, G], F32)
        th = sb("u_th", [64, G], F32)
        k.op("dve", lambda e: e.tensor_tensor(out=rho[:, :], in0=lre[:, :], in1=dt[:, :], op=ALU.mult), (B0,), (B0,))
        k.op("dve", lambda e: e.tensor_tensor(out=th[:, :], in0=lim[:, :], in1=dt[:, :], op=ALU.mult), (B0,), (B0,))
        nvi = sb("u_nvi", [64, 17], I32)
        nv = sb("u_nv", [64, 17], F32)
        k.op("pool", lambda e: e.iota(nvi[:, :], [[1, 17]], base=0, channel_multiplier=0), (B0,), (B0,))
        k.op("dve", lambda e: e.tensor_copy(out=nv[:, :], in_=nvi[:, :]), (B0,), (B0,))
        NP = G * 17
        argt = sb("u_argt", [64, NP], F32)
        argr = sb("u_argr", [64, NP], F32)
        a3 = lambda t: t[:, :].rearrange("p (g n) -> p g n", n=17)
        bth = th[:, :].unsqueeze(2).broadcast_to([64, G, 17])
        brho = rho[:, :].unsqueeze(2).broadcast_to([64, G, 17])
        bnv = nv[:, :].unsqueeze(1).broadcast_to([64, G, 17])
        k.op("dve", lambda e: e.tensor_tensor(out=a3(argt), in0=bth, in1=bnv, op=ALU.mult), (B0,), (B0,))
        k.op("dve", lambda e: e.tensor_tensor(out=a3(argr), in0=brho, in1=bnv, op=ALU.mult), (B0,), (B0,))
        cs = sb("u_cs", [64, NP], F32)
        sn = sb("u_sn", [64, NP], F32)
        scr = (sb("u_c1", [64, NP], F32)[:, :], sb("u_c2", [64, NP], I32)[:, :], sb("u_c3", [64, NP], F32)[:, :], sb("u_c4", [64, NP], F32)[:, :])
        cis(k, scr, lambda e, t: e.tensor_scalar(out=t, in0=argt[:, :], scalar1=1.0 / TWO_PI, scalar2=None, op0=ALU.mult), cs[:, :], sn[:, :], B0, B0)
        mg = sb("u_mg", [64, NP], F32)
        mgi = sb("u_mgi", [64, NP], F32)
        k.op("act", lambda e: e.activation(out=mg[:, :], in_=argr[:, :], func=AF.Exp), (B0,), (B0,))
        k.op("act", lambda e: e.activation(out=mgi[:, :], in_=argr[:, :], func=AF.Exp, scale=-1.0), (B0,), (B0,))
        Pr = sb("u_Pr", [64, G, 17], F32)
        Pi = sb("u_Pi", [64, G, 17], F32)
        Qr = sb("u_Qr", [64, G, 17], F32)
        Qi = sb("u_Qi", [64, G, 17], F32)
        f2 = lambda t: t[:, :, :].rearrange("p g n -> p (g n)")
        k.op("dve", lambda e: e.tensor_tensor(out=f2(Pr), in0=mg[:, :], in1=cs[:, :], op=ALU.mult), (B0,), (B0,))
        k.op("dve", lambda e: e.tensor_tensor(out=f2(Pi), in0=mg[:, :], in1=sn[:, :], op=ALU.mult), (B0,), (B0,))
        k.op("dve", lambda e: e.tensor_tensor(out=f2(Qr), in0=mgi[:, :], in1=cs[:, :], op=ALU.mult), (B0,), (B0,))
        k.op("dve", lambda e: e.scalar_tensor_tensor(out=f2(Qi), in0=mgi[:, :], scalar=-1.0, in1=sn[:, :], op0=ALU.mult, op1=ALU.mult), (B0,), (B0,))
        den = sb("u_den", [64, G], F32)
        tA = sb("u_tA", [64, G], F32)
        tB = sb("u_tB", [64, G], F32)
        nr = sb("u_nr", [64, G], F32)
        cr = sb("u_cr", [64, G], F32)
        ci = sb("u_ci", [64, G], F32)
        k.op("dve", lambda e: e.tensor_tensor(out=den[:, :], in0=lre[:, :], in1=lre[:, :], op=ALU.mult), (B0,), (B0,))
        k.op("dve", lambda e: e.tensor_tensor(out=tA[:, :], in0=lim[:, :], in1=lim[:, :], op=ALU.mult), (B0,), (B0,))
        k.op("dve", lambda e: e.tensor_tensor(out=den[:, :], in0=den[:, :], in1=tA[:, :], op=ALU.add), (B0,), (B0,))
        k.op("dve", lambda e: e.reciprocal(out=den[:, :], in_=den[:, :]), (B0,), (B0,))
        k.op("dve", lambda e: e.tensor_scalar(out=nr[:, :], in0=Pr[:, :, 1], scalar1=-1.0, scalar2=None, op0=ALU.add), (B0,), (B0,))
        k.op("dve", lambda e: e.tensor_tensor(out=tA[:, :], in0=nr[:, :], in1=lre[:, :], op=ALU.mult), (B0,), (B0,))
        k.op("dve", lambda e: e.tensor_tensor(out=tB[:, :], in0=Pi[:, :, 1], in1=lim[:, :], op=ALU.mult), (B0,), (B0,))
        k.op("dve", lambda e: e.tensor_tensor(out=tA[:, :], in0=tA[:, :], in1=tB[:, :], op=ALU.add), (B0,), (B0,))
        k.op("dve", lambda e: e.tensor_tensor(out=cr[:, :], in0=tA[:, :], in1=den[:, :], op=ALU.mult), (B0,), (B0,))
        k.op("dve", lambda e: e.tensor_tensor(out=tA[:, :], in0=Pi[:, :, 1], in1=lre[:, :], op=ALU.mult), (B0,), (B0,))
        k.op("dve", lambda e: e.tensor_tensor(out=tB[:, :], in0=nr[:, :], in1=lim[:, :], op=ALU.mult), (B0,), (B0,))
        k.op("dve", lambda e: e.tensor_tensor(out=tA[:, :], in0=tA[:, :], in1=tB[:, :], op=ALU.subtract), (B0,), (B0,))
        k.op("dve", lambda e: e.tensor_tensor(out=ci[:, :], in0=tA[:, :], in1=den[:, :], op=ALU.mult), (B0,), (B0,))
        bbr = sb("u_bbr", [64, G, 16], F32)
        bbi = sb("u_bbi", [64, G, 16], F32)
        w1 = sb("u_w1", [64, G, 16], F32)
        w2 = sb("u_w2", [64, G, 16], F32)
        bc16 = lambda t: t[:, :].unsqueeze(2).broadcast_to([64, G, 16])
        cmul(k, "dve", bbr[:, :, :], bbi[:, :, :], bc16(cr), bc16(ci), Bre[:, :, :], Bim[:, :, :], w1[:, :, :], w2[:, :, :], (B0,), (B0,), B0)
        mki = sb("u_mki", [128, 2, 256], I32)
        mask = sb("u_mask", [128, 2, 256], F32)
        idc = sb("u_idc", [128, 2, 256], F32)
        shid = sb("u_shid", [64, 128], F32)
        for ch in range(2):
            k.op("pool", lambda e, ch=ch: e.iota(mki[:, ch, :], [[16, 16], [0, 16]], base=15 - 128 * ch, channel_multiplier=-1), (B0,), (B0,))
        k.op("dve", lambda e: e.tensor_scalar(out=mask[:, :, :], in0=mki[:, :, :], scalar1=0.0, scalar2=None, op0=ALU.is_ge), (B0,), (B0,))
        for ch in range(2):
            k.op("pool", lambda e, ch=ch: e.iota(mki[:, ch, :], [[16, 16], [1, 16]], base=-128 * ch, channel_multiplier=-1), (B0,), (B0,))
        k.op("dve", lambda e: e.tensor_scalar(out=idc[:, :, :], in0=mki[:, :, :], scalar1=0.0, scalar2=None, op0=ALU.is_equal), (B0,), (B0,))
        k.op("pool", lambda e: e.iota(mki[0:64, 0, 0:128], [[1, 128]], base=-64, channel_multiplier=-1), (B0,), (B0,))
        k.op("dve", lambda e: e.tensor_scalar(out=shid[:, :], in0=mki[0:64, 0, 0:128], scalar1=0.0, scalar2=None, op0=ALU.is_equal), (B0,), (B0,))
        GC = 4
        Fr = sb("u_Fr", [64, GC, 16, 16], F32)
        Fi = sb("u_Fi", [64, GC, 16, 16], F32)
        Er = sb("u_Er", [64, GC, 17, 16], F32)
        nEi = sb("u_nEi", [64, GC, 17, 16], F32)
        Lr = sb("u_Lr", [64, GC, 16, 16], F32)
        Li = sb("u_Li", [64, GC, 16, 16], F32)
        x1 = sb("u_x1", [64, GC, 17, 16], F32)
        x2 = sb("u_x2", [64, GC, 17, 16], F32)
        tmask2 = sb("u_tmask2", [128, 512], F32)
        for gc in range(G // GC):
            gs = slice(gc * GC, (gc + 1) * GC)
            qr = Qr[:, gs, 0:16].unsqueeze(3).broadcast_to([64, GC, 16, 16])
            qi = Qi[:, gs, 0:16].unsqueeze(3).broadcast_to([64, GC, 16, 16])
            br_ = bbr[:, gs, :].unsqueeze(2).broadcast_to([64, GC, 16, 16])
            bi_ = bbi[:, gs, :].unsqueeze(2).broadcast_to([64, GC, 16, 16])
            cmul(k, "dve", Fr[:, :, :, :], Fi[:, :, :, :], qr, qi, br_, bi_, x1[:, :, 0:16, :], x2[:, :, 0:16, :], (B0,), (B0,), B0)
            pr = Pr[:, gs, :].unsqueeze(3).broadcast_to([64, GC, 17, 16])
            pi = Pi[:, gs, :].unsqueeze(3).broadcast_to([64, GC, 17, 16])
            cr_ = Cre[:, gs, :].unsqueeze(2).broadcast_to([64, GC, 17, 16])
            ci_ = Cim[:, gs, :].unsqueeze(2).broadcast_to([64, GC, 17, 16])
            cmul(k, "dve", Er[:, :, :, :], nEi[:, :, :, :], pr, pi, cr_, ci_, x1[:, :, :, :], x2[:, :, :, :], (B0,), (B0,), B0, neg_i=True)
            l15r = Pr[:, gs, 15:16].unsqueeze(3).broadcast_to([64, GC, 16, 16])
            l15i = Pi[:, gs, 15:16].unsqueeze(3).broadcast_to([64, GC, 16, 16])
            cmul(k, "dve", Lr[:, :, :, :], Li[:, :, :, :], l15r, l15i, Fr[:, :, :, :], Fi[:, :, :, :], x1[:, :, 0:16, :], x2[:, :, 0:16, :], (B0,), (B0,), B0)
            for gl in range(0, GC, 2):
                g = gc * GC + gl
                items = []
                for g2 in range(2):
                    for ch in range(2):
                        o0 = g2 * 256 + ch * 128
                        items.append((pp[:, o0:o0 + 64], Lr[:, gl + g2, ch * 8:(ch + 1) * 8, :].rearrange("p j c -> p (j c)"), c.identf[0:64, 0:64]))
                        items.append((pp[:, o0 + 64:o0 + 128], Li[:, gl + g2, ch * 8:(ch + 1) * 8, :].rearrange("p j c -> p (j c)"), c.identf[0:64, 0:64]))
                tr(k, items, (B0,), (Bp,))
                k.op("act", lambda e, g=g: e.copy(out=GT[:, g:g + 2, :, :], in_=pp[:, :].rearrange("p (g a b) -> p g a b", g=2, a=2)), (Bp,), (B0, Bp))
                for ch in range(2):
                    items = []
                    for g2 in range(2):
                        fr_ = Fr[:, gl + g2, ch * 8:(ch + 1) * 8, :].rearrange("p j c -> p (j c)")
                        fi_ = Fi[:, gl + g2, ch * 8:(ch + 1) * 8, :].rearrange("p j c -> p (j c)")
                        er_ = Er[:, gl + g2, 0:16, :].rearrange("p i c -> p (i c)")
                        ei_ = nEi[:, gl + g2, 0:16, :].rearrange("p i c -> p (i c)")
                        o = pq[:, g2 * 256:(g2 + 1) * 256]
                        items += [(o, fr_, er_, True, False), (o, fi_, ei_, False, True)]
                    Bq = Buf()
                    mm(k, items, (B0,), (Bq, Bp))
                    k.op("dve", lambda e, ch=ch: e.tensor_tensor(out=tmask2[:, :].rearrange("p (g x) -> p g x", g=2), in0=pq[:, :].rearrange("p (g x) -> p g x", g=2), in1=mask[:, ch, :].unsqueeze(1).broadcast_to([128, 2, 256]), op=ALU.mult), (Bq, B0), (B0, Bp))
                    for g2 in range(2):
                        k.op("dve", lambda e, ch=ch, g=g, g2=g2: e.scalar_tensor_tensor(out=TT[:, g + g2, ch, :], in0=idc[:, ch, :], scalar=dcol[:, g + g2:g + g2 + 1], in1=tmask2[:, g2 * 256:(g2 + 1) * 256], op0=ALU.mult, op1=ALU.add), (B0,), (B0,))
                items = []
                for g2 in range(2):
                    er1 = Er[:, gl + g2, 1:17, :].rearrange("p i c -> p (i c)")
                    ei1 = nEi[:, gl + g2, 1:17, :].rearrange("p i c -> p (i c)")
                    o = pq[:, g2 * 256:(g2 + 1) * 256]
                    items += [(o, c.identf[0:64, :], er1, True, False), (o, shid[:, :], ei1, False, True)]
                mm(k, items, (B0,), (Bp,))
                k.op("act", lambda e, g=g: e.copy(out=H[:, g:g + 2, :], in_=pq[:, :].rearrange("p (g x) -> p g x", g=2)), (Bp,), (B0, Bp))
        k.end("S_setup")
    with ExitStack() as ps:
        k.begin()
        sb = lambda n, s, d: ps.enter_context(nc.sbuf_tensor(n, s, d))
        B0 = Buf()
        lb = sb("w_lb", [128, 2, 2048], F32)
        k.dma("sp", lb[:, 0, :], io["ssm_lam_re"].rearrange("g p -> (g p)").partition_broadcast(128), B0, (), (B0,))
        k.dma("sp", lb[:, 1, :], io["ssm_lam_im"].rearrange("g p -> (g p)").partition_broadcast(128), B0, (), (B0,))
        dtb = sb("w_dtb", [128, G], F32)
        k.dma("sp", dtb[:, :], io["ssm_log_dt"].partition_broadcast(128), B0, (), (B0,))
        k.op("act", lambda e: e.activation(out=dtb[:, :], in_=dtb[:, :], func=AF.Exp), (B0,), (B0,))
        for i in range(2):
            k.op("dve", lambda e, i=i: e.tensor_tensor(out=lb[:, i, :].rearrange("p (g q) -> p g q", q=64), in0=lb[:, i, :].rearrange("p (g q) -> p g q", q=64), in1=dtb[:, :].unsqueeze(2).broadcast_to([128, G, 64]), op=ALU.mult), (B0,), (B0,))
        nki = sb("w_nki", [128, 2], I32)
        nk = sb("w_nk", [128, 2], F32)
        nk2 = sb("w_nk2", [128, 2], F32)
        k.op("pool", lambda e: e.iota(nki[:, 0:1], [[0, 1]], base=1024, channel_multiplier=-16), (B0,), (B0,))
        k.op("pool", lambda e: e.iota(nki[:, 1:2], [[0, 1]], base=-1040, channel_multiplier=16), (B0,), (B0,))
        k.op("dve", lambda e: e.tensor_copy(out=nk[:, :], in_=nki[:, :]), (B0,), (B0,))
        k.op("dve", lambda e: e.tensor_scalar(out=nk2[:, :], in0=nk[:, :], scalar1=1.0 / TWO_PI, scalar2=None, op0=ALU.mult), (B0,), (B0,))
        wc = sb("w_c", [128, 2048], F32)
        ws = sb("w_s", [128, 2048], F32)
        scr = (sb("w_c1", [128, 2048], F32)[:, :], sb("w_c2", [128, 2048], I32)[:, :], sb("w_c3", [128, 2048], F32)[:, :], sb("w_c4", [128, 2048], F32)[:, :])
        for i, Tb in enumerate((Mt, Dt)):
            cis(k, scr, lambda e, t, i=i: e.tensor_scalar(out=t, in0=lb[:, 1, :], scalar1=nk2[:, i:i + 1], scalar2=None, op0=ALU.mult), wc[:, :], ws[:, :], B0, B0)
            k.op("act", lambda e, i=i, Tb=Tb: e.activation(out=Tb[:, 1, :], in_=lb[:, 0, :], func=AF.Exp, scale=nk[:, i:i + 1]), (B0,), (B0,))
            k.op("dve", lambda e, Tb=Tb: e.tensor_tensor(out=Tb[:, 0, :], in0=Tb[:, 1, :], in1=wc[:, :], op=ALU.mult), (B0,), (B0,))
            k.op("dve", lambda e, Tb=Tb: e.tensor_tensor(out=Tb[:, 1, :], in0=Tb[:, 1, :], in1=ws[:, :], op=ALU.mult), (B0,), (B0,))
        k.end("S_tables")


def s5_main(k, c, io, GT, TT, H, Mt, Dt):
    nc = k.nc
    with ExitStack() as ps:
        k.begin()
        sb = lambda n, s, d: ps.enter_context(nc.sbuf_tensor(n, s, d))
        bTab = Buf()
        Wg = sb("S_Wg", [128, 4, 512], BF16)
        bWg = Buf()
        load_w_cast(k, ps, io["w_glu"], 4, 512, "S_wg", Wg, bWg, "dve")
        bgl = sb("S_bgl", [128, 4], F32)
        bbgl = Buf()
        k.dma("sp", bgl[:, :], io["b_glu"].rearrange("(m p) -> p m", p=128), bbgl, (), (bbgl,), allow_slow_non_contiguous=True)
        UA = sb("S_UA", [128, 8192], BF16)
        UB = sb("S_UB", [128, 8192], BF16)
        bUA, bUB = Buf(), Buf()
        U16 = sb("S_U16", [128, G, 2, 128], BF16)
        bU16 = Buf()
        X = sb("S_X", [128, G, 128], BF16)
        bX = [Buf() for _ in range(8)]
        Stm = sb("S_Stm", [128, G, 128], BF16)
        bStm = [Buf() for _ in range(8)]
        ST = sb("S_ST", [128, G, 128], BF16)
        bST = [Buf() for _ in range(4)]
        pT = [ps.enter_context(nc.psum_tensor("S_pT%d" % i, [128, 1024], BF16)) for i in range(2)]
        bpT = [Buf(), Buf()]
        NPD = 2
        pd = [ps.enter_context(nc.psum_tensor("S_pd%d" % i, [128, 512], F32)) for i in range(NPD)]
        bpd = [Buf() for _ in range(NPD)]
        py = [ps.enter_context(nc.psum_tensor("S_py%d" % i, [128, 512], F32)) for i in range(3)]
        bpy = [Buf() for _ in range(3)]
        pgl = ps.enter_context(nc.psum_tensor("S_pgl", [128, 512], F32))
        bpgl = Buf()
        tq = [sb("S_tq%d" % i, [128, 256], F32) for i in range(4)]
        btq = [Buf() for _ in range(4)]
        gx2 = [sb("S_gx2%d" % i, [128, 512], F32) for i in range(3)]
        gu = [sb("S_gu%d" % i, [128, 512], F32) for i in range(3)]
        bgx = [Buf() for _ in range(3)]
        bgu = [Buf() for _ in range(3)]
        sgl = sb("S_sgl", [128, 512], F32)
        bsgl = Buf()
        s5st = [sb("S_s5%d" % i, [128, 4, 512], BF16) for i in range(2)]
        bs5 = [Buf(), Buf()]
        ipT = 0
        ipd = 0
        ipy = 0

        def cmod(pbank, Tb, dst, g0, bsrc, bdst):
            src = pbank[:, :].rearrange("p (g x) -> p g x", g=4)
            sre, sim = src[:, :, 0:64], src[:, :, 64:128]
            tre = Tb[:, 0, g0 * 64:(g0 + 4) * 64].rearrange("p (g q) -> p g q", g=4)
            tim = Tb[:, 1, g0 * 64:(g0 + 4) * 64].rearrange("p (g q) -> p g q", g=4)
            v = lambda t: t[:, :].rearrange("p (g q) -> p g q", g=4)
            k.op("dve", lambda e: e.tensor_tensor(out=v(tq[0]), in0=sre, in1=tre, op=ALU.mult), (bsrc, bTab), (btq[0],))
            k.op("dve", lambda e: e.tensor_tensor(out=v(tq[1]), in0=sim, in1=tim, op=ALU.mult), (bsrc, bTab), (btq[1],))
            k.op("dve", lambda e: e.tensor_tensor(out=v(tq[2]), in0=sre, in1=tim, op=ALU.mult), (bsrc, bTab), (btq[2],))
            k.op("dve", lambda e: e.tensor_tensor(out=v(tq[3]), in0=sim, in1=tre, op=ALU.mult), (bsrc, bTab), (btq[3],))
            k.op("pool", lambda e: e.tensor_tensor(out=dst[:, g0:g0 + 4, 0:64], in0=v(tq[0]), in1=v(tq[1]), op=ALU.subtract), (btq[0], btq[1]), (bdst,))
            k.op("pool", lambda e: e.tensor_tensor(out=dst[:, g0:g0 + 4, 64:128], in0=v(tq[2]), in1=v(tq[3]), op=ALU.add), (btq[2], btq[3]), (bdst,))

        for s in range(BPC):
            tb = s * SEQ
            Utm = UA[:, :].rearrange("p (j c) -> p j c", j=16)
            k.dma("sp", Utm, io["z_s"][tb:tb + SEQ, 0:512].rearrange("(k j) c -> k j c", j=16), bUA, (), (bUA,))
            for hf in range(2):
                k.op("dve", lambda e, hf=hf: e.tensor_copy(
                    out=UB[:, hf * 4096:(hf + 1) * 4096].rearrange("p (g j c) -> p g j c", g=16, j=16),
                    in_=UA[:, :].rearrange("p (j g c) -> p g j c", j=16, g=32)[:, hf * 16:(hf + 1) * 16, :, :]), (bUA,), (bUB,))
            Ug = UB[:, :].rearrange("p (g x) -> p g x", g=32)
            for g4 in range(8):
                p = ipT % 2
                ipT += 1
                tr(k, [(pT[p][:, (gl * 2 + ch) * 128:(gl * 2 + ch + 1) * 128], Ug[:, g4 * 4 + gl, ch * 128:(ch + 1) * 128], c.ident[:, :]) for gl in range(4) for ch in range(2)], (bUB,), (bpT[p],))
                k.op("act", lambda e, p=p, g4=g4: e.copy(out=U16[:, g4 * 4:(g4 + 1) * 4, :, :], in_=pT[p][:, :].rearrange("p (g a k) -> p g a k", g=4, a=2)), (bpT[p],), (bU16,))
            for g4 in range(8):
                p = ipd % NPD
                ipd += 1
                items = []
                for gl in range(4):
                    g = g4 * 4 + gl
                    for ch in range(2):
                        items.append((pd[p][:, gl * 128:(gl + 1) * 128], U16[:, g, ch, :], GT[:, g, ch, :], ch == 0, ch == 1))
                mm(k, items, (bU16, bTab), (bpd[p],))
                cmod(pd[p], Mt, X, g4 * 4, bpd[p], bX[g4])
            for g4 in range(8):
                p = ipd % NPD
                ipd += 1
                mm(k, [(pd[p][:, :], c.tri[:, :], X[:, g4 * 4:(g4 + 1) * 4, :].rearrange("p g x -> p (g x)"), True, True)], (bX[g4],), (bpd[p],))
                cmod(pd[p], Dt, Stm, g4 * 4, bpd[p], bStm[g4])
            for g8 in range(4):
                p = ipT % 2
                ipT += 1
                tr(k, [(pT[p][:, gl * 128:(gl + 1) * 128], Stm[:, g8 * 8 + gl, :], c.ident[:, :]) for gl in range(8)], (bStm[2 * g8], bStm[2 * g8 + 1]), (bpT[p],))
                k.op("act", lambda e, p=p, g8=g8: e.copy(out=ST[:, g8 * 8:(g8 + 1) * 8, :], in_=pT[p][:, :].rearrange("p (g k) -> p g k", g=8)), (bpT[p],), (bST[g8],))
            ygtm = UA[:, :].rearrange("p (i c) -> p i c", i=16)
            def y_front(gp):
                p = gp % 3
                items = []
                for g2 in range(2):
                    g = gp * 2 + g2
                    o = py[p][:, g2 * 256:(g2 + 1) * 256]
                    items += [(o, U16[:, g, 0, :], TT[:, g, 0, :], True, False), (o, U16[:, g, 1, :], TT[:, g, 1, :], False, False), (o, ST[:, g, :], H[:, g, :], False, True)]
                mm(k, items, (bU16, bST[gp // 4], bTab), (bpy[p],))
                k.op("act", lambda e: e.activation(out=gx2[p][:, :], in_=py[p][:, :], func=AF.Square), (bpy[p],), (bgx[p],))
                k.op("pool", lambda e: e.tensor_scalar(out=gx2[p][:, :], in0=gx2[p][:, :], scalar1=0.044715, scalar2=1.0, op0=ALU.mult, op1=ALU.add), (bgx[p],), (bgx[p],))

            def y_back(gp):
                p = gp % 3
                k.op("dve", lambda e: e.tensor_tensor(out=gu[p][:, :], in0=py[p][:, :], in1=gx2[p][:, :], op=ALU.mult), (bpy[p], bgx[p]), (bgu[p],))
                k.op("act", lambda e: e.activation(out=gu[p][:, :], in_=gu[p][:, :], func=AF.Sigmoid, scale=1.5957691216057308), (bgu[p],), (bgu[p],))
                k.op("dve", lambda e: e.tensor_tensor(
                    out=ygtm[:, :, gp * 32:(gp + 1) * 32].rearrange("p i (g c) -> p g i c", g=2),
                    in0=py[p][:, :].rearrange("p (g i c) -> p g i c", g=2, i=16),
                    in1=gu[p][:, :].rearrange("p (g i c) -> p g i c", g=2, i=16), op=ALU.mult), (bpy[p], bgu[p]), (bUA,))

            y_front(0)
            for gp in range(16):
                if gp + 1 < 16:
                    y_front(gp + 1)
                y_back(gp)
            ygT = UB[:, :].rearrange("p (ct t) -> p ct t", ct=4)
            for ib in range(8):
                p = ipT % 2
                ipT += 1
                tr(k, [(pT[p][:, (i2 * 4 + ct) * 128:(i2 * 4 + ct + 1) * 128], ygtm[:, ib * 2 + i2, ct * 128:(ct + 1) * 128], c.ident[:, :]) for i2 in range(2) for ct in range(4)], (bUA,), (bpT[p],))
                k.op("act", lambda e, p=p, ib=ib: e.copy(
                    out=UB[:, :].rearrange("p (ct k i) -> p i ct k", ct=4, i=16)[:, ib * 2:(ib + 1) * 2, :, :],
                    in_=pT[p][:, :].rearrange("p (i ct k) -> p i ct k", i=2, ct=4)), (bpT[p],), (bUB,))
            for ch in range(4):
                sidx = (s * 4 + ch) % 2
                for m in range(4):
                    mm(k, [(pgl[:, :], Wg[:, f, m * 128:(m + 1) * 128], ygT[:, f, ch * 512:(ch + 1) * 512], f == 0, f == 3) for f in range(4)], (bUB, bWg), (bpgl,))
                    k.op("act", lambda e, m=m: e.activation(out=sgl[:, :], in_=pgl[:, :], func=AF.Sigmoid, bias=bgl[:, m:m + 1]), (bpgl, bbgl), (bsgl,))
                    k.op("dve", lambda e, m=m, ch=ch, sidx=sidx: e.tensor_tensor(out=s5st[sidx][:, m, :], in0=sgl[:, :], in1=ygT[:, m, ch * 512:(ch + 1) * 512], op=ALU.mult), (bsgl, bUB), (bs5[sidx],))
                k.dma("pool", io["s5_s"][:, tb + ch * 512:tb + (ch + 1) * 512].rearrange("(m p) t -> p m t", p=128), s5st[sidx][:, :, :], bs5[sidx], (bs5[sidx],), ())
        k.end("S_main")
```
